# Optimizing a Trainium2 kernel written in Bass

```python
import jax, jax.numpy as jnp
from jax import lax
import numpy as np

D_MODEL = 1024
BATCH = 8
SEQ = 8192
DEPTH = 1

GRID_W = 64
CTX_LEN = 256
D_MIX = D_MODEL
NA_HEADS = 8
NA_HEAD_DIM = 64
NA_WIDTH = NA_HEADS * NA_HEAD_DIM
NA_WIN_ROWS = 8
NA_WIN_COLS = 16
ML_HEADS = 4
ML_HEAD_DIM = 128
ML_WIDTH = ML_HEADS * ML_HEAD_DIM
ML_CHUNK = 128
CONV_K = 5
N_GROUPS = 8
EXPERTS_PER_GROUP = 8
N_EXPERTS = N_GROUPS * EXPERTS_PER_GROUP
TOP_K_IN_GROUP = 2
EXPERT_HIDDEN = 512
MOE_BLOCK = 128
ROPE_BASE = 10000.0
LN_EPS = 1e-5
DEEPNORM_ALPHA = (2.0 * DEPTH) ** 0.25
DEEPNORM_BETA = (8.0 * DEPTH) ** -0.25

COL_NA_Q = 0
COL_NA_K = COL_NA_Q + NA_WIDTH
COL_NA_V = COL_NA_K + NA_WIDTH
COL_ML_Q = COL_NA_V + NA_WIDTH
COL_ML_K = COL_ML_Q + ML_WIDTH
COL_ML_V = COL_ML_K + ML_WIDTH
COL_ML_O = COL_ML_V + ML_WIDTH
COL_GATES = COL_ML_O + ML_WIDTH
IN_COLS = COL_GATES + 4 * ML_HEADS

kernel_name = 'hybrid_na_mlstm_hmoe_layer'


def _layer_norm(x, gain=None, bias=None):
    x32 = x.astype(jnp.float32)
    mu = jnp.mean(x32, axis=-1, keepdims=True)
    var = jnp.mean(jnp.square(x32 - mu), axis=-1, keepdims=True)
    y = (x32 - mu) * lax.rsqrt(var + LN_EPS)
    if gain is not None:
        y = y * gain.astype(jnp.float32) + bias.astype(jnp.float32)
    return y.astype(x.dtype)


def _modulate(xn, shift, scale):
    return xn * (1.0 + scale) + shift


def _heads(z, col, n_heads, head_dim):
    B, T, _ = z.shape
    return z[..., col:col + n_heads * head_dim].reshape(B, T, n_heads, head_dim)


def _centred_depthwise_conv(u, w, b):
    pad = CONV_K // 2
    out = lax.conv_general_dilated(u, w[:, None, :].astype(u.dtype), window_strides=(1,),
                                   padding=[(pad, pad)], dimension_numbers=('NWC', 'WIO', 'NWC'),
                                   feature_group_count=u.shape[-1])
    return out + b


def _rope_axis(x, pos):
    half = x.shape[-1] // 2
    inv = ROPE_BASE ** (-jnp.arange(half, dtype=jnp.float32) / half)
    ang = pos.astype(jnp.float32)[:, None] * inv[None, :]
    cos = jnp.cos(ang)[None, :, None, :]
    sin = jnp.sin(ang)[None, :, None, :]
    x32 = x.astype(jnp.float32)
    x1, x2 = x32[..., :half], x32[..., half:]
    return jnp.concatenate([x1 * cos - x2 * sin, x1 * sin + x2 * cos], axis=-1).astype(x.dtype)


def _rope_2d(x, row_pos, col_pos):
    d2 = x.shape[-1] // 2
    return jnp.concatenate([_rope_axis(x[..., :d2], row_pos), _rope_axis(x[..., d2:], col_pos)], axis=-1)


def _neighbourhood_attention(q, k, v, k_ctx, v_ctx, rpb):
    B, N, H, d = q.shape
    rows = N // GRID_W
    kr = min(NA_WIN_ROWS, rows)
    n_win = kr * NA_WIN_COLS
    qg = (q * d ** -0.5).reshape(B, rows, GRID_W, H, d)
    kg = k.reshape(B, rows, GRID_W, H, d)
    vg = v.reshape(B, rows, GRID_W, H, d)
    cols = jnp.arange(GRID_W)
    col_start = jnp.clip(cols - NA_WIN_COLS // 2, 0, GRID_W - NA_WIN_COLS)
    col_idx = col_start[:, None] + jnp.arange(NA_WIN_COLS)[None, :]
    col_off = col_idx - cols[:, None] + (NA_WIN_COLS - 1)

    def row_block(r):
        r0 = jnp.clip(r - kr // 2, 0, rows - kr)
        q_r = lax.dynamic_index_in_dim(qg, r, axis=1, keepdims=False)
        k_band = lax.dynamic_slice_in_dim(kg, r0, kr, axis=1)
        v_band = lax.dynamic_slice_in_dim(vg, r0, kr, axis=1)
        k_win = k_band[:, :, col_idx]
        v_win = v_band[:, :, col_idx]
        row_off = r0 + jnp.arange(kr) - r + (NA_WIN_ROWS - 1)
        bias = rpb[:, row_off[:, None, None], col_off[None, :, :]]
        bias = bias.transpose(0, 2, 1, 3).reshape(H, GRID_W, n_win)
        s_win = jnp.einsum('bqhd,brqjhd->bhqrj', q_r, k_win).reshape(B, H, GRID_W, n_win)
        s_ctx = jnp.einsum('bqhd,bchd->bhqc', q_r, k_ctx)
        s = jnp.concatenate([s_win.astype(jnp.float32) + bias.astype(jnp.float32),
                             s_ctx.astype(jnp.float32)], axis=-1)
        p = jax.nn.softmax(s, axis=-1).astype(v.dtype)
        p_win = p[..., :n_win].reshape(B, H, GRID_W, kr, NA_WIN_COLS)
        p_ctx = p[..., n_win:]
        return (jnp.einsum('bhqrj,brqjhd->bqhd', p_win, v_win)
                + jnp.einsum('bhqc,bchd->bqhd', p_ctx, v_ctx))

    out = lax.map(row_block, jnp.arange(rows))
    return out.transpose(1, 0, 2, 3, 4).reshape(B, N, H * d)


def _context_attention(q, k, v):
    B, T, H, d = q.shape
    s = jnp.einsum('bqhd,bkhd->bhqk', q * d ** -0.5, k).astype(jnp.float32)
    p = jax.nn.softmax(s, axis=-1).astype(v.dtype)
    return jnp.einsum('bhqk,bkhd->bqhd', p, v).reshape(B, T, H * d)


def _mlstm_streams(z, conv_w, conv_b, gate_b, row_pos, col_pos):
    B, T, _ = z.shape
    qk = jax.nn.silu(_centred_depthwise_conv(z[..., COL_ML_Q:COL_ML_V], conv_w, conv_b))
    q = qk[..., :ML_WIDTH].reshape(B, T, ML_HEADS, ML_HEAD_DIM)
    k = qk[..., ML_WIDTH:].reshape(B, T, ML_HEADS, ML_HEAD_DIM)
    if row_pos is not None:
        q = _rope_2d(q, row_pos, col_pos)
        k = _rope_2d(k, row_pos, col_pos)
    v = _heads(z, COL_ML_V, ML_HEADS, ML_HEAD_DIM)
    gates = (z[..., COL_GATES:IN_COLS] + gate_b).astype(jnp.float32)
    gates = gates.reshape(B, T, 4, ML_HEADS).transpose(2, 0, 3, 1)
    to_bhtd = lambda a: a.transpose(0, 2, 1, 3)
    return to_bhtd(q), to_bhtd(k) * ML_HEAD_DIM ** -0.5, to_bhtd(v), gates


def _mlstm_chunkwise(q, k, v, i_pre, f_pre, state):
    B, H, T, dk = q.shape
    dv = v.shape[-1]
    L = ML_CHUNK
    nc = T // L

    def to_chunks(a):
        a = a.astype(jnp.float32)
        return jnp.moveaxis(a.reshape((B, H, nc, L) + a.shape[3:]), 2, 0)

    causal = jnp.tril(jnp.ones((L, L), dtype=bool))

    def step(carry, inp):
        C, n, m = carry
        q_c, k_c, v_c, i_c, lf_c = inp
        b = jnp.cumsum(lf_c, axis=-1)
        dlog = jnp.where(causal, b[..., :, None] - b[..., None, :] + i_c[..., None, :], -jnp.inf)
        m_t = jnp.maximum(b + m[..., None], jnp.max(dlog, axis=-1))
        dw = jnp.exp(dlog - m_t[..., None])
        inter = jnp.exp(b + m[..., None] - m_t)
        s = jnp.einsum('bhtd,bhsd->bhts', q_c, k_c) * dw
        num = jnp.einsum('bhts,bhsv->bhtv', s, v_c) + inter[..., None] * jnp.einsum('bhtd,bhdv->bhtv', q_c, C)
        den = jnp.sum(s, axis=-1) + inter * jnp.einsum('bhtd,bhd->bht', q_c, n)
        h = num / jnp.maximum(jnp.abs(den), jnp.exp(-m_t))[..., None]
        b_end = b[..., -1]
        g = b_end[..., None] - b + i_c
        m_new = jnp.maximum(b_end + m, jnp.max(g, axis=-1))
        decay = jnp.exp(b_end + m - m_new)
        wgt = jnp.exp(g - m_new[..., None])
        C_new = decay[..., None, None] * C + jnp.einsum('bhs,bhsd,bhsv->bhdv', wgt, k_c, v_c)
        n_new = decay[..., None] * n + jnp.einsum('bhs,bhsd->bhd', wgt, k_c)
        return (C_new, n_new, m_new), h

    xs = (to_chunks(q), to_chunks(k), to_chunks(v), to_chunks(i_pre), to_chunks(jax.nn.log_sigmoid(f_pre)))
    state, hs = lax.scan(step, state, xs)
    h = jnp.moveaxis(hs, 0, 2).reshape(B, H, T, dv)
    return h, state


def _mlstm_bidirectional(q, k, v, gates, state_fwd, state_bwd):
    i_f, f_f, i_b, f_b = gates
    h_f, st_f = _mlstm_chunkwise(q, k, v, i_f, f_f, state_fwd)
    rev = lambda a: jnp.flip(a, axis=2)
    h_b, st_b = _mlstm_chunkwise(rev(q), rev(k), rev(v), rev(i_b), rev(f_b), state_bwd)
    return h_f + rev(h_b), st_f, st_b


def _mlstm_readout(h, o_pre, gain):
    B, H, T, dv = h.shape
    hn = h * lax.rsqrt(jnp.mean(jnp.square(h), axis=-1, keepdims=True) + LN_EPS)
    hn = hn.transpose(0, 2, 1, 3).reshape(B, T, H * dv)
    return (hn * gain.astype(jnp.float32) * jax.nn.sigmoid(o_pre.astype(jnp.float32))).astype(o_pre.dtype)


def _zero_mlstm_state(B):
    return (jnp.zeros((B, ML_HEADS, ML_HEAD_DIM, ML_HEAD_DIM), jnp.float32),
            jnp.zeros((B, ML_HEADS, ML_HEAD_DIM), jnp.float32),
            jnp.zeros((B, ML_HEADS), jnp.float32))


def _hier_moe(h, w_rg, b_rg, w_re, b_re, w1, w3, w2):
    B, T, D = h.shape
    n_tok = B * T
    xt = h.reshape(n_tok, D)
    group_logits = (xt @ w_rg + b_rg).astype(jnp.float32)
    group_prob = jax.nn.softmax(group_logits, axis=-1)
    grp = jnp.argmax(group_logits, axis=-1)
    grp_w = jnp.take_along_axis(group_prob, grp[:, None], axis=1)
    exp_logits = (xt @ w_re + b_re).astype(jnp.float32).reshape(n_tok, N_GROUPS, EXPERTS_PER_GROUP)
    exp_logits = jnp.take_along_axis(exp_logits, grp[:, None, None], axis=1)[:, 0]
    top_val, top_idx = lax.top_k(exp_logits, TOP_K_IN_GROUP)
    weights = grp_w * jax.nn.softmax(top_val, axis=-1)
    expert = grp[:, None] * EXPERTS_PER_GROUP + top_idx

    n_assign = n_tok * TOP_K_IN_GROUP
    flat_e = expert.reshape(-1).astype(jnp.int32)
    flat_tok = jnp.repeat(jnp.arange(n_tok, dtype=jnp.int32), TOP_K_IN_GROUP)
    flat_w = weights.reshape(-1)
    order = jnp.argsort(flat_e)
    e_sorted, tok_sorted, w_sorted = flat_e[order], flat_tok[order], flat_w[order]
    sizes = jnp.bincount(flat_e, length=N_EXPERTS)
    padded = (sizes + MOE_BLOCK - 1) // MOE_BLOCK * MOE_BLOCK
    start = jnp.cumsum(sizes) - sizes
    pstart = jnp.cumsum(padded) - padded
    dest = pstart[e_sorted] + jnp.arange(n_assign, dtype=jnp.int32) - start[e_sorted]
    cap = -(-n_assign // MOE_BLOCK) * MOE_BLOCK + N_EXPERTS * MOE_BLOCK
    n_blocks = cap // MOE_BLOCK
    buf_tok = jnp.full((cap,), n_tok, jnp.int32).at[dest].set(tok_sorted)
    buf_w = jnp.zeros((cap,), jnp.float32).at[dest].set(w_sorted)
    block_e = jnp.minimum(jnp.searchsorted(jnp.cumsum(padded), jnp.arange(n_blocks) * MOE_BLOCK, side='right'),
                          N_EXPERTS - 1)

    def expert_block(args):
        tok, e = args
        xb = xt[jnp.minimum(tok, n_tok - 1)]
        return (jax.nn.silu(xb @ w1[e]) * (xb @ w3[e])) @ w2[e]

    ys = lax.map(expert_block, (buf_tok.reshape(n_blocks, MOE_BLOCK), block_e)).reshape(cap, D)
    out = jax.ops.segment_sum(ys * buf_w[:, None].astype(ys.dtype), buf_tok, num_segments=n_tok + 1)[:n_tok]
    return out.reshape(B, T, D)


def setup_inputs(seed: int = 0) -> dict:
    key = jax.random.key(seed)
    ks = jax.random.split(key, 25)
    f32 = jnp.float32
    nrm = lambda k, shape, s: jax.random.normal(k, shape, f32) * s
    x = nrm(ks[0], (BATCH, SEQ, D_MODEL), 1.0)
    c = nrm(ks[1], (BATCH, D_MODEL), 1.0)
    ctx = nrm(ks[2], (BATCH, CTX_LEN, D_MODEL), 1.0)
    c_ctx = nrm(ks[3], (D_MODEL,), 1.0)
    w_ada = nrm(ks[4], (DEPTH, D_MODEL, 6 * D_MODEL), D_MODEL ** -0.5)
    b_ada = nrm(ks[5], (DEPTH, 6 * D_MODEL), 0.02)
    w_in = nrm(ks[6], (DEPTH, D_MODEL, IN_COLS), D_MODEL ** -0.5)
    conv_w = nrm(ks[7], (DEPTH, CONV_K, 2 * ML_WIDTH), CONV_K ** -0.5)
    conv_b = nrm(ks[8], (DEPTH, 2 * ML_WIDTH), 0.02)
    i_bias = nrm(ks[9], (DEPTH, 2, ML_HEADS), 0.1)
    f_bias = jnp.linspace(3.0, 6.0, ML_HEADS, dtype=f32) + nrm(ks[10], (DEPTH, 2, ML_HEADS), 0.1)
    gate_b = jnp.stack([i_bias[:, 0], f_bias[:, 0], i_bias[:, 1], f_bias[:, 1]], axis=1).reshape(DEPTH, 4 * ML_HEADS)
    rpb = nrm(ks[11], (DEPTH, NA_HEADS, 2 * NA_WIN_ROWS - 1, 2 * NA_WIN_COLS - 1), 0.1)
    ml_norm_g = 1.0 + nrm(ks[12], (DEPTH, ML_WIDTH), 0.02)
    w_out = nrm(ks[13], (DEPTH, D_MIX, D_MODEL), D_MIX ** -0.5 * DEEPNORM_BETA)
    ln1_g = 1.0 + nrm(ks[14], (DEPTH, D_MODEL), 0.02)
    ln1_b = nrm(ks[15], (DEPTH, D_MODEL), 0.02)
    w_router_g = nrm(ks[16], (DEPTH, D_MODEL, N_GROUPS), D_MODEL ** -0.5)
    b_router_g = nrm(ks[17], (DEPTH, N_GROUPS), 0.01)
    w_router_e = nrm(ks[18], (DEPTH, D_MODEL, N_EXPERTS), D_MODEL ** -0.5)
    b_router_e = nrm(ks[19], (DEPTH, N_EXPERTS), 0.01)
    w1 = nrm(ks[20], (DEPTH, N_EXPERTS, D_MODEL, EXPERT_HIDDEN), D_MODEL ** -0.5)
    w3 = nrm(ks[21], (DEPTH, N_EXPERTS, D_MODEL, EXPERT_HIDDEN), D_MODEL ** -0.5)
    w2 = nrm(ks[22], (DEPTH, N_EXPERTS, EXPERT_HIDDEN, D_MODEL), EXPERT_HIDDEN ** -0.5 * DEEPNORM_BETA)
    ln2_g = 1.0 + nrm(ks[23], (DEPTH, D_MODEL), 0.02)
    ln2_b = nrm(ks[24], (DEPTH, D_MODEL), 0.02)
    return {'x': x, 'c': c, 'ctx': ctx, 'c_ctx': c_ctx, 'w_ada': w_ada, 'b_ada': b_ada, 'w_in': w_in,
            'conv_w': conv_w, 'conv_b': conv_b, 'gate_b': gate_b, 'rpb': rpb, 'ml_norm_g': ml_norm_g,
            'w_out': w_out, 'ln1_g': ln1_g, 'ln1_b': ln1_b, 'w_router_g': w_router_g, 'b_router_g': b_router_g,
            'w_router_e': w_router_e, 'b_router_e': b_router_e, 'w1': w1, 'w3': w3, 'w2': w2,
            'ln2_g': ln2_g, 'ln2_b': ln2_b}


def reference(x, c, ctx, c_ctx, w_ada, b_ada, w_in, conv_w, conv_b, gate_b, rpb, ml_norm_g, w_out,
              ln1_g, ln1_b, w_router_g, b_router_g, w_router_e, b_router_e, w1, w3, w2, ln2_g, ln2_b):
    B, N, _ = x.shape
    pos = jnp.arange(N)
    row_pos = pos // GRID_W
    col_pos = pos % GRID_W
    for l in range(DEPTH):
        last = l == DEPTH - 1
        ada = (jax.nn.silu(c) @ w_ada[l] + b_ada[l])[:, None, :]
        ada_c = jax.nn.silu(c_ctx) @ w_ada[l] + b_ada[l]
        sh1, sc1, g1, sh2, sc2, g2 = jnp.split(ada, 6, axis=-1)
        csh1, csc1, cg1, csh2, csc2, cg2 = jnp.split(ada_c, 6, axis=-1)

        z = _modulate(_layer_norm(x), sh1, sc1) @ w_in[l]
        zc = _modulate(_layer_norm(ctx), csh1, csc1) @ w_in[l]

        k_na_c = _heads(zc, COL_NA_K, NA_HEADS, NA_HEAD_DIM)
        v_na_c = _heads(zc, COL_NA_V, NA_HEADS, NA_HEAD_DIM)
        na = _neighbourhood_attention(_heads(z, COL_NA_Q, NA_HEADS, NA_HEAD_DIM),
                                      _heads(z, COL_NA_K, NA_HEADS, NA_HEAD_DIM),
                                      _heads(z, COL_NA_V, NA_HEADS, NA_HEAD_DIM),
                                      k_na_c, v_na_c, rpb[l])

        zero = _zero_mlstm_state(B)
        q_c, k_c, v_c, gates_c = _mlstm_streams(zc, conv_w[l], conv_b[l], gate_b[l], None, None)
        h_ml_c, st_f, st_b = _mlstm_bidirectional(q_c, k_c, v_c, gates_c, zero, zero)
        q_l, k_l, v_l, gates_l = _mlstm_streams(z, conv_w[l], conv_b[l], gate_b[l], row_pos, col_pos)
        h_ml, _, _ = _mlstm_bidirectional(q_l, k_l, v_l, gates_l, st_f, st_b)
        ml = _mlstm_readout(h_ml, z[..., COL_ML_O:COL_GATES], ml_norm_g[l])

        mix = jnp.concatenate([na, ml], axis=-1) @ w_out[l]
        x_mid = _layer_norm(DEEPNORM_ALPHA * x + g1 * mix, ln1_g[l], ln1_b[l])

        moe = _hier_moe(_modulate(_layer_norm(x_mid), sh2, sc2), w_router_g[l], b_router_g[l],
                        w_router_e[l], b_router_e[l], w1[l], w3[l], w2[l])
        x_next = _layer_norm(DEEPNORM_ALPHA * x_mid + g2 * moe, ln2_g[l], ln2_b[l])

        if not last:
            na_c = _context_attention(_heads(zc, COL_NA_Q, NA_HEADS, NA_HEAD_DIM), k_na_c, v_na_c)
            ml_cout = _mlstm_readout(h_ml_c, zc[..., COL_ML_O:COL_GATES], ml_norm_g[l])
            mix_c = jnp.concatenate([na_c, ml_cout], axis=-1) @ w_out[l]
            ctx_mid = _layer_norm(DEEPNORM_ALPHA * ctx + cg1 * mix_c, ln1_g[l], ln1_b[l])
            moe_c = _hier_moe(_modulate(_layer_norm(ctx_mid), csh2, csc2), w_router_g[l], b_router_g[l],
                              w_router_e[l], b_router_e[l], w1[l], w3[l], w2[l])
            ctx = _layer_norm(DEEPNORM_ALPHA * ctx_mid + cg2 * moe_c, ln2_g[l], ln2_b[l])
        x = x_next
    return x
```

```python
import contextlib
import numpy as np
import ml_dtypes
import concourse.bass as bass
import concourse.mybir as mybir
from concourse.bass_utils import run_bass_kernel_spmd

F32 = mybir.dt.float32
BF16 = mybir.dt.bfloat16
I32 = mybir.dt.int32
U32 = mybir.dt.uint32
AF = mybir.ActivationFunctionType
ALU = mybir.AluOpType
AX = mybir.AxisListType

D = 1024
T = 8192
CTX = 256
TT = T + CTX
NCOL = 3600
NT = T // 128
NCT = CTX // 128
GW = 64
ALPHA = 2.0 ** 0.25
EPS = 1e-5
NE = 64
CAP = 2 * T + NE * 128
NBLK = CAP // 128
HID = 512
NEG = -30000.0


class Buf:
    __slots__ = ("w", "r", "name")

    def __init__(self, name=""):
        self.w = None
        self.r = []
        self.name = name


class Sched:
    ENG = ("pe", "act", "dve", "pool", "sp")
    LIM = 16000
    NDS = 24
    NDS_SP = 16

    def __init__(self, nc, es):
        self.nc = nc
        self.es = es
        self.e = dict(pe=nc.tensor, act=nc.scalar, dve=nc.vector, pool=nc.gpsimd, sp=nc.sync)
        self.nsem = 0
        self.sems = {}
        self.epoch = {k: 0 for k in self.ENG}
        self.cnt = {k: 0 for k in self.ENG}
        self.seen = {k: {} for k in self.ENG}
        self.pending = {k: [] for k in self.ENG}
        self.dep = [0] * self.NDS
        self.dcnt = [0] * self.NDS
        self.dnext = 0
        self.dnext_q = {}
        self.n_inst = 0

    def semobj(self, key):
        if key not in self.sems:
            self.sems[key] = self.es.enter_context(self.nc.semaphore("s%d" % self.nsem))
            self.nsem += 1
        return self.sems[key]

    def _wait(self, eng, ev):
        key, val = ev
        if self.seen[eng].get(key, 0) >= val:
            return
        self.e[eng].wait_ge(self.semobj(key), val)
        self.seen[eng][key] = val
        self.n_inst += 1

    def _deps(self, eng, reads, writes):
        deps = set()
        for b in reads:
            if b.w is not None:
                deps.add(b.w)
        for b in writes:
            if b.w is not None:
                deps.add(b.w)
            deps.update(b.r)
        for ev in deps:
            if ev is None:
                continue
            if eng == "pe" and ev[0][0] == "pe":
                continue
            self._wait(eng, ev)

    def _record(self, ev, reads, writes):
        for b in reads:
            b.r.append(ev)
            if len(b.r) > 12:
                b.r = b.r[-12:] if False else b.r
        for b in writes:
            b.w = ev
            b.r = []

    def op(self, eng, fn, reads=(), writes=(), inc=True):
        self._deps(eng, reads, writes)
        inst = fn(self.e[eng])
        self.n_inst += 1
        if not inc:
            self.pending[eng].append((list(reads), list(writes)))
            return inst
        if self.cnt[eng] >= self.LIM:
            self.epoch[eng] += 1
            self.cnt[eng] = 0
        self.cnt[eng] += 1
        key = (eng, self.epoch[eng])
        inst.then_inc(self.semobj(key), 1)
        ev = (key, self.cnt[eng])
        for (r, w) in self.pending[eng]:
            self._record(ev, r, w)
        self.pending[eng] = []
        self._record(ev, reads, writes)
        return inst

    def dma(self, out, in_, reads=(), writes=(), eng="sp", indirect=None):
        lo, hi = (0, self.NDS_SP) if eng == "sp" else (self.NDS_SP, self.NDS)
        i = self.dnext_q.get(eng, lo)
        self.dnext_q[eng] = lo + ((i + 1 - lo) % (hi - lo))
        if self.dcnt[i] > 0:
            self._wait(eng, (("d", i, self.dep[i]), 16 * self.dcnt[i]))
        if self.dcnt[i] >= self.LIM // 16:
            self.dep[i] += 1
            self.dcnt[i] = 0
        self._deps(eng, reads, writes)
        if indirect is None:
            inst = self.e[eng].dma_start(out=out, in_=in_)
        else:
            inst = indirect(self.e[eng])
        self.n_inst += 1
        self.dcnt[i] += 1
        key = ("d", i, self.dep[i])
        inst.then_inc(self.semobj(key), 16)
        ev = (key, 16 * self.dcnt[i])
        self._record(ev, reads, writes)
        return ev

    def barrier(self):
        for k in self.ENG:
            assert not self.pending[k], "pending group at barrier"
        evs = []
        for k in self.ENG:
            if self.cnt[k] > 0:
                evs.append(((k, self.epoch[k]), self.cnt[k]))
        for i in range(self.NDS):
            if self.dcnt[i] > 0:
                evs.append((("d", i, self.dep[i]), 16 * self.dcnt[i]))
        for k in self.ENG:
            for ev in evs:
                if ev[0][0] == k:
                    continue
                self._wait(k, ev)

    def final_wait(self, eng="sp"):
        for i in range(self.NDS):
            if self.dcnt[i] > 0:
                self._wait(eng, (("d", i, self.dep[i]), 16 * self.dcnt[i]))


def interleave(*gens):
    gens = [g for g in gens if g is not None]
    while gens:
        for g in list(gens):
            try:
                next(g)
            except StopIteration:
                gens.remove(g)


def weighted(gen, n):
    def g():
        done = False
        while not done:
            for _ in range(n):
                try:
                    next(gen)
                except StopIteration:
                    done = True
                    break
            yield
    return g()


def build_program(dbg=None, upto="all", phases="ABCDEFGHI"):
    dbg = dbg or []
    nc = bass.Bass("TRN2", target_bir_lowering=False)

    def din(name, shape, dt=F32):
        return nc.dram_tensor(name, list(shape), dt, kind="ExternalInput").ap()

    def dscr(name, shape, dt):
        kind = "ExternalOutput" if name in dbg else "Internal"
        return nc.dram_tensor(name, list(shape), dt, kind=kind).ap()

    x_d = din("x", [T, D])
    ctx_d = din("ctx", [CTX, D])
    cc_d = din("cc", [128, 8, 2])
    wada_d = din("w_ada", [D, 6 * D])
    bada_d = din("b_ada_l", [128, 48, 2])
    badar_d = din("b_ada_r", [1, 6 * D])
    win_d = din("w_in", [D, NCOL])
    gateb_d = din("gate_b", [16, 1])
    out_d = nc.dram_tensor("out", [T, D], F32, kind="ExternalOutput").ap()

    zT_d = dscr("zT", [16, 128, TT], BF16)
    vna_d = dscr("vna", [TT, 8 * 65], BF16)
    mlv_d = dscr("mlv", [TT, 512], BF16)
    mlo_d = dscr("mlo", [TT, 512], F32)
    gT_d = dscr("gT", [16, TT], F32)
    na_d = dscr("na", [T, 512], BF16)
    ml_d = dscr("ml", [T, 512], BF16)
    biasT_d = din("biasT", [5, 128, 8 * 5 * 128], BF16)
    qkT_d = dscr("qkT", [8, 128, TT], BF16)
    kml_d = dscr("kml", [TT, 512], BF16)
    hf_d = dscr("hf", [T, 512], F32)
    rope_d = din("rope", [4, 128, T])
    rperm_d = din("rperm", [128, 128], BF16)
    cmask_d = din("cmask", [3, 128, 128], BF16)
    convw_d = din("convw", [128, 8, 5])
    convb_d = din("convb", [128, 8])
    mlg_d = din("mlg", [128, 512])
    wout_d = din("w_out", [D, D])
    wr_d = din("w_r", [D, 72])
    rbias_d = din("rbias", [128, 72])
    lnp_d = din("lnp", [4, 128, D])
    bvals_d = din("bvals", [128, 3])
    w1_d = din("w1", [NE * 128, 8 * HID])
    w3_d = din("w3", [NE * 128, 8 * HID])
    w2_d = din("w2", [NE * 128, 4 * D])
    wbf_d = [dscr("wbf%d" % m, [NE * 128, 4096], BF16) for m in range(3)]
    xmid_d = dscr("xmid", [T, D], F32)
    h2_d = dscr("h2", [T, D], BF16)
    xperm_d = dscr("xperm", [CAP, D], BF16)
    yperm_d = dscr("yperm", [CAP, D], F32)
    rt_dbg = dscr("rt_dbg", [128, NT, 4], F32) if "rt_dbg" in dbg else None
    eb_dbg = dscr("eb_dbg", [1, 256], I32) if "eb_dbg" in dbg else None
    ix_dbg = dscr("ix_dbg", [128, 256], I32) if "ix_dbg" in dbg else None
    dbgD_d = dscr("dbgD", [2, 3, 4, TT], F32) if "dbgD" in dbg else None
    ada_dbg = dscr("ada_dbg", [128, 48, 2], F32) if "ada_dbg" in dbg else None
    gb_dbg = dscr("gb_dbg", [128, 2048], F32) if "gb_dbg" in dbg else None

    with contextlib.ExitStack() as es:
        S = Sched(nc, es)

        def sb(st, name, shape, dt):
            return st.enter_context(nc.sbuf_tensor("sb_" + name, list(shape), dt))

        def ps(st, name, shape, dt):
            return st.enter_context(nc.psum_tensor("ps_" + name, list(shape), dt))

        ident_bf = sb(es, "ident_bf", [128, 128], BF16)
        ident_f = sb(es, "ident_f", [128, 128], F32)
        ones_f = sb(es, "ones_f", [128, 128], F32)
        adaT = sb(es, "adaT", [128, 48, 2], F32)
        g_b = sb(es, "g_b", [128, 2048], F32)
        B_const = Buf("const")
        B_ada = Buf("ada")
        B_gb = Buf("gb")

        def mk_ident(tile):
            S.op("pool", lambda e: e.memset(tile[:], 1.0), writes=[B_const])
            S.op("pool", lambda e: e.affine_select(out=tile[:], in_=tile[:], pattern=[[1, 128]],
                                                   compare_op=ALU.is_equal, fill=0.0, base=0,
                                                   channel_multiplier=-1), reads=[B_const], writes=[B_const])
        mk_ident(ident_f)
        S.op("dve", lambda e: e.tensor_copy(out=ident_bf[:], in_=ident_f[:]), reads=[B_const], writes=[B_const])
        S.op("dve", lambda e: e.memset(ones_f[:], 1.0), writes=[B_const])
        neghalf_c = sb(es, "neghalf_c", [128, 1], F32)
        S.op("dve", lambda e: e.memset(neghalf_c[:], -0.5), writes=[B_const])


        def conv_task():
            srcs = (w1_d, w3_d, w2_d)
            for r0 in range(0, NE * 128, 128):
                for m in range(3):
                    S.dma(wbf_d[m][r0:r0 + 128, :], srcs[m][r0:r0 + 128, :], eng="pool")
                    yield
        bg_task = conv_task() if "H" in phases else iter(())

        def bg_step(n):
            for _ in range(n):
                try:
                    next(bg_task)
                except StopIteration:
                    return

        with contextlib.ExitStack() as pa:
            cc = sb(pa, "cc", [128, 8, 2], F32)
            scc = sb(pa, "scc", [128, 8, 2], F32)
            badal = sb(pa, "badal", [128, 48, 2], F32)
            badar = sb(pa, "badar", [1, 6 * D], F32)
            wp = [sb(pa, "wadap%d" % i, [128, 8, 1024], F32) for i in range(2)]
            grow = sb(pa, "grow", [1, 2048], F32)
            adaps = ps(pa, "adaps", [128, 512], F32)
            rowps = ps(pa, "rowps", [1, 512], F32)
            bcps = ps(pa, "bcps", [128, 512], F32)
            B_cc, B_scc, B_bl, B_br = Buf(), Buf(), Buf(), Buf()
            B_wp = [Buf(), Buf()]
            B_adaps, B_rowps, B_bcps, B_grow = Buf(), Buf(), Buf(), Buf()

            S.dma(cc[:], cc_d, writes=[B_cc])
            S.dma(badal[:], bada_d, writes=[B_bl])
            S.dma(badar[:], badar_d, writes=[B_br])
            S.op("act", lambda e: e.activation(out=scc[:], in_=cc[:], func=AF.Silu), reads=[B_cc], writes=[B_scc])
            wada_v = wada_d.rearrange("(k p) n -> p k n", p=128)
            for pc in range(6):
                S.dma(wp[pc % 2][:], wada_v[:, :, pc * 1024:(pc + 1) * 1024], writes=[B_wp[pc % 2]])
                w = wp[pc % 2]
                if pc in (2, 5):
                    gi = 0 if pc == 2 else 1
                    for grp in range(2):
                        for k in range(8):
                            S.op("pe", lambda e, k=k, grp=grp: e.matmul(
                                rowps[0:1, :], lhsT=scc[:, k, 0:1], rhs=w[:, k, grp * 512:(grp + 1) * 512],
                                start=(k == 0), stop=(k == 7)),
                                reads=[B_scc, B_wp[pc % 2]], writes=[B_rowps], inc=(k == 7))
                        S.op("dve", lambda e, grp=grp: e.tensor_tensor(
                            out=grow[0:1, gi * 1024 + grp * 512: gi * 1024 + (grp + 1) * 512], in0=rowps[0:1, :],
                            in1=badar[0:1, pc * 1024 + grp * 512: pc * 1024 + (grp + 1) * 512], op=ALU.add),
                            reads=[B_rowps, B_br], writes=[B_grow])
                        S.op("pe", lambda e, grp=grp: e.matmul(
                            bcps[:, :], lhsT=ones_f[0:1, :], rhs=grow[0:1, gi * 1024 + grp * 512: gi * 1024 + (grp + 1) * 512],
                            start=True, stop=True), reads=[B_grow, B_const], writes=[B_bcps])
                        S.op("act", lambda e, grp=grp: e.activation(
                            out=g_b[:, gi * 1024 + grp * 512: gi * 1024 + (grp + 1) * 512], in_=bcps[:, :], func=AF.Copy),
                            reads=[B_bcps], writes=[B_gb])
                else:
                    for jj in range(8):
                        for k in range(8):
                            S.op("pe", lambda e, k=k, jj=jj: e.matmul(
                                adaps[:, jj * 2:(jj + 1) * 2], lhsT=w[:, k, jj * 128:(jj + 1) * 128], rhs=scc[:, k, :],
                                start=(k == 0), stop=(k == 7)),
                                reads=[B_scc, B_wp[pc % 2]], writes=[B_adaps], inc=(k == 7 and jj == 7))
                    S.op("dve", lambda e: e.tensor_tensor(
                        out=adaT[:, pc * 8:(pc + 1) * 8, :], in0=adaps[:, 0:16].rearrange("p (j s) -> p j s", s=2),
                        in1=badal[:, pc * 8:(pc + 1) * 8, :], op=ALU.add),
                        reads=[B_adaps, B_bl], writes=[B_ada])
                    if pc in (1, 4):
                        S.op("dve", lambda e: e.tensor_scalar(
                            out=adaT[:, pc * 8:(pc + 1) * 8, :], in0=adaT[:, pc * 8:(pc + 1) * 8, :],
                            scalar1=1.0, scalar2=None, op0=ALU.add), reads=[B_ada], writes=[B_ada])
            if ada_dbg is not None:
                S.dma(ada_dbg, adaT[:], reads=[B_ada])
                S.dma(gb_dbg, g_b[:], reads=[B_gb])
            S.barrier()
        if upto == "A":
            S.final_wait()
            return nc

        with contextlib.ExitStack() as pb:
            win_sb = sb(pb, "win_sb", [128, 8, NCOL], BF16)
            gateb = sb(pb, "gateb", [16, 1], F32)
            B_win, B_gateb = Buf(), Buf()
            win_v = win_d.rearrange("(k p) n -> p k n", p=128)
            for k in range(8):
                S.dma(win_sb[:, k, :], win_v[:, k, :], writes=[B_win], eng="pool")
            S.dma(gateb[:], gateb_d, writes=[B_gateb])

            NXB = 3
            xt = [sb(pb, "xt%d" % i, [128, D], F32) for i in range(NXB)]
            B_xt = [Buf() for _ in range(NXB)]
            xn = [sb(pb, "xn%d" % i, [128, D], BF16) for i in range(2)]
            B_xn = [Buf(), Buf()]
            st6 = sb(pb, "st6", [128, 2, 6], F32)
            mv = sb(pb, "mv", [128, 2], F32)
            rstd = sb(pb, "rstd", [128, 1], F32)
            nmr = sb(pb, "nmr", [128, 1], F32)
            neghalf = sb(pb, "neghalf", [128, 1], F32)
            B_st, B_mv, B_rstd, B_nmr = Buf(), Buf(), Buf(), Buf()
            S.op("dve", lambda e: e.memset(neghalf[:], -0.5), writes=[B_const])
            xmT = [sb(pb, "xmT%d" % i, [128, 8, 512], BF16) for i in range(2)]
            B_xmT = [Buf(), Buf()]
            tp = [ps(pb, "tp%d" % i, [128, 1024], BF16) for i in range(2)]
            B_tp = [Buf(), Buf()]
            fps = [ps(pb, "fps%d" % i, [128, 512], F32) for i in range(2)]
            B_fps = [Buf(), Buf()]
            tps = [ps(pb, "tps%d" % i, [128, 512], F32) for i in range(2)]
            B_tps = [Buf(), Buf()]
            gps = ps(pb, "gps", [16, 512], F32)
            B_gps = Buf()
            zst = [sb(pb, "zst%d" % i, [128, 16, 512], BF16) for i in range(2)]
            B_zst = [Buf(), Buf()]
            vst = [sb(pb, "vst%d" % i, [128, 4, 8 * 65], BF16) for i in range(2)]
            B_vst = [Buf(), Buf()]
            mvst = [sb(pb, "mvst%d" % i, [128, 4, 512], BF16) for i in range(2)]
            B_mvst = [Buf(), Buf()]
            ost = [sb(pb, "ost%d" % i, [128, 4, 512], F32) for i in range(2)]
            B_ost = [Buf(), Buf()]
            gst = [sb(pb, "gst%d" % i, [16, 512], F32) for i in range(2)]
            B_gst = [Buf(), Buf()]
            for i in range(2):
                S.op("dve", lambda e, i=i: e.memset(vst[i][:], 1.0), writes=[B_vst[i]])

            supers = [(0, NCT, 1, ctx_d)] + [(CTX + s * 512, 4, 0, x_d[s * 512:(s + 1) * 512, :]) for s in range(T // 512)]
            tile_list = []
            for si, (t0, ntl, stream, src) in enumerate(supers):
                for j in range(ntl):
                    tile_list.append((si, j))
            load_idx = [0]

            def issue_load(n):
                while load_idx[0] <= n and load_idx[0] < len(tile_list):
                    si, j = tile_list[load_idx[0]]
                    src = supers[si][3]
                    b = load_idx[0] % NXB
                    S.dma(xt[b][:], src[j * 128:(j + 1) * 128, :], writes=[B_xt[b]])
                    load_idx[0] += 1

            gtile = {}
            cnt = 0
            for si, (t0, ntl, stream, src) in enumerate(supers):
                for j in range(ntl):
                    gtile[(si, j)] = cnt
                    cnt += 1

            evac_rr = [0]

            def evac(out, in_, reads, writes, scale=None, eng=None):
                if eng is None:
                    eng = ("act", "dve")[evac_rr[0] % 2]
                    evac_rr[0] += 1
                if eng == "act":
                    if scale is None:
                        S.op("act", lambda e: e.activation(out=out, in_=in_, func=AF.Copy), reads=reads, writes=writes)
                    else:
                        S.op("act", lambda e: e.activation(out=out, in_=in_, func=AF.Copy, scale=float(scale)),
                             reads=reads, writes=writes)
                else:
                    if scale is None:
                        S.op("dve", lambda e: e.tensor_copy(out=out, in_=in_), reads=reads, writes=writes)
                    else:
                        S.op("dve", lambda e: e.tensor_scalar(out=out, in0=in_, scalar1=float(scale), scalar2=None,
                                                              op0=ALU.mult), reads=reads, writes=writes)

            def prep(si):
                t0, ntl, stream, src = supers[si]
                xm = xmT[si % 2]
                Bxm = B_xmT[si % 2]
                for j in range(ntl):
                    g = gtile[(si, j)]
                    issue_load(g + 2)
                    b = g % NXB
                    x_t = xt[b]
                    S.op("dve", lambda e: e.bn_stats(out=st6[:, 0, :], in_=x_t[:, 0:512]), reads=[B_xt[b]], writes=[B_st])
                    S.op("dve", lambda e: e.bn_stats(out=st6[:, 1, :], in_=x_t[:, 512:1024]), reads=[B_xt[b]], writes=[B_st])
                    S.op("dve", lambda e: e.bn_aggr(out=mv[:], in_=st6[:].rearrange("p a b -> p (a b)")), reads=[B_st], writes=[B_mv])
                    S.op("dve", lambda e: e.tensor_scalar(out=rstd[:], in0=mv[:, 1:2], scalar1=EPS, scalar2=None, op0=ALU.add),
                         reads=[B_mv], writes=[B_rstd])
                    S.op("pool", lambda e: e.tensor_tensor(out=rstd[:], in0=rstd[:], in1=neghalf[:], op=ALU.pow),
                         reads=[B_rstd, B_const], writes=[B_rstd])
                    S.op("dve", lambda e: e.scalar_tensor_tensor(out=nmr[:], in0=mv[:, 0:1], scalar=-1.0, in1=rstd[:],
                                                                 op0=ALU.mult, op1=ALU.mult), reads=[B_mv, B_rstd], writes=[B_nmr])
                    xnb = xn[g % 2]
                    S.op("act", lambda e: e.activation(out=xnb[:], in_=x_t[:], func=AF.Identity, bias=nmr[:, 0:1], scale=rstd[:, 0:1]),
                         reads=[B_xt[b], B_nmr, B_rstd], writes=[B_xn[g % 2]])
                    bg_step(3)
                    yield
                    tpp = tp[g % 2]
                    for k in range(8):
                        S.op("pe", lambda e, k=k: e.transpose(out=tpp[:, k * 128:(k + 1) * 128], in_=xnb[:, k * 128:(k + 1) * 128],
                                                              identity=ident_bf[:]),
                             reads=[B_xn[g % 2], B_const], writes=[B_tp[g % 2]], inc=(k == 7))
                    for k in range(8):
                        o = xm[:, k, j * 128:(j + 1) * 128]
                        i_ = tpp[:, k * 128:(k + 1) * 128]
                        sc = adaT[:, 8 + k, stream:stream + 1]
                        sh = adaT[:, 0 + k, stream:stream + 1]
                        if k % 2 == 0:
                            S.op("act", lambda e, o=o, i_=i_, sc=sc, sh=sh: e.activation(out=o, in_=i_, func=AF.Identity, bias=sh, scale=sc),
                                 reads=[B_tp[g % 2], B_ada], writes=[Bxm])
                        else:
                            S.op("dve", lambda e, o=o, i_=i_, sc=sc, sh=sh: e.tensor_scalar(out=o, in0=i_, scalar1=sc, scalar2=sh,
                                                                                           op0=ALU.mult, op1=ALU.add),
                                 reads=[B_tp[g % 2], B_ada], writes=[Bxm])
                    yield

            FM_CH = [(c * 128) for c in range(0, 8)] + [1536 + c * 128 for c in range(0, 8)]

            def mm(si):
                t0, ntl, stream, src = supers[si]
                ntok = ntl * 128
                xm = xmT[si % 2]
                Bxm = B_xmT[si % 2]
                zs = zst[si % 2]
                for ci, c0 in enumerate(FM_CH):
                    p = fps[ci % 2]
                    for k in range(8):
                        S.op("pe", lambda e, k=k: e.matmul(p[:, 0:ntok], lhsT=win_sb[:, k, c0:c0 + 128], rhs=xm[:, k, 0:ntok],
                                                           start=(k == 0), stop=(k == 7)),
                             reads=[B_win, Bxm], writes=[B_fps[ci % 2]], inc=(k == 7))
                    evac(zs[:, ci, 0:ntok], p[:, 0:ntok], [B_fps[ci % 2]], [B_zst[si % 2]], scale=(0.125 if ci < 4 else None))
                    yield
                S.dma(zT_d[:, :, t0:t0 + ntok].rearrange("c p t -> p c t"), zs[:, :, 0:ntok], reads=[B_zst[si % 2]])
                for k in range(8):
                    S.op("pe", lambda e, k=k: e.matmul(gps[:, 0:ntok], lhsT=win_sb[:, k, 3584:3600], rhs=xm[:, k, 0:ntok],
                                                       start=(k == 0), stop=(k == 7)),
                         reads=[B_win, Bxm], writes=[B_gps], inc=(k == 7))
                gs = gst[si % 2]
                S.op("act", lambda e: e.activation(out=gs[:, 0:ntok], in_=gps[:, 0:ntok], func=AF.Identity, bias=gateb[:, 0:1], scale=1.0),
                     reads=[B_gps, B_gateb], writes=[B_gst[si % 2]])
                S.dma(gT_d[:, t0:t0 + ntok], gs[:, 0:ntok], reads=[B_gst[si % 2]])
                yield
                for j in range(ntl):
                    lhs = lambda k: xm[:, k, j * 128:(j + 1) * 128]
                    for gi, c0 in enumerate((1024, 2560, 3072)):
                        q = (j * 3 + gi) % 2
                        p = tps[q]
                        for k in range(8):
                            S.op("pe", lambda e, k=k: e.matmul(p[:, :], lhsT=lhs(k), rhs=win_sb[:, k, c0:c0 + 512],
                                                               start=(k == 0), stop=(k == 7)),
                                 reads=[B_win, Bxm], writes=[B_tps[q]], inc=(k == 7))
                        if gi == 0:
                            evac(vst[si % 2][:, j, :].rearrange("p (h d) -> p h d", d=65)[:, :, 0:64],
                                 p[:, :].rearrange("p (h d) -> p h d", d=64), [B_tps[q]], [B_vst[si % 2]])
                        elif gi == 1:
                            evac(mvst[si % 2][:, j, :], p[:, :], [B_tps[q]], [B_mvst[si % 2]])
                        else:
                            S.op("act", lambda e: e.activation(out=ost[si % 2][:, j, :], in_=p[:, :], func=AF.Sigmoid),
                                 reads=[B_tps[q]], writes=[B_ost[si % 2]])
                        yield
                tv = lambda d_: d_[t0:t0 + ntok, :].rearrange("(j p) f -> p j f", p=128)
                S.dma(tv(vna_d), vst[si % 2][:, 0:ntl, :], reads=[B_vst[si % 2]])
                S.dma(tv(mlv_d), mvst[si % 2][:, 0:ntl, :], reads=[B_mvst[si % 2]])
                S.dma(tv(mlo_d), ost[si % 2][:, 0:ntl, :], reads=[B_ost[si % 2]])
                yield

            nsup = len(supers) if upto != "B1" else 2
            issue_load(1)
            interleave(prep(0))
            for si in range(nsup):
                interleave(mm(si), weighted(prep(si + 1), 1) if si + 1 < nsup else None)
            S.barrier()
        if upto in ("B", "B1"):
            S.final_wait()
            return nc


        if "F" in phases:
          with contextlib.ExitStack() as pf:
            kT_sb = sb(pf, "kT_sb", [128, 4, TT], BF16)
            v_sb = sb(pf, "v_sb", [128, TT // 128, 520], BF16)
            biasI = sb(pf, "biasI", [128, 8, 5, 128], BF16)
            biasE = sb(pf, "biasE", [128, 8, 5, 128], BF16)
            B_kT, B_v, B_bI, B_bE = Buf(), Buf(), Buf(), Buf()
            B_kTs = [Buf() for _ in range(4)]
            B_vs = [Buf() for _ in range(6)]
            for c in range(4):
                S.dma(kT_sb[:, c, :], zT_d[4 + c, :, :], writes=[B_kTs[c]])
            vv = vna_d.rearrange("(j p) f -> p j f", p=128)
            for j0 in range(0, TT // 128, 11):
                S.dma(v_sb[:, j0:j0 + 11, :], vv[:, j0:j0 + 11, :], writes=[B_vs[j0 // 11]])
            S.dma(biasI[:].rearrange("p h n q -> p (h n q)"), biasT_d[0], writes=[B_bI])
            qT = [sb(pf, "qT%d" % i, [128, 4, 128], BF16) for i in range(2)]
            B_qT = [Buf(), Buf()]
            NPT = 3
            pT = [sb(pf, "pT%d" % i, [128, 896], BF16) for i in range(NPT)]
            B_pT = [Buf() for _ in range(NPT)]
            sT = [ps(pf, "sT%d" % i, [128, 1024], F32) for i in range(2)]
            B_sT = [Buf(), Buf()]
            po = [ps(pf, "po%d" % i, [128, 2, 512], F32) for i in range(2)]
            B_po = [Buf(), Buf()]
            rec = sb(pf, "rec", [128, 8], F32)
            B_rec = Buf()
            ostg = [sb(pf, "ostg%d" % i, [128, 8, 64], BF16) for i in range(2)]
            B_ostg = [Buf(), Buf()]
            ntiles_f = NT if upto != "F1" else 3
            tiles_f = list(range(NT)) if upto != "F1" else [0, 1, 2, 30, 62, 63]

            def load_q(idx):
                if idx < len(tiles_f):
                    i = tiles_f[idx]
                    t0 = CTX + i * 128
                    S.dma(qT[idx % 2][:], zT_d[0:4, :, t0:t0 + 128].rearrange("c p t -> p c t"), writes=[B_qT[idx % 2]])
            load_q(0)
            hcount = 0
            for idx, i in enumerate(tiles_f):
                load_q(idx + 1)
                variant = {0: 1, 1: 2, 62: 3, 63: 4}.get(i, 0)
                if variant:
                    S.dma(biasE[:].rearrange("p h n q -> p (h n q)"), biasT_d[variant], writes=[B_bE])
                    bias, B_bias = biasE, B_bE
                else:
                    bias, B_bias = biasI, B_bI
                jb0 = min(max(i - 2, 0), 59)
                q = qT[idx % 2]
                pob = po[idx % 2]
                def emit_qk(h, hc):
                    c = h // 2
                    pb = (h % 2) * 64
                    sTb = sT[hc % 2]
                    for n in range(7):
                        if n < 5:
                            k0 = CTX + (jb0 + n) * 128
                        else:
                            k0 = (n - 5) * 128
                        S.op("pe", lambda e: e.matmul(sTb[:, n * 128:(n + 1) * 128], lhsT=kT_sb[pb:pb + 64, c, k0:k0 + 128],
                                                      rhs=q[pb:pb + 64, c, :], start=True, stop=(n >= 5)),
                             reads=B_kTs + [B_qT[idx % 2]], writes=[B_sT[hc % 2]], inc=(n == 6))
                        if n < 5:
                            S.op("pe", lambda e: e.matmul(sTb[:, n * 128:(n + 1) * 128], lhsT=ident_bf[:, :],
                                                          rhs=bias[:, h, n, :], start=False, stop=True),
                                 reads=[B_bias, B_const], writes=[B_sT[hc % 2]], inc=False)

                emit_qk(0, hcount)
                for h in range(8):
                    sTb = sT[hcount % 2]
                    pTb = pT[hcount % NPT]
                    S.op("act", lambda e: e.activation(out=pTb[:, :], in_=sTb[:, 0:896], func=AF.Exp),
                         reads=[B_sT[hcount % 2]], writes=[B_pT[hcount % NPT]])
                    if h + 1 < 8:
                        emit_qk(h + 1, hcount + 1)
                    for n in range(7):
                        blk = (2 + jb0 + n) if n < 5 else (n - 5)
                        S.op("pe", lambda e: e.matmul(pob[:, h // 4, (h % 4) * 65:(h % 4) * 65 + 65], lhsT=pTb[:, n * 128:(n + 1) * 128],
                                                      rhs=v_sb[:, blk, h * 65:(h + 1) * 65], start=(n == 0), stop=(n == 6)),
                             reads=[B_pT[hcount % NPT]] + B_vs, writes=[B_po[idx % 2]], inc=(n == 6))
                    hcount += 1
                pov = pob[:, :, 0:260].rearrange("p a (h d) -> p a h d", d=65)
                S.op("dve", lambda e: e.reciprocal(out=rec[:].rearrange("p (a h) -> p a h", a=2), in_=pov[:, :, :, 64]),
                     reads=[B_po[idx % 2]], writes=[B_rec])
                og = ostg[idx % 2]
                for a in range(2):
                    S.op("dve", lambda e: e.tensor_tensor(out=og[:, a * 4:(a + 1) * 4, :], in0=pov[:, a, :, 0:64],
                                                          in1=rec[:, a * 4:(a + 1) * 4].unsqueeze(2).to_broadcast([128, 4, 64]),
                                                          op=ALU.mult),
                         reads=[B_po[idx % 2], B_rec], writes=[B_ostg[idx % 2]])
                S.dma(na_d[i * 128:(i + 1) * 128, :], og[:].rearrange("p h d -> p (h d)"), reads=[B_ostg[idx % 2]])
            S.barrier()
        if upto in ("F", "F1"):
            S.final_wait()
            return nc

        KSC = 128.0 ** -0.5
        if "C" in phases:
          with contextlib.ExitStack() as pc_:
            convw = sb(pc_, "convw", [128, 8, 5], F32)
            convb = sb(pc_, "convb", [128, 8], F32)
            diagw = sb(pc_, "diagw", [128, 8, 5, 128], BF16)
            rperm = sb(pc_, "rperm", [128, 128], BF16)
            B_cw, B_cb, B_dw, B_rp = Buf(), Buf(), Buf(), Buf()
            S.dma(convw[:], convw_d, writes=[B_cw])
            S.dma(convb[:], convb_d, writes=[B_cb])
            S.dma(rperm[:], rperm_d, writes=[B_rp])
            for ch in range(8):
                for j in range(5):
                    S.op("dve", lambda e: e.tensor_scalar(out=diagw[:, ch, j, :], in0=ident_f[:, :], scalar1=convw[:, ch, j:j + 1],
                                                          scalar2=None, op0=ALU.mult), reads=[B_cw, B_const], writes=[B_dw])
            u8 = [sb(pc_, "u8_%d" % i, [128, 8, 516], BF16) for i in range(2)]
            B_u8 = [Buf(), Buf()]
            rt = [sb(pc_, "rt%d" % i, [128, 4, 512], F32) for i in range(2)]
            B_rt = [Buf(), Buf()]
            qs = [sb(pc_, "qs%d" % i, [128, 512], BF16) for i in range(2)]
            B_qs = [Buf(), Buf()]
            t1 = [sb(pc_, "t1_%d" % i, [128, 512], F32) for i in range(2)]
            B_t1 = [Buf(), Buf()]
            t2 = [sb(pc_, "t2_%d" % i, [128, 512], F32) for i in range(2)]
            B_t2 = [Buf(), Buf()]
            qko = [sb(pc_, "qko%d" % i, [128, 8, 512], BF16) for i in range(2)]
            B_qko = [Buf(), Buf()]
            kst = [sb(pc_, "kst%d" % i, [128, 4, 4, 128], BF16) for i in range(2)]
            B_kst = [Buf(), Buf()]
            cps_ = [ps(pc_, "cvps%d" % i, [128, 512], F32) for i in range(2)]
            B_cps = [Buf(), Buf()]
            rps = [ps(pc_, "rps%d" % i, [128, 512], F32) for i in range(2)]
            B_rps = [Buf(), Buf()]
            trp = [ps(pc_, "trp%d" % i, [128, 4, 128], BF16) for i in range(2)]
            B_trp = [Buf(), Buf()]
            groups = [(0, CTX, False, 0)] + [(CTX + g * 512, 512, True, g * 512) for g in range(T // 512)]
            if upto == "C1":
                groups = groups[:2] + groups[-1:]

            def load_group(gi):
                if gi >= len(groups):
                    return
                tt0, n, lat, lo = groups[gi]
                u = u8[gi % 2]
                seg0, seg1 = (CTX, TT) if lat else (0, CTX)
                a = max(tt0 - 2, seg0)
                b_ = min(tt0 + n + 2, seg1)
                if a > tt0 - 2:
                    S.op("pool", lambda e: e.memset(u[:, :, 0:2], 0.0), writes=[B_u8[gi % 2]])
                if b_ < tt0 + n + 2:
                    S.op("pool", lambda e: e.memset(u[:, :, n + 2:n + 4], 0.0), writes=[B_u8[gi % 2]])
                S.dma(u[:, :, a - (tt0 - 2):b_ - (tt0 - 2)], zT_d[8:16, :, a:b_].rearrange("c p t -> p c t"), writes=[B_u8[gi % 2]])
                if lat:
                    S.dma(rt[gi % 2][:], rope_d[:, :, lo:lo + 512].rearrange("c p t -> p c t"), writes=[B_rt[gi % 2]])
            load_group(0)
            cc_ = 0
            for gi, (tt0, n, lat, lo) in enumerate(groups):
                load_group(gi + 1)
                u = u8[gi % 2]
                qo = qko[gi % 2]
                for ch in range(8):
                    cp = cps_[cc_ % 2]
                    for j in range(5):
                        S.op("pe", lambda e: e.matmul(cp[:, 0:n], lhsT=diagw[:, ch, j, :], rhs=u[:, ch, j:j + n], start=(j == 0), stop=(j == 4)),
                             reads=[B_dw, B_u8[gi % 2]], writes=[B_cps[cc_ % 2]], inc=(j == 4))
                    isk = ch >= 4
                    if not lat:
                        if isk:
                            S.op("act", lambda e: e.activation(out=t1[0][:, 0:n], in_=cp[:, 0:n], func=AF.Silu, bias=convb[:, ch:ch + 1], scale=1.0),
                                 reads=[B_cps[cc_ % 2], B_cb], writes=[B_t1[0]])
                            S.op("dve", lambda e: e.tensor_scalar(out=qo[:, ch, 0:n], in0=t1[0][:, 0:n], scalar1=KSC, scalar2=None, op0=ALU.mult),
                                 reads=[B_t1[0]], writes=[B_qko[gi % 2]])
                        else:
                            S.op("act", lambda e: e.activation(out=qo[:, ch, 0:n], in_=cp[:, 0:n], func=AF.Silu, bias=convb[:, ch:ch + 1], scale=1.0),
                                 reads=[B_cps[cc_ % 2], B_cb], writes=[B_qko[gi % 2]])
                    else:
                        q_ = qs[cc_ % 2]
                        S.op("act", lambda e: e.activation(out=q_[:, 0:n], in_=cp[:, 0:n], func=AF.Silu, bias=convb[:, ch:ch + 1], scale=1.0),
                             reads=[B_cps[cc_ % 2], B_cb], writes=[B_qs[cc_ % 2]])
                        rp_ = rps[cc_ % 2]
                        S.op("pe", lambda e: e.matmul(rp_[:, 0:n], lhsT=rperm[:, :], rhs=q_[:, 0:n], start=True, stop=True),
                             reads=[B_rp, B_qs[cc_ % 2]], writes=[B_rps[cc_ % 2]])
                        tb = 2 if isk else 0
                        S.op("pool", lambda e: e.tensor_tensor(out=t1[cc_ % 2][:, 0:n], in0=q_[:, 0:n], in1=rt[gi % 2][:, tb, 0:n], op=ALU.mult),
                             reads=[B_qs[cc_ % 2], B_rt[gi % 2]], writes=[B_t1[cc_ % 2]])
                        S.op("dve", lambda e: e.tensor_tensor(out=t2[cc_ % 2][:, 0:n], in0=rp_[:, 0:n], in1=rt[gi % 2][:, tb + 1, 0:n], op=ALU.mult),
                             reads=[B_rps[cc_ % 2], B_rt[gi % 2]], writes=[B_t2[cc_ % 2]])
                        S.op("dve", lambda e: e.tensor_tensor(out=qo[:, ch, 0:n], in0=t1[cc_ % 2][:, 0:n], in1=t2[cc_ % 2][:, 0:n], op=ALU.add),
                             reads=[B_t1[cc_ % 2], B_t2[cc_ % 2]], writes=[B_qko[gi % 2]])
                    cc_ += 1
                S.dma(qkT_d[:, :, tt0:tt0 + n].rearrange("c p t -> p c t"), qo[:, :, 0:n], reads=[B_qko[gi % 2]])
                ks = kst[gi % 2]
                for j in range(n // 128):
                    tr = trp[j % 2]
                    for h in range(4):
                        S.op("pe", lambda e: e.transpose(out=tr[:, h, :], in_=qo[:, 4 + h, j * 128:(j + 1) * 128], identity=ident_bf[:]),
                             reads=[B_qko[gi % 2], B_const], writes=[B_trp[j % 2]], inc=(h == 3))
                    S.op("act", lambda e: e.activation(out=ks[:, j, :, :], in_=tr[:, :, :], func=AF.Copy),
                         reads=[B_trp[j % 2]], writes=[B_kst[gi % 2]])
                S.dma(kml_d[tt0:tt0 + n, :].rearrange("(j p) f -> p j f", p=128), ks[:, 0:n // 128, :, :].rearrange("p j h d -> p j (h d)"),
                      reads=[B_kst[gi % 2]])
            S.barrier()
        if upto in ("C", "C1"):
            S.final_wait()
            return nc

        NCH = TT // 128
        if "E" in phases:
          with contextlib.ExitStack() as pde:
            wgtT = [sb(pde, "wgtT%d" % d, [128, NCH, 4], F32) for d in range(2)]
            thrT = [sb(pde, "thrT%d" % d, [128, NCH, 4], F32) for d in range(2)]
            decB = [sb(pde, "decB%d" % d, [128, 4, NCH], F32) for d in range(2)]
            B_wgtT, B_thrT, B_decB = [Buf(), Buf()], [Buf(), Buf()], [Buf(), Buf()]
            with contextlib.ExitStack() as pd:
                X1 = sb(pd, "X1", [4, TT], F32)
                X2 = sb(pd, "X2", [4, TT], F32)
                X3 = sb(pd, "X3", [4, TT], F32)
                Z0 = sb(pd, "Z0", [4, TT], F32)
                ngd = sb(pd, "ngd", [4, NCH], F32)
                dec = sb(pd, "dec", [4, NCH], F32)
                sel = sb(pd, "sel", [4, 4, 128], F32)
                ptw = ps(pd, "ptw", [128, 512], F32)
                ptt = ps(pd, "ptt", [128, 512], F32)
                pdc = ps(pd, "pdc", [128, 512], F32)
                B1, B2, B3, BZ, Bng, Bdec, Bsel, Bptw, Bptt, Bpdc = [Buf() for _ in range(10)]
                S.op("pool", lambda e: e.memset(Z0[:], 0.0), writes=[BZ])
                for h in range(4):
                    S.op("dve", lambda e: e.tensor_copy(out=sel[0:4, h, :], in_=ident_f[0:4, h:h + 1].to_broadcast([4, 128])),
                         reads=[B_const], writes=[Bsel])
                segs = [(0, CTX), (CTX, TT)]
                for d in range(2):
                    if d == 0:
                        S.dma(X1[:], gT_d[0:4, :], writes=[B1])
                        S.dma(X2[:], gT_d[4:8, :], writes=[B2])
                    else:
                        S.dma(X3[:], gT_d[8:12, :], writes=[B3])
                        for (a, b_) in segs:
                            S.op("dve", lambda e: e.tensor_copy(out=X1[:, a:b_], in_=X3[:, a:b_][:, ::-1]), reads=[B3], writes=[B1])
                        S.dma(X3[:], gT_d[12:16, :], writes=[B3])
                        for (a, b_) in segs:
                            S.op("dve", lambda e: e.tensor_copy(out=X2[:, a:b_], in_=X3[:, a:b_][:, ::-1]), reads=[B3], writes=[B2])
                    S.op("act", lambda e: e.activation(out=X2[:], in_=X2[:], func=AF.Exp, scale=-1.0), reads=[B2], writes=[B2])
                    S.op("act", lambda e: e.activation(out=X2[:], in_=X2[:], func=AF.Ln, bias=1.0, scale=1.0), reads=[B2], writes=[B2])
                    S.op("dve", lambda e: e.tensor_tensor_scan(out=X3[:], data0=Z0[:], data1=X2[:], initial=0.0, op0=ALU.add, op1=ALU.add),
                         reads=[BZ, B2], writes=[B3])
                    S.op("dve", lambda e: e.tensor_tensor(out=X1[:], in0=X1[:], in1=X3[:], op=ALU.add), reads=[B1, B3], writes=[B1])
                    S.op("dve", lambda e: e.tensor_tensor_scan(out=X2[:], data0=X1[:], data1=X1[:], initial=0.0, op0=ALU.max, op1=ALU.max),
                         reads=[B1], writes=[B2])
                    S.op("dve", lambda e: e.tensor_scalar(out=ngd[:], in0=X2[:, 127:TT:128], scalar1=-1.0, scalar2=None, op0=ALU.mult),
                         reads=[B2], writes=[Bng])
                    S.op("dve", lambda e: e.tensor_copy(out=dec[:, 0:1], in_=ngd[:, 0:1]), reads=[Bng], writes=[Bdec])
                    S.op("dve", lambda e: e.tensor_tensor(out=dec[:, 1:NCH], in0=X2[:, 127:TT - 128:128], in1=ngd[:, 1:NCH], op=ALU.add),
                         reads=[B2, Bng], writes=[Bdec])
                    S.op("act", lambda e: e.activation(out=dec[:], in_=dec[:], func=AF.Exp), reads=[Bdec], writes=[Bdec])
                    for c in range(NCH):
                        sl = slice(c * 128, (c + 1) * 128)
                        S.op("act", lambda e: e.activation(out=X1[:, sl], in_=X1[:, sl], func=AF.Exp, bias=ngd[:, c:c + 1], scale=1.0),
                             reads=[B1, Bng], writes=[B1])
                        S.op("act", lambda e: e.activation(out=X3[:, sl], in_=X3[:, sl], func=AF.Exp, bias=ngd[:, c:c + 1], scale=1.0),
                             reads=[B3, Bng], writes=[B3])
                    if d == 0:
                        Wt, Bw, Tt, Bt = X1, B1, X3, B3
                    else:
                        for (a, b_) in segs:
                            S.op("dve", lambda e: e.tensor_copy(out=X2[:, a:b_], in_=X1[:, a:b_][:, ::-1]), reads=[B1], writes=[B2])
                        for (a, b_) in segs:
                            S.op("dve", lambda e: e.tensor_copy(out=X1[:, a:b_], in_=X3[:, a:b_][:, ::-1]), reads=[B3], writes=[B1])
                        Wt, Bw, Tt, Bt = X2, B2, X1, B1
                    for blk in range(NCH):
                        sl = slice(blk * 128, (blk + 1) * 128)
                        S.op("pe", lambda e: e.matmul(ptw[:, blk * 4:(blk + 1) * 4], lhsT=Wt[0:4, sl], rhs=ident_f[0:4, 0:4], start=True, stop=True),
                             reads=[Bw, B_const], writes=[Bptw], inc=False)
                        S.op("pe", lambda e: e.matmul(ptt[:, blk * 4:(blk + 1) * 4], lhsT=Tt[0:4, sl], rhs=ident_f[0:4, 0:4], start=True, stop=True),
                             reads=[Bt, B_const], writes=[Bptt], inc=(blk == NCH - 1))
                    S.op("dve", lambda e: e.tensor_copy(out=wgtT[d][:].rearrange("p c h -> p (c h)"), in_=ptw[:, 0:NCH * 4]),
                         reads=[Bptw], writes=[B_wgtT[d]])
                    S.op("dve", lambda e: e.tensor_copy(out=thrT[d][:].rearrange("p c h -> p (c h)"), in_=ptt[:, 0:NCH * 4]),
                         reads=[Bptt], writes=[B_thrT[d]])
                    for h in range(4):
                        S.op("pe", lambda e: e.matmul(pdc[:, h * NCH:(h + 1) * NCH], lhsT=sel[0:4, h, :], rhs=dec[0:4, :], start=True, stop=True),
                             reads=[Bsel, Bdec], writes=[Bpdc], inc=(h == 3))
                    S.op("dve", lambda e: e.tensor_copy(out=decB[d][:].rearrange("p h c -> p (h c)"), in_=pdc[:, 0:4 * NCH]),
                         reads=[Bpdc], writes=[B_decB[d]])
                if dbgD_d is not None:
                    pass
                S.barrier()

            with contextlib.ExitStack() as pe_:
                cmask = sb(pe_, "cmask", [128, 3, 128], BF16)
                mlg = sb(pe_, "mlg", [128, 512], F32)
                B_cm, B_mlg = Buf(), Buf()
                S.dma(cmask[:], cmask_d.rearrange("a p q -> p a q"), writes=[B_cm])
                S.dma(mlg[:], mlg_d, writes=[B_mlg])
                QT = [sb(pe_, "QT%d" % i, [128, 4, 128], BF16) for i in range(2)]
                KT = [sb(pe_, "KT%d" % i, [128, 4, 128], BF16) for i in range(2)]
                Kt = [sb(pe_, "Kt%d" % i, [128, 4, 128], BF16) for i in range(2)]
                Vt = [sb(pe_, "Vt%d" % i, [128, 4, 128], BF16) for i in range(2)]
                HF = [sb(pe_, "HF%d" % i, [128, 512], F32) for i in range(2)]
                SO = [sb(pe_, "SO%d" % i, [128, 512], F32) for i in range(2)]
                B_QT, B_KT, B_Kt, B_Vt, B_HF, B_SO = [[Buf(), Buf()] for _ in range(6)]
                vp = [sb(pe_, "vp%d" % i, [128, 4, 129], BF16) for i in range(2)]
                B_vp = [Buf(), Buf()]
                sm = [sb(pe_, "sm%d" % i, [128, 128], BF16) for i in range(4)]
                B_sm = [Buf() for _ in range(4)]
                Cst = sb(pe_, "Cst", [128, 4, 129], F32)
                B_Cst = [Buf() for _ in range(4)]
                Cdb = [sb(pe_, "Cdb%d" % i, [128, 4, 129], BF16) for i in range(2)]
                B_Cdb = [[Buf() for _ in range(4)] for _ in range(2)]
                hst = [sb(pe_, "hst%d" % i, [128, 4, 128], F32) for i in range(2)]
                B_hst = [Buf(), Buf()]
                dn = sb(pe_, "dn", [128, 4], F32)
                B_dn = Buf()
                hsq = sb(pe_, "hsq", [128, 512], F32)
                ss = sb(pe_, "ss", [128, 4], F32)
                go = sb(pe_, "go", [128, 512], F32)
                mlo_t = [sb(pe_, "mlo_t%d" % i, [128, 512], BF16) for i in range(2)]
                B_hsq, B_ss, B_go = Buf(), Buf(), Buf()
                B_mlo = [Buf(), Buf()]
                sps = [ps(pe_, "sps%d" % i, [128, 4, 128], F32) for i in range(2)]
                B_sps = [Buf(), Buf()]
                hps = [ps(pe_, "hps%d" % i, [128, 2, 512], F32) for i in range(2)]
                B_hps = [Buf(), Buf()]
                cps2 = ps(pe_, "cps2", [128, 2, 512], F32)
                B_cps2 = [Buf() for _ in range(4)]

                def blk_of(d, c):
                    if d == 0:
                        return c
                    return (1 - c) if c < 2 else (67 - c)

                nch_run = NCH if upto != "E1" else 5
                for d in range(2):
                    S.op("dve", lambda e: e.memset(Cst[:], 0.0), writes=B_Cst)

                    def load_chunk(c):
                        if c >= nch_run:
                            return
                        blk = blk_of(d, c)
                        b = c % 2
                        rows = slice(blk * 128, (blk + 1) * 128)
                        S.dma(Kt[b][:].rearrange("p h d -> p (h d)"), kml_d[rows, :], writes=[B_Kt[b]])
                        S.dma(Vt[b][:].rearrange("p h d -> p (h d)"), mlv_d[rows, :], writes=[B_Vt[b]])
                        if blk >= 2:
                            S.dma(QT[b][:], qkT_d[0:4, :, rows].rearrange("c p t -> p c t"), writes=[B_QT[b]])
                            S.dma(KT[b][:], qkT_d[4:8, :, rows].rearrange("c p t -> p c t"), writes=[B_KT[b]])
                            if d == 1:
                                S.dma(HF[b][:], hf_d[(blk - 2) * 128:(blk - 1) * 128, :], writes=[B_HF[b]])
                                S.dma(SO[b][:], mlo_d[rows, :], writes=[B_SO[b]])
                    load_chunk(0)
                    for c in range(nch_run):
                        load_chunk(c + 1)
                        blk = blk_of(d, c)
                        b = c % 2
                        lat = blk >= 2
                        vpb = vp[b]
                        S.op("dve", lambda e: e.tensor_tensor(out=vpb[:, :, 0:128], in0=Vt[b][:, :, :],
                                                              in1=wgtT[d][:, blk, :].unsqueeze(2).to_broadcast([128, 4, 128]), op=ALU.mult),
                             reads=[B_Vt[b], B_wgtT[d]], writes=[B_vp[b]])
                        S.op("dve", lambda e: e.tensor_copy(out=vpb[:, :, 128], in_=wgtT[d][:, blk, :]),
                             reads=[B_wgtT[d]], writes=[B_vp[b]])
                        hp = hps[b]
                        cdb = Cdb[b]
                        for h in range(4):
                            S.op("act", lambda e: e.activation(out=cdb[:, h, :], in_=Cst[:, h, :], func=AF.Copy, scale=decB[d][:, h, c:c + 1]),
                                 reads=[B_Cst[h], B_decB[d]], writes=[B_Cdb[b][h]])
                            if lat:
                                S.op("pe", lambda e: e.matmul(sps[b][:, h, :], lhsT=KT[b][:, h, :], rhs=QT[b][:, h, :], start=True, stop=True),
                                     reads=[B_KT[b], B_QT[b]], writes=[B_sps[b]], inc=(h == 3))
                        for h in range(4):
                            co = cps2[:, h // 2, (h % 2) * 129:(h % 2) * 129 + 129]
                            S.op("pe", lambda e: e.matmul(co, lhsT=Kt[b][:, h, :], rhs=vpb[:, h, :], start=True, stop=True),
                                 reads=[B_Kt[b], B_vp[b]], writes=[B_cps2[h]])
                            if lat:
                                S.op("dve", lambda e: e.tensor_tensor(out=sm[h][:, :], in0=sps[b][:, h, :], in1=cmask[:, d, :], op=ALU.mult),
                                     reads=[B_sps[b], B_cm], writes=[B_sm[h]])
                        if lat:
                            for h in range(4):
                                ho = hp[:, h // 2, (h % 2) * 129:(h % 2) * 129 + 129]
                                S.op("pe", lambda e: e.matmul(ho, lhsT=sm[h][:, :], rhs=vpb[:, h, :], start=True, stop=False),
                                     reads=[B_sm[h], B_vp[b]], writes=[B_hps[b]], inc=False)
                                S.op("pe", lambda e: e.matmul(ho, lhsT=QT[b][:, h, :], rhs=cdb[:, h, :], start=False, stop=True),
                                     reads=[B_QT[b], B_Cdb[b][h]], writes=[B_hps[b]], inc=(h == 3))
                        for h in range(4):
                            co = cps2[:, h // 2, (h % 2) * 129:(h % 2) * 129 + 129]
                            S.op("dve", lambda e: e.scalar_tensor_tensor(out=Cst[:, h, :], in0=Cst[:, h, :], scalar=decB[d][:, h, c:c + 1], in1=co,
                                                                         op0=ALU.mult, op1=ALU.add),
                                 reads=[B_Cst[h], B_decB[d], B_cps2[h], B_Cdb[b][h]], writes=[B_Cst[h]])
                        if not lat:
                            continue
                        hv = hp[:, :, 0:258].rearrange("p a (h d) -> p a h d", d=129)
                        S.op("act", lambda e: e.activation(out=dn[:].rearrange("p (a h) -> p a h", a=2), in_=hv[:, :, :, 128], func=AF.Abs),
                             reads=[B_hps[b]], writes=[B_dn])
                        S.op("dve", lambda e: e.tensor_tensor(out=dn[:], in0=dn[:], in1=thrT[d][:, blk, :], op=ALU.max),
                             reads=[B_dn, B_thrT[d]], writes=[B_dn])
                        S.op("dve", lambda e: e.reciprocal(out=dn[:], in_=dn[:]), reads=[B_dn], writes=[B_dn])
                        hs = hst[b]
                        for a in range(2):
                            S.op("dve", lambda e: e.tensor_tensor(out=hs[:, a * 2:(a + 1) * 2, :], in0=hv[:, a, :, 0:128],
                                                                  in1=dn[:, a * 2:(a + 1) * 2].unsqueeze(2).to_broadcast([128, 2, 128]), op=ALU.mult),
                                 reads=[B_hps[b], B_dn], writes=[B_hst[b]])
                        lt = blk - 2
                        if d == 0:
                            S.dma(hf_d[lt * 128:(lt + 1) * 128, :], hs[:].rearrange("p h d -> p (h d)"), reads=[B_hst[b]])
                        else:
                            hsf = hs[:].rearrange("p h d -> p (h d)")
                            S.op("pool", lambda e: e.tensor_tensor(out=hsf, in0=hsf, in1=HF[b][:, :], op=ALU.add),
                                 reads=[B_hst[b], B_HF[b]], writes=[B_hst[b]])
                            S.op("pool", lambda e: e.tensor_tensor(out=hsq[:, :], in0=hsf, in1=hsf, op=ALU.mult),
                                 reads=[B_hst[b]], writes=[B_hsq])
                            S.op("dve", lambda e: e.tensor_reduce(out=ss[:, :], in_=hsq[:].rearrange("p (h d) -> p h d", d=128), axis=AX.X, op=ALU.add),
                                 reads=[B_hsq], writes=[B_ss])
                            S.op("dve", lambda e: e.tensor_scalar(out=ss[:, :], in0=ss[:, :], scalar1=1.0 / 128.0, scalar2=EPS, op0=ALU.mult, op1=ALU.add),
                                 reads=[B_ss], writes=[B_ss])
                            S.op("pool", lambda e: e.tensor_tensor(out=ss[:, :], in0=ss[:, :], in1=neghalf_c[:, 0:1].to_broadcast([128, 4]), op=ALU.pow),
                                 reads=[B_ss, B_const], writes=[B_ss])
                            S.op("pool", lambda e: e.tensor_tensor(out=go[:, :], in0=SO[b][:, :], in1=mlg[:, :], op=ALU.mult),
                                 reads=[B_SO[b], B_mlg], writes=[B_go])
                            S.op("dve", lambda e: e.tensor_tensor(out=hs[:, :, :], in0=hs[:, :, :],
                                                                  in1=ss[:, :].unsqueeze(2).to_broadcast([128, 4, 128]), op=ALU.mult),
                                 reads=[B_hst[b], B_ss], writes=[B_hst[b]])
                            S.op("dve", lambda e: e.tensor_tensor(out=mlo_t[b][:, :], in0=hsf, in1=go[:, :], op=ALU.mult),
                                 reads=[B_hst[b], B_go], writes=[B_mlo[b]])
                            S.dma(ml_d[lt * 128:(lt + 1) * 128, :], mlo_t[b][:, :], reads=[B_mlo[b]])
                    S.barrier()
        if upto in ("E", "E1"):
            S.final_wait()
            return nc

        if "G" in phases:
          bg_step(100000)
          with contextlib.ExitStack() as pg0:
            W12 = sb(pg0, "W12", [128, NT, 2], F32)
            D1i = sb(pg0, "D1i", [128, NT], I32)
            D2i = sb(pg0, "D2i", [128, NT], I32)
            ebrow = sb(pg0, "ebrow", [1, 256], I32)
            idxw = sb(pg0, "idxw", [128, 256], I32)
            B_idxw = Buf()
            lnp = sb(pg0, "lnp", [128, 4, D], F32)
            B_W12, B_D1, B_D2, B_eb, B_lnp = Buf(), Buf(), Buf(), Buf(), Buf()
            S.dma(lnp[:], lnp_d.rearrange("a p f -> p a f"), writes=[B_lnp])

            def layer_norm_stats(x_ap, Bx, st6, mv, rstd, nmr, Bs):
                S.op("dve", lambda e: e.bn_stats(out=st6[:, 0, :], in_=x_ap[:, 0:512]), reads=[Bx], writes=[Bs])
                S.op("dve", lambda e: e.bn_stats(out=st6[:, 1, :], in_=x_ap[:, 512:1024]), reads=[Bx], writes=[Bs])
                S.op("dve", lambda e: e.bn_aggr(out=mv[:], in_=st6[:].rearrange("p a b -> p (a b)")), reads=[Bs], writes=[Bs])
                S.op("dve", lambda e: e.tensor_scalar(out=rstd[:], in0=mv[:, 1:2], scalar1=EPS, scalar2=None, op0=ALU.add), reads=[Bs], writes=[Bs])
                S.op("pool", lambda e: e.tensor_tensor(out=rstd[:], in0=rstd[:], in1=neghalf_c[:], op=ALU.pow), reads=[Bs, B_const], writes=[Bs])
                S.op("dve", lambda e: e.scalar_tensor_tensor(out=nmr[:], in0=mv[:, 0:1], scalar=-1.0, in1=rstd[:], op0=ALU.mult, op1=ALU.mult),
                     reads=[Bs], writes=[Bs])

            with contextlib.ExitStack() as pg:
                wout_sb = sb(pg, "wout_sb", [128, 8, D], BF16)
                wstg = [sb(pg, "wstg%d" % i, [128, D], F32) for i in range(2)]
                B_wout, B_wstg = Buf(), [Buf(), Buf()]
                wout_v = wout_d.rearrange("(k p) n -> p k n", p=128)
                for k in range(8):
                    S.dma(wstg[k % 2][:], wout_v[:, k, :], writes=[B_wstg[k % 2]])
                    S.op("dve", lambda e: e.tensor_tensor(out=wout_sb[:, k, :], in0=wstg[k % 2][:], in1=g_b[:, 0:1024], op=ALU.mult),
                         reads=[B_wstg[k % 2], B_gb], writes=[B_wout])
                wr_sb = sb(pg, "wr_sb", [128, 8, 72], BF16)
                rbias = sb(pg, "rbias", [128, 72], F32)
                SU = sb(pg, "SU", [128, 128], BF16)
                ones_bf = sb(pg, "ones_bf", [128, 128], BF16)
                bvals = sb(pg, "bvals", [128, 3], F32)
                B_wr, B_rb, B_SU, B_bv = Buf(), Buf(), Buf(), Buf()
                S.dma(wr_sb[:], wr_d.rearrange("(k p) n -> p k n", p=128), writes=[B_wr], eng="pool")
                S.dma(rbias[:], rbias_d, writes=[B_rb])
                S.dma(SU[:], cmask_d[2], writes=[B_SU])
                S.dma(bvals[:], bvals_d, writes=[B_bv])
                S.op("dve", lambda e: e.memset(ones_bf[:], 1.0), writes=[B_const])
                M1 = sb(pg, "M1", [128, NT, 64], BF16)
                M2 = sb(pg, "M2", [128, NT, 64], BF16)
                RK = sb(pg, "RK", [128, NT, 64], F32)
                big = sb(pg, "big", [128, NT, 64], F32)
                run = sb(pg, "run", [128, 64], F32)
                B_M1, B_M2, B_RK, B_big, B_run = Buf(), Buf(), Buf(), Buf(), Buf()
                S.op("dve", lambda e: e.memset(run[:], 0.0), writes=[B_run])
                xg = [sb(pg, "xg%d" % i, [128, D], F32) for i in range(2)]
                mix = [sb(pg, "mix%d" % i, [128, D], BF16) for i in range(2)]
                B_xg, B_mix = [Buf(), Buf()], [Buf(), Buf()]
                mixT = sb(pg, "mixT", [128, 8, 128], BF16)
                xm_ = sb(pg, "xm_", [128, D], F32)
                xmid = [sb(pg, "xmid%d" % i, [128, D], F32) for i in range(2)]
                xn2 = sb(pg, "xn2", [128, D], BF16)
                h2T = sb(pg, "h2T", [128, 8, 128], BF16)
                h2 = [sb(pg, "h2_%d" % i, [128, D], BF16) for i in range(2)]
                B_mixT, B_xm, B_xmid, B_xn2, B_h2T, B_h2 = Buf(), Buf(), [Buf(), Buf()], Buf(), Buf(), [Buf(), Buf()]
                st6 = sb(pg, "gst6", [128, 2, 6], F32)
                mv = sb(pg, "gmv", [128, 2], F32)
                rstd = sb(pg, "grstd", [128, 1], F32)
                nmr = sb(pg, "gnmr", [128, 1], F32)
                B_s1 = Buf()
                lg = sb(pg, "lg", [128, 72], F32)
                sm8 = sb(pg, "sm8", [128, 16], F32)
                gm = sb(pg, "gm", [128, 8], F32)
                ge = sb(pg, "ge", [128, 8], F32)
                elm = sb(pg, "elm", [128, 64], F32)
                top8 = sb(pg, "top8", [128, 8], F32)
                m12 = sb(pg, "m12", [128, 64], BF16)
                B_lg, B_sm8, B_gm, B_elm, B_top8, B_m12 = Buf(), Buf(), Buf(), Buf(), Buf(), Buf()
                tpg = [ps(pg, "tpg%d" % i, [128, 1024], BF16) for i in range(2)]
                B_tpg = [Buf(), Buf()]
                ops_ = ps(pg, "ops_", [128, 2, 512], F32)
                B_ops = Buf()
                tpb = ps(pg, "tpb", [128, 1024], BF16)
                B_tpb = Buf()
                lps = ps(pg, "lps", [128, 512], F32)
                B_lps = Buf()
                rkps = ps(pg, "rkps", [128, 512], F32)
                B_rkps = Buf()

                nt_run = NT if upto not in ("G1",) else 2

                def load_tile(i):
                    if i >= nt_run:
                        return
                    rows = slice(i * 128, (i + 1) * 128)
                    S.dma(xg[i % 2][:], x_d[rows, :], writes=[B_xg[i % 2]])
                    S.dma(mix[i % 2][:, 0:512], na_d[rows, :], writes=[B_mix[i % 2]])
                    S.dma(mix[i % 2][:, 512:1024], ml_d[rows, :], writes=[B_mix[i % 2]])
                load_tile(0)
                for i in range(nt_run):
                    load_tile(i + 1)
                    rows = slice(i * 128, (i + 1) * 128)
                    mx = mix[i % 2]
                    tp_ = tpg[0]
                    for k in range(8):
                        S.op("pe", lambda e: e.transpose(out=tp_[:, k * 128:(k + 1) * 128], in_=mx[:, k * 128:(k + 1) * 128], identity=ident_bf[:]),
                             reads=[B_mix[i % 2], B_const], writes=[B_tpg[0]], inc=(k == 7))
                    S.op("act", lambda e: e.activation(out=mixT[:, 0:4, :], in_=tp_[:, 0:512].rearrange("p (k t) -> p k t", t=128), func=AF.Copy),
                         reads=[B_tpg[0]], writes=[B_mixT])
                    S.op("dve", lambda e: e.tensor_copy(out=mixT[:, 4:8, :], in_=tp_[:, 512:1024].rearrange("p (k t) -> p k t", t=128)),
                         reads=[B_tpg[0]], writes=[B_mixT])
                    for n in range(2):
                        for k in range(8):
                            S.op("pe", lambda e: e.matmul(ops_[:, n, :], lhsT=mixT[:, k, :], rhs=wout_sb[:, k, n * 512:(n + 1) * 512],
                                                          start=(k == 0), stop=(k == 7)),
                                 reads=[B_mixT, B_wout], writes=[B_ops], inc=(k == 7 and n == 1))
                    S.op("dve", lambda e: e.scalar_tensor_tensor(out=xm_[:].rearrange("p (n f) -> p n f", n=2), in0=xg[i % 2][:].rearrange("p (n f) -> p n f", n=2),
                                                                 scalar=ALPHA, in1=ops_[:, :, :], op0=ALU.mult, op1=ALU.add),
                         reads=[B_xg[i % 2], B_ops], writes=[B_xm])
                    layer_norm_stats(xm_, B_xm, st6, mv, rstd, nmr, B_s1)
                    xmd = xmid[i % 2]
                    S.op("act", lambda e: e.activation(out=xmd[:], in_=xm_[:], func=AF.Identity, bias=nmr[:, 0:1], scale=rstd[:, 0:1]),
                         reads=[B_xm, B_s1], writes=[B_xmid[i % 2]])
                    S.op("pool", lambda e: e.tensor_tensor(out=xmd[:], in0=xmd[:], in1=lnp[:, 0, :], op=ALU.mult),
                         reads=[B_xmid[i % 2], B_lnp], writes=[B_xmid[i % 2]])
                    S.op("dve", lambda e: e.tensor_tensor(out=xmd[:], in0=xmd[:], in1=lnp[:, 1, :], op=ALU.add),
                         reads=[B_xmid[i % 2], B_lnp], writes=[B_xmid[i % 2]])
                    S.dma(xmid_d[rows, :], xmd[:], reads=[B_xmid[i % 2]])
                    layer_norm_stats(xmd, B_xmid[i % 2], st6, mv, rstd, nmr, B_s1)
                    S.op("act", lambda e: e.activation(out=xn2[:], in_=xmd[:], func=AF.Identity, bias=nmr[:, 0:1], scale=rstd[:, 0:1]),
                         reads=[B_xmid[i % 2], B_s1], writes=[B_xn2])
                    tp2 = tpg[1]
                    for k in range(8):
                        S.op("pe", lambda e: e.transpose(out=tp2[:, k * 128:(k + 1) * 128], in_=xn2[:, k * 128:(k + 1) * 128], identity=ident_bf[:]),
                             reads=[B_xn2, B_const], writes=[B_tpg[1]], inc=(k == 7))
                    for k in range(8):
                        o = h2T[:, k, :]
                        i_ = tp2[:, k * 128:(k + 1) * 128]
                        sc = adaT[:, 32 + k, 0:1]
                        sh = adaT[:, 24 + k, 0:1]
                        if k % 2 == 0:
                            S.op("act", lambda e: e.activation(out=o, in_=i_, func=AF.Identity, bias=sh, scale=sc),
                                 reads=[B_tpg[1], B_ada], writes=[B_h2T])
                        else:
                            S.op("dve", lambda e: e.tensor_scalar(out=o, in0=i_, scalar1=sc, scalar2=sh, op0=ALU.mult, op1=ALU.add),
                                 reads=[B_tpg[1], B_ada], writes=[B_h2T])
                    for k in range(8):
                        S.op("pe", lambda e: e.transpose(out=tpb[:, k * 128:(k + 1) * 128], in_=h2T[:, k, :], identity=ident_bf[:]),
                             reads=[B_h2T, B_const], writes=[B_tpb], inc=(k == 7))
                    h2b = h2[i % 2]
                    S.op("act", lambda e: e.activation(out=h2b[:, 0:512], in_=tpb[:, 0:512], func=AF.Copy), reads=[B_tpb], writes=[B_h2[i % 2]])
                    S.op("dve", lambda e: e.tensor_copy(out=h2b[:, 512:1024], in_=tpb[:, 512:1024]), reads=[B_tpb], writes=[B_h2[i % 2]])
                    S.dma(h2_d[rows, :], h2b[:], reads=[B_h2[i % 2]])
                    for k in range(8):
                        S.op("pe", lambda e: e.matmul(lps[:, 0:72], lhsT=h2T[:, k, :], rhs=wr_sb[:, k, :], start=(k == 0), stop=(k == 7)),
                             reads=[B_h2T, B_wr], writes=[B_lps], inc=(k == 7))
                    S.op("dve", lambda e: e.tensor_tensor(out=lg[:], in0=lps[:, 0:72], in1=rbias[:], op=ALU.add), reads=[B_lps, B_rb], writes=[B_lg])
                    S.op("dve", lambda e: e.reduce_max(out=sm8[:, 0:1], in_=lg[:, 0:8], axis=AX.X), reads=[B_lg], writes=[B_sm8])
                    S.op("dve", lambda e: e.tensor_scalar(out=sm8[:, 1:2], in0=sm8[:, 0:1], scalar1=-1.0, scalar2=None, op0=ALU.mult),
                         reads=[B_sm8], writes=[B_sm8])
                    S.op("act", lambda e: e.activation(out=ge[:], in_=lg[:, 0:8], func=AF.Exp, bias=sm8[:, 1:2], scale=1.0, accum_out=sm8[:, 2:3]),
                         reads=[B_lg, B_sm8], writes=[B_sm8, B_gm])
                    S.op("dve", lambda e: e.tensor_scalar(out=gm[:], in0=lg[:, 0:8], scalar1=sm8[:, 0:1], scalar2=None, op0=ALU.is_ge),
                         reads=[B_lg, B_sm8, B_gm], writes=[B_gm])
                    S.op("dve", lambda e: e.tensor_scalar(out=gm[:], in0=gm[:], scalar1=1e9, scalar2=-1e9, op0=ALU.mult, op1=ALU.add),
                         reads=[B_gm], writes=[B_gm])
                    S.op("dve", lambda e: e.tensor_tensor(out=elm[:].rearrange("p (g e) -> p g e", e=8), in0=lg[:, 8:72].rearrange("p (g e) -> p g e", e=8),
                                                          in1=gm[:, :].unsqueeze(2).to_broadcast([128, 8, 8]), op=ALU.add),
                         reads=[B_lg, B_gm], writes=[B_elm])
                    S.op("dve", lambda e: e.max(out=top8[:], in_=elm[:]), reads=[B_elm], writes=[B_top8])
                    S.op("dve", lambda e: e.tensor_scalar(out=M1[:, i, :], in0=elm[:], scalar1=top8[:, 0:1], scalar2=None, op0=ALU.is_ge),
                         reads=[B_elm, B_top8], writes=[B_M1])
                    S.op("dve", lambda e: e.tensor_scalar(out=m12[:], in0=elm[:], scalar1=top8[:, 1:2], scalar2=None, op0=ALU.is_ge),
                         reads=[B_elm, B_top8], writes=[B_m12])
                    S.op("dve", lambda e: e.tensor_tensor(out=M2[:, i, :], in0=m12[:], in1=M1[:, i, :], op=ALU.subtract),
                         reads=[B_m12, B_M1], writes=[B_M2])
                    S.op("dve", lambda e: e.tensor_tensor(out=sm8[:, 3:4], in0=top8[:, 1:2], in1=top8[:, 0:1], op=ALU.subtract),
                         reads=[B_top8, B_sm8], writes=[B_sm8])
                    S.op("act", lambda e: e.activation(out=sm8[:, 4:5], in_=sm8[:, 3:4], func=AF.Exp), reads=[B_sm8], writes=[B_sm8])
                    S.op("dve", lambda e: e.tensor_scalar(out=sm8[:, 5:6], in0=sm8[:, 4:5], scalar1=1.0, scalar2=sm8[:, 2:3], op0=ALU.add, op1=ALU.mult),
                         reads=[B_sm8], writes=[B_sm8])
                    S.op("dve", lambda e: e.reciprocal(out=W12[:, i, 0:1], in_=sm8[:, 5:6]), reads=[B_sm8], writes=[B_W12])
                    S.op("dve", lambda e: e.tensor_tensor(out=W12[:, i, 1:2], in0=W12[:, i, 0:1], in1=sm8[:, 4:5], op=ALU.mult),
                         reads=[B_sm8, B_W12], writes=[B_W12])
                    S.op("pe", lambda e: e.matmul(rkps[:, 0:64], lhsT=SU[:, :], rhs=m12[:, :], start=True, stop=True),
                         reads=[B_SU, B_m12], writes=[B_rkps], inc=False)
                    S.op("pe", lambda e: e.matmul(rkps[:, 64:128], lhsT=ones_bf[:, :], rhs=m12[:, :], start=True, stop=True),
                         reads=[B_const, B_m12], writes=[B_rkps])
                    S.op("dve", lambda e: e.tensor_tensor(out=RK[:, i, :], in0=rkps[:, 0:64], in1=run[:], op=ALU.add),
                         reads=[B_rkps, B_run], writes=[B_RK])
                    S.op("dve", lambda e: e.tensor_tensor(out=run[:], in0=rkps[:, 64:128], in1=run[:], op=ALU.add),
                         reads=[B_rkps, B_run, B_RK], writes=[B_run])

                szi = sb(pg, "szi", [128, 64], I32)
                pad = sb(pg, "pad", [128, 64], F32)
                cum = sb(pg, "cum", [128, 64], F32)
                pst = sb(pg, "pst", [128, 64], F32)
                z64 = sb(pg, "z64", [128, 64], F32)
                cmpb = sb(pg, "cmpb", [128, 64], F32)
                ebf = sb(pg, "ebf", [128, 2], F32)
                ebr = sb(pg, "ebr", [1, 256], F32)
                B_bk = Buf()
                S.op("dve", lambda e: e.memset(z64[:], 0.0), writes=[B_bk])
                S.op("dve", lambda e: e.tensor_scalar(out=szi[:], in0=run[:], scalar1=127.0, scalar2=None, op0=ALU.add), reads=[B_run], writes=[B_bk])
                S.op("dve", lambda e: e.tensor_scalar(out=szi[:], in0=szi[:], scalar1=7, scalar2=None, op0=ALU.arith_shift_right), reads=[B_bk], writes=[B_bk])
                S.op("dve", lambda e: e.tensor_scalar(out=szi[:], in0=szi[:], scalar1=7, scalar2=None, op0=ALU.logical_shift_left), reads=[B_bk], writes=[B_bk])
                S.op("dve", lambda e: e.tensor_copy(out=pad[:], in_=szi[:]), reads=[B_bk], writes=[B_bk])
                S.op("dve", lambda e: e.tensor_tensor_scan(out=cum[:], data0=z64[:], data1=pad[:], initial=0.0, op0=ALU.add, op1=ALU.add),
                     reads=[B_bk], writes=[B_bk])
                S.op("dve", lambda e: e.tensor_tensor(out=pst[:], in0=cum[:], in1=pad[:], op=ALU.subtract), reads=[B_bk], writes=[B_bk])
                for j in range(2):
                    S.op("dve", lambda e: e.tensor_scalar(out=cmpb[:], in0=cum[:], scalar1=bvals[:, j:j + 1], scalar2=None, op0=ALU.is_le),
                         reads=[B_bk, B_bv], writes=[B_bk])
                    S.op("dve", lambda e: e.reduce_sum(out=ebf[:, j:j + 1], in_=cmpb[:], axis=AX.X), reads=[B_bk], writes=[B_bk])
                S.op("dve", lambda e: e.tensor_scalar(out=ebf[:], in0=ebf[:], scalar1=63.0, scalar2=None, op0=ALU.min), reads=[B_bk], writes=[B_bk])
                for j in range(2):
                    S.op("pe", lambda e: e.matmul(lps[0:1, j * 128:(j + 1) * 128], lhsT=ebf[:, j:j + 1], rhs=ident_f[:, :], start=True, stop=True),
                         reads=[B_bk, B_const], writes=[B_lps], inc=(j == 1))
                S.op("dve", lambda e: e.tensor_copy(out=ebr[0:1, :], in_=lps[0:1, 0:256]), reads=[B_lps], writes=[B_bk])
                S.op("dve", lambda e: e.tensor_copy(out=ebrow[0:1, :], in_=ebr[0:1, :]), reads=[B_bk], writes=[B_eb])
                idr = sb(pg, "idr", [1, 256], F32)
                samer = sb(pg, "samer", [1, 256], F32)
                S.op("dve", lambda e: e.memset(samer[0:1, :], 0.0), writes=[B_bk])
                S.op("dve", lambda e: e.tensor_tensor(out=samer[0:1, 2:256], in0=ebr[0:1, 2:256], in1=ebr[0:1, 0:254], op=ALU.is_equal),
                     reads=[B_bk], writes=[B_bk])
                S.op("dve", lambda e: e.tensor_scalar(out=idr[0:1, :], in0=ebr[0:1, :], scalar1=128.0, scalar2=None, op0=ALU.mult),
                     reads=[B_bk], writes=[B_bk])
                S.op("dve", lambda e: e.scalar_tensor_tensor(out=idr[0:1, :], in0=samer[0:1, :], scalar=1048576.0, in1=idr[0:1, :],
                                                             op0=ALU.mult, op1=ALU.add), reads=[B_bk], writes=[B_bk])
                S.op("pe", lambda e: e.matmul(lps[:, 0:256], lhsT=ones_f[0:1, :], rhs=idr[0:1, :], start=True, stop=True),
                     reads=[B_bk, B_const], writes=[B_lps])
                idxf = sb(pg, "idxf", [128, 256], F32)
                S.op("dve", lambda e: e.tensor_scalar(out=idxf[:], in0=lps[:, 0:256], scalar1=bvals[:, 2:3], scalar2=None, op0=ALU.add),
                     reads=[B_lps, B_bv], writes=[B_bk])
                S.op("dve", lambda e: e.tensor_copy(out=idxw[:], in_=idxf[:]), reads=[B_bk], writes=[B_idxw])
                S.op("dve", lambda e: e.tensor_tensor(out=RK[:], in0=RK[:], in1=pst[:, :].unsqueeze(1).to_broadcast([128, NT, 64]), op=ALU.add),
                     reads=[B_RK, B_bk], writes=[B_RK])
                dsf = sb(pg, "dsf", [128, NT], F32)
                for (Mx, Bm, Dx, Bd) in ((M1, B_M1, D1i, B_D1), (M2, B_M2, D2i, B_D2)):
                    S.op("dve", lambda e: e.tensor_tensor(out=big[:], in0=RK[:], in1=Mx[:], op=ALU.mult), reads=[B_RK, Bm], writes=[B_big])
                    S.op("dve", lambda e: e.tensor_reduce(out=dsf[:], in_=big[:], axis=AX.X, op=ALU.add), reads=[B_big], writes=[B_bk])
                    S.op("dve", lambda e: e.tensor_copy(out=Dx[:], in_=dsf[:]), reads=[B_bk], writes=[Bd])
                if rt_dbg is not None:
                    S.op("dve", lambda e: e.tensor_copy(out=big[:, :, 0:1].rearrange("p t o -> p (t o)"), in_=D1i[:]), reads=[B_D1], writes=[B_big])
                    S.op("dve", lambda e: e.tensor_copy(out=big[:, :, 1:2].rearrange("p t o -> p (t o)"), in_=D2i[:]), reads=[B_D2], writes=[B_big])
                    S.op("dve", lambda e: e.tensor_copy(out=big[:, :, 2:4], in_=W12[:]), reads=[B_W12], writes=[B_big])
                    S.dma(rt_dbg, big[:, :, 0:4], reads=[B_big])
                    S.dma(eb_dbg, ebrow[:], reads=[B_eb])
                    if ix_dbg is not None:
                        S.dma(ix_dbg, idxw[:], reads=[B_idxw])
                for i in range(nt_run):
                    rows = slice(i * 128, (i + 1) * 128)
                    hb = h2[i % 2]
                    S.dma(hb[:], h2_d[rows, :], writes=[B_h2[i % 2]])
                    for (Dx, Bd) in ((D1i, B_D1), (D2i, B_D2)):
                        S.dma(None, None, reads=[B_h2[i % 2], Bd], eng="pool",
                              indirect=lambda e: e.indirect_dma_start(out=xperm_d[:, :], out_offset=bass.IndirectOffsetOnAxis(ap=Dx[:, i:i + 1], axis=0),
                                                                      in_=hb[:, :], in_offset=None))
                S.barrier()
            if upto in ("G", "G1"):
                S.final_wait()
                return nc

            with contextlib.ExitStack() as ph:
                w1b = [sb(ph, "w1b%d" % i, [128, 8, HID], BF16) for i in range(2)]
                w3b = [sb(ph, "w3b%d" % i, [128, 8, HID], BF16) for i in range(2)]
                w2b = [sb(ph, "w2b%d" % i, [128, 4, D], BF16) for i in range(2)]
                B_wb = [[Buf(), Buf(), Buf()] for _ in range(2)]
                xb = [sb(ph, "xb%d" % i, [128, D], BF16) for i in range(3)]
                B_xb = [Buf(), Buf(), Buf()]
                xbT = [sb(ph, "xbT%d" % i, [128, 8, 128], BF16) for i in range(2)]
                sa = [sb(ph, "sa%d" % i, [128, HID], F32) for i in range(2)]
                hh = [sb(ph, "hh%d" % i, [128, HID], BF16) for i in range(2)]
                hhT = [sb(ph, "hhT%d" % i, [128, 4, 128], BF16) for i in range(2)]
                yb = [sb(ph, "yb%d" % i, [128, D], F32) for i in range(2)]
                B_xbT, B_sa, B_hh, B_hhT, B_yb = [[Buf(), Buf()] for _ in range(5)]
                tpp = [ps(ph, "tpp%d" % i, [128, 1024], BF16) for i in range(2)]
                B_tpp = [Buf(), Buf()]
                agp = [ps(ph, "agp%d" % i, [128, 2, 512], F32) for i in range(2)]
                B_agp = [Buf(), Buf()]
                yps = ps(ph, "yps", [128, 2, 512], F32)
                B_yps = Buf()
                nb_run = NBLK if upto != "H1" else 4
                tpc = [0]
                bnd_reg = nc.gpsimd.to_reg(NE * 128 - 1)

                def load_w(b, which):
                    if b >= nb_run or b < 0:
                        return
                    q = b % 2
                    for m, wt_ in enumerate((w1b[q], w3b[q], w2b[q])):
                        if m not in which:
                            continue
                        S.dma(None, None, reads=[B_idxw], writes=[B_wb[q][m]], eng="pool",
                              indirect=lambda e: e.indirect_dma_start(out=wt_[:].rearrange("p k n -> p (k n)"), out_offset=None, in_=wbf_d[m][:, :],
                                                                      in_offset=bass.IndirectOffsetOnAxis(ap=idxw[:, b:b + 1], axis=0),
                                                                      bounds_check=bnd_reg, oob_is_err=False))

                def load_x(b):
                    if b >= nb_run:
                        return
                    S.dma(xb[b % 3][:], xperm_d[b * 128:(b + 1) * 128, :], writes=[B_xb[b % 3]])

                def st_T(b):
                    q = b % 2
                    t_ = tpc[0] % 2
                    tpc[0] += 1
                    for k in range(8):
                        S.op("pe", lambda e: e.transpose(out=tpp[t_][:, k * 128:(k + 1) * 128], in_=xb[b % 3][:, k * 128:(k + 1) * 128], identity=ident_bf[:]),
                             reads=[B_xb[b % 3], B_const], writes=[B_tpp[t_]], inc=(k == 7))
                    S.op("act", lambda e: e.activation(out=xbT[q][:, 0:4, :], in_=tpp[t_][:, 0:512].rearrange("p (k t) -> p k t", t=128), func=AF.Copy),
                         reads=[B_tpp[t_]], writes=[B_xbT[q]])
                    S.op("dve", lambda e: e.tensor_copy(out=xbT[q][:, 4:8, :], in_=tpp[t_][:, 512:1024].rearrange("p (k t) -> p k t", t=128)),
                         reads=[B_tpp[t_]], writes=[B_xbT[q]])

                def st_mm1(b):
                    q = b % 2
                    for k in range(8):
                        S.op("pe", lambda e: e.matmul(agp[q][:, 0, :], lhsT=xbT[q][:, k, :], rhs=w1b[q][:, k, :], start=(k == 0), stop=(k == 7)),
                             reads=[B_xbT[q], B_wb[q][0]], writes=[B_agp[q]], inc=False)
                    for k in range(8):
                        S.op("pe", lambda e: e.matmul(agp[q][:, 1, :], lhsT=xbT[q][:, k, :], rhs=w3b[q][:, k, :], start=(k == 0), stop=(k == 7)),
                             reads=[B_xbT[q], B_wb[q][1]], writes=[B_agp[q]], inc=(k == 7))
                    S.op("act", lambda e: e.activation(out=sa[q][:], in_=agp[q][:, 0, :], func=AF.Silu), reads=[B_agp[q]], writes=[B_sa[q]])
                    S.op("dve", lambda e: e.tensor_tensor(out=hh[q][:], in0=sa[q][:], in1=agp[q][:, 1, :], op=ALU.mult),
                         reads=[B_sa[q], B_agp[q]], writes=[B_hh[q]])

                def st_Th(b):
                    q = b % 2
                    t_ = tpc[0] % 2
                    tpc[0] += 1
                    for k in range(4):
                        S.op("pe", lambda e: e.transpose(out=tpp[t_][:, k * 128:(k + 1) * 128], in_=hh[q][:, k * 128:(k + 1) * 128], identity=ident_bf[:]),
                             reads=[B_hh[q], B_const], writes=[B_tpp[t_]], inc=(k == 3))
                    S.op("act", lambda e: e.activation(out=hhT[q][:, :, :], in_=tpp[t_][:, 0:512].rearrange("p (k t) -> p k t", t=128), func=AF.Copy),
                         reads=[B_tpp[t_]], writes=[B_hhT[q]])

                def st_mm2(b):
                    q = b % 2
                    for n in range(2):
                        for k in range(4):
                            S.op("pe", lambda e: e.matmul(yps[:, n, :], lhsT=hhT[q][:, k, :], rhs=w2b[q][:, k, n * 512:(n + 1) * 512],
                                                          start=(k == 0), stop=(k == 3)),
                                 reads=[B_hhT[q], B_wb[q][2]], writes=[B_yps], inc=(k == 3 and n == 1))
                    S.op("act", lambda e: e.activation(out=yb[q][:, 0:512], in_=yps[:, 0, :], func=AF.Copy), reads=[B_yps], writes=[B_yb[q]])
                    S.op("dve", lambda e: e.tensor_copy(out=yb[q][:, 512:1024], in_=yps[:, 1, :]), reads=[B_yps], writes=[B_yb[q]])
                    S.dma(yperm_d[b * 128:(b + 1) * 128, :], yb[q][:], reads=[B_yb[q]])

                load_w(0, (0, 1))
                load_w(1, (0, 1))
                load_w(0, (2,))
                load_x(0)
                load_x(1)
                load_x(2)
                st_T(0)
                for t in range(nb_run + 1):
                    load_x(t + 3)
                    if t >= 1:
                        st_Th(t - 1)
                    if t + 1 < nb_run:
                        st_T(t + 1)
                    if t < nb_run:
                        st_mm1(t)
                        load_w(t + 2, (0, 1))
                    if t >= 1:
                        st_mm2(t - 1)
                    load_w(t + 1, (2,))
                S.barrier()
            if upto in ("H", "H1"):
                S.final_wait()
                return nc

            with contextlib.ExitStack() as pi:
                y1 = [sb(pi, "y1_%d" % i, [128, D], F32) for i in range(2)]
                y2 = [sb(pi, "y2_%d" % i, [128, D], F32) for i in range(2)]
                xmt = [sb(pi, "xmt%d" % i, [128, D], F32) for i in range(2)]
                B_y1, B_y2, B_xmt = [Buf(), Buf()], [Buf(), Buf()], [Buf(), Buf()]
                acc = sb(pi, "acc", [128, D], F32)
                ot = [sb(pi, "ot%d" % i, [128, D], F32) for i in range(2)]
                B_acc, B_ot = Buf(), [Buf(), Buf()]
                st6 = sb(pi, "ist6", [128, 2, 6], F32)
                mv = sb(pi, "imv", [128, 2], F32)
                rstd = sb(pi, "irstd", [128, 1], F32)
                nmr = sb(pi, "inmr", [128, 1], F32)
                B_s2 = Buf()

                def load_i(i):
                    if i >= NT:
                        return
                    q = i % 2
                    S.dma(xmt[q][:], xmid_d[i * 128:(i + 1) * 128, :], writes=[B_xmt[q]])
                    for (yt, By, Dx, Bd) in ((y1[q], B_y1[q], D1i, B_D1), (y2[q], B_y2[q], D2i, B_D2)):
                        S.dma(None, None, reads=[Bd], writes=[By], eng="pool",
                              indirect=lambda e: e.indirect_dma_start(out=yt[:, :], out_offset=None, in_=yperm_d[:, :],
                                                                      in_offset=bass.IndirectOffsetOnAxis(ap=Dx[:, i:i + 1], axis=0)))
                load_i(0)
                for i in range(NT):
                    load_i(i + 1)
                    q = i % 2
                    S.op("dve", lambda e: e.tensor_scalar(out=acc[:], in0=y1[q][:], scalar1=W12[:, i, 0:1], scalar2=None, op0=ALU.mult),
                         reads=[B_y1[q], B_W12], writes=[B_acc])
                    S.op("dve", lambda e: e.scalar_tensor_tensor(out=acc[:], in0=y2[q][:], scalar=W12[:, i, 1:2], in1=acc[:], op0=ALU.mult, op1=ALU.add),
                         reads=[B_y2[q], B_W12, B_acc], writes=[B_acc])
                    S.op("pool", lambda e: e.tensor_tensor(out=acc[:], in0=acc[:], in1=g_b[:, 1024:2048], op=ALU.mult),
                         reads=[B_acc, B_gb], writes=[B_acc])
                    S.op("dve", lambda e: e.scalar_tensor_tensor(out=acc[:], in0=xmt[q][:], scalar=ALPHA, in1=acc[:], op0=ALU.mult, op1=ALU.add),
                         reads=[B_xmt[q], B_acc], writes=[B_acc])
                    layer_norm_stats(acc, B_acc, st6, mv, rstd, nmr, B_s2)
                    S.op("act", lambda e: e.activation(out=ot[q][:], in_=acc[:], func=AF.Identity, bias=nmr[:, 0:1], scale=rstd[:, 0:1]),
                         reads=[B_acc, B_s2], writes=[B_ot[q]])
                    S.op("pool", lambda e: e.tensor_tensor(out=ot[q][:], in0=ot[q][:], in1=lnp[:, 2, :], op=ALU.mult),
                         reads=[B_ot[q], B_lnp], writes=[B_ot[q]])
                    S.op("dve", lambda e: e.tensor_tensor(out=ot[q][:], in0=ot[q][:], in1=lnp[:, 3, :], op=ALU.add),
                         reads=[B_ot[q], B_lnp], writes=[B_ot[q]])
                    S.dma(out_d[i * 128:(i + 1) * 128, :], ot[q][:], reads=[B_ot[q]])
                S.barrier()

        S.final_wait()
    return nc


def make_in_maps(inputs, n_cores=8):
    x = np.asarray(inputs["x"], np.float32)
    c = np.asarray(inputs["c"], np.float32)
    ctx = np.asarray(inputs["ctx"], np.float32)
    c_ctx = np.asarray(inputs["c_ctx"], np.float32)
    w_ada = np.ascontiguousarray(np.asarray(inputs["w_ada"], np.float32)[0])
    b_ada = np.asarray(inputs["b_ada"], np.float32)[0]
    w_in = np.ascontiguousarray(np.asarray(inputs["w_in"], np.float32)[0])
    gate_b = np.asarray(inputs["gate_b"], np.float32)[0]
    b_ada_l = np.ascontiguousarray(np.repeat(b_ada.reshape(48, 128).T[:, :, None], 2, axis=2))
    rope = make_rope_tables()
    rperm = make_rperm()
    sidx = np.arange(128)
    cmask = np.stack([(sidx[None, :] >= sidx[:, None]), (sidx[:, None] >= sidx[None, :]), (sidx[None, :] > sidx[:, None])]).astype(np.float32).astype(ml_dtypes.bfloat16)
    conv_w = np.asarray(inputs["conv_w"], np.float32)[0]
    conv_b = np.asarray(inputs["conv_b"], np.float32)[0]
    convw_l = np.ascontiguousarray(conv_w.reshape(5, 8, 128).transpose(2, 1, 0))
    convb_l = np.ascontiguousarray(conv_b.reshape(8, 128).T)
    mlg = np.ascontiguousarray(np.broadcast_to(np.asarray(inputs["ml_norm_g"], np.float32)[0][None, :], (128, 512)))
    w_out = np.ascontiguousarray(np.asarray(inputs["w_out"], np.float32)[0])
    w_r = np.ascontiguousarray(np.concatenate([np.asarray(inputs["w_router_g"], np.float32)[0],
                                               np.asarray(inputs["w_router_e"], np.float32)[0]], axis=1))
    rb = np.concatenate([np.asarray(inputs["b_router_g"], np.float32)[0], np.asarray(inputs["b_router_e"], np.float32)[0]])
    rbias = np.ascontiguousarray(np.broadcast_to(rb[None, :], (128, 72)))
    lnp = np.stack([np.broadcast_to(np.asarray(inputs[k], np.float32)[0][None, :], (128, D))
                    for k in ("ln1_g", "ln1_b", "ln2_g", "ln2_b")]).astype(np.float32)
    bvals = np.concatenate([128.0 * (np.arange(128)[:, None] + 128 * np.arange(2)[None, :]), np.arange(128)[:, None]], axis=1).astype(np.float32)
    relay = lambda w, kc, n: np.ascontiguousarray(np.asarray(w, np.float32)[0].reshape(NE, kc, 128, n).transpose(0, 2, 1, 3).reshape(NE * 128, kc * n))
    w1 = relay(inputs["w1"], 8, HID)
    w3 = relay(inputs["w3"], 8, HID)
    w2 = relay(inputs["w2"], 4, D)
    biasT = make_bias_tables(np.asarray(inputs["rpb"], np.float32)[0])
    maps = []
    for b in range(n_cores):
        cc = np.stack([c[b].reshape(8, 128).T, c_ctx.reshape(8, 128).T], axis=-1)
        maps.append({
            "x": np.ascontiguousarray(x[b]),
            "ctx": np.ascontiguousarray(ctx[b]),
            "cc": np.ascontiguousarray(cc),
            "w_ada": w_ada,
            "b_ada_l": b_ada_l,
            "b_ada_r": np.ascontiguousarray(b_ada.reshape(1, -1)),
            "w_in": w_in,
            "gate_b": np.ascontiguousarray(gate_b.reshape(16, 1)),
            "biasT": biasT,
            "w_out": w_out, "w_r": w_r, "rbias": rbias, "lnp": np.ascontiguousarray(lnp), "bvals": bvals,
            "w1": w1, "w3": w3, "w2": w2,
            "rope": rope, "rperm": rperm, "cmask": cmask, "convw": convw_l, "convb": convb_l, "mlg": mlg,
        })
    return maps


def make_bias_tables(rpb):
    rpb = np.asarray(rpb, np.float32)
    out = np.empty((5, 128, 8, 5, 128), np.float32)
    q = np.arange(128)
    k = np.arange(128)
    for v, i in enumerate((2, 0, 1, 62, 63)):
        jb0 = min(max(i - 2, 0), 59)
        qr = 2 * i + q // 64
        qc = q % 64
        r0 = np.clip(qr - 4, 0, 120)
        c0 = np.clip(qc - 8, 0, 48)
        for n in range(5):
            kr = 2 * (jb0 + n) + k // 64
            kc = k % 64
            inside = ((kr[:, None] >= r0[None, :]) & (kr[:, None] < r0[None, :] + 8) &
                      (kc[:, None] >= c0[None, :]) & (kc[:, None] < c0[None, :] + 16))
            ri = np.clip(kr[:, None] - qr[None, :] + 7, 0, 14)
            ci = np.clip(kc[:, None] - qc[None, :] + 15, 0, 30)
            g = rpb[:, ri, ci]
            out[v, :, :, n, :] = np.where(inside[None], g, np.float32(NEG)).transpose(1, 0, 2)
    return out.reshape(5, 128, 8 * 5 * 128).astype(ml_dtypes.bfloat16)


def make_rope_tables():
    t = np.arange(T)
    row = (t // GW).astype(np.float64)
    col = (t % GW).astype(np.float64)
    f = np.arange(128)
    inv = 10000.0 ** (-(f % 32).astype(np.float64) / 32.0)
    pos = np.where((f // 64 == 0)[:, None], row[None, :], col[None, :])
    ang = (pos.astype(np.float32) * inv.astype(np.float32)[:, None]).astype(np.float32)
    cs, sn = np.cos(ang).astype(np.float32), np.sin(ang).astype(np.float32)
    ksc = np.float32(128.0 ** -0.5)
    return np.stack([cs, sn, cs * ksc, sn * ksc]).astype(np.float32)


def make_rperm():
    r = np.zeros((128, 128), np.float32)
    for f in range(128):
        if f % 64 < 32:
            r[f + 32, f] = -1.0
        else:
            r[f - 32, f] = 1.0
    return r.astype(ml_dtypes.bfloat16)


def kernel(**inputs):
    nc = build_program()
    maps = make_in_maps(inputs)
    res = run_bass_kernel_spmd(nc, maps, core_ids=list(range(8)))
    out = np.stack([np.asarray(r["out"]) for r in res.results], axis=0)
    return out.astype(np.float32)
```

```python
import contextlib
import numpy as np
import ml_dtypes
import concourse.bass as bass
import concourse.mybir as mybir
from concourse.bass_utils import run_bass_kernel_spmd

F32 = mybir.dt.float32
BF16 = mybir.dt.bfloat16
I32 = mybir.dt.int32
U32 = mybir.dt.uint32
AF = mybir.ActivationFunctionType
ALU = mybir.AluOpType
AX = mybir.AxisListType

D = 1024
T = 8192
CTX = 256
TT = T + CTX
NCOL = 3600
NT = T // 128
NCT = CTX // 128
GW = 64
ALPHA = 2.0 ** 0.25
EPS = 1e-5
NE = 64
CAP = 2 * T + NE * 128
NBLK = CAP // 128
HID = 512
NEG = -30000.0


class Buf:
    __slots__ = ("w", "r", "name")

    def __init__(self, name=""):
        self.w = None
        self.r = []
        self.name = name


class Sched:
    ENG = ("pe", "act", "dve", "pool", "sp")
    LIM = 16000
    NDS = 24
    NDS_SP = 16

    def __init__(self, nc, es):
        self.nc = nc
        self.es = es
        self.e = dict(pe=nc.tensor, act=nc.scalar, dve=nc.vector, pool=nc.gpsimd, sp=nc.sync)
        self.nsem = 0
        self.sems = {}
        self.epoch = {k: 0 for k in self.ENG}
        self.cnt = {k: 0 for k in self.ENG}
        self.seen = {k: {} for k in self.ENG}
        self.pending = {k: [] for k in self.ENG}
        self.dep = [0] * self.NDS
        self.dcnt = [0] * self.NDS
        self.dnext = 0
        self.dnext_q = {}
        self.n_inst = 0

    def semobj(self, key):
        if key not in self.sems:
            self.sems[key] = self.es.enter_context(self.nc.semaphore("s%d" % self.nsem))
            self.nsem += 1
        return self.sems[key]

    def _wait(self, eng, ev):
        key, val = ev
        if self.seen[eng].get(key, 0) >= val:
            return
        self.e[eng].wait_ge(self.semobj(key), val)
        self.seen[eng][key] = val
        self.n_inst += 1

    def _deps(self, eng, reads, writes):
        deps = set()
        for b in reads:
            if b.w is not None:
                deps.add(b.w)
        for b in writes:
            if b.w is not None:
                deps.add(b.w)
            deps.update(b.r)
        for ev in deps:
            if ev is None:
                continue
            if eng == "pe" and ev[0][0] == "pe":
                continue
            self._wait(eng, ev)

    def _record(self, ev, reads, writes):
        for b in reads:
            b.r.append(ev)
            if len(b.r) > 12:
                b.r = b.r[-12:] if False else b.r
        for b in writes:
            b.w = ev
            b.r = []

    def op(self, eng, fn, reads=(), writes=(), inc=True):
        self._deps(eng, reads, writes)
        inst = fn(self.e[eng])
        self.n_inst += 1
        if not inc:
            self.pending[eng].append((list(reads), list(writes)))
            return inst
        if self.cnt[eng] >= self.LIM:
            self.epoch[eng] += 1
            self.cnt[eng] = 0
        self.cnt[eng] += 1
        key = (eng, self.epoch[eng])
        inst.then_inc(self.semobj(key), 1)
        ev = (key, self.cnt[eng])
        for (r, w) in self.pending[eng]:
            self._record(ev, r, w)
        self.pending[eng] = []
        self._record(ev, reads, writes)
        return inst

    def dma(self, out, in_, reads=(), writes=(), eng="sp", indirect=None):
        lo, hi = (0, self.NDS_SP) if eng == "sp" else (self.NDS_SP, self.NDS)
        i = self.dnext_q.get(eng, lo)
        self.dnext_q[eng] = lo + ((i + 1 - lo) % (hi - lo))
        if self.dcnt[i] > 0:
            self._wait(eng, (("d", i, self.dep[i]), 16 * self.dcnt[i]))
        if self.dcnt[i] >= self.LIM // 16:
            self.dep[i] += 1
            self.dcnt[i] = 0
        self._deps(eng, reads, writes)
        if indirect is None:
            inst = self.e[eng].dma_start(out=out, in_=in_)
        else:
            inst = indirect(self.e[eng])
        self.n_inst += 1
        self.dcnt[i] += 1
        key = ("d", i, self.dep[i])
        inst.then_inc(self.semobj(key), 16)
        ev = (key, 16 * self.dcnt[i])
        self._record(ev, reads, writes)
        return ev

    def barrier(self):
        for k in self.ENG:
            assert not self.pending[k], "pending group at barrier"
        evs = []
        for k in self.ENG:
            if self.cnt[k] > 0:
                evs.append(((k, self.epoch[k]), self.cnt[k]))
        for i in range(self.NDS):
            if self.dcnt[i] > 0:
                evs.append((("d", i, self.dep[i]), 16 * self.dcnt[i]))
        for k in self.ENG:
            for ev in evs:
                if ev[0][0] == k:
                    continue
                self._wait(k, ev)

    def final_wait(self, eng="sp"):
        for i in range(self.NDS):
            if self.dcnt[i] > 0:
                self._wait(eng, (("d", i, self.dep[i]), 16 * self.dcnt[i]))


def interleave(*gens):
    gens = [g for g in gens if g is not None]
    while gens:
        for g in list(gens):
            try:
                next(g)
            except StopIteration:
                gens.remove(g)


def weighted(gen, n):
    def g():
        done = False
        while not done:
            for _ in range(n):
                try:
                    next(gen)
                except StopIteration:
                    done = True
                    break
            yield
    return g()


def build_program(dbg=None, upto="all", phases="ABCDEFGHI"):
    dbg = dbg or []
    nc = bass.Bass("TRN2", target_bir_lowering=False)

    def din(name, shape, dt=F32):
        return nc.dram_tensor(name, list(shape), dt, kind="ExternalInput").ap()

    def dscr(name, shape, dt):
        kind = "ExternalOutput" if name in dbg else "Internal"
        return nc.dram_tensor(name, list(shape), dt, kind=kind).ap()

    x_d = din("x", [T, D])
    ctx_d = din("ctx", [CTX, D])
    cc_d = din("cc", [128, 8, 2])
    wada_d = din("w_ada", [D, 6 * D])
    bada_d = din("b_ada_l", [128, 48, 2])
    badar_d = din("b_ada_r", [1, 6 * D])
    win_d = din("w_in", [D, NCOL])
    gateb_d = din("gate_b", [16, 1])
    out_d = nc.dram_tensor("out", [T, D], F32, kind="ExternalOutput").ap()

    zT_d = dscr("zT", [16, 128, TT], BF16)
    vna_d = dscr("vna", [TT, 8 * 65], BF16)
    mlv_d = dscr("mlv", [TT, 512], BF16)
    mlo_d = dscr("mlo", [TT, 512], F32)
    gT_d = dscr("gT", [16, TT], F32)
    na_d = dscr("na", [T, 512], BF16)
    ml_d = dscr("ml", [T, 512], BF16)
    biasT_d = din("biasT", [5, 128, 8 * 5 * 128], BF16)
    qkT_d = dscr("qkT", [8, 128, TT], BF16)
    kml_d = dscr("kml", [TT, 512], BF16)
    hf_d = dscr("hf", [T, 512], F32)
    rope_d = din("rope", [4, 128, T])
    rperm_d = din("rperm", [128, 128], BF16)
    cmask_d = din("cmask", [3, 128, 128], BF16)
    convw_d = din("convw", [128, 8, 5])
    convb_d = din("convb", [128, 8])
    mlg_d = din("mlg", [128, 512])
    wout_d = din("w_out", [D, D])
    wr_d = din("w_r", [D, 72])
    rbias_d = din("rbias", [128, 72])
    lnp_d = din("lnp", [4, 128, D])
    bvals_d = din("bvals", [128, 3])
    w1_d = din("w1", [NE * 128, 8 * HID])
    w3_d = din("w3", [NE * 128, 8 * HID])
    w2_d = din("w2", [NE * 128, 4 * D])
    wbf_d = [dscr("wbf%d" % m, [NE * 128, 4096], BF16) for m in range(3)]
    xmid_d = dscr("xmid", [T, D], F32)
    h2_d = dscr("h2", [T, D], BF16)
    xperm_d = dscr("xperm", [CAP, D], BF16)
    yperm_d = dscr("yperm", [CAP, D], F32)
    rt_dbg = dscr("rt_dbg", [128, NT, 4], F32) if "rt_dbg" in dbg else None
    eb_dbg = dscr("eb_dbg", [1, 256], I32) if "eb_dbg" in dbg else None
    ix_dbg = dscr("ix_dbg", [128, 256], I32) if "ix_dbg" in dbg else None
    dbgD_d = dscr("dbgD", [2, 3, 4, TT], F32) if "dbgD" in dbg else None
    ada_dbg = dscr("ada_dbg", [128, 48, 2], F32) if "ada_dbg" in dbg else None
    gb_dbg = dscr("gb_dbg", [128, 2048], F32) if "gb_dbg" in dbg else None

    with contextlib.ExitStack() as es:
        S = Sched(nc, es)

        def sb(st, name, shape, dt):
            return st.enter_context(nc.sbuf_tensor("sb_" + name, list(shape), dt))

        def ps(st, name, shape, dt):
            return st.enter_context(nc.psum_tensor("ps_" + name, list(shape), dt))

        ident_bf = sb(es, "ident_bf", [128, 128], BF16)
        ident_f = sb(es, "ident_f", [128, 128], F32)
        ones_f = sb(es, "ones_f", [128, 128], F32)
        adaT = sb(es, "adaT", [128, 48, 2], F32)
        g_b = sb(es, "g_b", [128, 2048], F32)
        B_const = Buf("const")
        B_ada = Buf("ada")
        B_gb = Buf("gb")

        def mk_ident(tile):
            S.op("pool", lambda e: e.memset(tile[:], 1.0), writes=[B_const])
            S.op("pool", lambda e: e.affine_select(out=tile[:], in_=tile[:], pattern=[[1, 128]],
                                                   compare_op=ALU.is_equal, fill=0.0, base=0,
                                                   channel_multiplier=-1), reads=[B_const], writes=[B_const])
        mk_ident(ident_f)
        S.op("dve", lambda e: e.tensor_copy(out=ident_bf[:], in_=ident_f[:]), reads=[B_const], writes=[B_const])
        S.op("dve", lambda e: e.memset(ones_f[:], 1.0), writes=[B_const])
        neghalf_c = sb(es, "neghalf_c", [128, 1], F32)
        S.op("dve", lambda e: e.memset(neghalf_c[:], -0.5), writes=[B_const])


        def conv_task():
            srcs = (w1_d, w3_d, w2_d)
            for r0 in range(0, NE * 128, 128):
                for m in range(3):
                    S.dma(wbf_d[m][r0:r0 + 128, :], srcs[m][r0:r0 + 128, :], eng="pool")
                    yield
        bg_task = conv_task() if "H" in phases else iter(())

        def bg_step(n):
            for _ in range(n):
                try:
                    next(bg_task)
                except StopIteration:
                    return

        with contextlib.ExitStack() as pa:
            cc = sb(pa, "cc", [128, 8, 2], F32)
            scc = sb(pa, "scc", [128, 8, 2], F32)
            badal = sb(pa, "badal", [128, 48, 2], F32)
            badar = sb(pa, "badar", [1, 6 * D], F32)
            wp = [sb(pa, "wadap%d" % i, [128, 8, 1024], F32) for i in range(2)]
            grow = sb(pa, "grow", [1, 2048], F32)
            adaps = ps(pa, "adaps", [128, 512], F32)
            rowps = ps(pa, "rowps", [1, 512], F32)
            bcps = ps(pa, "bcps", [128, 512], F32)
            B_cc, B_scc, B_bl, B_br = Buf(), Buf(), Buf(), Buf()
            B_wp = [Buf(), Buf()]
            B_adaps, B_rowps, B_bcps, B_grow = Buf(), Buf(), Buf(), Buf()

            S.dma(cc[:], cc_d, writes=[B_cc])
            S.dma(badal[:], bada_d, writes=[B_bl])
            S.dma(badar[:], badar_d, writes=[B_br])
            S.op("act", lambda e: e.activation(out=scc[:], in_=cc[:], func=AF.Silu), reads=[B_cc], writes=[B_scc])
            wada_v = wada_d.rearrange("(k p) n -> p k n", p=128)
            for pc in range(6):
                S.dma(wp[pc % 2][:], wada_v[:, :, pc * 1024:(pc + 1) * 1024], writes=[B_wp[pc % 2]])
                w = wp[pc % 2]
                if pc in (2, 5):
                    gi = 0 if pc == 2 else 1
                    for grp in range(2):
                        for k in range(8):
                            S.op("pe", lambda e, k=k, grp=grp: e.matmul(
                                rowps[0:1, :], lhsT=scc[:, k, 0:1], rhs=w[:, k, grp * 512:(grp + 1) * 512],
                                start=(k == 0), stop=(k == 7)),
                                reads=[B_scc, B_wp[pc % 2]], writes=[B_rowps], inc=(k == 7))
                        S.op("dve", lambda e, grp=grp: e.tensor_tensor(
                            out=grow[0:1, gi * 1024 + grp * 512: gi * 1024 + (grp + 1) * 512], in0=rowps[0:1, :],
                            in1=badar[0:1, pc * 1024 + grp * 512: pc * 1024 + (grp + 1) * 512], op=ALU.add),
                            reads=[B_rowps, B_br], writes=[B_grow])
                        S.op("pe", lambda e, grp=grp: e.matmul(
                            bcps[:, :], lhsT=ones_f[0:1, :], rhs=grow[0:1, gi * 1024 + grp * 512: gi * 1024 + (grp + 1) * 512],
                            start=True, stop=True), reads=[B_grow, B_const], writes=[B_bcps])
                        S.op("act", lambda e, grp=grp: e.activation(
                            out=g_b[:, gi * 1024 + grp * 512: gi * 1024 + (grp + 1) * 512], in_=bcps[:, :], func=AF.Copy),
                            reads=[B_bcps], writes=[B_gb])
                else:
                    for jj in range(8):
                        for k in range(8):
                            S.op("pe", lambda e, k=k, jj=jj: e.matmul(
                                adaps[:, jj * 2:(jj + 1) * 2], lhsT=w[:, k, jj * 128:(jj + 1) * 128], rhs=scc[:, k, :],
                                start=(k == 0), stop=(k == 7)),
                                reads=[B_scc, B_wp[pc % 2]], writes=[B_adaps], inc=(k == 7 and jj == 7))
                    S.op("dve", lambda e: e.tensor_tensor(
                        out=adaT[:, pc * 8:(pc + 1) * 8, :], in0=adaps[:, 0:16].rearrange("p (j s) -> p j s", s=2),
                        in1=badal[:, pc * 8:(pc + 1) * 8, :], op=ALU.add),
                        reads=[B_adaps, B_bl], writes=[B_ada])
                    if pc in (1, 4):
                        S.op("dve", lambda e: e.tensor_scalar(
                            out=adaT[:, pc * 8:(pc + 1) * 8, :], in0=adaT[:, pc * 8:(pc + 1) * 8, :],
                            scalar1=1.0, scalar2=None, op0=ALU.add), reads=[B_ada], writes=[B_ada])
            if ada_dbg is not None:
                S.dma(ada_dbg, adaT[:], reads=[B_ada])
                S.dma(gb_dbg, g_b[:], reads=[B_gb])
            S.barrier()
        if upto == "A":
            S.final_wait()
            return nc

        with contextlib.ExitStack() as pb:
            win_sb = sb(pb, "win_sb", [128, 8, NCOL], BF16)
            gateb = sb(pb, "gateb", [16, 1], F32)
            B_win, B_gateb = Buf(), Buf()
            win_v = win_d.rearrange("(k p) n -> p k n", p=128)
            for k in range(8):
                S.dma(win_sb[:, k, :], win_v[:, k, :], writes=[B_win], eng="pool")
            S.dma(gateb[:], gateb_d, writes=[B_gateb])

            NXB = 3
            xt = [sb(pb, "xt%d" % i, [128, D], F32) for i in range(NXB)]
            B_xt = [Buf() for _ in range(NXB)]
            xn = [sb(pb, "xn%d" % i, [128, D], BF16) for i in range(2)]
            B_xn = [Buf(), Buf()]
            st6 = sb(pb, "st6", [128, 2, 6], F32)
            mv = sb(pb, "mv", [128, 2], F32)
            rstd = sb(pb, "rstd", [128, 1], F32)
            nmr = sb(pb, "nmr", [128, 1], F32)
            neghalf = sb(pb, "neghalf", [128, 1], F32)
            B_st, B_mv, B_rstd, B_nmr = Buf(), Buf(), Buf(), Buf()
            S.op("dve", lambda e: e.memset(neghalf[:], -0.5), writes=[B_const])
            xmT = [sb(pb, "xmT%d" % i, [128, 8, 512], BF16) for i in range(2)]
            B_xmT = [Buf(), Buf()]
            tp = [ps(pb, "tp%d" % i, [128, 1024], BF16) for i in range(2)]
            B_tp = [Buf(), Buf()]
            fps = [ps(pb, "fps%d" % i, [128, 512], F32) for i in range(2)]
            B_fps = [Buf(), Buf()]
            tps = [ps(pb, "tps%d" % i, [128, 512], F32) for i in range(2)]
            B_tps = [Buf(), Buf()]
            gps = ps(pb, "gps", [16, 512], F32)
            B_gps = Buf()
            zst = [sb(pb, "zst%d" % i, [128, 16, 512], BF16) for i in range(2)]
            B_zst = [Buf(), Buf()]
            vst = [sb(pb, "vst%d" % i, [128, 4, 8 * 65], BF16) for i in range(2)]
            B_vst = [Buf(), Buf()]
            mvst = [sb(pb, "mvst%d" % i, [128, 4, 512], BF16) for i in range(2)]
            B_mvst = [Buf(), Buf()]
            ost = [sb(pb, "ost%d" % i, [128, 4, 512], F32) for i in range(2)]
            B_ost = [Buf(), Buf()]
            gst = [sb(pb, "gst%d" % i, [16, 512], F32) for i in range(2)]
            B_gst = [Buf(), Buf()]
            for i in range(2):
                S.op("dve", lambda e, i=i: e.memset(vst[i][:], 1.0), writes=[B_vst[i]])

            supers = [(0, NCT, 1, ctx_d)] + [(CTX + s * 512, 4, 0, x_d[s * 512:(s + 1) * 512, :]) for s in range(T // 512)]
            tile_list = []
            for si, (t0, ntl, stream, src) in enumerate(supers):
                for j in range(ntl):
                    tile_list.append((si, j))
            load_idx = [0]

            def issue_load(n):
                while load_idx[0] <= n and load_idx[0] < len(tile_list):
                    si, j = tile_list[load_idx[0]]
                    src = supers[si][3]
                    b = load_idx[0] % NXB
                    S.dma(xt[b][:], src[j * 128:(j + 1) * 128, :], writes=[B_xt[b]])
                    load_idx[0] += 1

            gtile = {}
            cnt = 0
            for si, (t0, ntl, stream, src) in enumerate(supers):
                for j in range(ntl):
                    gtile[(si, j)] = cnt
                    cnt += 1

            evac_rr = [0]

            def evac(out, in_, reads, writes, scale=None, eng=None):
                if eng is None:
                    eng = ("act", "dve")[evac_rr[0] % 2]
                    evac_rr[0] += 1
                if eng == "act":
                    if scale is None:
                        S.op("act", lambda e: e.activation(out=out, in_=in_, func=AF.Copy), reads=reads, writes=writes)
                    else:
                        S.op("act", lambda e: e.activation(out=out, in_=in_, func=AF.Copy, scale=float(scale)),
                             reads=reads, writes=writes)
                else:
                    if scale is None:
                        S.op("dve", lambda e: e.tensor_copy(out=out, in_=in_), reads=reads, writes=writes)
                    else:
                        S.op("dve", lambda e: e.tensor_scalar(out=out, in0=in_, scalar1=float(scale), scalar2=None,
                                                              op0=ALU.mult), reads=reads, writes=writes)

            def prep(si):
                t0, ntl, stream, src = supers[si]
                xm = xmT[si % 2]
                Bxm = B_xmT[si % 2]
                for j in range(ntl):
                    g = gtile[(si, j)]
                    issue_load(g + 2)
                    b = g % NXB
                    x_t = xt[b]
                    S.op("dve", lambda e: e.bn_stats(out=st6[:, 0, :], in_=x_t[:, 0:512]), reads=[B_xt[b]], writes=[B_st])
                    S.op("dve", lambda e: e.bn_stats(out=st6[:, 1, :], in_=x_t[:, 512:1024]), reads=[B_xt[b]], writes=[B_st])
                    S.op("dve", lambda e: e.bn_aggr(out=mv[:], in_=st6[:].rearrange("p a b -> p (a b)")), reads=[B_st], writes=[B_mv])
                    S.op("dve", lambda e: e.tensor_scalar(out=rstd[:], in0=mv[:, 1:2], scalar1=EPS, scalar2=None, op0=ALU.add),
                         reads=[B_mv], writes=[B_rstd])
                    S.op("pool", lambda e: e.tensor_tensor(out=rstd[:], in0=rstd[:], in1=neghalf[:], op=ALU.pow),
                         reads=[B_rstd, B_const], writes=[B_rstd])
                    S.op("dve", lambda e: e.scalar_tensor_tensor(out=nmr[:], in0=mv[:, 0:1], scalar=-1.0, in1=rstd[:],
                                                                 op0=ALU.mult, op1=ALU.mult), reads=[B_mv, B_rstd], writes=[B_nmr])
                    xnb = xn[g % 2]
                    S.op("act", lambda e: e.activation(out=xnb[:], in_=x_t[:], func=AF.Identity, bias=nmr[:, 0:1], scale=rstd[:, 0:1]),
                         reads=[B_xt[b], B_nmr, B_rstd], writes=[B_xn[g % 2]])
                    bg_step(3)
                    yield
                    tpp = tp[g % 2]
                    for k in range(8):
                        S.op("pe", lambda e, k=k: e.transpose(out=tpp[:, k * 128:(k + 1) * 128], in_=xnb[:, k * 128:(k + 1) * 128],
                                                              identity=ident_bf[:]),
                             reads=[B_xn[g % 2], B_const], writes=[B_tp[g % 2]], inc=(k == 7))
                    for k in range(8):
                        o = xm[:, k, j * 128:(j + 1) * 128]
                        i_ = tpp[:, k * 128:(k + 1) * 128]
                        sc = adaT[:, 8 + k, stream:stream + 1]
                        sh = adaT[:, 0 + k, stream:stream + 1]
                        if k % 2 == 0:
                            S.op("act", lambda e, o=o, i_=i_, sc=sc, sh=sh: e.activation(out=o, in_=i_, func=AF.Identity, bias=sh, scale=sc),
                                 reads=[B_tp[g % 2], B_ada], writes=[Bxm])
                        else:
                            S.op("dve", lambda e, o=o, i_=i_, sc=sc, sh=sh: e.tensor_scalar(out=o, in0=i_, scalar1=sc, scalar2=sh,
                                                                                           op0=ALU.mult, op1=ALU.add),
                                 reads=[B_tp[g % 2], B_ada], writes=[Bxm])
                    yield

            FM_CH = [(c * 128) for c in range(0, 8)] + [1536 + c * 128 for c in range(0, 8)]

            def mm(si):
                t0, ntl, stream, src = supers[si]
                ntok = ntl * 128
                xm = xmT[si % 2]
                Bxm = B_xmT[si % 2]
                zs = zst[si % 2]
                for ci, c0 in enumerate(FM_CH):
                    p = fps[ci % 2]
                    for k in range(8):
                        S.op("pe", lambda e, k=k: e.matmul(p[:, 0:ntok], lhsT=win_sb[:, k, c0:c0 + 128], rhs=xm[:, k, 0:ntok],
                                                           start=(k == 0), stop=(k == 7)),
                             reads=[B_win, Bxm], writes=[B_fps[ci % 2]], inc=(k == 7))
                    evac(zs[:, ci, 0:ntok], p[:, 0:ntok], [B_fps[ci % 2]], [B_zst[si % 2]], scale=(0.125 if ci < 4 else None))
                    yield
                S.dma(zT_d[:, :, t0:t0 + ntok].rearrange("c p t -> p c t"), zs[:, :, 0:ntok], reads=[B_zst[si % 2]])
                for k in range(8):
                    S.op("pe", lambda e, k=k: e.matmul(gps[:, 0:ntok], lhsT=win_sb[:, k, 3584:3600], rhs=xm[:, k, 0:ntok],
                                                       start=(k == 0), stop=(k == 7)),
                         reads=[B_win, Bxm], writes=[B_gps], inc=(k == 7))
                gs = gst[si % 2]
                S.op("act", lambda e: e.activation(out=gs[:, 0:ntok], in_=gps[:, 0:ntok], func=AF.Identity, bias=gateb[:, 0:1], scale=1.0),
                     reads=[B_gps, B_gateb], writes=[B_gst[si % 2]])
                S.dma(gT_d[:, t0:t0 + ntok], gs[:, 0:ntok], reads=[B_gst[si % 2]])
                yield
                for j in range(ntl):
                    lhs = lambda k: xm[:, k, j * 128:(j + 1) * 128]
                    for gi, c0 in enumerate((1024, 2560, 3072)):
                        q = (j * 3 + gi) % 2
                        p = tps[q]
                        for k in range(8):
                            S.op("pe", lambda e, k=k: e.matmul(p[:, :], lhsT=lhs(k), rhs=win_sb[:, k, c0:c0 + 512],
                                                               start=(k == 0), stop=(k == 7)),
                                 reads=[B_win, Bxm], writes=[B_tps[q]], inc=(k == 7))
                        if gi == 0:
                            evac(vst[si % 2][:, j, :].rearrange("p (h d) -> p h d", d=65)[:, :, 0:64],
                                 p[:, :].rearrange("p (h d) -> p h d", d=64), [B_tps[q]], [B_vst[si % 2]])
                        elif gi == 1:
                            evac(mvst[si % 2][:, j, :], p[:, :], [B_tps[q]], [B_mvst[si % 2]])
                        else:
                            S.op("act", lambda e: e.activation(out=ost[si % 2][:, j, :], in_=p[:, :], func=AF.Sigmoid),
                                 reads=[B_tps[q]], writes=[B_ost[si % 2]])
                        yield
                tv = lambda d_: d_[t0:t0 + ntok, :].rearrange("(j p) f -> p j f", p=128)
                S.dma(tv(vna_d), vst[si % 2][:, 0:ntl, :], reads=[B_vst[si % 2]])
                S.dma(tv(mlv_d), mvst[si % 2][:, 0:ntl, :], reads=[B_mvst[si % 2]])
                S.dma(tv(mlo_d), ost[si % 2][:, 0:ntl, :], reads=[B_ost[si % 2]])
                yield

            nsup = len(supers) if upto != "B1" else 2
            issue_load(1)
            interleave(prep(0))
            for si in range(nsup):
                interleave(mm(si), weighted(prep(si + 1), 1) if si + 1 < nsup else None)
            S.barrier()
        if upto in ("B", "B1"):
            S.final_wait()
            return nc


        if "F" in phases:
          with contextlib.ExitStack() as pf:
            kT_sb = sb(pf, "kT_sb", [128, 4, TT], BF16)
            v_sb = sb(pf, "v_sb", [128, TT // 128, 520], BF16)
            biasI = sb(pf, "biasI", [128, 8, 5, 128], BF16)
            biasE = sb(pf, "biasE", [128, 8, 5, 128], BF16)
            B_kT, B_v, B_bI, B_bE = Buf(), Buf(), Buf(), Buf()
            B_kTs = [Buf() for _ in range(4)]
            B_vs = [Buf() for _ in range(6)]
            for c in range(4):
                S.dma(kT_sb[:, c, :], zT_d[4 + c, :, :], writes=[B_kTs[c]])
            vv = vna_d.rearrange("(j p) f -> p j f", p=128)
            for j0 in range(0, TT // 128, 11):
                S.dma(v_sb[:, j0:j0 + 11, :], vv[:, j0:j0 + 11, :], writes=[B_vs[j0 // 11]])
            S.dma(biasI[:].rearrange("p h n q -> p (h n q)"), biasT_d[0], writes=[B_bI])
            qT = [sb(pf, "qT%d" % i, [128, 4, 128], BF16) for i in range(2)]
            B_qT = [Buf(), Buf()]
            NPT = 3
            pT = [sb(pf, "pT%d" % i, [128, 896], BF16) for i in range(NPT)]
            B_pT = [Buf() for _ in range(NPT)]
            sT = [ps(pf, "sT%d" % i, [128, 1024], F32) for i in range(2)]
            B_sT = [Buf(), Buf()]
            po = [ps(pf, "po%d" % i, [128, 2, 512], F32) for i in range(2)]
            B_po = [Buf(), Buf()]
            rec = sb(pf, "rec", [128, 8], F32)
            B_rec = Buf()
            ostg = [sb(pf, "ostg%d" % i, [128, 8, 64], BF16) for i in range(2)]
            B_ostg = [Buf(), Buf()]
            ntiles_f = NT if upto != "F1" else 3
            tiles_f = list(range(NT)) if upto != "F1" else [0, 1, 2, 30, 62, 63]

            def load_q(idx):
                if idx < len(tiles_f):
                    i = tiles_f[idx]
                    t0 = CTX + i * 128
                    S.dma(qT[idx % 2][:], zT_d[0:4, :, t0:t0 + 128].rearrange("c p t -> p c t"), writes=[B_qT[idx % 2]])
            load_q(0)
            hcount = 0
            for idx, i in enumerate(tiles_f):
                load_q(idx + 1)
                variant = {0: 1, 1: 2, 62: 3, 63: 4}.get(i, 0)
                if variant:
                    S.dma(biasE[:].rearrange("p h n q -> p (h n q)"), biasT_d[variant], writes=[B_bE])
                    bias, B_bias = biasE, B_bE
                else:
                    bias, B_bias = biasI, B_bI
                jb0 = min(max(i - 2, 0), 59)
                q = qT[idx % 2]
                pob = po[idx % 2]
                def emit_qk(h, hc):
                    c = h // 2
                    pb = (h % 2) * 64
                    sTb = sT[hc % 2]
                    for n in range(7):
                        if n < 5:
                            k0 = CTX + (jb0 + n) * 128
                        else:
                            k0 = (n - 5) * 128
                        S.op("pe", lambda e: e.matmul(sTb[:, n * 128:(n + 1) * 128], lhsT=kT_sb[pb:pb + 64, c, k0:k0 + 128],
                                                      rhs=q[pb:pb + 64, c, :], start=True, stop=(n >= 5)),
                             reads=B_kTs + [B_qT[idx % 2]], writes=[B_sT[hc % 2]], inc=(n == 6))
                        if n < 5:
                            S.op("pe", lambda e: e.matmul(sTb[:, n * 128:(n + 1) * 128], lhsT=ident_bf[:, :],
                                                          rhs=bias[:, h, n, :], start=False, stop=True),
                                 reads=[B_bias, B_const], writes=[B_sT[hc % 2]], inc=False)

                emit_qk(0, hcount)
                for h in range(8):
                    sTb = sT[hcount % 2]
                    pTb = pT[hcount % NPT]
                    S.op("act", lambda e: e.activation(out=pTb[:, :], in_=sTb[:, 0:896], func=AF.Exp),
                         reads=[B_sT[hcount % 2]], writes=[B_pT[hcount % NPT]])
                    if h + 1 < 8:
                        emit_qk(h + 1, hcount + 1)
                    for n in range(7):
                        blk = (2 + jb0 + n) if n < 5 else (n - 5)
                        S.op("pe", lambda e: e.matmul(pob[:, h // 4, (h % 4) * 65:(h % 4) * 65 + 65], lhsT=pTb[:, n * 128:(n + 1) * 128],
                                                      rhs=v_sb[:, blk, h * 65:(h + 1) * 65], start=(n == 0), stop=(n == 6)),
                             reads=[B_pT[hcount % NPT]] + B_vs, writes=[B_po[idx % 2]], inc=(n == 6))
                    hcount += 1
                pov = pob[:, :, 0:260].rearrange("p a (h d) -> p a h d", d=65)
                S.op("dve", lambda e: e.reciprocal(out=rec[:].rearrange("p (a h) -> p a h", a=2), in_=pov[:, :, :, 64]),
                     reads=[B_po[idx % 2]], writes=[B_rec])
                og = ostg[idx % 2]
                for a in range(2):
                    S.op("dve", lambda e: e.tensor_tensor(out=og[:, a * 4:(a + 1) * 4, :], in0=pov[:, a, :, 0:64],
                                                          in1=rec[:, a * 4:(a + 1) * 4].unsqueeze(2).to_broadcast([128, 4, 64]),
                                                          op=ALU.mult),
                         reads=[B_po[idx % 2], B_rec], writes=[B_ostg[idx % 2]])
                S.dma(na_d[i * 128:(i + 1) * 128, :], og[:].rearrange("p h d -> p (h d)"), reads=[B_ostg[idx % 2]])
            S.barrier()
        if upto in ("F", "F1"):
            S.final_wait()
            return nc

        KSC = 128.0 ** -0.5
        if "C" in phases:
          with contextlib.ExitStack() as pc_:
            convw = sb(pc_, "convw", [128, 8, 5], F32)
            convb = sb(pc_, "convb", [128, 8], F32)
            diagw = sb(pc_, "diagw", [128, 8, 5, 128], BF16)
            rperm = sb(pc_, "rperm", [128, 128], BF16)
            B_cw, B_cb, B_dw, B_rp = Buf(), Buf(), Buf(), Buf()
            S.dma(convw[:], convw_d, writes=[B_cw])
            S.dma(convb[:], convb_d, writes=[B_cb])
            S.dma(rperm[:], rperm_d, writes=[B_rp])
            for ch in range(8):
                for j in range(5):
                    S.op("dve", lambda e: e.tensor_scalar(out=diagw[:, ch, j, :], in0=ident_f[:, :], scalar1=convw[:, ch, j:j + 1],
                                                          scalar2=None, op0=ALU.mult), reads=[B_cw, B_const], writes=[B_dw])
            u8 = [sb(pc_, "u8_%d" % i, [128, 8, 516], BF16) for i in range(2)]
            B_u8 = [Buf(), Buf()]
            rt = [sb(pc_, "rt%d" % i, [128, 4, 512], F32) for i in range(2)]
            B_rt = [Buf(), Buf()]
            qs = [sb(pc_, "qs%d" % i, [128, 512], BF16) for i in range(2)]
            B_qs = [Buf(), Buf()]
            t1 = [sb(pc_, "t1_%d" % i, [128, 512], F32) for i in range(2)]
            B_t1 = [Buf(), Buf()]
            t2 = [sb(pc_, "t2_%d" % i, [128, 512], F32) for i in range(2)]
            B_t2 = [Buf(), Buf()]
            qko = [sb(pc_, "qko%d" % i, [128, 8, 512], BF16) for i in range(2)]
            B_qko = [Buf(), Buf()]
            kst = [sb(pc_, "kst%d" % i, [128, 4, 4, 128], BF16) for i in range(2)]
            B_kst = [Buf(), Buf()]
            cps_ = [ps(pc_, "cvps%d" % i, [128, 512], F32) for i in range(2)]
            B_cps = [Buf(), Buf()]
            rps = [ps(pc_, "rps%d" % i, [128, 512], F32) for i in range(2)]
            B_rps = [Buf(), Buf()]
            trp = [ps(pc_, "trp%d" % i, [128, 4, 128], BF16) for i in range(2)]
            B_trp = [Buf(), Buf()]
            groups = [(0, CTX, False, 0)] + [(CTX + g * 512, 512, True, g * 512) for g in range(T // 512)]
            if upto == "C1":
                groups = groups[:2] + groups[-1:]

            def load_group(gi):
                if gi >= len(groups):
                    return
                tt0, n, lat, lo = groups[gi]
                u = u8[gi % 2]
                seg0, seg1 = (CTX, TT) if lat else (0, CTX)
                a = max(tt0 - 2, seg0)
                b_ = min(tt0 + n + 2, seg1)
                if a > tt0 - 2:
                    S.op("pool", lambda e: e.memset(u[:, :, 0:2], 0.0), writes=[B_u8[gi % 2]])
                if b_ < tt0 + n + 2:
                    S.op("pool", lambda e: e.memset(u[:, :, n + 2:n + 4], 0.0), writes=[B_u8[gi % 2]])
                S.dma(u[:, :, a - (tt0 - 2):b_ - (tt0 - 2)], zT_d[8:16, :, a:b_].rearrange("c p t -> p c t"), writes=[B_u8[gi % 2]])
                if lat:
                    S.dma(rt[gi % 2][:], rope_d[:, :, lo:lo + 512].rearrange("c p t -> p c t"), writes=[B_rt[gi % 2]])
            load_group(0)
            cc_ = 0
            for gi, (tt0, n, lat, lo) in enumerate(groups):
                load_group(gi + 1)
                u = u8[gi % 2]
                qo = qko[gi % 2]
                for ch in range(8):
                    cp = cps_[cc_ % 2]
                    for j in range(5):
                        S.op("pe", lambda e: e.matmul(cp[:, 0:n], lhsT=diagw[:, ch, j, :], rhs=u[:, ch, j:j + n], start=(j == 0), stop=(j == 4)),
                             reads=[B_dw, B_u8[gi % 2]], writes=[B_cps[cc_ % 2]], inc=(j == 4))
                    isk = ch >= 4
                    if not lat:
                        if isk:
                            S.op("act", lambda e: e.activation(out=t1[0][:, 0:n], in_=cp[:, 0:n], func=AF.Silu, bias=convb[:, ch:ch + 1], scale=1.0),
                                 reads=[B_cps[cc_ % 2], B_cb], writes=[B_t1[0]])
                            S.op("dve", lambda e: e.tensor_scalar(out=qo[:, ch, 0:n], in0=t1[0][:, 0:n], scalar1=KSC, scalar2=None, op0=ALU.mult),
                                 reads=[B_t1[0]], writes=[B_qko[gi % 2]])
                        else:
                            S.op("act", lambda e: e.activation(out=qo[:, ch, 0:n], in_=cp[:, 0:n], func=AF.Silu, bias=convb[:, ch:ch + 1], scale=1.0),
                                 reads=[B_cps[cc_ % 2], B_cb], writes=[B_qko[gi % 2]])
                    else:
                        q_ = qs[cc_ % 2]
                        S.op("act", lambda e: e.activation(out=q_[:, 0:n], in_=cp[:, 0:n], func=AF.Silu, bias=convb[:, ch:ch + 1], scale=1.0),
                             reads=[B_cps[cc_ % 2], B_cb], writes=[B_qs[cc_ % 2]])
                        rp_ = rps[cc_ % 2]
                        S.op("pe", lambda e: e.matmul(rp_[:, 0:n], lhsT=rperm[:, :], rhs=q_[:, 0:n], start=True, stop=True),
                             reads=[B_rp, B_qs[cc_ % 2]], writes=[B_rps[cc_ % 2]])
                        tb = 2 if isk else 0
                        S.op("pool", lambda e: e.tensor_tensor(out=t1[cc_ % 2][:, 0:n], in0=q_[:, 0:n], in1=rt[gi % 2][:, tb, 0:n], op=ALU.mult),
                             reads=[B_qs[cc_ % 2], B_rt[gi % 2]], writes=[B_t1[cc_ % 2]])
                        S.op("dve", lambda e: e.tensor_tensor(out=t2[cc_ % 2][:, 0:n], in0=rp_[:, 0:n], in1=rt[gi % 2][:, tb + 1, 0:n], op=ALU.mult),
                             reads=[B_rps[cc_ % 2], B_rt[gi % 2]], writes=[B_t2[cc_ % 2]])
                        S.op("dve", lambda e: e.tensor_tensor(out=qo[:, ch, 0:n], in0=t1[cc_ % 2][:, 0:n], in1=t2[cc_ % 2][:, 0:n], op=ALU.add),
                             reads=[B_t1[cc_ % 2], B_t2[cc_ % 2]], writes=[B_qko[gi % 2]])
                    cc_ += 1
                S.dma(qkT_d[:, :, tt0:tt0 + n].rearrange("c p t -> p c t"), qo[:, :, 0:n], reads=[B_qko[gi % 2]])
                ks = kst[gi % 2]
                for j in range(n // 128):
                    tr = trp[j % 2]
                    for h in range(4):
                        S.op("pe", lambda e: e.transpose(out=tr[:, h, :], in_=qo[:, 4 + h, j * 128:(j + 1) * 128], identity=ident_bf[:]),
                             reads=[B_qko[gi % 2], B_const], writes=[B_trp[j % 2]], inc=(h == 3))
                    S.op("act", lambda e: e.activation(out=ks[:, j, :, :], in_=tr[:, :, :], func=AF.Copy),
                         reads=[B_trp[j % 2]], writes=[B_kst[gi % 2]])
                S.dma(kml_d[tt0:tt0 + n, :].rearrange("(j p) f -> p j f", p=128), ks[:, 0:n // 128, :, :].rearrange("p j h d -> p j (h d)"),
                      reads=[B_kst[gi % 2]])
            S.barrier()
        if upto in ("C", "C1"):
            S.final_wait()
            return nc

        NCH = TT // 128
        if "E" in phases:
          with contextlib.ExitStack() as pde:
            wgtT = [sb(pde, "wgtT%d" % d, [128, NCH, 4], F32) for d in range(2)]
            thrT = [sb(pde, "thrT%d" % d, [128, NCH, 4], F32) for d in range(2)]
            decB = [sb(pde, "decB%d" % d, [128, 4, NCH], F32) for d in range(2)]
            B_wgtT, B_thrT, B_decB = [Buf(), Buf()], [Buf(), Buf()], [Buf(), Buf()]
            with contextlib.ExitStack() as pd:
                X1 = sb(pd, "X1", [4, TT], F32)
                X2 = sb(pd, "X2", [4, TT], F32)
                X3 = sb(pd, "X3", [4, TT], F32)
                Z0 = sb(pd, "Z0", [4, TT], F32)
                ngd = sb(pd, "ngd", [4, NCH], F32)
                dec = sb(pd, "dec", [4, NCH], F32)
                sel = sb(pd, "sel", [4, 4, 128], F32)
                ptw = ps(pd, "ptw", [128, 512], F32)
                ptt = ps(pd, "ptt", [128, 512], F32)
                pdc = ps(pd, "pdc", [128, 512], F32)
                B1, B2, B3, BZ, Bng, Bdec, Bsel, Bptw, Bptt, Bpdc = [Buf() for _ in range(10)]
                S.op("pool", lambda e: e.memset(Z0[:], 0.0), writes=[BZ])
                for h in range(4):
                    S.op("dve", lambda e: e.tensor_copy(out=sel[0:4, h, :], in_=ident_f[0:4, h:h + 1].to_broadcast([4, 128])),
                         reads=[B_const], writes=[Bsel])
                segs = [(0, CTX), (CTX, TT)]
                for d in range(2):
                    if d == 0:
                        S.dma(X1[:], gT_d[0:4, :], writes=[B1])
                        S.dma(X2[:], gT_d[4:8, :], writes=[B2])
                    else:
                        S.dma(X3[:], gT_d[8:12, :], writes=[B3])
                        for (a, b_) in segs:
                            S.op("dve", lambda e: e.tensor_copy(out=X1[:, a:b_], in_=X3[:, a:b_][:, ::-1]), reads=[B3], writes=[B1])
                        S.dma(X3[:], gT_d[12:16, :], writes=[B3])
                        for (a, b_) in segs:
                            S.op("dve", lambda e: e.tensor_copy(out=X2[:, a:b_], in_=X3[:, a:b_][:, ::-1]), reads=[B3], writes=[B2])
                    S.op("act", lambda e: e.activation(out=X2[:], in_=X2[:], func=AF.Exp, scale=-1.0), reads=[B2], writes=[B2])
                    S.op("act", lambda e: e.activation(out=X2[:], in_=X2[:], func=AF.Ln, bias=1.0, scale=1.0), reads=[B2], writes=[B2])
                    S.op("dve", lambda e: e.tensor_tensor_scan(out=X3[:], data0=Z0[:], data1=X2[:], initial=0.0, op0=ALU.add, op1=ALU.add),
                         reads=[BZ, B2], writes=[B3])
                    S.op("dve", lambda e: e.tensor_tensor(out=X1[:], in0=X1[:], in1=X3[:], op=ALU.add), reads=[B1, B3], writes=[B1])
                    S.op("dve", lambda e: e.tensor_tensor_scan(out=X2[:], data0=X1[:], data1=X1[:], initial=0.0, op0=ALU.max, op1=ALU.max),
                         reads=[B1], writes=[B2])
                    S.op("dve", lambda e: e.tensor_scalar(out=ngd[:], in0=X2[:, 127:TT:128], scalar1=-1.0, scalar2=None, op0=ALU.mult),
                         reads=[B2], writes=[Bng])
                    S.op("dve", lambda e: e.tensor_copy(out=dec[:, 0:1], in_=ngd[:, 0:1]), reads=[Bng], writes=[Bdec])
                    S.op("dve", lambda e: e.tensor_tensor(out=dec[:, 1:NCH], in0=X2[:, 127:TT - 128:128], in1=ngd[:, 1:NCH], op=ALU.add),
                         reads=[B2, Bng], writes=[Bdec])
                    S.op("act", lambda e: e.activation(out=dec[:], in_=dec[:], func=AF.Exp), reads=[Bdec], writes=[Bdec])
                    for c in range(NCH):
                        sl = slice(c * 128, (c + 1) * 128)
                        S.op("act", lambda e: e.activation(out=X1[:, sl], in_=X1[:, sl], func=AF.Exp, bias=ngd[:, c:c + 1], scale=1.0),
                             reads=[B1, Bng], writes=[B1])
                        S.op("act", lambda e: e.activation(out=X3[:, sl], in_=X3[:, sl], func=AF.Exp, bias=ngd[:, c:c + 1], scale=1.0),
                             reads=[B3, Bng], writes=[B3])
                    if d == 0:
                        Wt, Bw, Tt, Bt = X1, B1, X3, B3
                    else:
                        for (a, b_) in segs:
                            S.op("dve", lambda e: e.tensor_copy(out=X2[:, a:b_], in_=X1[:, a:b_][:, ::-1]), reads=[B1], writes=[B2])
                        for (a, b_) in segs:
                            S.op("dve", lambda e: e.tensor_copy(out=X1[:, a:b_], in_=X3[:, a:b_][:, ::-1]), reads=[B3], writes=[B1])
                        Wt, Bw, Tt, Bt = X2, B2, X1, B1
                    for blk in range(NCH):
                        sl = slice(blk * 128, (blk + 1) * 128)
                        S.op("pe", lambda e: e.matmul(ptw[:, blk * 4:(blk + 1) * 4], lhsT=Wt[0:4, sl], rhs=ident_f[0:4, 0:4], start=True, stop=True),
                             reads=[Bw, B_const], writes=[Bptw], inc=False)
                        S.op("pe", lambda e: e.matmul(ptt[:, blk * 4:(blk + 1) * 4], lhsT=Tt[0:4, sl], rhs=ident_f[0:4, 0:4], start=True, stop=True),
                             reads=[Bt, B_const], writes=[Bptt], inc=(blk == NCH - 1))
                    S.op("dve", lambda e: e.tensor_copy(out=wgtT[d][:].rearrange("p c h -> p (c h)"), in_=ptw[:, 0:NCH * 4]),
                         reads=[Bptw], writes=[B_wgtT[d]])
                    S.op("dve", lambda e: e.tensor_copy(out=thrT[d][:].rearrange("p c h -> p (c h)"), in_=ptt[:, 0:NCH * 4]),
                         reads=[Bptt], writes=[B_thrT[d]])
                    for h in range(4):
                        S.op("pe", lambda e: e.matmul(pdc[:, h * NCH:(h + 1) * NCH], lhsT=sel[0:4, h, :], rhs=dec[0:4, :], start=True, stop=True),
                             reads=[Bsel, Bdec], writes=[Bpdc], inc=(h == 3))
                    S.op("dve", lambda e: e.tensor_copy(out=decB[d][:].rearrange("p h c -> p (h c)"), in_=pdc[:, 0:4 * NCH]),
                         reads=[Bpdc], writes=[B_decB[d]])
                if dbgD_d is not None:
                    pass
                S.barrier()

            with contextlib.ExitStack() as pe_:
                cmask = sb(pe_, "cmask", [128, 3, 128], BF16)
                mlg = sb(pe_, "mlg", [128, 512], F32)
                B_cm, B_mlg = Buf(), Buf()
                S.dma(cmask[:], cmask_d.rearrange("a p q -> p a q"), writes=[B_cm])
                S.dma(mlg[:], mlg_d, writes=[B_mlg])
                QT = [sb(pe_, "QT%d" % i, [128, 4, 128], BF16) for i in range(2)]
                KT = [sb(pe_, "KT%d" % i, [128, 4, 128], BF16) for i in range(2)]
                Kt = [sb(pe_, "Kt%d" % i, [128, 4, 128], BF16) for i in range(2)]
                Vt = [sb(pe_, "Vt%d" % i, [128, 4, 128], BF16) for i in range(2)]
                HF = [sb(pe_, "HF%d" % i, [128, 512], F32) for i in range(2)]
                SO = [sb(pe_, "SO%d" % i, [128, 512], F32) for i in range(2)]
                B_QT, B_KT, B_Kt, B_Vt, B_HF, B_SO = [[Buf(), Buf()] for _ in range(6)]
                vp = [sb(pe_, "vp%d" % i, [128, 4, 129], BF16) for i in range(2)]
                B_vp = [Buf(), Buf()]
                sm = [sb(pe_, "sm%d" % i, [128, 128], BF16) for i in range(4)]
                B_sm = [Buf() for _ in range(4)]
                Cst = sb(pe_, "Cst", [128, 4, 129], F32)
                B_Cst = [Buf() for _ in range(4)]
                Cdb = [sb(pe_, "Cdb%d" % i, [128, 4, 129], BF16) for i in range(2)]
                B_Cdb = [[Buf() for _ in range(4)] for _ in range(2)]
                hst = [sb(pe_, "hst%d" % i, [128, 4, 128], F32) for i in range(2)]
                B_hst = [Buf(), Buf()]
                dn = sb(pe_, "dn", [128, 4], F32)
                B_dn = Buf()
                hsq = sb(pe_, "hsq", [128, 512], F32)
                ss = sb(pe_, "ss", [128, 4], F32)
                go = sb(pe_, "go", [128, 512], F32)
                mlo_t = [sb(pe_, "mlo_t%d" % i, [128, 512], BF16) for i in range(2)]
                B_hsq, B_ss, B_go = Buf(), Buf(), Buf()
                B_mlo = [Buf(), Buf()]
                sps = [ps(pe_, "sps%d" % i, [128, 4, 128], F32) for i in range(2)]
                B_sps = [Buf(), Buf()]
                hps = [ps(pe_, "hps%d" % i, [128, 2, 512], F32) for i in range(2)]
                B_hps = [Buf(), Buf()]
                cps2 = ps(pe_, "cps2", [128, 2, 512], F32)
                B_cps2 = [Buf() for _ in range(4)]

                def blk_of(d, c):
                    if d == 0:
                        return c
                    return (1 - c) if c < 2 else (67 - c)

                nch_run = NCH if upto != "E1" else 5
                for d in range(2):
                    S.op("dve", lambda e: e.memset(Cst[:], 0.0), writes=B_Cst)

                    def load_chunk(c):
                        if c >= nch_run:
                            return
                        blk = blk_of(d, c)
                        b = c % 2
                        rows = slice(blk * 128, (blk + 1) * 128)
                        S.dma(Kt[b][:].rearrange("p h d -> p (h d)"), kml_d[rows, :], writes=[B_Kt[b]])
                        S.dma(Vt[b][:].rearrange("p h d -> p (h d)"), mlv_d[rows, :], writes=[B_Vt[b]])
                        if blk >= 2:
                            S.dma(QT[b][:], qkT_d[0:4, :, rows].rearrange("c p t -> p c t"), writes=[B_QT[b]])
                            S.dma(KT[b][:], qkT_d[4:8, :, rows].rearrange("c p t -> p c t"), writes=[B_KT[b]])
                            if d == 1:
                                S.dma(HF[b][:], hf_d[(blk - 2) * 128:(blk - 1) * 128, :], writes=[B_HF[b]])
                                S.dma(SO[b][:], mlo_d[rows, :], writes=[B_SO[b]])
                    load_chunk(0)
                    for c in range(nch_run):
                        load_chunk(c + 1)
                        blk = blk_of(d, c)
                        b = c % 2
                        lat = blk >= 2
                        vpb = vp[b]
                        S.op("dve", lambda e: e.tensor_tensor(out=vpb[:, :, 0:128], in0=Vt[b][:, :, :],
                                                              in1=wgtT[d][:, blk, :].unsqueeze(2).to_broadcast([128, 4, 128]), op=ALU.mult),
                             reads=[B_Vt[b], B_wgtT[d]], writes=[B_vp[b]])
                        S.op("dve", lambda e: e.tensor_copy(out=vpb[:, :, 128], in_=wgtT[d][:, blk, :]),
                             reads=[B_wgtT[d]], writes=[B_vp[b]])
                        hp = hps[b]
                        cdb = Cdb[b]
                        for h in range(4):
                            S.op("act", lambda e: e.activation(out=cdb[:, h, :], in_=Cst[:, h, :], func=AF.Copy, scale=decB[d][:, h, c:c + 1]),
                                 reads=[B_Cst[h], B_decB[d]], writes=[B_Cdb[b][h]])
                            if lat:
                                S.op("pe", lambda e: e.matmul(sps[b][:, h, :], lhsT=KT[b][:, h, :], rhs=QT[b][:, h, :], start=True, stop=True),
                                     reads=[B_KT[b], B_QT[b]], writes=[B_sps[b]], inc=(h == 3))
                        for h in range(4):
                            co = cps2[:, h // 2, (h % 2) * 129:(h % 2) * 129 + 129]
                            S.op("pe", lambda e: e.matmul(co, lhsT=Kt[b][:, h, :], rhs=vpb[:, h, :], start=True, stop=True),
                                 reads=[B_Kt[b], B_vp[b]], writes=[B_cps2[h]])
                            if lat:
                                S.op("dve", lambda e: e.tensor_tensor(out=sm[h][:, :], in0=sps[b][:, h, :], in1=cmask[:, d, :], op=ALU.mult),
                                     reads=[B_sps[b], B_cm], writes=[B_sm[h]])
                        if lat:
                            for h in range(4):
                                ho = hp[:, h // 2, (h % 2) * 129:(h % 2) * 129 + 129]
                                S.op("pe", lambda e: e.matmul(ho, lhsT=sm[h][:, :], rhs=vpb[:, h, :], start=True, stop=False),
                                     reads=[B_sm[h], B_vp[b]], writes=[B_hps[b]], inc=False)
                                S.op("pe", lambda e: e.matmul(ho, lhsT=QT[b][:, h, :], rhs=cdb[:, h, :], start=False, stop=True),
                                     reads=[B_QT[b], B_Cdb[b][h]], writes=[B_hps[b]], inc=(h == 3))
                        for h in range(4):
                            co = cps2[:, h // 2, (h % 2) * 129:(h % 2) * 129 + 129]
                            S.op("dve", lambda e: e.scalar_tensor_tensor(out=Cst[:, h, :], in0=Cst[:, h, :], scalar=decB[d][:, h, c:c + 1], in1=co,
                                                                         op0=ALU.mult, op1=ALU.add),
                                 reads=[B_Cst[h], B_decB[d], B_cps2[h], B_Cdb[b][h]], writes=[B_Cst[h]])
                        if not lat:
                            continue
                        hv = hp[:, :, 0:258].rearrange("p a (h d) -> p a h d", d=129)
                        S.op("act", lambda e: e.activation(out=dn[:].rearrange("p (a h) -> p a h", a=2), in_=hv[:, :, :, 128], func=AF.Abs),
                             reads=[B_hps[b]], writes=[B_dn])
                        S.op("dve", lambda e: e.tensor_tensor(out=dn[:], in0=dn[:], in1=thrT[d][:, blk, :], op=ALU.max),
                             reads=[B_dn, B_thrT[d]], writes=[B_dn])
                        S.op("dve", lambda e: e.reciprocal(out=dn[:], in_=dn[:]), reads=[B_dn], writes=[B_dn])
                        hs = hst[b]
                        for a in range(2):
                            S.op("dve", lambda e: e.tensor_tensor(out=hs[:, a * 2:(a + 1) * 2, :], in0=hv[:, a, :, 0:128],
                                                                  in1=dn[:, a * 2:(a + 1) * 2].unsqueeze(2).to_broadcast([128, 2, 128]), op=ALU.mult),
                                 reads=[B_hps[b], B_dn], writes=[B_hst[b]])
                        lt = blk - 2
                        if d == 0:
                            S.dma(hf_d[lt * 128:(lt + 1) * 128, :], hs[:].rearrange("p h d -> p (h d)"), reads=[B_hst[b]])
                        else:
                            hsf = hs[:].rearrange("p h d -> p (h d)")
                            S.op("pool", lambda e: e.tensor_tensor(out=hsf, in0=hsf, in1=HF[b][:, :], op=ALU.add),
                                 reads=[B_hst[b], B_HF[b]], writes=[B_hst[b]])
                            S.op("pool", lambda e: e.tensor_tensor(out=hsq[:, :], in0=hsf, in1=hsf, op=ALU.mult),
                                 reads=[B_hst[b]], writes=[B_hsq])
                            S.op("dve", lambda e: e.tensor_reduce(out=ss[:, :], in_=hsq[:].rearrange("p (h d) -> p h d", d=128), axis=AX.X, op=ALU.add),
                                 reads=[B_hsq], writes=[B_ss])
                            S.op("dve", lambda e: e.tensor_scalar(out=ss[:, :], in0=ss[:, :], scalar1=1.0 / 128.0, scalar2=EPS, op0=ALU.mult, op1=ALU.add),
                                 reads=[B_ss], writes=[B_ss])
                            S.op("pool", lambda e: e.tensor_tensor(out=ss[:, :], in0=ss[:, :], in1=neghalf_c[:, 0:1].to_broadcast([128, 4]), op=ALU.pow),
                                 reads=[B_ss, B_const], writes=[B_ss])
                            S.op("pool", lambda e: e.tensor_tensor(out=go[:, :], in0=SO[b][:, :], in1=mlg[:, :], op=ALU.mult),
                                 reads=[B_SO[b], B_mlg], writes=[B_go])
                            S.op("dve", lambda e: e.tensor_tensor(out=hs[:, :, :], in0=hs[:, :, :],
                                                                  in1=ss[:, :].unsqueeze(2).to_broadcast([128, 4, 128]), op=ALU.mult),
                                 reads=[B_hst[b], B_ss], writes=[B_hst[b]])
                            S.op("dve", lambda e: e.tensor_tensor(out=mlo_t[b][:, :], in0=hsf, in1=go[:, :], op=ALU.mult),
                                 reads=[B_hst[b], B_go], writes=[B_mlo[b]])
                            S.dma(ml_d[lt * 128:(lt + 1) * 128, :], mlo_t[b][:, :], reads=[B_mlo[b]])
                    S.barrier()
        if upto in ("E", "E1"):
            S.final_wait()
            return nc

        if "G" in phases:
          bg_step(100000)
          with contextlib.ExitStack() as pg0:
            W12 = sb(pg0, "W12", [128, NT, 2], F32)
            D1i = sb(pg0, "D1i", [128, NT], I32)
            D2i = sb(pg0, "D2i", [128, NT], I32)
            ebrow = sb(pg0, "ebrow", [1, 256], I32)
            idxw = sb(pg0, "idxw", [128, 256], I32)
            B_idxw = Buf()
            lnp = sb(pg0, "lnp", [128, 4, D], F32)
            B_W12, B_D1, B_D2, B_eb, B_lnp = Buf(), Buf(), Buf(), Buf(), Buf()
            S.dma(lnp[:], lnp_d.rearrange("a p f -> p a f"), writes=[B_lnp])

            def layer_norm_stats(x_ap, Bx, st6, mv, rstd, nmr, Bs):
                S.op("dve", lambda e: e.bn_stats(out=st6[:, 0, :], in_=x_ap[:, 0:512]), reads=[Bx], writes=[Bs])
                S.op("dve", lambda e: e.bn_stats(out=st6[:, 1, :], in_=x_ap[:, 512:1024]), reads=[Bx], writes=[Bs])
                S.op("dve", lambda e: e.bn_aggr(out=mv[:], in_=st6[:].rearrange("p a b -> p (a b)")), reads=[Bs], writes=[Bs])
                S.op("dve", lambda e: e.tensor_scalar(out=rstd[:], in0=mv[:, 1:2], scalar1=EPS, scalar2=None, op0=ALU.add), reads=[Bs], writes=[Bs])
                S.op("pool", lambda e: e.tensor_tensor(out=rstd[:], in0=rstd[:], in1=neghalf_c[:], op=ALU.pow), reads=[Bs, B_const], writes=[Bs])
                S.op("dve", lambda e: e.scalar_tensor_tensor(out=nmr[:], in0=mv[:, 0:1], scalar=-1.0, in1=rstd[:], op0=ALU.mult, op1=ALU.mult),
                     reads=[Bs], writes=[Bs])

            with contextlib.ExitStack() as pg:
                wout_sb = sb(pg, "wout_sb", [128, 8, D], BF16)
                wstg = [sb(pg, "wstg%d" % i, [128, D], F32) for i in range(2)]
                B_wout, B_wstg = Buf(), [Buf(), Buf()]
                wout_v = wout_d.rearrange("(k p) n -> p k n", p=128)
                for k in range(8):
                    S.dma(wstg[k % 2][:], wout_v[:, k, :], writes=[B_wstg[k % 2]])
                    S.op("dve", lambda e: e.tensor_tensor(out=wout_sb[:, k, :], in0=wstg[k % 2][:], in1=g_b[:, 0:1024], op=ALU.mult),
                         reads=[B_wstg[k % 2], B_gb], writes=[B_wout])
                wr_sb = sb(pg, "wr_sb", [128, 8, 72], BF16)
                rbias = sb(pg, "rbias", [128, 72], F32)
                SU = sb(pg, "SU", [128, 128], BF16)
                ones_bf = sb(pg, "ones_bf", [128, 128], BF16)
                bvals = sb(pg, "bvals", [128, 3], F32)
                B_wr, B_rb, B_SU, B_bv = Buf(), Buf(), Buf(), Buf()
                S.dma(wr_sb[:], wr_d.rearrange("(k p) n -> p k n", p=128), writes=[B_wr], eng="pool")
                S.dma(rbias[:], rbias_d, writes=[B_rb])
                S.dma(SU[:], cmask_d[2], writes=[B_SU])
                S.dma(bvals[:], bvals_d, writes=[B_bv])
                S.op("dve", lambda e: e.memset(ones_bf[:], 1.0), writes=[B_const])
                M1 = sb(pg, "M1", [128, NT, 64], BF16)
                M2 = sb(pg, "M2", [128, NT, 64], BF16)
                RK = sb(pg, "RK", [128, NT, 64], F32)
                big = sb(pg, "big", [128, NT, 64], F32)
                run = sb(pg, "run", [128, 64], F32)
                B_M1, B_M2, B_RK, B_big, B_run = Buf(), Buf(), Buf(), Buf(), Buf()
                S.op("dve", lambda e: e.memset(run[:], 0.0), writes=[B_run])
                xg = [sb(pg, "xg%d" % i, [128, D], F32) for i in range(2)]
                mix = [sb(pg, "mix%d" % i, [128, D], BF16) for i in range(2)]
                B_xg, B_mix = [Buf(), Buf()], [Buf(), Buf()]
                mixT = sb(pg, "mixT", [128, 8, 128], BF16)
                xm_ = sb(pg, "xm_", [128, D], F32)
                xmid = [sb(pg, "xmid%d" % i, [128, D], F32) for i in range(2)]
                xn2 = sb(pg, "xn2", [128, D], BF16)
                h2T = sb(pg, "h2T", [128, 8, 128], BF16)
                h2 = [sb(pg, "h2_%d" % i, [128, D], BF16) for i in range(2)]
                B_mixT, B_xm, B_xmid, B_xn2, B_h2T, B_h2 = Buf(), Buf(), [Buf(), Buf()], Buf(), Buf(), [Buf(), Buf()]
                st6 = sb(pg, "gst6", [128, 2, 6], F32)
                mv = sb(pg, "gmv", [128, 2], F32)
                rstd = sb(pg, "grstd", [128, 1], F32)
                nmr = sb(pg, "gnmr", [128, 1], F32)
                B_s1 = Buf()
                lg = sb(pg, "lg", [128, 72], F32)
                sm8 = sb(pg, "sm8", [128, 16], F32)
                gm = sb(pg, "gm", [128, 8], F32)
                ge = sb(pg, "ge", [128, 8], F32)
                elm = sb(pg, "elm", [128, 64], F32)
                top8 = sb(pg, "top8", [128, 8], F32)
                m12 = sb(pg, "m12", [128, 64], BF16)
                B_lg, B_sm8, B_gm, B_elm, B_top8, B_m12 = Buf(), Buf(), Buf(), Buf(), Buf(), Buf()
                tpg = [ps(pg, "tpg%d" % i, [128, 1024], BF16) for i in range(2)]
                B_tpg = [Buf(), Buf()]
                ops_ = ps(pg, "ops_", [128, 2, 512], F32)
                B_ops = Buf()
                tpb = ps(pg, "tpb", [128, 1024], BF16)
                B_tpb = Buf()
                lps = ps(pg, "lps", [128, 512], F32)
                B_lps = Buf()
                rkps = ps(pg, "rkps", [128, 512], F32)
                B_rkps = Buf()

                nt_run = NT if upto not in ("G1",) else 2

                def load_tile(i):
                    if i >= nt_run:
                        return
                    rows = slice(i * 128, (i + 1) * 128)
                    S.dma(xg[i % 2][:], x_d[rows, :], writes=[B_xg[i % 2]])
                    S.dma(mix[i % 2][:, 0:512], na_d[rows, :], writes=[B_mix[i % 2]])
                    S.dma(mix[i % 2][:, 512:1024], ml_d[rows, :], writes=[B_mix[i % 2]])
                st6b = sb(pg, "gst6b", [128, 2, 6], F32)
                mvb = sb(pg, "gmvb", [128, 2], F32)
                rstdb = sb(pg, "grstdb", [128, 1], F32)
                nmrb = sb(pg, "gnmrb", [128, 1], F32)
                B_s1b = Buf()
                lps2 = ps(pg, "lps2", [128, 512], F32)
                lpsb = [lps, lps2]
                B_lpsb = [B_lps, Buf()]

                def stage_a(i):
                    rows = slice(i * 128, (i + 1) * 128)
                    mx = mix[i % 2]
                    tp_ = tpg[0]
                    for k in range(8):
                        S.op("pe", lambda e: e.transpose(out=tp_[:, k * 128:(k + 1) * 128], in_=mx[:, k * 128:(k + 1) * 128], identity=ident_bf[:]),
                             reads=[B_mix[i % 2], B_const], writes=[B_tpg[0]], inc=(k == 7))
                    yield
                    S.op("act", lambda e: e.activation(out=mixT[:, 0:4, :], in_=tp_[:, 0:512].rearrange("p (k t) -> p k t", t=128), func=AF.Copy),
                         reads=[B_tpg[0]], writes=[B_mixT])
                    yield
                    S.op("dve", lambda e: e.tensor_copy(out=mixT[:, 4:8, :], in_=tp_[:, 512:1024].rearrange("p (k t) -> p k t", t=128)),
                         reads=[B_tpg[0]], writes=[B_mixT])
                    yield
                    for n in range(2):
                        for k in range(8):
                            S.op("pe", lambda e: e.matmul(ops_[:, n, :], lhsT=mixT[:, k, :], rhs=wout_sb[:, k, n * 512:(n + 1) * 512],
                                                          start=(k == 0), stop=(k == 7)),
                                 reads=[B_mixT, B_wout], writes=[B_ops], inc=(k == 7 and n == 1))
                    yield
                    S.op("dve", lambda e: e.scalar_tensor_tensor(out=xm_[:].rearrange("p (n f) -> p n f", n=2), in0=xg[i % 2][:].rearrange("p (n f) -> p n f", n=2),
                                                                 scalar=ALPHA, in1=ops_[:, :, :], op0=ALU.mult, op1=ALU.add),
                         reads=[B_xg[i % 2], B_ops], writes=[B_xm])
                    yield
                    load_tile(i + 2)
                    for _ in layer_norm_stats_g(xm_, B_xm, st6, mv, rstd, nmr, B_s1):
                        yield
                    xmd = xmid[i % 2]
                    S.op("act", lambda e: e.activation(out=xmd[:], in_=xm_[:], func=AF.Identity, bias=nmr[:, 0:1], scale=rstd[:, 0:1]),
                         reads=[B_xm, B_s1], writes=[B_xmid[i % 2]])
                    yield
                    S.op("pool", lambda e: e.tensor_tensor(out=xmd[:], in0=xmd[:], in1=lnp[:, 0, :], op=ALU.mult),
                         reads=[B_xmid[i % 2], B_lnp], writes=[B_xmid[i % 2]])
                    yield
                    S.op("dve", lambda e: e.tensor_tensor(out=xmd[:], in0=xmd[:], in1=lnp[:, 1, :], op=ALU.add),
                         reads=[B_xmid[i % 2], B_lnp], writes=[B_xmid[i % 2]])
                    yield
                    S.dma(xmid_d[rows, :], xmd[:], reads=[B_xmid[i % 2]])
                    yield

                def layer_norm_stats_g(x_ap, Bx, st6_, mv_, rstd_, nmr_, Bs):
                    S.op("dve", lambda e: e.bn_stats(out=st6_[:, 0, :], in_=x_ap[:, 0:512]), reads=[Bx], writes=[Bs])
                    yield
                    S.op("dve", lambda e: e.bn_stats(out=st6_[:, 1, :], in_=x_ap[:, 512:1024]), reads=[Bx], writes=[Bs])
                    yield
                    S.op("dve", lambda e: e.bn_aggr(out=mv_[:], in_=st6_[:].rearrange("p a b -> p (a b)")), reads=[Bs], writes=[Bs])
                    S.op("dve", lambda e: e.tensor_scalar(out=rstd_[:], in0=mv_[:, 1:2], scalar1=EPS, scalar2=None, op0=ALU.add), reads=[Bs], writes=[Bs])
                    yield
                    S.op("pool", lambda e: e.tensor_tensor(out=rstd_[:], in0=rstd_[:], in1=neghalf_c[:], op=ALU.pow), reads=[Bs, B_const], writes=[Bs])
                    yield
                    S.op("dve", lambda e: e.scalar_tensor_tensor(out=nmr_[:], in0=mv_[:, 0:1], scalar=-1.0, in1=rstd_[:], op0=ALU.mult, op1=ALU.mult),
                         reads=[Bs], writes=[Bs])
                    yield

                def stage_b(i):
                    rows = slice(i * 128, (i + 1) * 128)
                    xmd = xmid[i % 2]
                    for _ in layer_norm_stats_g(xmd, B_xmid[i % 2], st6b, mvb, rstdb, nmrb, B_s1b):
                        yield
                    S.op("act", lambda e: e.activation(out=xn2[:], in_=xmd[:], func=AF.Identity, bias=nmrb[:, 0:1], scale=rstdb[:, 0:1]),
                         reads=[B_xmid[i % 2], B_s1b], writes=[B_xn2])
                    yield
                    tp2 = tpg[1]
                    for k in range(8):
                        S.op("pe", lambda e: e.transpose(out=tp2[:, k * 128:(k + 1) * 128], in_=xn2[:, k * 128:(k + 1) * 128], identity=ident_bf[:]),
                             reads=[B_xn2, B_const], writes=[B_tpg[1]], inc=(k == 7))
                    yield
                    for k in range(8):
                        o = h2T[:, k, :]
                        i_ = tp2[:, k * 128:(k + 1) * 128]
                        sc = adaT[:, 32 + k, 0:1]
                        sh = adaT[:, 24 + k, 0:1]
                        if k % 2 == 0:
                            S.op("act", lambda e: e.activation(out=o, in_=i_, func=AF.Identity, bias=sh, scale=sc),
                                 reads=[B_tpg[1], B_ada], writes=[B_h2T])
                        else:
                            S.op("dve", lambda e: e.tensor_scalar(out=o, in0=i_, scalar1=sc, scalar2=sh, op0=ALU.mult, op1=ALU.add),
                                 reads=[B_tpg[1], B_ada], writes=[B_h2T])
                        yield
                    for k in range(8):
                        S.op("pe", lambda e: e.transpose(out=tpb[:, k * 128:(k + 1) * 128], in_=h2T[:, k, :], identity=ident_bf[:]),
                             reads=[B_h2T, B_const], writes=[B_tpb], inc=(k == 7))
                    lp = lpsb[i % 2]
                    for k in range(8):
                        S.op("pe", lambda e: e.matmul(lp[:, 0:72], lhsT=h2T[:, k, :], rhs=wr_sb[:, k, :], start=(k == 0), stop=(k == 7)),
                             reads=[B_h2T, B_wr], writes=[B_lpsb[i % 2]], inc=(k == 7))
                    yield
                    h2b = h2[i % 2]
                    S.op("act", lambda e: e.activation(out=h2b[:, 0:512], in_=tpb[:, 0:512], func=AF.Copy), reads=[B_tpb], writes=[B_h2[i % 2]])
                    yield
                    S.op("dve", lambda e: e.tensor_copy(out=h2b[:, 512:1024], in_=tpb[:, 512:1024]), reads=[B_tpb], writes=[B_h2[i % 2]])
                    yield
                    S.dma(h2_d[rows, :], h2b[:], reads=[B_h2[i % 2]])
                    yield

                def stage_c(i):
                    lp = lpsb[i % 2]
                    Bl = B_lpsb[i % 2]
                    S.op("dve", lambda e: e.tensor_tensor(out=lg[:], in0=lp[:, 0:72], in1=rbias[:], op=ALU.add), reads=[Bl, B_rb], writes=[B_lg])
                    yield
                    S.op("dve", lambda e: e.reduce_max(out=sm8[:, 0:1], in_=lg[:, 0:8], axis=AX.X), reads=[B_lg], writes=[B_sm8])
                    yield
                    S.op("dve", lambda e: e.tensor_scalar(out=sm8[:, 1:2], in0=sm8[:, 0:1], scalar1=-1.0, scalar2=None, op0=ALU.mult),
                         reads=[B_sm8], writes=[B_sm8])
                    yield
                    S.op("act", lambda e: e.activation(out=ge[:], in_=lg[:, 0:8], func=AF.Exp, bias=sm8[:, 1:2], scale=1.0, accum_out=sm8[:, 2:3]),
                         reads=[B_lg, B_sm8], writes=[B_sm8, B_gm])
                    yield
                    S.op("dve", lambda e: e.tensor_scalar(out=gm[:], in0=lg[:, 0:8], scalar1=sm8[:, 0:1], scalar2=None, op0=ALU.is_ge),
                         reads=[B_lg, B_sm8, B_gm], writes=[B_gm])
                    yield
                    S.op("dve", lambda e: e.tensor_scalar(out=gm[:], in0=gm[:], scalar1=1e9, scalar2=-1e9, op0=ALU.mult, op1=ALU.add),
                         reads=[B_gm], writes=[B_gm])
                    yield
                    S.op("dve", lambda e: e.tensor_tensor(out=elm[:].rearrange("p (g e) -> p g e", e=8), in0=lg[:, 8:72].rearrange("p (g e) -> p g e", e=8),
                                                          in1=gm[:, :].unsqueeze(2).to_broadcast([128, 8, 8]), op=ALU.add),
                         reads=[B_lg, B_gm], writes=[B_elm])
                    yield
                    S.op("dve", lambda e: e.max(out=top8[:], in_=elm[:]), reads=[B_elm], writes=[B_top8])
                    yield
                    S.op("dve", lambda e: e.tensor_scalar(out=M1[:, i, :], in0=elm[:], scalar1=top8[:, 0:1], scalar2=None, op0=ALU.is_ge),
                         reads=[B_elm, B_top8], writes=[B_M1])
                    yield
                    S.op("dve", lambda e: e.tensor_scalar(out=m12[:], in0=elm[:], scalar1=top8[:, 1:2], scalar2=None, op0=ALU.is_ge),
                         reads=[B_elm, B_top8], writes=[B_m12])
                    yield
                    S.op("dve", lambda e: e.tensor_tensor(out=M2[:, i, :], in0=m12[:], in1=M1[:, i, :], op=ALU.subtract),
                         reads=[B_m12, B_M1], writes=[B_M2])
                    yield
                    S.op("dve", lambda e: e.tensor_tensor(out=sm8[:, 3:4], in0=top8[:, 1:2], in1=top8[:, 0:1], op=ALU.subtract),
                         reads=[B_top8, B_sm8], writes=[B_sm8])
                    yield
                    S.op("act", lambda e: e.activation(out=sm8[:, 4:5], in_=sm8[:, 3:4], func=AF.Exp), reads=[B_sm8], writes=[B_sm8])
                    yield
                    S.op("dve", lambda e: e.tensor_scalar(out=sm8[:, 5:6], in0=sm8[:, 4:5], scalar1=1.0, scalar2=sm8[:, 2:3], op0=ALU.add, op1=ALU.mult),
                         reads=[B_sm8], writes=[B_sm8])
                    yield
                    S.op("dve", lambda e: e.reciprocal(out=W12[:, i, 0:1], in_=sm8[:, 5:6]), reads=[B_sm8], writes=[B_W12])
                    yield
                    S.op("dve", lambda e: e.tensor_tensor(out=W12[:, i, 1:2], in0=W12[:, i, 0:1], in1=sm8[:, 4:5], op=ALU.mult),
                         reads=[B_sm8, B_W12], writes=[B_W12])
                    yield
                    S.op("pe", lambda e: e.matmul(rkps[:, 0:64], lhsT=SU[:, :], rhs=m12[:, :], start=True, stop=True),
                         reads=[B_SU, B_m12], writes=[B_rkps], inc=False)
                    S.op("pe", lambda e: e.matmul(rkps[:, 64:128], lhsT=ones_bf[:, :], rhs=m12[:, :], start=True, stop=True),
                         reads=[B_const, B_m12], writes=[B_rkps])
                    yield
                    S.op("dve", lambda e: e.tensor_tensor(out=RK[:, i, :], in0=rkps[:, 0:64], in1=run[:], op=ALU.add),
                         reads=[B_rkps, B_run], writes=[B_RK])
                    yield
                    S.op("dve", lambda e: e.tensor_tensor(out=run[:], in0=rkps[:, 64:128], in1=run[:], op=ALU.add),
                         reads=[B_rkps, B_run, B_RK], writes=[B_run])
                    yield

                load_tile(0)
                load_tile(1)
                for step in range(nt_run + 2):
                    gens = []
                    if step < nt_run:
                        gens.append(stage_a(step))
                    if 0 <= step - 1 < nt_run:
                        gens.append(stage_b(step - 1))
                    if 0 <= step - 2 < nt_run:
                        gens.append(stage_c(step - 2))
                    interleave(*gens)

                szi = sb(pg, "szi", [128, 64], I32)
                pad = sb(pg, "pad", [128, 64], F32)
                cum = sb(pg, "cum", [128, 64], F32)
                pst = sb(pg, "pst", [128, 64], F32)
                z64 = sb(pg, "z64", [128, 64], F32)
                cmpb = sb(pg, "cmpb", [128, 64], F32)
                ebf = sb(pg, "ebf", [128, 2], F32)
                ebr = sb(pg, "ebr", [1, 256], F32)
                B_bk = Buf()
                S.op("dve", lambda e: e.memset(z64[:], 0.0), writes=[B_bk])
                S.op("dve", lambda e: e.tensor_scalar(out=szi[:], in0=run[:], scalar1=127.0, scalar2=None, op0=ALU.add), reads=[B_run], writes=[B_bk])
                S.op("dve", lambda e: e.tensor_scalar(out=szi[:], in0=szi[:], scalar1=7, scalar2=None, op0=ALU.arith_shift_right), reads=[B_bk], writes=[B_bk])
                S.op("dve", lambda e: e.tensor_scalar(out=szi[:], in0=szi[:], scalar1=7, scalar2=None, op0=ALU.logical_shift_left), reads=[B_bk], writes=[B_bk])
                S.op("dve", lambda e: e.tensor_copy(out=pad[:], in_=szi[:]), reads=[B_bk], writes=[B_bk])
                S.op("dve", lambda e: e.tensor_tensor_scan(out=cum[:], data0=z64[:], data1=pad[:], initial=0.0, op0=ALU.add, op1=ALU.add),
                     reads=[B_bk], writes=[B_bk])
                S.op("dve", lambda e: e.tensor_tensor(out=pst[:], in0=cum[:], in1=pad[:], op=ALU.subtract), reads=[B_bk], writes=[B_bk])
                for j in range(2):
                    S.op("dve", lambda e: e.tensor_scalar(out=cmpb[:], in0=cum[:], scalar1=bvals[:, j:j + 1], scalar2=None, op0=ALU.is_le),
                         reads=[B_bk, B_bv], writes=[B_bk])
                    S.op("dve", lambda e: e.reduce_sum(out=ebf[:, j:j + 1], in_=cmpb[:], axis=AX.X), reads=[B_bk], writes=[B_bk])
                S.op("dve", lambda e: e.tensor_scalar(out=ebf[:], in0=ebf[:], scalar1=63.0, scalar2=None, op0=ALU.min), reads=[B_bk], writes=[B_bk])
                for j in range(2):
                    S.op("pe", lambda e: e.matmul(lps[0:1, j * 128:(j + 1) * 128], lhsT=ebf[:, j:j + 1], rhs=ident_f[:, :], start=True, stop=True),
                         reads=[B_bk, B_const], writes=[B_lps], inc=(j == 1))
                S.op("dve", lambda e: e.tensor_copy(out=ebr[0:1, :], in_=lps[0:1, 0:256]), reads=[B_lps], writes=[B_bk])
                S.op("dve", lambda e: e.tensor_copy(out=ebrow[0:1, :], in_=ebr[0:1, :]), reads=[B_bk], writes=[B_eb])
                idr = sb(pg, "idr", [1, 256], F32)
                samer = sb(pg, "samer", [1, 256], F32)
                S.op("dve", lambda e: e.memset(samer[0:1, :], 0.0), writes=[B_bk])
                S.op("dve", lambda e: e.tensor_tensor(out=samer[0:1, 2:256], in0=ebr[0:1, 2:256], in1=ebr[0:1, 0:254], op=ALU.is_equal),
                     reads=[B_bk], writes=[B_bk])
                S.op("dve", lambda e: e.tensor_scalar(out=idr[0:1, :], in0=ebr[0:1, :], scalar1=128.0, scalar2=None, op0=ALU.mult),
                     reads=[B_bk], writes=[B_bk])
                S.op("dve", lambda e: e.scalar_tensor_tensor(out=idr[0:1, :], in0=samer[0:1, :], scalar=1048576.0, in1=idr[0:1, :],
                                                             op0=ALU.mult, op1=ALU.add), reads=[B_bk], writes=[B_bk])
                S.op("pe", lambda e: e.matmul(lps[:, 0:256], lhsT=ones_f[0:1, :], rhs=idr[0:1, :], start=True, stop=True),
                     reads=[B_bk, B_const], writes=[B_lps])
                idxf = sb(pg, "idxf", [128, 256], F32)
                S.op("dve", lambda e: e.tensor_scalar(out=idxf[:], in0=lps[:, 0:256], scalar1=bvals[:, 2:3], scalar2=None, op0=ALU.add),
                     reads=[B_lps, B_bv], writes=[B_bk])
                S.op("dve", lambda e: e.tensor_copy(out=idxw[:], in_=idxf[:]), reads=[B_bk], writes=[B_idxw])
                S.op("dve", lambda e: e.tensor_tensor(out=RK[:], in0=RK[:], in1=pst[:, :].unsqueeze(1).to_broadcast([128, NT, 64]), op=ALU.add),
                     reads=[B_RK, B_bk], writes=[B_RK])
                dsf = sb(pg, "dsf", [128, NT], F32)
                for (Mx, Bm, Dx, Bd) in ((M1, B_M1, D1i, B_D1), (M2, B_M2, D2i, B_D2)):
                    S.op("dve", lambda e: e.tensor_tensor(out=big[:], in0=RK[:], in1=Mx[:], op=ALU.mult), reads=[B_RK, Bm], writes=[B_big])
                    S.op("dve", lambda e: e.tensor_reduce(out=dsf[:], in_=big[:], axis=AX.X, op=ALU.add), reads=[B_big], writes=[B_bk])
                    S.op("dve", lambda e: e.tensor_copy(out=Dx[:], in_=dsf[:]), reads=[B_bk], writes=[Bd])
                if rt_dbg is not None:
                    S.op("dve", lambda e: e.tensor_copy(out=big[:, :, 0:1].rearrange("p t o -> p (t o)"), in_=D1i[:]), reads=[B_D1], writes=[B_big])
                    S.op("dve", lambda e: e.tensor_copy(out=big[:, :, 1:2].rearrange("p t o -> p (t o)"), in_=D2i[:]), reads=[B_D2], writes=[B_big])
                    S.op("dve", lambda e: e.tensor_copy(out=big[:, :, 2:4], in_=W12[:]), reads=[B_W12], writes=[B_big])
                    S.dma(rt_dbg, big[:, :, 0:4], reads=[B_big])
                    S.dma(eb_dbg, ebrow[:], reads=[B_eb])
                    if ix_dbg is not None:
                        S.dma(ix_dbg, idxw[:], reads=[B_idxw])
                for i in range(nt_run):
                    rows = slice(i * 128, (i + 1) * 128)
                    hb = h2[i % 2]
                    S.dma(hb[:], h2_d[rows, :], writes=[B_h2[i % 2]])
                    for (Dx, Bd) in ((D1i, B_D1), (D2i, B_D2)):
                        S.dma(None, None, reads=[B_h2[i % 2], Bd], eng="pool",
                              indirect=lambda e: e.indirect_dma_start(out=xperm_d[:, :], out_offset=bass.IndirectOffsetOnAxis(ap=Dx[:, i:i + 1], axis=0),
                                                                      in_=hb[:, :], in_offset=None))
                S.barrier()
            if upto in ("G", "G1"):
                S.final_wait()
                return nc

            with contextlib.ExitStack() as ph:
                w1b = [sb(ph, "w1b%d" % i, [128, 8, HID], BF16) for i in range(2)]
                w3b = [sb(ph, "w3b%d" % i, [128, 8, HID], BF16) for i in range(2)]
                w2b = [sb(ph, "w2b%d" % i, [128, 4, D], BF16) for i in range(2)]
                B_wb = [[Buf(), Buf(), Buf()] for _ in range(2)]
                xb = [sb(ph, "xb%d" % i, [128, D], BF16) for i in range(3)]
                B_xb = [Buf(), Buf(), Buf()]
                xbT = [sb(ph, "xbT%d" % i, [128, 8, 128], BF16) for i in range(2)]
                sa = [sb(ph, "sa%d" % i, [128, HID], F32) for i in range(2)]
                hh = [sb(ph, "hh%d" % i, [128, HID], BF16) for i in range(2)]
                hhT = [sb(ph, "hhT%d" % i, [128, 4, 128], BF16) for i in range(2)]
                yb = [sb(ph, "yb%d" % i, [128, D], F32) for i in range(2)]
                B_xbT, B_sa, B_hh, B_hhT, B_yb = [[Buf(), Buf()] for _ in range(5)]
                tpp = [ps(ph, "tpp%d" % i, [128, 1024], BF16) for i in range(2)]
                B_tpp = [Buf(), Buf()]
                agp = [ps(ph, "agp%d" % i, [128, 2, 512], F32) for i in range(2)]
                B_agp = [Buf(), Buf()]
                yps = ps(ph, "yps", [128, 2, 512], F32)
                B_yps = Buf()
                nb_run = NBLK if upto != "H1" else 4
                tpc = [0]
                bnd_reg = nc.gpsimd.to_reg(NE * 128 - 1)

                def load_w(b, which):
                    if b >= nb_run or b < 0:
                        return
                    q = b % 2
                    for m, wt_ in enumerate((w1b[q], w3b[q], w2b[q])):
                        if m not in which:
                            continue
                        S.dma(None, None, reads=[B_idxw], writes=[B_wb[q][m]], eng="pool",
                              indirect=lambda e: e.indirect_dma_start(out=wt_[:].rearrange("p k n -> p (k n)"), out_offset=None, in_=wbf_d[m][:, :],
                                                                      in_offset=bass.IndirectOffsetOnAxis(ap=idxw[:, b:b + 1], axis=0),
                                                                      bounds_check=bnd_reg, oob_is_err=False))

                def load_x(b):
                    if b >= nb_run:
                        return
                    S.dma(xb[b % 3][:], xperm_d[b * 128:(b + 1) * 128, :], writes=[B_xb[b % 3]])

                def st_T(b):
                    q = b % 2
                    t_ = tpc[0] % 2
                    tpc[0] += 1
                    for k in range(8):
                        S.op("pe", lambda e: e.transpose(out=tpp[t_][:, k * 128:(k + 1) * 128], in_=xb[b % 3][:, k * 128:(k + 1) * 128], identity=ident_bf[:]),
                             reads=[B_xb[b % 3], B_const], writes=[B_tpp[t_]], inc=(k == 7))
                    S.op("act", lambda e: e.activation(out=xbT[q][:, 0:4, :], in_=tpp[t_][:, 0:512].rearrange("p (k t) -> p k t", t=128), func=AF.Copy),
                         reads=[B_tpp[t_]], writes=[B_xbT[q]])
                    S.op("dve", lambda e: e.tensor_copy(out=xbT[q][:, 4:8, :], in_=tpp[t_][:, 512:1024].rearrange("p (k t) -> p k t", t=128)),
                         reads=[B_tpp[t_]], writes=[B_xbT[q]])

                def st_mm1(b):
                    q = b % 2
                    for k in range(8):
                        S.op("pe", lambda e: e.matmul(agp[q][:, 0, :], lhsT=xbT[q][:, k, :], rhs=w1b[q][:, k, :], start=(k == 0), stop=(k == 7)),
                             reads=[B_xbT[q], B_wb[q][0]], writes=[B_agp[q]], inc=False)
                    for k in range(8):
                        S.op("pe", lambda e: e.matmul(agp[q][:, 1, :], lhsT=xbT[q][:, k, :], rhs=w3b[q][:, k, :], start=(k == 0), stop=(k == 7)),
                             reads=[B_xbT[q], B_wb[q][1]], writes=[B_agp[q]], inc=(k == 7))
                    S.op("act", lambda e: e.activation(out=sa[q][:], in_=agp[q][:, 0, :], func=AF.Silu), reads=[B_agp[q]], writes=[B_sa[q]])
                    S.op("dve", lambda e: e.tensor_tensor(out=hh[q][:], in0=sa[q][:], in1=agp[q][:, 1, :], op=ALU.mult),
                         reads=[B_sa[q], B_agp[q]], writes=[B_hh[q]])

                def st_Th(b):
                    q = b % 2
                    t_ = tpc[0] % 2
                    tpc[0] += 1
                    for k in range(4):
                        S.op("pe", lambda e: e.transpose(out=tpp[t_][:, k * 128:(k + 1) * 128], in_=hh[q][:, k * 128:(k + 1) * 128], identity=ident_bf[:]),
                             reads=[B_hh[q], B_const], writes=[B_tpp[t_]], inc=(k == 3))
                    S.op("act", lambda e: e.activation(out=hhT[q][:, :, :], in_=tpp[t_][:, 0:512].rearrange("p (k t) -> p k t", t=128), func=AF.Copy),
                         reads=[B_tpp[t_]], writes=[B_hhT[q]])

                def st_mm2(b):
                    q = b % 2
                    for n in range(2):
                        for k in range(4):
                            S.op("pe", lambda e: e.matmul(yps[:, n, :], lhsT=hhT[q][:, k, :], rhs=w2b[q][:, k, n * 512:(n + 1) * 512],
                                                          start=(k == 0), stop=(k == 3)),
                                 reads=[B_hhT[q], B_wb[q][2]], writes=[B_yps], inc=(k == 3 and n == 1))
                    S.op("act", lambda e: e.activation(out=yb[q][:, 0:512], in_=yps[:, 0, :], func=AF.Copy), reads=[B_yps], writes=[B_yb[q]])
                    S.op("dve", lambda e: e.tensor_copy(out=yb[q][:, 512:1024], in_=yps[:, 1, :]), reads=[B_yps], writes=[B_yb[q]])
                    S.dma(yperm_d[b * 128:(b + 1) * 128, :], yb[q][:], reads=[B_yb[q]])

                load_w(0, (0, 1))
                load_w(1, (0, 1))
                load_w(0, (2,))
                load_x(0)
                load_x(1)
                load_x(2)
                st_T(0)
                for t in range(nb_run + 1):
                    load_x(t + 3)
                    if t >= 1:
                        st_Th(t - 1)
                    if t + 1 < nb_run:
                        st_T(t + 1)
                    if t < nb_run:
                        st_mm1(t)
                        load_w(t + 2, (0, 1))
                    if t >= 1:
                        st_mm2(t - 1)
                    load_w(t + 1, (2,))
                S.barrier()
            if upto in ("H", "H1"):
                S.final_wait()
                return nc

            with contextlib.ExitStack() as pi:
                y1 = [sb(pi, "y1_%d" % i, [128, D], F32) for i in range(2)]
                y2 = [sb(pi, "y2_%d" % i, [128, D], F32) for i in range(2)]
                xmt = [sb(pi, "xmt%d" % i, [128, D], F32) for i in range(2)]
                B_y1, B_y2, B_xmt = [Buf(), Buf()], [Buf(), Buf()], [Buf(), Buf()]
                acc = sb(pi, "acc", [128, D], F32)
                ot = [sb(pi, "ot%d" % i, [128, D], F32) for i in range(2)]
                B_acc, B_ot = Buf(), [Buf(), Buf()]
                st6 = sb(pi, "ist6", [128, 2, 6], F32)
                mv = sb(pi, "imv", [128, 2], F32)
                rstd = sb(pi, "irstd", [128, 1], F32)
                nmr = sb(pi, "inmr", [128, 1], F32)
                B_s2 = Buf()

                def load_i(i):
                    if i >= NT:
                        return
                    q = i % 2
                    S.dma(xmt[q][:], xmid_d[i * 128:(i + 1) * 128, :], writes=[B_xmt[q]])
                    for (yt, By, Dx, Bd) in ((y1[q], B_y1[q], D1i, B_D1), (y2[q], B_y2[q], D2i, B_D2)):
                        S.dma(None, None, reads=[Bd], writes=[By], eng="pool",
                              indirect=lambda e: e.indirect_dma_start(out=yt[:, :], out_offset=None, in_=yperm_d[:, :],
                                                                      in_offset=bass.IndirectOffsetOnAxis(ap=Dx[:, i:i + 1], axis=0)))
                acc2 = [acc, sb(pi, "acc_b", [128, D], F32)]
                B_acc2 = [B_acc, Buf()]

                def ln_stats_i(x_ap, Bx, Bs):
                    S.op("dve", lambda e: e.bn_stats(out=st6[:, 0, :], in_=x_ap[:, 0:512]), reads=[Bx], writes=[Bs])
                    yield
                    S.op("dve", lambda e: e.bn_stats(out=st6[:, 1, :], in_=x_ap[:, 512:1024]), reads=[Bx], writes=[Bs])
                    yield
                    S.op("dve", lambda e: e.bn_aggr(out=mv[:], in_=st6[:].rearrange("p a b -> p (a b)")), reads=[Bs], writes=[Bs])
                    S.op("dve", lambda e: e.tensor_scalar(out=rstd[:], in0=mv[:, 1:2], scalar1=EPS, scalar2=None, op0=ALU.add), reads=[Bs], writes=[Bs])
                    yield
                    S.op("pool", lambda e: e.tensor_tensor(out=rstd[:], in0=rstd[:], in1=neghalf_c[:], op=ALU.pow), reads=[Bs, B_const], writes=[Bs])
                    yield
                    S.op("dve", lambda e: e.scalar_tensor_tensor(out=nmr[:], in0=mv[:, 0:1], scalar=-1.0, in1=rstd[:], op0=ALU.mult, op1=ALU.mult),
                         reads=[Bs], writes=[Bs])
                    yield

                def stage_1(i):
                    q = i % 2
                    ac, Ba = acc2[q], B_acc2[q]
                    S.op("dve", lambda e: e.tensor_scalar(out=ac[:], in0=y1[q][:], scalar1=W12[:, i, 0:1], scalar2=None, op0=ALU.mult),
                         reads=[B_y1[q], B_W12], writes=[Ba])
                    yield
                    S.op("dve", lambda e: e.scalar_tensor_tensor(out=ac[:], in0=y2[q][:], scalar=W12[:, i, 1:2], in1=ac[:], op0=ALU.mult, op1=ALU.add),
                         reads=[B_y2[q], B_W12, Ba], writes=[Ba])
                    yield
                    S.op("pool", lambda e: e.tensor_tensor(out=ac[:], in0=ac[:], in1=g_b[:, 1024:2048], op=ALU.mult),
                         reads=[Ba, B_gb], writes=[Ba])
                    yield
                    S.op("dve", lambda e: e.scalar_tensor_tensor(out=ac[:], in0=xmt[q][:], scalar=ALPHA, in1=ac[:], op0=ALU.mult, op1=ALU.add),
                         reads=[B_xmt[q], Ba], writes=[Ba])
                    yield
                    load_i(i + 2)
                    yield

                def stage_2(i):
                    q = i % 2
                    ac, Ba = acc2[q], B_acc2[q]
                    for _ in ln_stats_i(ac, Ba, B_s2):
                        yield
                    S.op("act", lambda e: e.activation(out=ot[q][:], in_=ac[:], func=AF.Identity, bias=nmr[:, 0:1], scale=rstd[:, 0:1]),
                         reads=[Ba, B_s2], writes=[B_ot[q]])
                    yield
                    S.op("pool", lambda e: e.tensor_tensor(out=ot[q][:], in0=ot[q][:], in1=lnp[:, 2, :], op=ALU.mult),
                         reads=[B_ot[q], B_lnp], writes=[B_ot[q]])
                    yield
                    S.op("dve", lambda e: e.tensor_tensor(out=ot[q][:], in0=ot[q][:], in1=lnp[:, 3, :], op=ALU.add),
                         reads=[B_ot[q], B_lnp], writes=[B_ot[q]])
                    yield
                    S.dma(out_d[i * 128:(i + 1) * 128, :], ot[q][:], reads=[B_ot[q]])
                    yield

                load_i(0)
                load_i(1)
                for step in range(NT + 1):
                    gens = []
                    if step < NT:
                        gens.append(stage_1(step))
                    if 0 <= step - 1 < NT:
                        gens.append(stage_2(step - 1))
                    interleave(*gens)
                S.barrier()

        S.final_wait()
    return nc


def make_in_maps(inputs, n_cores=8):
    x = np.asarray(inputs["x"], np.float32)
    c = np.asarray(inputs["c"], np.float32)
    ctx = np.asarray(inputs["ctx"], np.float32)
    c_ctx = np.asarray(inputs["c_ctx"], np.float32)
    w_ada = np.ascontiguousarray(np.asarray(inputs["w_ada"], np.float32)[0])
    b_ada = np.asarray(inputs["b_ada"], np.float32)[0]
    w_in = np.ascontiguousarray(np.asarray(inputs["w_in"], np.float32)[0])
    gate_b = np.asarray(inputs["gate_b"], np.float32)[0]
    b_ada_l = np.ascontiguousarray(np.repeat(b_ada.reshape(48, 128).T[:, :, None], 2, axis=2))
    rope = make_rope_tables()
    rperm = make_rperm()
    sidx = np.arange(128)
    cmask = np.stack([(sidx[None, :] >= sidx[:, None]), (sidx[:, None] >= sidx[None, :]), (sidx[None, :] > sidx[:, None])]).astype(np.float32).astype(ml_dtypes.bfloat16)
    conv_w = np.asarray(inputs["conv_w"], np.float32)[0]
    conv_b = np.asarray(inputs["conv_b"], np.float32)[0]
    convw_l = np.ascontiguousarray(conv_w.reshape(5, 8, 128).transpose(2, 1, 0))
    convb_l = np.ascontiguousarray(conv_b.reshape(8, 128).T)
    mlg = np.ascontiguousarray(np.broadcast_to(np.asarray(inputs["ml_norm_g"], np.float32)[0][None, :], (128, 512)))
    w_out = np.ascontiguousarray(np.asarray(inputs["w_out"], np.float32)[0])
    w_r = np.ascontiguousarray(np.concatenate([np.asarray(inputs["w_router_g"], np.float32)[0],
                                               np.asarray(inputs["w_router_e"], np.float32)[0]], axis=1))
    rb = np.concatenate([np.asarray(inputs["b_router_g"], np.float32)[0], np.asarray(inputs["b_router_e"], np.float32)[0]])
    rbias = np.ascontiguousarray(np.broadcast_to(rb[None, :], (128, 72)))
    lnp = np.stack([np.broadcast_to(np.asarray(inputs[k], np.float32)[0][None, :], (128, D))
                    for k in ("ln1_g", "ln1_b", "ln2_g", "ln2_b")]).astype(np.float32)
    bvals = np.concatenate([128.0 * (np.arange(128)[:, None] + 128 * np.arange(2)[None, :]), np.arange(128)[:, None]], axis=1).astype(np.float32)
    relay = lambda w, kc, n: np.ascontiguousarray(np.asarray(w, np.float32)[0].reshape(NE, kc, 128, n).transpose(0, 2, 1, 3).reshape(NE * 128, kc * n))
    w1 = relay(inputs["w1"], 8, HID)
    w3 = relay(inputs["w3"], 8, HID)
    w2 = relay(inputs["w2"], 4, D)
    biasT = make_bias_tables(np.asarray(inputs["rpb"], np.float32)[0])
    maps = []
    for b in range(n_cores):
        cc = np.stack([c[b].reshape(8, 128).T, c_ctx.reshape(8, 128).T], axis=-1)
        maps.append({
            "x": np.ascontiguousarray(x[b]),
            "ctx": np.ascontiguousarray(ctx[b]),
            "cc": np.ascontiguousarray(cc),
            "w_ada": w_ada,
            "b_ada_l": b_ada_l,
            "b_ada_r": np.ascontiguousarray(b_ada.reshape(1, -1)),
            "w_in": w_in,
            "gate_b": np.ascontiguousarray(gate_b.reshape(16, 1)),
            "biasT": biasT,
            "w_out": w_out, "w_r": w_r, "rbias": rbias, "lnp": np.ascontiguousarray(lnp), "bvals": bvals,
            "w1": w1, "w3": w3, "w2": w2,
            "rope": rope, "rperm": rperm, "cmask": cmask, "convw": convw_l, "convb": convb_l, "mlg": mlg,
        })
    return maps


def make_bias_tables(rpb):
    rpb = np.asarray(rpb, np.float32)
    out = np.empty((5, 128, 8, 5, 128), np.float32)
    q = np.arange(128)
    k = np.arange(128)
    for v, i in enumerate((2, 0, 1, 62, 63)):
        jb0 = min(max(i - 2, 0), 59)
        qr = 2 * i + q // 64
        qc = q % 64
        r0 = np.clip(qr - 4, 0, 120)
        c0 = np.clip(qc - 8, 0, 48)
        for n in range(5):
            kr = 2 * (jb0 + n) + k // 64
            kc = k % 64
            inside = ((kr[:, None] >= r0[None, :]) & (kr[:, None] < r0[None, :] + 8) &
                      (kc[:, None] >= c0[None, :]) & (kc[:, None] < c0[None, :] + 16))
            ri = np.clip(kr[:, None] - qr[None, :] + 7, 0, 14)
            ci = np.clip(kc[:, None] - qc[None, :] + 15, 0, 30)
            g = rpb[:, ri, ci]
            out[v, :, :, n, :] = np.where(inside[None], g, np.float32(NEG)).transpose(1, 0, 2)
    return out.reshape(5, 128, 8 * 5 * 128).astype(ml_dtypes.bfloat16)


def make_rope_tables():
    t = np.arange(T)
    row = (t // GW).astype(np.float64)
    col = (t % GW).astype(np.float64)
    f = np.arange(128)
    inv = 10000.0 ** (-(f % 32).astype(np.float64) / 32.0)
    pos = np.where((f // 64 == 0)[:, None], row[None, :], col[None, :])
    ang = (pos.astype(np.float32) * inv.astype(np.float32)[:, None]).astype(np.float32)
    cs, sn = np.cos(ang).astype(np.float32), np.sin(ang).astype(np.float32)
    ksc = np.float32(128.0 ** -0.5)
    return np.stack([cs, sn, cs * ksc, sn * ksc]).astype(np.float32)


def make_rperm():
    r = np.zeros((128, 128), np.float32)
    for f in range(128):
        if f % 64 < 32:
            r[f + 32, f] = -1.0
        else:
            r[f - 32, f] = 1.0
    return r.astype(ml_dtypes.bfloat16)


def kernel(**inputs):
    nc = build_program()
    maps = make_in_maps(inputs)
    res = run_bass_kernel_spmd(nc, maps, core_ids=list(range(8)))
    out = np.stack([np.asarray(r["out"]) for r in res.results], axis=0)
    return out.astype(np.float32)
```

```python
import contextlib
import numpy as np
import ml_dtypes
import concourse.bass as bass
import concourse.mybir as mybir
from concourse.bass_utils import run_bass_kernel_spmd

F32 = mybir.dt.float32
BF16 = mybir.dt.bfloat16
I32 = mybir.dt.int32
U32 = mybir.dt.uint32
AF = mybir.ActivationFunctionType
ALU = mybir.AluOpType
AX = mybir.AxisListType

D = 1024
T = 8192
CTX = 256
TT = T + CTX
NCOL = 3600
NT = T // 128
NCT = CTX // 128
GW = 64
ALPHA = 2.0 ** 0.25
EPS = 1e-5
NE = 64
CAP = 2 * T + NE * 128
NBLK = CAP // 128
HID = 512
NEG = -30000.0


class Buf:
    __slots__ = ("w", "r", "name")

    def __init__(self, name=""):
        self.w = None
        self.r = []
        self.name = name


class Sched:
    ENG = ("pe", "act", "dve", "pool", "sp")
    LIM = 16000
    NDS = 24
    NDS_SP = 16

    def __init__(self, nc, es):
        self.nc = nc
        self.es = es
        self.e = dict(pe=nc.tensor, act=nc.scalar, dve=nc.vector, pool=nc.gpsimd, sp=nc.sync)
        self.nsem = 0
        self.sems = {}
        self.epoch = {k: 0 for k in self.ENG}
        self.cnt = {k: 0 for k in self.ENG}
        self.seen = {k: {} for k in self.ENG}
        self.pending = {k: [] for k in self.ENG}
        self.dep = [0] * self.NDS
        self.dcnt = [0] * self.NDS
        self.dnext = 0
        self.dnext_q = {}
        self.n_inst = 0

    def semobj(self, key):
        if key not in self.sems:
            self.sems[key] = self.es.enter_context(self.nc.semaphore("s%d" % self.nsem))
            self.nsem += 1
        return self.sems[key]

    def _wait(self, eng, ev):
        key, val = ev
        if self.seen[eng].get(key, 0) >= val:
            return
        self.e[eng].wait_ge(self.semobj(key), val)
        self.seen[eng][key] = val
        self.n_inst += 1

    def _deps(self, eng, reads, writes):
        deps = set()
        for b in reads:
            if b.w is not None:
                deps.add(b.w)
        for b in writes:
            if b.w is not None:
                deps.add(b.w)
            deps.update(b.r)
        for ev in deps:
            if ev is None:
                continue
            if eng == "pe" and ev[0][0] == "pe":
                continue
            self._wait(eng, ev)

    def _record(self, ev, reads, writes):
        for b in reads:
            b.r.append(ev)
            if len(b.r) > 12:
                b.r = b.r[-12:] if False else b.r
        for b in writes:
            b.w = ev
            b.r = []

    def op(self, eng, fn, reads=(), writes=(), inc=True):
        self._deps(eng, reads, writes)
        inst = fn(self.e[eng])
        self.n_inst += 1
        if not inc:
            self.pending[eng].append((list(reads), list(writes)))
            return inst
        if self.cnt[eng] >= self.LIM:
            self.epoch[eng] += 1
            self.cnt[eng] = 0
        self.cnt[eng] += 1
        key = (eng, self.epoch[eng])
        inst.then_inc(self.semobj(key), 1)
        ev = (key, self.cnt[eng])
        for (r, w) in self.pending[eng]:
            self._record(ev, r, w)
        self.pending[eng] = []
        self._record(ev, reads, writes)
        return inst

    def dma(self, out, in_, reads=(), writes=(), eng="sp", indirect=None):
        lo, hi = (0, self.NDS_SP) if eng == "sp" else (self.NDS_SP, self.NDS)
        i = self.dnext_q.get(eng, lo)
        self.dnext_q[eng] = lo + ((i + 1 - lo) % (hi - lo))
        if self.dcnt[i] > 0:
            self._wait(eng, (("d", i, self.dep[i]), 16 * self.dcnt[i]))
        if self.dcnt[i] >= self.LIM // 16:
            self.dep[i] += 1
            self.dcnt[i] = 0
        self._deps(eng, reads, writes)
        if indirect is None:
            inst = self.e[eng].dma_start(out=out, in_=in_)
        else:
            inst = indirect(self.e[eng])
        self.n_inst += 1
        self.dcnt[i] += 1
        key = ("d", i, self.dep[i])
        inst.then_inc(self.semobj(key), 16)
        ev = (key, 16 * self.dcnt[i])
        self._record(ev, reads, writes)
        return ev

    def barrier(self):
        for k in self.ENG:
            assert not self.pending[k], "pending group at barrier"
        evs = []
        for k in self.ENG:
            if self.cnt[k] > 0:
                evs.append(((k, self.epoch[k]), self.cnt[k]))
        for i in range(self.NDS):
            if self.dcnt[i] > 0:
                evs.append((("d", i, self.dep[i]), 16 * self.dcnt[i]))
        for k in self.ENG:
            for ev in evs:
                if ev[0][0] == k:
                    continue
                self._wait(k, ev)

    def final_wait(self, eng="sp"):
        for i in range(self.NDS):
            if self.dcnt[i] > 0:
                self._wait(eng, (("d", i, self.dep[i]), 16 * self.dcnt[i]))


def interleave(*gens):
    gens = [g for g in gens if g is not None]
    while gens:
        for g in list(gens):
            try:
                next(g)
            except StopIteration:
                gens.remove(g)


def weighted(gen, n):
    def g():
        done = False
        while not done:
            for _ in range(n):
                try:
                    next(gen)
                except StopIteration:
                    done = True
                    break
            yield
    return g()


def build_program(dbg=None, upto="all", phases="ABCDEFGHI"):
    dbg = dbg or []
    nc = bass.Bass("TRN2", target_bir_lowering=False)

    def din(name, shape, dt=F32):
        return nc.dram_tensor(name, list(shape), dt, kind="ExternalInput").ap()

    def dscr(name, shape, dt):
        kind = "ExternalOutput" if name in dbg else "Internal"
        return nc.dram_tensor(name, list(shape), dt, kind=kind).ap()

    x_d = din("x", [T, D])
    ctx_d = din("ctx", [CTX, D])
    cc_d = din("cc", [128, 8, 2])
    wada_d = din("w_ada", [D, 6 * D])
    bada_d = din("b_ada_l", [128, 48, 2])
    badar_d = din("b_ada_r", [1, 6 * D])
    win_d = din("w_in", [D, NCOL])
    gateb_d = din("gate_b", [16, 1])
    out_d = nc.dram_tensor("out", [T, D], F32, kind="ExternalOutput").ap()

    zT_d = dscr("zT", [16, 128, TT], BF16)
    vna_d = dscr("vna", [TT, 8 * 65], BF16)
    mlv_d = dscr("mlv", [TT, 512], BF16)
    mlo_d = dscr("mlo", [TT, 512], F32)
    gT_d = dscr("gT", [16, TT], F32)
    na_d = dscr("na", [T, 512], BF16)
    ml_d = dscr("ml", [T, 512], BF16)
    biasT_d = din("biasT", [5, 128, 8 * 5 * 128], BF16)
    qkT_d = dscr("qkT", [8, 128, TT], BF16)
    kml_d = dscr("kml", [TT, 512], BF16)
    hf_d = dscr("hf", [T, 512], F32)
    rope_d = din("rope", [4, 128, T])
    rperm_d = din("rperm", [128, 128], BF16)
    cmask_d = din("cmask", [3, 128, 128], BF16)
    convw_d = din("convw", [128, 8, 5])
    convb_d = din("convb", [128, 8])
    mlg_d = din("mlg", [128, 512])
    wout_d = din("w_out", [D, D])
    wr_d = din("w_r", [D, 72])
    rbias_d = din("rbias", [128, 72])
    lnp_d = din("lnp", [4, 128, D])
    bvals_d = din("bvals", [128, 3])
    w1_d = din("w1", [NE * 128, 8 * HID])
    w3_d = din("w3", [NE * 128, 8 * HID])
    w2_d = din("w2", [NE * 128, 4 * D])
    wbf_d = [dscr("wbf%d" % m, [NE * 128, 4096], BF16) for m in range(3)]
    xmid_d = dscr("xmid", [T, D], F32)
    h2_d = dscr("h2", [T, D], BF16)
    xperm_d = dscr("xperm", [CAP, D], BF16)
    yperm_d = dscr("yperm", [CAP, D], F32)
    rt_dbg = dscr("rt_dbg", [128, NT, 4], F32) if "rt_dbg" in dbg else None
    eb_dbg = dscr("eb_dbg", [1, 256], I32) if "eb_dbg" in dbg else None
    ix_dbg = dscr("ix_dbg", [128, 256], I32) if "ix_dbg" in dbg else None
    dbgD_d = dscr("dbgD", [2, 3, 4, TT], F32) if "dbgD" in dbg else None
    ada_dbg = dscr("ada_dbg", [128, 48, 2], F32) if "ada_dbg" in dbg else None
    gb_dbg = dscr("gb_dbg", [128, 2048], F32) if "gb_dbg" in dbg else None

    with contextlib.ExitStack() as es:
        S = Sched(nc, es)

        def sb(st, name, shape, dt):
            return st.enter_context(nc.sbuf_tensor("sb_" + name, list(shape), dt))

        def ps(st, name, shape, dt):
            return st.enter_context(nc.psum_tensor("ps_" + name, list(shape), dt))

        ident_bf = sb(es, "ident_bf", [128, 128], BF16)
        ident_f = sb(es, "ident_f", [128, 128], F32)
        ones_f = sb(es, "ones_f", [128, 128], F32)
        adaT = sb(es, "adaT", [128, 48, 2], F32)
        g_b = sb(es, "g_b", [128, 2048], F32)
        B_const = Buf("const")
        B_ada = Buf("ada")
        B_gb = Buf("gb")

        def mk_ident(tile):
            S.op("pool", lambda e: e.memset(tile[:], 1.0), writes=[B_const])
            S.op("pool", lambda e: e.affine_select(out=tile[:], in_=tile[:], pattern=[[1, 128]],
                                                   compare_op=ALU.is_equal, fill=0.0, base=0,
                                                   channel_multiplier=-1), reads=[B_const], writes=[B_const])
        mk_ident(ident_f)
        S.op("dve", lambda e: e.tensor_copy(out=ident_bf[:], in_=ident_f[:]), reads=[B_const], writes=[B_const])
        S.op("dve", lambda e: e.memset(ones_f[:], 1.0), writes=[B_const])
        neghalf_c = sb(es, "neghalf_c", [128, 1], F32)
        S.op("dve", lambda e: e.memset(neghalf_c[:], -0.5), writes=[B_const])


        def conv_task():
            srcs = (w1_d, w3_d, w2_d)
            for r0 in range(0, NE * 128, 128):
                for m in range(3):
                    S.dma(wbf_d[m][r0:r0 + 128, :], srcs[m][r0:r0 + 128, :], reads=bg_pace, eng="pool")
                    yield
        bg_task = conv_task() if "H" in phases else iter(())

        bg_pace = []

        def bg_step(n, pace=None):
            bg_pace[:] = [pace] if pace is not None else []
            for _ in range(n):
                try:
                    next(bg_task)
                except StopIteration:
                    return

        with contextlib.ExitStack() as pa:
            cc = sb(pa, "cc", [128, 8, 2], F32)
            scc = sb(pa, "scc", [128, 8, 2], F32)
            badal = sb(pa, "badal", [128, 48, 2], F32)
            badar = sb(pa, "badar", [1, 6 * D], F32)
            wp = [sb(pa, "wadap%d" % i, [128, 8, 1024], F32) for i in range(2)]
            grow = sb(pa, "grow", [1, 2048], F32)
            adaps = ps(pa, "adaps", [128, 512], F32)
            rowps = ps(pa, "rowps", [1, 512], F32)
            bcps = ps(pa, "bcps", [128, 512], F32)
            B_cc, B_scc, B_bl, B_br = Buf(), Buf(), Buf(), Buf()
            B_wp = [Buf(), Buf()]
            B_adaps, B_rowps, B_bcps, B_grow = Buf(), Buf(), Buf(), Buf()

            S.dma(cc[:], cc_d, writes=[B_cc])
            S.dma(badal[:], bada_d, writes=[B_bl])
            S.dma(badar[:], badar_d, writes=[B_br])
            S.op("act", lambda e: e.activation(out=scc[:], in_=cc[:], func=AF.Silu), reads=[B_cc], writes=[B_scc])
            wada_v = wada_d.rearrange("(k p) n -> p k n", p=128)
            for pc in range(6):
                S.dma(wp[pc % 2][:], wada_v[:, :, pc * 1024:(pc + 1) * 1024], writes=[B_wp[pc % 2]])
                w = wp[pc % 2]
                if pc in (2, 5):
                    gi = 0 if pc == 2 else 1
                    for grp in range(2):
                        for k in range(8):
                            S.op("pe", lambda e, k=k, grp=grp: e.matmul(
                                rowps[0:1, :], lhsT=scc[:, k, 0:1], rhs=w[:, k, grp * 512:(grp + 1) * 512],
                                start=(k == 0), stop=(k == 7)),
                                reads=[B_scc, B_wp[pc % 2]], writes=[B_rowps], inc=(k == 7))
                        S.op("dve", lambda e, grp=grp: e.tensor_tensor(
                            out=grow[0:1, gi * 1024 + grp * 512: gi * 1024 + (grp + 1) * 512], in0=rowps[0:1, :],
                            in1=badar[0:1, pc * 1024 + grp * 512: pc * 1024 + (grp + 1) * 512], op=ALU.add),
                            reads=[B_rowps, B_br], writes=[B_grow])
                        S.op("pe", lambda e, grp=grp: e.matmul(
                            bcps[:, :], lhsT=ones_f[0:1, :], rhs=grow[0:1, gi * 1024 + grp * 512: gi * 1024 + (grp + 1) * 512],
                            start=True, stop=True), reads=[B_grow, B_const], writes=[B_bcps])
                        S.op("act", lambda e, grp=grp: e.activation(
                            out=g_b[:, gi * 1024 + grp * 512: gi * 1024 + (grp + 1) * 512], in_=bcps[:, :], func=AF.Copy),
                            reads=[B_bcps], writes=[B_gb])
                else:
                    for jj in range(8):
                        for k in range(8):
                            S.op("pe", lambda e, k=k, jj=jj: e.matmul(
                                adaps[:, jj * 2:(jj + 1) * 2], lhsT=w[:, k, jj * 128:(jj + 1) * 128], rhs=scc[:, k, :],
                                start=(k == 0), stop=(k == 7)),
                                reads=[B_scc, B_wp[pc % 2]], writes=[B_adaps], inc=(k == 7 and jj == 7))
                    S.op("dve", lambda e: e.tensor_tensor(
                        out=adaT[:, pc * 8:(pc + 1) * 8, :], in0=adaps[:, 0:16].rearrange("p (j s) -> p j s", s=2),
                        in1=badal[:, pc * 8:(pc + 1) * 8, :], op=ALU.add),
                        reads=[B_adaps, B_bl], writes=[B_ada])
                    if pc in (1, 4):
                        S.op("dve", lambda e: e.tensor_scalar(
                            out=adaT[:, pc * 8:(pc + 1) * 8, :], in0=adaT[:, pc * 8:(pc + 1) * 8, :],
                            scalar1=1.0, scalar2=None, op0=ALU.add), reads=[B_ada], writes=[B_ada])
            if ada_dbg is not None:
                S.dma(ada_dbg, adaT[:], reads=[B_ada])
                S.dma(gb_dbg, g_b[:], reads=[B_gb])
            S.barrier()
        if upto == "A":
            S.final_wait()
            return nc

        with contextlib.ExitStack() as pb:
            win_sb = sb(pb, "win_sb", [128, 8, NCOL], BF16)
            gateb = sb(pb, "gateb", [16, 1], F32)
            B_win, B_gateb = Buf(), Buf()
            win_v = win_d.rearrange("(k p) n -> p k n", p=128)
            for k in range(8):
                S.dma(win_sb[:, k, :], win_v[:, k, :], writes=[B_win], eng="pool")
            S.dma(gateb[:], gateb_d, writes=[B_gateb])

            NXB = 3
            xt = [sb(pb, "xt%d" % i, [128, D], F32) for i in range(NXB)]
            B_xt = [Buf() for _ in range(NXB)]
            xn = [sb(pb, "xn%d" % i, [128, D], BF16) for i in range(2)]
            B_xn = [Buf(), Buf()]
            st6 = sb(pb, "st6", [128, 2, 6], F32)
            mv = sb(pb, "mv", [128, 2], F32)
            rstd = sb(pb, "rstd", [128, 1], F32)
            nmr = sb(pb, "nmr", [128, 1], F32)
            neghalf = sb(pb, "neghalf", [128, 1], F32)
            B_st, B_mv, B_rstd, B_nmr = Buf(), Buf(), Buf(), Buf()
            S.op("dve", lambda e: e.memset(neghalf[:], -0.5), writes=[B_const])
            xmT = [sb(pb, "xmT%d" % i, [128, 8, 512], BF16) for i in range(2)]
            B_xmT = [Buf(), Buf()]
            tp = [ps(pb, "tp%d" % i, [128, 1024], BF16) for i in range(2)]
            B_tp = [Buf(), Buf()]
            fps = [ps(pb, "fps%d" % i, [128, 512], F32) for i in range(2)]
            B_fps = [Buf(), Buf()]
            tps = [ps(pb, "tps%d" % i, [128, 512], F32) for i in range(2)]
            B_tps = [Buf(), Buf()]
            gps = ps(pb, "gps", [16, 512], F32)
            B_gps = Buf()
            zst = [sb(pb, "zst%d" % i, [128, 16, 512], BF16) for i in range(2)]
            B_zst = [Buf(), Buf()]
            vst = [sb(pb, "vst%d" % i, [128, 4, 8 * 65], BF16) for i in range(2)]
            B_vst = [Buf(), Buf()]
            mvst = [sb(pb, "mvst%d" % i, [128, 4, 512], BF16) for i in range(2)]
            B_mvst = [Buf(), Buf()]
            ost = [sb(pb, "ost%d" % i, [128, 4, 512], F32) for i in range(2)]
            B_ost = [Buf(), Buf()]
            gst = [sb(pb, "gst%d" % i, [16, 512], F32) for i in range(2)]
            B_gst = [Buf(), Buf()]
            for i in range(2):
                S.op("dve", lambda e, i=i: e.memset(vst[i][:], 1.0), writes=[B_vst[i]])

            supers = [(0, NCT, 1, ctx_d)] + [(CTX + s * 512, 4, 0, x_d[s * 512:(s + 1) * 512, :]) for s in range(T // 512)]
            tile_list = []
            for si, (t0, ntl, stream, src) in enumerate(supers):
                for j in range(ntl):
                    tile_list.append((si, j))
            load_idx = [0]

            def issue_load(n):
                while load_idx[0] <= n and load_idx[0] < len(tile_list):
                    si, j = tile_list[load_idx[0]]
                    src = supers[si][3]
                    b = load_idx[0] % NXB
                    S.dma(xt[b][:], src[j * 128:(j + 1) * 128, :], writes=[B_xt[b]])
                    load_idx[0] += 1

            gtile = {}
            cnt = 0
            for si, (t0, ntl, stream, src) in enumerate(supers):
                for j in range(ntl):
                    gtile[(si, j)] = cnt
                    cnt += 1

            evac_rr = [0]

            def evac(out, in_, reads, writes, scale=None, eng=None):
                if eng is None:
                    eng = ("act", "dve")[evac_rr[0] % 2]
                    evac_rr[0] += 1
                if eng == "act":
                    if scale is None:
                        S.op("act", lambda e: e.activation(out=out, in_=in_, func=AF.Copy), reads=reads, writes=writes)
                    else:
                        S.op("act", lambda e: e.activation(out=out, in_=in_, func=AF.Copy, scale=float(scale)),
                             reads=reads, writes=writes)
                else:
                    if scale is None:
                        S.op("dve", lambda e: e.tensor_copy(out=out, in_=in_), reads=reads, writes=writes)
                    else:
                        S.op("dve", lambda e: e.tensor_scalar(out=out, in0=in_, scalar1=float(scale), scalar2=None,
                                                              op0=ALU.mult), reads=reads, writes=writes)

            def prep(si):
                t0, ntl, stream, src = supers[si]
                xm = xmT[si % 2]
                Bxm = B_xmT[si % 2]
                for j in range(ntl):
                    g = gtile[(si, j)]
                    issue_load(g + 2)
                    b = g % NXB
                    x_t = xt[b]
                    S.op("dve", lambda e: e.bn_stats(out=st6[:, 0, :], in_=x_t[:, 0:512]), reads=[B_xt[b]], writes=[B_st])
                    S.op("dve", lambda e: e.bn_stats(out=st6[:, 1, :], in_=x_t[:, 512:1024]), reads=[B_xt[b]], writes=[B_st])
                    S.op("dve", lambda e: e.bn_aggr(out=mv[:], in_=st6[:].rearrange("p a b -> p (a b)")), reads=[B_st], writes=[B_mv])
                    S.op("dve", lambda e: e.tensor_scalar(out=rstd[:], in0=mv[:, 1:2], scalar1=EPS, scalar2=None, op0=ALU.add),
                         reads=[B_mv], writes=[B_rstd])
                    S.op("pool", lambda e: e.tensor_tensor(out=rstd[:], in0=rstd[:], in1=neghalf[:], op=ALU.pow),
                         reads=[B_rstd, B_const], writes=[B_rstd])
                    S.op("dve", lambda e: e.scalar_tensor_tensor(out=nmr[:], in0=mv[:, 0:1], scalar=-1.0, in1=rstd[:],
                                                                 op0=ALU.mult, op1=ALU.mult), reads=[B_mv, B_rstd], writes=[B_nmr])
                    xnb = xn[g % 2]
                    pace = Buf()
                    S.op("act", lambda e: e.activation(out=xnb[:], in_=x_t[:], func=AF.Identity, bias=nmr[:, 0:1], scale=rstd[:, 0:1]),
                         reads=[B_xt[b], B_nmr, B_rstd], writes=[B_xn[g % 2], pace])
                    bg_step(2, pace)
                    yield
                    tpp = tp[g % 2]
                    for k in range(8):
                        S.op("pe", lambda e, k=k: e.transpose(out=tpp[:, k * 128:(k + 1) * 128], in_=xnb[:, k * 128:(k + 1) * 128],
                                                              identity=ident_bf[:]),
                             reads=[B_xn[g % 2], B_const], writes=[B_tp[g % 2]], inc=(k == 7))
                    for k in range(8):
                        o = xm[:, k, j * 128:(j + 1) * 128]
                        i_ = tpp[:, k * 128:(k + 1) * 128]
                        sc = adaT[:, 8 + k, stream:stream + 1]
                        sh = adaT[:, 0 + k, stream:stream + 1]
                        if k % 2 == 0:
                            S.op("act", lambda e, o=o, i_=i_, sc=sc, sh=sh: e.activation(out=o, in_=i_, func=AF.Identity, bias=sh, scale=sc),
                                 reads=[B_tp[g % 2], B_ada], writes=[Bxm])
                        else:
                            S.op("dve", lambda e, o=o, i_=i_, sc=sc, sh=sh: e.tensor_scalar(out=o, in0=i_, scalar1=sc, scalar2=sh,
                                                                                           op0=ALU.mult, op1=ALU.add),
                                 reads=[B_tp[g % 2], B_ada], writes=[Bxm])
                    yield

            FM_CH = [(c * 128) for c in range(0, 8)] + [1536 + c * 128 for c in range(0, 8)]

            def mm(si):
                t0, ntl, stream, src = supers[si]
                ntok = ntl * 128
                xm = xmT[si % 2]
                Bxm = B_xmT[si % 2]
                zs = zst[si % 2]
                for ci, c0 in enumerate(FM_CH):
                    p = fps[ci % 2]
                    for k in range(8):
                        S.op("pe", lambda e, k=k: e.matmul(p[:, 0:ntok], lhsT=win_sb[:, k, c0:c0 + 128], rhs=xm[:, k, 0:ntok],
                                                           start=(k == 0), stop=(k == 7)),
                             reads=[B_win, Bxm], writes=[B_fps[ci % 2]], inc=(k == 7))
                    evac(zs[:, ci, 0:ntok], p[:, 0:ntok], [B_fps[ci % 2]], [B_zst[si % 2]], scale=(0.125 if ci < 4 else None))
                    yield
                S.dma(zT_d[:, :, t0:t0 + ntok].rearrange("c p t -> p c t"), zs[:, :, 0:ntok], reads=[B_zst[si % 2]])
                for k in range(8):
                    S.op("pe", lambda e, k=k: e.matmul(gps[:, 0:ntok], lhsT=win_sb[:, k, 3584:3600], rhs=xm[:, k, 0:ntok],
                                                       start=(k == 0), stop=(k == 7)),
                         reads=[B_win, Bxm], writes=[B_gps], inc=(k == 7))
                gs = gst[si % 2]
                S.op("act", lambda e: e.activation(out=gs[:, 0:ntok], in_=gps[:, 0:ntok], func=AF.Identity, bias=gateb[:, 0:1], scale=1.0),
                     reads=[B_gps, B_gateb], writes=[B_gst[si % 2]])
                S.dma(gT_d[:, t0:t0 + ntok], gs[:, 0:ntok], reads=[B_gst[si % 2]])
                yield
                for j in range(ntl):
                    lhs = lambda k: xm[:, k, j * 128:(j + 1) * 128]
                    for gi, c0 in enumerate((1024, 2560, 3072)):
                        q = (j * 3 + gi) % 2
                        p = tps[q]
                        for k in range(8):
                            S.op("pe", lambda e, k=k: e.matmul(p[:, :], lhsT=lhs(k), rhs=win_sb[:, k, c0:c0 + 512],
                                                               start=(k == 0), stop=(k == 7)),
                                 reads=[B_win, Bxm], writes=[B_tps[q]], inc=(k == 7))
                        if gi == 0:
                            evac(vst[si % 2][:, j, :].rearrange("p (h d) -> p h d", d=65)[:, :, 0:64],
                                 p[:, :].rearrange("p (h d) -> p h d", d=64), [B_tps[q]], [B_vst[si % 2]])
                        elif gi == 1:
                            evac(mvst[si % 2][:, j, :], p[:, :], [B_tps[q]], [B_mvst[si % 2]])
                        else:
                            S.op("act", lambda e: e.activation(out=ost[si % 2][:, j, :], in_=p[:, :], func=AF.Sigmoid),
                                 reads=[B_tps[q]], writes=[B_ost[si % 2]])
                        yield
                tv = lambda d_: d_[t0:t0 + ntok, :].rearrange("(j p) f -> p j f", p=128)
                S.dma(tv(vna_d), vst[si % 2][:, 0:ntl, :], reads=[B_vst[si % 2]])
                S.dma(tv(mlv_d), mvst[si % 2][:, 0:ntl, :], reads=[B_mvst[si % 2]])
                S.dma(tv(mlo_d), ost[si % 2][:, 0:ntl, :], reads=[B_ost[si % 2]])
                yield

            nsup = len(supers) if upto != "B1" else 2
            issue_load(1)
            interleave(prep(0))
            for si in range(nsup):
                interleave(mm(si), weighted(prep(si + 1), 1) if si + 1 < nsup else None)
            S.barrier()
        if upto in ("B", "B1"):
            S.final_wait()
            return nc


        if "F" in phases:
          with contextlib.ExitStack() as pf:
            kT_sb = sb(pf, "kT_sb", [128, 4, TT], BF16)
            v_sb = sb(pf, "v_sb", [128, TT // 128, 520], BF16)
            biasI = sb(pf, "biasI", [128, 8, 5, 128], BF16)
            biasE = sb(pf, "biasE", [128, 8, 5, 128], BF16)
            B_kT, B_v, B_bI, B_bE = Buf(), Buf(), Buf(), Buf()
            B_kTs = [Buf() for _ in range(4)]
            B_vs = [Buf() for _ in range(6)]
            for c in range(4):
                S.dma(kT_sb[:, c, :], zT_d[4 + c, :, :], writes=[B_kTs[c]])
            vv = vna_d.rearrange("(j p) f -> p j f", p=128)
            for j0 in range(0, TT // 128, 11):
                S.dma(v_sb[:, j0:j0 + 11, :], vv[:, j0:j0 + 11, :], writes=[B_vs[j0 // 11]])
            S.dma(biasI[:].rearrange("p h n q -> p (h n q)"), biasT_d[0], writes=[B_bI])
            qT = [sb(pf, "qT%d" % i, [128, 4, 128], BF16) for i in range(4)]
            B_qT = [Buf() for _ in range(4)]
            NPT = 3
            pT = [sb(pf, "pT%d" % i, [128, 896], BF16) for i in range(NPT)]
            B_pT = [Buf() for _ in range(NPT)]
            sT = [ps(pf, "sT%d" % i, [128, 1024], F32) for i in range(3)]
            B_sT = [Buf(), Buf(), Buf()]
            sS = [sb(pf, "sS%d" % i, [128, 896], F32) for i in range(4)]
            B_sS = [Buf(), Buf(), Buf(), Buf()]
            biasE_b = sb(pf, "biasE_b", [128, 8, 5, 128], BF16)
            biasE2 = [biasE, biasE_b]
            B_bE2 = [B_bE, Buf()]
            po = [ps(pf, "po%d" % i, [128, 2, 512], F32) for i in range(1)]
            B_po = [Buf()]
            rec = sb(pf, "rec", [128, 8], F32)
            B_rec = Buf()
            ostg = [sb(pf, "ostg%d" % i, [128, 8, 64], BF16) for i in range(2)]
            B_ostg = [Buf(), Buf()]
            ntiles_f = NT if upto != "F1" else 3
            tiles_f = list(range(NT)) if upto != "F1" else [0, 1, 2, 30, 62, 63]

            def load_q(idx):
                if idx < len(tiles_f):
                    i = tiles_f[idx]
                    t0 = CTX + i * 128
                    S.dma(qT[idx % 4][:], zT_d[0:4, :, t0:t0 + 128].rearrange("c p t -> p c t"), writes=[B_qT[idx % 4]])

            steps = [(idx, i, h) for idx, i in enumerate(tiles_f) for h in range(8)]
            tile_bias = {}
            nedge = 0
            for idx, i in enumerate(tiles_f):
                variant = {0: 1, 1: 2, 62: 3, 63: 4}.get(i, 0)
                if variant:
                    tile_bias[idx] = (biasE2[nedge % 2], B_bE2[nedge % 2], variant)
                    nedge += 1
                else:
                    tile_bias[idx] = (biasI, B_bI, 0)
            started = set()
            tile_pace = {}

            def tile_start(idx):
                if idx in started or idx >= len(tiles_f):
                    return
                started.add(idx)
                load_q(idx + 3)
                if idx in tile_pace:
                    bg_step(1, tile_pace[idx])
                bt, Bb, variant = tile_bias[idx]
                if variant:
                    S.dma(bt[:].rearrange("p h n q -> p (h n q)"), biasT_d[variant], writes=[Bb])

            def emit_qk(sidx):
                if sidx >= len(steps):
                    return
                idx, i, h = steps[sidx]
                tile_start(idx)
                jb0 = min(max(i - 2, 0), 59)
                q = qT[idx % 4]
                bt, Bb, _ = tile_bias[idx]
                c = h // 2
                pb = (h % 2) * 64
                sTb = sT[sidx % 3]
                for n in range(7):
                    if n < 5:
                        k0 = CTX + (jb0 + n) * 128
                    else:
                        k0 = (n - 5) * 128
                    S.op("pe", lambda e: e.matmul(sTb[:, n * 128:(n + 1) * 128], lhsT=kT_sb[pb:pb + 64, c, k0:k0 + 128],
                                                  rhs=q[pb:pb + 64, c, :], start=True, stop=True),
                         reads=B_kTs + [B_qT[idx % 4]], writes=[B_sT[sidx % 3]], inc=(n == 6))
                ssb = sS[sidx % 4]
                S.op("dve", lambda e: e.tensor_tensor(out=ssb[:, 0:640], in0=sTb[:, 0:640], in1=bt[:, h, :, :].rearrange("p n q -> p (n q)"), op=ALU.add),
                     reads=[B_sT[sidx % 3], Bb], writes=[B_sS[sidx % 4]])
                S.op("dve", lambda e: e.tensor_copy(out=ssb[:, 640:896], in_=sTb[:, 640:896]),
                     reads=[B_sT[sidx % 3]], writes=[B_sS[sidx % 4]])

            load_q(0)
            load_q(1)
            load_q(2)
            emit_qk(0)
            emit_qk(1)
            emit_qk(2)
            for sidx, (idx, i, h) in enumerate(steps):
                jb0 = min(max(i - 2, 0), 59)
                ssb = sS[sidx % 4]
                pTb = pT[sidx % NPT]
                wl = [B_pT[sidx % NPT]]
                if h == 0:
                    tile_pace[idx + 1] = Buf()
                    wl.append(tile_pace[idx + 1])
                S.op("act", lambda e: e.activation(out=pTb[:, :], in_=ssb[:, :], func=AF.Exp),
                     reads=[B_sS[sidx % 4]], writes=wl)
                emit_qk(sidx + 3)
                pob = po[0]
                for n in range(7):
                    blk = (2 + jb0 + n) if n < 5 else (n - 5)
                    S.op("pe", lambda e: e.matmul(pob[:, h // 4, (h % 4) * 65:(h % 4) * 65 + 65], lhsT=pTb[:, n * 128:(n + 1) * 128],
                                                  rhs=v_sb[:, blk, h * 65:(h + 1) * 65], start=(n == 0), stop=(n == 6)),
                         reads=[B_pT[sidx % NPT]] + B_vs, writes=[B_po[0]], inc=(n == 6))
                if h == 7:
                    pov = pob[:, :, 0:260].rearrange("p a (h d) -> p a h d", d=65)
                    S.op("dve", lambda e: e.reciprocal(out=rec[:].rearrange("p (a h) -> p a h", a=2), in_=pov[:, :, :, 64]),
                         reads=[B_po[0]], writes=[B_rec])
                    og = ostg[idx % 2]
                    for a_ in range(2):
                        S.op("dve", lambda e: e.tensor_tensor(out=og[:, a_ * 4:(a_ + 1) * 4, :], in0=pov[:, a_, :, 0:64],
                                                              in1=rec[:, a_ * 4:(a_ + 1) * 4].unsqueeze(2).to_broadcast([128, 4, 64]),
                                                              op=ALU.mult),
                             reads=[B_po[0], B_rec], writes=[B_ostg[idx % 2]])
                    S.dma(na_d[i * 128:(i + 1) * 128, :], og[:].rearrange("p h d -> p (h d)"), reads=[B_ostg[idx % 2]])
            S.barrier()
        if upto in ("F", "F1"):
            S.final_wait()
            return nc

        KSC = 128.0 ** -0.5
        if "C" in phases:
          with contextlib.ExitStack() as pc_:
            convw = sb(pc_, "convw", [128, 8, 5], F32)
            convb = sb(pc_, "convb", [128, 8], F32)
            diagw = sb(pc_, "diagw", [128, 8, 5, 128], BF16)
            rperm = sb(pc_, "rperm", [128, 128], BF16)
            B_cw, B_cb, B_dw, B_rp = Buf(), Buf(), Buf(), Buf()
            S.dma(convw[:], convw_d, writes=[B_cw])
            S.dma(convb[:], convb_d, writes=[B_cb])
            S.dma(rperm[:], rperm_d, writes=[B_rp])
            for ch in range(8):
                for j in range(5):
                    S.op("dve", lambda e: e.tensor_scalar(out=diagw[:, ch, j, :], in0=ident_f[:, :], scalar1=convw[:, ch, j:j + 1],
                                                          scalar2=None, op0=ALU.mult), reads=[B_cw, B_const], writes=[B_dw])
            u8 = [sb(pc_, "u8_%d" % i, [128, 8, 516], BF16) for i in range(2)]
            B_u8 = [Buf(), Buf()]
            rt = [sb(pc_, "rt%d" % i, [128, 4, 512], F32) for i in range(2)]
            B_rt = [Buf(), Buf()]
            qs = [sb(pc_, "qs%d" % i, [128, 512], BF16) for i in range(2)]
            B_qs = [Buf(), Buf()]
            t1 = [sb(pc_, "t1_%d" % i, [128, 512], F32) for i in range(2)]
            B_t1 = [Buf(), Buf()]
            t2 = [sb(pc_, "t2_%d" % i, [128, 512], F32) for i in range(2)]
            B_t2 = [Buf(), Buf()]
            qko = [sb(pc_, "qko%d" % i, [128, 8, 512], BF16) for i in range(2)]
            B_qko = [Buf(), Buf()]
            kst = [sb(pc_, "kst%d" % i, [128, 4, 4, 128], BF16) for i in range(2)]
            B_kst = [Buf(), Buf()]
            cps_ = [ps(pc_, "cvps%d" % i, [128, 512], F32) for i in range(2)]
            B_cps = [Buf(), Buf()]
            rps = [ps(pc_, "rps%d" % i, [128, 512], F32) for i in range(2)]
            B_rps = [Buf(), Buf()]
            trp = [ps(pc_, "trp%d" % i, [128, 4, 128], BF16) for i in range(2)]
            B_trp = [Buf(), Buf()]
            groups = [(0, CTX, False, 0)] + [(CTX + g * 512, 512, True, g * 512) for g in range(T // 512)]
            if upto == "C1":
                groups = groups[:2] + groups[-1:]

            def load_group(gi):
                if gi >= len(groups):
                    return
                tt0, n, lat, lo = groups[gi]
                u = u8[gi % 2]
                seg0, seg1 = (CTX, TT) if lat else (0, CTX)
                a = max(tt0 - 2, seg0)
                b_ = min(tt0 + n + 2, seg1)
                if a > tt0 - 2:
                    S.op("pool", lambda e: e.memset(u[:, :, 0:2], 0.0), writes=[B_u8[gi % 2]])
                if b_ < tt0 + n + 2:
                    S.op("pool", lambda e: e.memset(u[:, :, n + 2:n + 4], 0.0), writes=[B_u8[gi % 2]])
                S.dma(u[:, :, a - (tt0 - 2):b_ - (tt0 - 2)], zT_d[8:16, :, a:b_].rearrange("c p t -> p c t"), writes=[B_u8[gi % 2]])
                if lat:
                    S.dma(rt[gi % 2][:], rope_d[:, :, lo:lo + 512].rearrange("c p t -> p c t"), writes=[B_rt[gi % 2]])
            load_group(0)
            cc_ = 0
            for gi, (tt0, n, lat, lo) in enumerate(groups):
                load_group(gi + 1)
                u = u8[gi % 2]
                qo = qko[gi % 2]
                for ch in range(8):
                    cp = cps_[cc_ % 2]
                    for j in range(5):
                        S.op("pe", lambda e: e.matmul(cp[:, 0:n], lhsT=diagw[:, ch, j, :], rhs=u[:, ch, j:j + n], start=(j == 0), stop=(j == 4)),
                             reads=[B_dw, B_u8[gi % 2]], writes=[B_cps[cc_ % 2]], inc=(j == 4))
                    isk = ch >= 4
                    if not lat:
                        if isk:
                            S.op("act", lambda e: e.activation(out=t1[0][:, 0:n], in_=cp[:, 0:n], func=AF.Silu, bias=convb[:, ch:ch + 1], scale=1.0),
                                 reads=[B_cps[cc_ % 2], B_cb], writes=[B_t1[0]])
                            S.op("dve", lambda e: e.tensor_scalar(out=qo[:, ch, 0:n], in0=t1[0][:, 0:n], scalar1=KSC, scalar2=None, op0=ALU.mult),
                                 reads=[B_t1[0]], writes=[B_qko[gi % 2]])
                        else:
                            S.op("act", lambda e: e.activation(out=qo[:, ch, 0:n], in_=cp[:, 0:n], func=AF.Silu, bias=convb[:, ch:ch + 1], scale=1.0),
                                 reads=[B_cps[cc_ % 2], B_cb], writes=[B_qko[gi % 2]])
                    else:
                        q_ = qs[cc_ % 2]
                        S.op("act", lambda e: e.activation(out=q_[:, 0:n], in_=cp[:, 0:n], func=AF.Silu, bias=convb[:, ch:ch + 1], scale=1.0),
                             reads=[B_cps[cc_ % 2], B_cb], writes=[B_qs[cc_ % 2]])
                        rp_ = rps[cc_ % 2]
                        S.op("pe", lambda e: e.matmul(rp_[:, 0:n], lhsT=rperm[:, :], rhs=q_[:, 0:n], start=True, stop=True),
                             reads=[B_rp, B_qs[cc_ % 2]], writes=[B_rps[cc_ % 2]])
                        tb = 2 if isk else 0
                        S.op("pool", lambda e: e.tensor_tensor(out=t1[cc_ % 2][:, 0:n], in0=q_[:, 0:n], in1=rt[gi % 2][:, tb, 0:n], op=ALU.mult),
                             reads=[B_qs[cc_ % 2], B_rt[gi % 2]], writes=[B_t1[cc_ % 2]])
                        S.op("dve", lambda e: e.tensor_tensor(out=t2[cc_ % 2][:, 0:n], in0=rp_[:, 0:n], in1=rt[gi % 2][:, tb + 1, 0:n], op=ALU.mult),
                             reads=[B_rps[cc_ % 2], B_rt[gi % 2]], writes=[B_t2[cc_ % 2]])
                        S.op("dve", lambda e: e.tensor_tensor(out=qo[:, ch, 0:n], in0=t1[cc_ % 2][:, 0:n], in1=t2[cc_ % 2][:, 0:n], op=ALU.add),
                             reads=[B_t1[cc_ % 2], B_t2[cc_ % 2]], writes=[B_qko[gi % 2]])
                    cc_ += 1
                S.dma(qkT_d[:, :, tt0:tt0 + n].rearrange("c p t -> p c t"), qo[:, :, 0:n], reads=[B_qko[gi % 2]])
                ks = kst[gi % 2]
                for j in range(n // 128):
                    tr = trp[j % 2]
                    for h in range(4):
                        S.op("pe", lambda e: e.transpose(out=tr[:, h, :], in_=qo[:, 4 + h, j * 128:(j + 1) * 128], identity=ident_bf[:]),
                             reads=[B_qko[gi % 2], B_const], writes=[B_trp[j % 2]], inc=(h == 3))
                    S.op("act", lambda e: e.activation(out=ks[:, j, :, :], in_=tr[:, :, :], func=AF.Copy),
                         reads=[B_trp[j % 2]], writes=[B_kst[gi % 2]])
                S.dma(kml_d[tt0:tt0 + n, :].rearrange("(j p) f -> p j f", p=128), ks[:, 0:n // 128, :, :].rearrange("p j h d -> p j (h d)"),
                      reads=[B_kst[gi % 2]])
            S.barrier()
        if upto in ("C", "C1"):
            S.final_wait()
            return nc

        NCH = TT // 128
        if "E" in phases:
          with contextlib.ExitStack() as pde:
            wgtT = [sb(pde, "wgtT%d" % d, [128, NCH, 4], F32) for d in range(2)]
            thrT = [sb(pde, "thrT%d" % d, [128, NCH, 4], F32) for d in range(2)]
            decB = [sb(pde, "decB%d" % d, [128, 4, NCH], F32) for d in range(2)]
            B_wgtT, B_thrT, B_decB = [Buf(), Buf()], [Buf(), Buf()], [Buf(), Buf()]
            with contextlib.ExitStack() as pd:
                X1 = sb(pd, "X1", [4, TT], F32)
                X2 = sb(pd, "X2", [4, TT], F32)
                X3 = sb(pd, "X3", [4, TT], F32)
                Z0 = sb(pd, "Z0", [4, TT], F32)
                ngd = sb(pd, "ngd", [4, NCH], F32)
                dec = sb(pd, "dec", [4, NCH], F32)
                sel = sb(pd, "sel", [4, 4, 128], F32)
                ptw = ps(pd, "ptw", [128, 512], F32)
                ptt = ps(pd, "ptt", [128, 512], F32)
                pdc = ps(pd, "pdc", [128, 512], F32)
                B1, B2, B3, BZ, Bng, Bdec, Bsel, Bptw, Bptt, Bpdc = [Buf() for _ in range(10)]
                S.op("pool", lambda e: e.memset(Z0[:], 0.0), writes=[BZ])
                for h in range(4):
                    S.op("dve", lambda e: e.tensor_copy(out=sel[0:4, h, :], in_=ident_f[0:4, h:h + 1].to_broadcast([4, 128])),
                         reads=[B_const], writes=[Bsel])
                segs = [(0, CTX), (CTX, TT)]
                for d in range(2):
                    if d == 0:
                        S.dma(X1[:], gT_d[0:4, :], writes=[B1])
                        S.dma(X2[:], gT_d[4:8, :], writes=[B2])
                    else:
                        S.dma(X3[:], gT_d[8:12, :], writes=[B3])
                        for (a, b_) in segs:
                            S.op("dve", lambda e: e.tensor_copy(out=X1[:, a:b_], in_=X3[:, a:b_][:, ::-1]), reads=[B3], writes=[B1])
                        S.dma(X3[:], gT_d[12:16, :], writes=[B3])
                        for (a, b_) in segs:
                            S.op("dve", lambda e: e.tensor_copy(out=X2[:, a:b_], in_=X3[:, a:b_][:, ::-1]), reads=[B3], writes=[B2])
                    S.op("act", lambda e: e.activation(out=X2[:], in_=X2[:], func=AF.Exp, scale=-1.0), reads=[B2], writes=[B2])
                    S.op("act", lambda e: e.activation(out=X2[:], in_=X2[:], func=AF.Ln, bias=1.0, scale=1.0), reads=[B2], writes=[B2])
                    S.op("dve", lambda e: e.tensor_tensor_scan(out=X3[:], data0=Z0[:], data1=X2[:], initial=0.0, op0=ALU.add, op1=ALU.add),
                         reads=[BZ, B2], writes=[B3])
                    S.op("dve", lambda e: e.tensor_tensor(out=X1[:], in0=X1[:], in1=X3[:], op=ALU.add), reads=[B1, B3], writes=[B1])
                    S.op("dve", lambda e: e.tensor_tensor_scan(out=X2[:], data0=X1[:], data1=X1[:], initial=0.0, op0=ALU.max, op1=ALU.max),
                         reads=[B1], writes=[B2])
                    S.op("dve", lambda e: e.tensor_scalar(out=ngd[:], in0=X2[:, 127:TT:128], scalar1=-1.0, scalar2=None, op0=ALU.mult),
                         reads=[B2], writes=[Bng])
                    S.op("dve", lambda e: e.tensor_copy(out=dec[:, 0:1], in_=ngd[:, 0:1]), reads=[Bng], writes=[Bdec])
                    S.op("dve", lambda e: e.tensor_tensor(out=dec[:, 1:NCH], in0=X2[:, 127:TT - 128:128], in1=ngd[:, 1:NCH], op=ALU.add),
                         reads=[B2, Bng], writes=[Bdec])
                    S.op("act", lambda e: e.activation(out=dec[:], in_=dec[:], func=AF.Exp), reads=[Bdec], writes=[Bdec])
                    for c in range(NCH):
                        sl = slice(c * 128, (c + 1) * 128)
                        S.op("act", lambda e: e.activation(out=X1[:, sl], in_=X1[:, sl], func=AF.Exp, bias=ngd[:, c:c + 1], scale=1.0),
                             reads=[B1, Bng], writes=[B1])
                        S.op("act", lambda e: e.activation(out=X3[:, sl], in_=X3[:, sl], func=AF.Exp, bias=ngd[:, c:c + 1], scale=1.0),
                             reads=[B3, Bng], writes=[B3])
                    if d == 0:
                        Wt, Bw, Tt, Bt = X1, B1, X3, B3
                    else:
                        for (a, b_) in segs:
                            S.op("dve", lambda e: e.tensor_copy(out=X2[:, a:b_], in_=X1[:, a:b_][:, ::-1]), reads=[B1], writes=[B2])
                        for (a, b_) in segs:
                            S.op("dve", lambda e: e.tensor_copy(out=X1[:, a:b_], in_=X3[:, a:b_][:, ::-1]), reads=[B3], writes=[B1])
                        Wt, Bw, Tt, Bt = X2, B2, X1, B1
                    for blk in range(NCH):
                        sl = slice(blk * 128, (blk + 1) * 128)
                        S.op("pe", lambda e: e.matmul(ptw[:, blk * 4:(blk + 1) * 4], lhsT=Wt[0:4, sl], rhs=ident_f[0:4, 0:4], start=True, stop=True),
                             reads=[Bw, B_const], writes=[Bptw], inc=False)
                        S.op("pe", lambda e: e.matmul(ptt[:, blk * 4:(blk + 1) * 4], lhsT=Tt[0:4, sl], rhs=ident_f[0:4, 0:4], start=True, stop=True),
                             reads=[Bt, B_const], writes=[Bptt], inc=(blk == NCH - 1))
                    S.op("dve", lambda e: e.tensor_copy(out=wgtT[d][:].rearrange("p c h -> p (c h)"), in_=ptw[:, 0:NCH * 4]),
                         reads=[Bptw], writes=[B_wgtT[d]])
                    S.op("dve", lambda e: e.tensor_copy(out=thrT[d][:].rearrange("p c h -> p (c h)"), in_=ptt[:, 0:NCH * 4]),
                         reads=[Bptt], writes=[B_thrT[d]])
                    for h in range(4):
                        S.op("pe", lambda e: e.matmul(pdc[:, h * NCH:(h + 1) * NCH], lhsT=sel[0:4, h, :], rhs=dec[0:4, :], start=True, stop=True),
                             reads=[Bsel, Bdec], writes=[Bpdc], inc=(h == 3))
                    S.op("dve", lambda e: e.tensor_copy(out=decB[d][:].rearrange("p h c -> p (h c)"), in_=pdc[:, 0:4 * NCH]),
                         reads=[Bpdc], writes=[B_decB[d]])
                if dbgD_d is not None:
                    pass
                S.barrier()

            with contextlib.ExitStack() as pe_:
                cmask = sb(pe_, "cmask", [128, 3, 128], BF16)
                mlg = sb(pe_, "mlg", [128, 512], F32)
                B_cm, B_mlg = Buf(), Buf()
                S.dma(cmask[:], cmask_d.rearrange("a p q -> p a q"), writes=[B_cm])
                S.dma(mlg[:], mlg_d, writes=[B_mlg])
                QT = [sb(pe_, "QT%d" % i, [128, 4, 128], BF16) for i in range(2)]
                KT = [sb(pe_, "KT%d" % i, [128, 4, 128], BF16) for i in range(2)]
                Kt = [sb(pe_, "Kt%d" % i, [128, 4, 128], BF16) for i in range(2)]
                Vt = [sb(pe_, "Vt%d" % i, [128, 4, 128], BF16) for i in range(2)]
                HF = [sb(pe_, "HF%d" % i, [128, 512], F32) for i in range(2)]
                SO = [sb(pe_, "SO%d" % i, [128, 512], F32) for i in range(2)]
                B_QT, B_KT, B_Kt, B_Vt, B_HF, B_SO = [[Buf(), Buf()] for _ in range(6)]
                vp = [sb(pe_, "vp%d" % i, [128, 4, 129], BF16) for i in range(2)]
                B_vp = [Buf(), Buf()]
                sm = [sb(pe_, "sm%d" % i, [128, 128], BF16) for i in range(4)]
                B_sm = [Buf() for _ in range(4)]
                Cst = sb(pe_, "Cst", [128, 4, 129], F32)
                B_Cst = [Buf() for _ in range(4)]
                Cdb = [sb(pe_, "Cdb%d" % i, [128, 4, 129], BF16) for i in range(2)]
                B_Cdb = [[Buf() for _ in range(4)] for _ in range(2)]
                hst = [sb(pe_, "hst%d" % i, [128, 4, 128], F32) for i in range(2)]
                B_hst = [Buf(), Buf()]
                dn = sb(pe_, "dn", [128, 4], F32)
                B_dn = Buf()
                hsq = sb(pe_, "hsq", [128, 512], F32)
                ss = sb(pe_, "ss", [128, 4], F32)
                go = sb(pe_, "go", [128, 512], F32)
                mlo_t = [sb(pe_, "mlo_t%d" % i, [128, 512], BF16) for i in range(2)]
                B_hsq, B_ss, B_go = Buf(), Buf(), Buf()
                B_mlo = [Buf(), Buf()]
                sps = [ps(pe_, "sps%d" % i, [128, 4, 128], F32) for i in range(2)]
                B_sps = [Buf(), Buf()]
                hps = [ps(pe_, "hps%d" % i, [128, 2, 512], F32) for i in range(2)]
                B_hps = [Buf(), Buf()]
                cps2 = ps(pe_, "cps2", [128, 2, 512], F32)
                B_cps2 = [Buf() for _ in range(4)]

                def blk_of(d, c):
                    if d == 0:
                        return c
                    return (1 - c) if c < 2 else (67 - c)

                nch_run = NCH if upto != "E1" else 5
                for d in range(2):
                    S.op("dve", lambda e: e.memset(Cst[:], 0.0), writes=B_Cst)

                    def load_chunk(c):
                        if c >= nch_run:
                            return
                        blk = blk_of(d, c)
                        b = c % 2
                        rows = slice(blk * 128, (blk + 1) * 128)
                        S.dma(Kt[b][:].rearrange("p h d -> p (h d)"), kml_d[rows, :], writes=[B_Kt[b]])
                        S.dma(Vt[b][:].rearrange("p h d -> p (h d)"), mlv_d[rows, :], writes=[B_Vt[b]])
                        if blk >= 2:
                            S.dma(QT[b][:], qkT_d[0:4, :, rows].rearrange("c p t -> p c t"), writes=[B_QT[b]])
                            S.dma(KT[b][:], qkT_d[4:8, :, rows].rearrange("c p t -> p c t"), writes=[B_KT[b]])
                            if d == 1:
                                S.dma(HF[b][:], hf_d[(blk - 2) * 128:(blk - 1) * 128, :], writes=[B_HF[b]])
                                S.dma(SO[b][:], mlo_d[rows, :], writes=[B_SO[b]])
                    load_chunk(0)
                    for c in range(nch_run):
                        load_chunk(c + 1)
                        blk = blk_of(d, c)
                        b = c % 2
                        lat = blk >= 2
                        vpb = vp[b]
                        S.op("dve", lambda e: e.tensor_tensor(out=vpb[:, :, 0:128], in0=Vt[b][:, :, :],
                                                              in1=wgtT[d][:, blk, :].unsqueeze(2).to_broadcast([128, 4, 128]), op=ALU.mult),
                             reads=[B_Vt[b], B_wgtT[d]], writes=[B_vp[b]])
                        S.op("dve", lambda e: e.tensor_copy(out=vpb[:, :, 128], in_=wgtT[d][:, blk, :]),
                             reads=[B_wgtT[d]], writes=[B_vp[b]])
                        hp = hps[b]
                        cdb = Cdb[b]
                        for h in range(4):
                            S.op("act", lambda e: e.activation(out=cdb[:, h, :], in_=Cst[:, h, :], func=AF.Copy, scale=decB[d][:, h, c:c + 1]),
                                 reads=[B_Cst[h], B_decB[d]], writes=[B_Cdb[b][h]])
                            if lat:
                                S.op("pe", lambda e: e.matmul(sps[b][:, h, :], lhsT=KT[b][:, h, :], rhs=QT[b][:, h, :], start=True, stop=True),
                                     reads=[B_KT[b], B_QT[b]], writes=[B_sps[b]], inc=(h == 3))
                        for h in range(4):
                            co = cps2[:, h // 2, (h % 2) * 129:(h % 2) * 129 + 129]
                            S.op("pe", lambda e: e.matmul(co, lhsT=Kt[b][:, h, :], rhs=vpb[:, h, :], start=True, stop=True),
                                 reads=[B_Kt[b], B_vp[b]], writes=[B_cps2[h]])
                            if lat:
                                S.op("dve", lambda e: e.tensor_tensor(out=sm[h][:, :], in0=sps[b][:, h, :], in1=cmask[:, d, :], op=ALU.mult),
                                     reads=[B_sps[b], B_cm], writes=[B_sm[h]])
                        if lat:
                            for h in range(4):
                                ho = hp[:, h // 2, (h % 2) * 129:(h % 2) * 129 + 129]
                                S.op("pe", lambda e: e.matmul(ho, lhsT=sm[h][:, :], rhs=vpb[:, h, :], start=True, stop=False),
                                     reads=[B_sm[h], B_vp[b]], writes=[B_hps[b]], inc=False)
                                S.op("pe", lambda e: e.matmul(ho, lhsT=QT[b][:, h, :], rhs=cdb[:, h, :], start=False, stop=True),
                                     reads=[B_QT[b], B_Cdb[b][h]], writes=[B_hps[b]], inc=(h == 3))
                        for h in range(4):
                            co = cps2[:, h // 2, (h % 2) * 129:(h % 2) * 129 + 129]
                            S.op("dve", lambda e: e.scalar_tensor_tensor(out=Cst[:, h, :], in0=Cst[:, h, :], scalar=decB[d][:, h, c:c + 1], in1=co,
                                                                         op0=ALU.mult, op1=ALU.add),
                                 reads=[B_Cst[h], B_decB[d], B_cps2[h], B_Cdb[b][h]], writes=[B_Cst[h]])
                        if not lat:
                            continue
                        hv = hp[:, :, 0:258].rearrange("p a (h d) -> p a h d", d=129)
                        S.op("act", lambda e: e.activation(out=dn[:].rearrange("p (a h) -> p a h", a=2), in_=hv[:, :, :, 128], func=AF.Abs),
                             reads=[B_hps[b]], writes=[B_dn])
                        S.op("dve", lambda e: e.tensor_tensor(out=dn[:], in0=dn[:], in1=thrT[d][:, blk, :], op=ALU.max),
                             reads=[B_dn, B_thrT[d]], writes=[B_dn])
                        S.op("dve", lambda e: e.reciprocal(out=dn[:], in_=dn[:]), reads=[B_dn], writes=[B_dn])
                        hs = hst[b]
                        for a in range(2):
                            S.op("dve", lambda e: e.tensor_tensor(out=hs[:, a * 2:(a + 1) * 2, :], in0=hv[:, a, :, 0:128],
                                                                  in1=dn[:, a * 2:(a + 1) * 2].unsqueeze(2).to_broadcast([128, 2, 128]), op=ALU.mult),
                                 reads=[B_hps[b], B_dn], writes=[B_hst[b]])
                        lt = blk - 2
                        if d == 0:
                            S.dma(hf_d[lt * 128:(lt + 1) * 128, :], hs[:].rearrange("p h d -> p (h d)"), reads=[B_hst[b]])
                        else:
                            hsf = hs[:].rearrange("p h d -> p (h d)")
                            S.op("pool", lambda e: e.tensor_tensor(out=hsf, in0=hsf, in1=HF[b][:, :], op=ALU.add),
                                 reads=[B_hst[b], B_HF[b]], writes=[B_hst[b]])
                            S.op("pool", lambda e: e.tensor_tensor(out=hsq[:, :], in0=hsf, in1=hsf, op=ALU.mult),
                                 reads=[B_hst[b]], writes=[B_hsq])
                            S.op("dve", lambda e: e.tensor_reduce(out=ss[:, :], in_=hsq[:].rearrange("p (h d) -> p h d", d=128), axis=AX.X, op=ALU.add),
                                 reads=[B_hsq], writes=[B_ss])
                            S.op("dve", lambda e: e.tensor_scalar(out=ss[:, :], in0=ss[:, :], scalar1=1.0 / 128.0, scalar2=EPS, op0=ALU.mult, op1=ALU.add),
                                 reads=[B_ss], writes=[B_ss])
                            S.op("pool", lambda e: e.tensor_tensor(out=ss[:, :], in0=ss[:, :], in1=neghalf_c[:, 0:1].to_broadcast([128, 4]), op=ALU.pow),
                                 reads=[B_ss, B_const], writes=[B_ss])
                            S.op("pool", lambda e: e.tensor_tensor(out=go[:, :], in0=SO[b][:, :], in1=mlg[:, :], op=ALU.mult),
                                 reads=[B_SO[b], B_mlg], writes=[B_go])
                            S.op("dve", lambda e: e.tensor_tensor(out=hs[:, :, :], in0=hs[:, :, :],
                                                                  in1=ss[:, :].unsqueeze(2).to_broadcast([128, 4, 128]), op=ALU.mult),
                                 reads=[B_hst[b], B_ss], writes=[B_hst[b]])
                            S.op("dve", lambda e: e.tensor_tensor(out=mlo_t[b][:, :], in0=hsf, in1=go[:, :], op=ALU.mult),
                                 reads=[B_hst[b], B_go], writes=[B_mlo[b]])
                            S.dma(ml_d[lt * 128:(lt + 1) * 128, :], mlo_t[b][:, :], reads=[B_mlo[b]])
                    S.barrier()
        if upto in ("E", "E1"):
            S.final_wait()
            return nc

        if "G" in phases:
          bg_step(100000)
          with contextlib.ExitStack() as pg0:
            W12 = sb(pg0, "W12", [128, NT, 2], F32)
            D1i = sb(pg0, "D1i", [128, NT], I32)
            D2i = sb(pg0, "D2i", [128, NT], I32)
            ebrow = sb(pg0, "ebrow", [1, 256], I32)
            idxw = sb(pg0, "idxw", [128, 256], I32)
            B_idxw = Buf()
            lnp = sb(pg0, "lnp", [128, 4, D], F32)
            B_W12, B_D1, B_D2, B_eb, B_lnp = Buf(), Buf(), Buf(), Buf(), Buf()
            S.dma(lnp[:], lnp_d.rearrange("a p f -> p a f"), writes=[B_lnp])

            def layer_norm_stats(x_ap, Bx, st6, mv, rstd, nmr, Bs):
                S.op("dve", lambda e: e.bn_stats(out=st6[:, 0, :], in_=x_ap[:, 0:512]), reads=[Bx], writes=[Bs])
                S.op("dve", lambda e: e.bn_stats(out=st6[:, 1, :], in_=x_ap[:, 512:1024]), reads=[Bx], writes=[Bs])
                S.op("dve", lambda e: e.bn_aggr(out=mv[:], in_=st6[:].rearrange("p a b -> p (a b)")), reads=[Bs], writes=[Bs])
                S.op("dve", lambda e: e.tensor_scalar(out=rstd[:], in0=mv[:, 1:2], scalar1=EPS, scalar2=None, op0=ALU.add), reads=[Bs], writes=[Bs])
                S.op("pool", lambda e: e.tensor_tensor(out=rstd[:], in0=rstd[:], in1=neghalf_c[:], op=ALU.pow), reads=[Bs, B_const], writes=[Bs])
                S.op("dve", lambda e: e.scalar_tensor_tensor(out=nmr[:], in0=mv[:, 0:1], scalar=-1.0, in1=rstd[:], op0=ALU.mult, op1=ALU.mult),
                     reads=[Bs], writes=[Bs])

            with contextlib.ExitStack() as pg:
                wout_sb = sb(pg, "wout_sb", [128, 8, D], BF16)
                wstg = [sb(pg, "wstg%d" % i, [128, D], F32) for i in range(2)]
                B_wout, B_wstg = Buf(), [Buf(), Buf()]
                wout_v = wout_d.rearrange("(k p) n -> p k n", p=128)
                for k in range(8):
                    S.dma(wstg[k % 2][:], wout_v[:, k, :], writes=[B_wstg[k % 2]])
                    S.op("dve", lambda e: e.tensor_tensor(out=wout_sb[:, k, :], in0=wstg[k % 2][:], in1=g_b[:, 0:1024], op=ALU.mult),
                         reads=[B_wstg[k % 2], B_gb], writes=[B_wout])
                wr_sb = sb(pg, "wr_sb", [128, 8, 72], BF16)
                rbias = sb(pg, "rbias", [128, 72], F32)
                SU = sb(pg, "SU", [128, 128], BF16)
                ones_bf = sb(pg, "ones_bf", [128, 128], BF16)
                bvals = sb(pg, "bvals", [128, 3], F32)
                B_wr, B_rb, B_SU, B_bv = Buf(), Buf(), Buf(), Buf()
                S.dma(wr_sb[:], wr_d.rearrange("(k p) n -> p k n", p=128), writes=[B_wr], eng="pool")
                S.dma(rbias[:], rbias_d, writes=[B_rb])
                S.dma(SU[:], cmask_d[2], writes=[B_SU])
                S.dma(bvals[:], bvals_d, writes=[B_bv])
                S.op("dve", lambda e: e.memset(ones_bf[:], 1.0), writes=[B_const])
                M1 = sb(pg, "M1", [128, NT, 64], BF16)
                M2 = sb(pg, "M2", [128, NT, 64], BF16)
                RK = sb(pg, "RK", [128, NT, 64], F32)
                big = sb(pg, "big", [128, NT, 64], F32)
                run = sb(pg, "run", [128, 64], F32)
                B_M1, B_M2, B_RK, B_big, B_run = Buf(), Buf(), Buf(), Buf(), Buf()
                S.op("dve", lambda e: e.memset(run[:], 0.0), writes=[B_run])
                xg = [sb(pg, "xg%d" % i, [128, D], F32) for i in range(2)]
                mix = [sb(pg, "mix%d" % i, [128, D], BF16) for i in range(2)]
                B_xg, B_mix = [Buf(), Buf()], [Buf(), Buf()]
                mixT = sb(pg, "mixT", [128, 8, 128], BF16)
                xm_ = sb(pg, "xm_", [128, D], F32)
                xmid = [sb(pg, "xmid%d" % i, [128, D], F32) for i in range(2)]
                xn2 = sb(pg, "xn2", [128, D], BF16)
                h2T = sb(pg, "h2T", [128, 8, 128], BF16)
                h2 = [sb(pg, "h2_%d" % i, [128, D], BF16) for i in range(2)]
                B_mixT, B_xm, B_xmid, B_xn2, B_h2T, B_h2 = Buf(), Buf(), [Buf(), Buf()], Buf(), Buf(), [Buf(), Buf()]
                st6 = sb(pg, "gst6", [128, 2, 6], F32)
                mv = sb(pg, "gmv", [128, 2], F32)
                rstd = sb(pg, "grstd", [128, 1], F32)
                nmr = sb(pg, "gnmr", [128, 1], F32)
                B_s1 = Buf()
                lg = sb(pg, "lg", [128, 72], F32)
                sm8 = sb(pg, "sm8", [128, 16], F32)
                gm = sb(pg, "gm", [128, 8], F32)
                ge = sb(pg, "ge", [128, 8], F32)
                elm = sb(pg, "elm", [128, 64], F32)
                top8 = sb(pg, "top8", [128, 8], F32)
                m12 = sb(pg, "m12", [128, 64], BF16)
                B_lg, B_sm8, B_gm, B_elm, B_top8, B_m12 = Buf(), Buf(), Buf(), Buf(), Buf(), Buf()
                tpg = [ps(pg, "tpg%d" % i, [128, 1024], BF16) for i in range(2)]
                B_tpg = [Buf(), Buf()]
                ops_ = ps(pg, "ops_", [128, 2, 512], F32)
                B_ops = Buf()
                tpb = ps(pg, "tpb", [128, 1024], BF16)
                B_tpb = Buf()
                lps = ps(pg, "lps", [128, 512], F32)
                B_lps = Buf()
                rkps = ps(pg, "rkps", [128, 512], F32)
                B_rkps = Buf()

                nt_run = NT if upto not in ("G1",) else 2

                def load_tile(i):
                    if i >= nt_run:
                        return
                    rows = slice(i * 128, (i + 1) * 128)
                    S.dma(xg[i % 2][:], x_d[rows, :], writes=[B_xg[i % 2]])
                    S.dma(mix[i % 2][:, 0:512], na_d[rows, :], writes=[B_mix[i % 2]])
                    S.dma(mix[i % 2][:, 512:1024], ml_d[rows, :], writes=[B_mix[i % 2]])
                st6b = sb(pg, "gst6b", [128, 2, 6], F32)
                mvb = sb(pg, "gmvb", [128, 2], F32)
                rstdb = sb(pg, "grstdb", [128, 1], F32)
                nmrb = sb(pg, "gnmrb", [128, 1], F32)
                B_s1b = Buf()
                lps2 = ps(pg, "lps2", [128, 512], F32)
                lpsb = [lps, lps2]
                B_lpsb = [B_lps, Buf()]

                def stage_a(i):
                    rows = slice(i * 128, (i + 1) * 128)
                    mx = mix[i % 2]
                    tp_ = tpg[0]
                    for k in range(8):
                        S.op("pe", lambda e: e.transpose(out=tp_[:, k * 128:(k + 1) * 128], in_=mx[:, k * 128:(k + 1) * 128], identity=ident_bf[:]),
                             reads=[B_mix[i % 2], B_const], writes=[B_tpg[0]], inc=(k == 7))
                    yield
                    S.op("act", lambda e: e.activation(out=mixT[:, 0:4, :], in_=tp_[:, 0:512].rearrange("p (k t) -> p k t", t=128), func=AF.Copy),
                         reads=[B_tpg[0]], writes=[B_mixT])
                    yield
                    S.op("dve", lambda e: e.tensor_copy(out=mixT[:, 4:8, :], in_=tp_[:, 512:1024].rearrange("p (k t) -> p k t", t=128)),
                         reads=[B_tpg[0]], writes=[B_mixT])
                    yield
                    for n in range(2):
                        for k in range(8):
                            S.op("pe", lambda e: e.matmul(ops_[:, n, :], lhsT=mixT[:, k, :], rhs=wout_sb[:, k, n * 512:(n + 1) * 512],
                                                          start=(k == 0), stop=(k == 7)),
                                 reads=[B_mixT, B_wout], writes=[B_ops], inc=(k == 7 and n == 1))
                    yield
                    S.op("dve", lambda e: e.scalar_tensor_tensor(out=xm_[:].rearrange("p (n f) -> p n f", n=2), in0=xg[i % 2][:].rearrange("p (n f) -> p n f", n=2),
                                                                 scalar=ALPHA, in1=ops_[:, :, :], op0=ALU.mult, op1=ALU.add),
                         reads=[B_xg[i % 2], B_ops], writes=[B_xm])
                    yield
                    load_tile(i + 2)
                    for _ in layer_norm_stats_g(xm_, B_xm, st6, mv, rstd, nmr, B_s1):
                        yield
                    xmd = xmid[i % 2]
                    S.op("act", lambda e: e.activation(out=xmd[:], in_=xm_[:], func=AF.Identity, bias=nmr[:, 0:1], scale=rstd[:, 0:1]),
                         reads=[B_xm, B_s1], writes=[B_xmid[i % 2]])
                    yield
                    S.op("pool", lambda e: e.tensor_tensor(out=xmd[:], in0=xmd[:], in1=lnp[:, 0, :], op=ALU.mult),
                         reads=[B_xmid[i % 2], B_lnp], writes=[B_xmid[i % 2]])
                    yield
                    S.op("dve", lambda e: e.tensor_tensor(out=xmd[:], in0=xmd[:], in1=lnp[:, 1, :], op=ALU.add),
                         reads=[B_xmid[i % 2], B_lnp], writes=[B_xmid[i % 2]])
                    yield
                    S.dma(xmid_d[rows, :], xmd[:], reads=[B_xmid[i % 2]])
                    yield

                def layer_norm_stats_g(x_ap, Bx, st6_, mv_, rstd_, nmr_, Bs):
                    S.op("dve", lambda e: e.bn_stats(out=st6_[:, 0, :], in_=x_ap[:, 0:512]), reads=[Bx], writes=[Bs])
                    yield
                    S.op("dve", lambda e: e.bn_stats(out=st6_[:, 1, :], in_=x_ap[:, 512:1024]), reads=[Bx], writes=[Bs])
                    yield
                    S.op("dve", lambda e: e.bn_aggr(out=mv_[:], in_=st6_[:].rearrange("p a b -> p (a b)")), reads=[Bs], writes=[Bs])
                    S.op("dve", lambda e: e.tensor_scalar(out=rstd_[:], in0=mv_[:, 1:2], scalar1=EPS, scalar2=None, op0=ALU.add), reads=[Bs], writes=[Bs])
                    yield
                    S.op("pool", lambda e: e.tensor_tensor(out=rstd_[:], in0=rstd_[:], in1=neghalf_c[:], op=ALU.pow), reads=[Bs, B_const], writes=[Bs])
                    yield
                    S.op("dve", lambda e: e.scalar_tensor_tensor(out=nmr_[:], in0=mv_[:, 0:1], scalar=-1.0, in1=rstd_[:], op0=ALU.mult, op1=ALU.mult),
                         reads=[Bs], writes=[Bs])
                    yield

                def stage_b(i):
                    rows = slice(i * 128, (i + 1) * 128)
                    xmd = xmid[i % 2]
                    for _ in layer_norm_stats_g(xmd, B_xmid[i % 2], st6b, mvb, rstdb, nmrb, B_s1b):
                        yield
                    S.op("act", lambda e: e.activation(out=xn2[:], in_=xmd[:], func=AF.Identity, bias=nmrb[:, 0:1], scale=rstdb[:, 0:1]),
                         reads=[B_xmid[i % 2], B_s1b], writes=[B_xn2])
                    yield
                    tp2 = tpg[1]
                    for k in range(8):
                        S.op("pe", lambda e: e.transpose(out=tp2[:, k * 128:(k + 1) * 128], in_=xn2[:, k * 128:(k + 1) * 128], identity=ident_bf[:]),
                             reads=[B_xn2, B_const], writes=[B_tpg[1]], inc=(k == 7))
                    yield
                    for k in range(8):
                        o = h2T[:, k, :]
                        i_ = tp2[:, k * 128:(k + 1) * 128]
                        sc = adaT[:, 32 + k, 0:1]
                        sh = adaT[:, 24 + k, 0:1]
                        if k % 2 == 0:
                            S.op("act", lambda e: e.activation(out=o, in_=i_, func=AF.Identity, bias=sh, scale=sc),
                                 reads=[B_tpg[1], B_ada], writes=[B_h2T])
                        else:
                            S.op("dve", lambda e: e.tensor_scalar(out=o, in0=i_, scalar1=sc, scalar2=sh, op0=ALU.mult, op1=ALU.add),
                                 reads=[B_tpg[1], B_ada], writes=[B_h2T])
                        yield
                    for k in range(8):
                        S.op("pe", lambda e: e.transpose(out=tpb[:, k * 128:(k + 1) * 128], in_=h2T[:, k, :], identity=ident_bf[:]),
                             reads=[B_h2T, B_const], writes=[B_tpb], inc=(k == 7))
                    lp = lpsb[i % 2]
                    for k in range(8):
                        S.op("pe", lambda e: e.matmul(lp[:, 0:72], lhsT=h2T[:, k, :], rhs=wr_sb[:, k, :], start=(k == 0), stop=(k == 7)),
                             reads=[B_h2T, B_wr], writes=[B_lpsb[i % 2]], inc=(k == 7))
                    yield
                    h2b = h2[i % 2]
                    S.op("act", lambda e: e.activation(out=h2b[:, 0:512], in_=tpb[:, 0:512], func=AF.Copy), reads=[B_tpb], writes=[B_h2[i % 2]])
                    yield
                    S.op("dve", lambda e: e.tensor_copy(out=h2b[:, 512:1024], in_=tpb[:, 512:1024]), reads=[B_tpb], writes=[B_h2[i % 2]])
                    yield
                    S.dma(h2_d[rows, :], h2b[:], reads=[B_h2[i % 2]])
                    yield

                def stage_c(i):
                    lp = lpsb[i % 2]
                    Bl = B_lpsb[i % 2]
                    S.op("dve", lambda e: e.tensor_tensor(out=lg[:], in0=lp[:, 0:72], in1=rbias[:], op=ALU.add), reads=[Bl, B_rb], writes=[B_lg])
                    yield
                    S.op("dve", lambda e: e.reduce_max(out=sm8[:, 0:1], in_=lg[:, 0:8], axis=AX.X), reads=[B_lg], writes=[B_sm8])
                    yield
                    S.op("dve", lambda e: e.tensor_scalar(out=sm8[:, 1:2], in0=sm8[:, 0:1], scalar1=-1.0, scalar2=None, op0=ALU.mult),
                         reads=[B_sm8], writes=[B_sm8])
                    yield
                    S.op("act", lambda e: e.activation(out=ge[:], in_=lg[:, 0:8], func=AF.Exp, bias=sm8[:, 1:2], scale=1.0, accum_out=sm8[:, 2:3]),
                         reads=[B_lg, B_sm8], writes=[B_sm8, B_gm])
                    yield
                    S.op("dve", lambda e: e.tensor_scalar(out=gm[:], in0=lg[:, 0:8], scalar1=sm8[:, 0:1], scalar2=None, op0=ALU.is_ge),
                         reads=[B_lg, B_sm8, B_gm], writes=[B_gm])
                    yield
                    S.op("dve", lambda e: e.tensor_scalar(out=gm[:], in0=gm[:], scalar1=1e9, scalar2=-1e9, op0=ALU.mult, op1=ALU.add),
                         reads=[B_gm], writes=[B_gm])
                    yield
                    S.op("dve", lambda e: e.tensor_tensor(out=elm[:].rearrange("p (g e) -> p g e", e=8), in0=lg[:, 8:72].rearrange("p (g e) -> p g e", e=8),
                                                          in1=gm[:, :].unsqueeze(2).to_broadcast([128, 8, 8]), op=ALU.add),
                         reads=[B_lg, B_gm], writes=[B_elm])
                    yield
                    S.op("dve", lambda e: e.max(out=top8[:], in_=elm[:]), reads=[B_elm], writes=[B_top8])
                    yield
                    S.op("dve", lambda e: e.tensor_scalar(out=M1[:, i, :], in0=elm[:], scalar1=top8[:, 0:1], scalar2=None, op0=ALU.is_ge),
                         reads=[B_elm, B_top8], writes=[B_M1])
                    yield
                    S.op("dve", lambda e: e.tensor_scalar(out=m12[:], in0=elm[:], scalar1=top8[:, 1:2], scalar2=None, op0=ALU.is_ge),
                         reads=[B_elm, B_top8], writes=[B_m12])
                    yield
                    S.op("dve", lambda e: e.tensor_tensor(out=M2[:, i, :], in0=m12[:], in1=M1[:, i, :], op=ALU.subtract),
                         reads=[B_m12, B_M1], writes=[B_M2])
                    yield
                    S.op("dve", lambda e: e.tensor_tensor(out=sm8[:, 3:4], in0=top8[:, 1:2], in1=top8[:, 0:1], op=ALU.subtract),
                         reads=[B_top8, B_sm8], writes=[B_sm8])
                    yield
                    S.op("act", lambda e: e.activation(out=sm8[:, 4:5], in_=sm8[:, 3:4], func=AF.Exp), reads=[B_sm8], writes=[B_sm8])
                    yield
                    S.op("dve", lambda e: e.tensor_scalar(out=sm8[:, 5:6], in0=sm8[:, 4:5], scalar1=1.0, scalar2=sm8[:, 2:3], op0=ALU.add, op1=ALU.mult),
                         reads=[B_sm8], writes=[B_sm8])
                    yield
                    S.op("dve", lambda e: e.reciprocal(out=W12[:, i, 0:1], in_=sm8[:, 5:6]), reads=[B_sm8], writes=[B_W12])
                    yield
                    S.op("dve", lambda e: e.tensor_tensor(out=W12[:, i, 1:2], in0=W12[:, i, 0:1], in1=sm8[:, 4:5], op=ALU.mult),
                         reads=[B_sm8, B_W12], writes=[B_W12])
                    yield
                    S.op("pe", lambda e: e.matmul(rkps[:, 0:64], lhsT=SU[:, :], rhs=m12[:, :], start=True, stop=True),
                         reads=[B_SU, B_m12], writes=[B_rkps], inc=False)
                    S.op("pe", lambda e: e.matmul(rkps[:, 64:128], lhsT=ones_bf[:, :], rhs=m12[:, :], start=True, stop=True),
                         reads=[B_const, B_m12], writes=[B_rkps])
                    yield
                    S.op("dve", lambda e: e.tensor_tensor(out=RK[:, i, :], in0=rkps[:, 0:64], in1=run[:], op=ALU.add),
                         reads=[B_rkps, B_run], writes=[B_RK])
                    yield
                    S.op("dve", lambda e: e.tensor_tensor(out=run[:], in0=rkps[:, 64:128], in1=run[:], op=ALU.add),
                         reads=[B_rkps, B_run, B_RK], writes=[B_run])
                    yield

                load_tile(0)
                load_tile(1)
                for step in range(nt_run + 2):
                    gens = []
                    if step < nt_run:
                        gens.append(stage_a(step))
                    if 0 <= step - 1 < nt_run:
                        gens.append(stage_b(step - 1))
                    if 0 <= step - 2 < nt_run:
                        gens.append(stage_c(step - 2))
                    interleave(*gens)

                szi = sb(pg, "szi", [128, 64], I32)
                pad = sb(pg, "pad", [128, 64], F32)
                cum = sb(pg, "cum", [128, 64], F32)
                pst = sb(pg, "pst", [128, 64], F32)
                z64 = sb(pg, "z64", [128, 64], F32)
                cmpb = sb(pg, "cmpb", [128, 64], F32)
                ebf = sb(pg, "ebf", [128, 2], F32)
                ebr = sb(pg, "ebr", [1, 256], F32)
                B_bk = Buf()
                S.op("dve", lambda e: e.memset(z64[:], 0.0), writes=[B_bk])
                S.op("dve", lambda e: e.tensor_scalar(out=szi[:], in0=run[:], scalar1=127.0, scalar2=None, op0=ALU.add), reads=[B_run], writes=[B_bk])
                S.op("dve", lambda e: e.tensor_scalar(out=szi[:], in0=szi[:], scalar1=7, scalar2=None, op0=ALU.arith_shift_right), reads=[B_bk], writes=[B_bk])
                S.op("dve", lambda e: e.tensor_scalar(out=szi[:], in0=szi[:], scalar1=7, scalar2=None, op0=ALU.logical_shift_left), reads=[B_bk], writes=[B_bk])
                S.op("dve", lambda e: e.tensor_copy(out=pad[:], in_=szi[:]), reads=[B_bk], writes=[B_bk])
                S.op("dve", lambda e: e.tensor_tensor_scan(out=cum[:], data0=z64[:], data1=pad[:], initial=0.0, op0=ALU.add, op1=ALU.add),
                     reads=[B_bk], writes=[B_bk])
                S.op("dve", lambda e: e.tensor_tensor(out=pst[:], in0=cum[:], in1=pad[:], op=ALU.subtract), reads=[B_bk], writes=[B_bk])
                for j in range(2):
                    S.op("dve", lambda e: e.tensor_scalar(out=cmpb[:], in0=cum[:], scalar1=bvals[:, j:j + 1], scalar2=None, op0=ALU.is_le),
                         reads=[B_bk, B_bv], writes=[B_bk])
                    S.op("dve", lambda e: e.reduce_sum(out=ebf[:, j:j + 1], in_=cmpb[:], axis=AX.X), reads=[B_bk], writes=[B_bk])
                S.op("dve", lambda e: e.tensor_scalar(out=ebf[:], in0=ebf[:], scalar1=63.0, scalar2=None, op0=ALU.min), reads=[B_bk], writes=[B_bk])
                for j in range(2):
                    S.op("pe", lambda e: e.matmul(lps[0:1, j * 128:(j + 1) * 128], lhsT=ebf[:, j:j + 1], rhs=ident_f[:, :], start=True, stop=True),
                         reads=[B_bk, B_const], writes=[B_lps], inc=(j == 1))
                S.op("dve", lambda e: e.tensor_copy(out=ebr[0:1, :], in_=lps[0:1, 0:256]), reads=[B_lps], writes=[B_bk])
                S.op("dve", lambda e: e.tensor_copy(out=ebrow[0:1, :], in_=ebr[0:1, :]), reads=[B_bk], writes=[B_eb])
                idr = sb(pg, "idr", [1, 256], F32)
                samer = sb(pg, "samer", [1, 256], F32)
                S.op("dve", lambda e: e.memset(samer[0:1, :], 0.0), writes=[B_bk])
                S.op("dve", lambda e: e.tensor_tensor(out=samer[0:1, 2:256], in0=ebr[0:1, 2:256], in1=ebr[0:1, 0:254], op=ALU.is_equal),
                     reads=[B_bk], writes=[B_bk])
                S.op("dve", lambda e: e.tensor_scalar(out=idr[0:1, :], in0=ebr[0:1, :], scalar1=128.0, scalar2=None, op0=ALU.mult),
                     reads=[B_bk], writes=[B_bk])
                S.op("dve", lambda e: e.scalar_tensor_tensor(out=idr[0:1, :], in0=samer[0:1, :], scalar=1048576.0, in1=idr[0:1, :],
                                                             op0=ALU.mult, op1=ALU.add), reads=[B_bk], writes=[B_bk])
                S.op("pe", lambda e: e.matmul(lps[:, 0:256], lhsT=ones_f[0:1, :], rhs=idr[0:1, :], start=True, stop=True),
                     reads=[B_bk, B_const], writes=[B_lps])
                idxf = sb(pg, "idxf", [128, 256], F32)
                S.op("dve", lambda e: e.tensor_scalar(out=idxf[:], in0=lps[:, 0:256], scalar1=bvals[:, 2:3], scalar2=None, op0=ALU.add),
                     reads=[B_lps, B_bv], writes=[B_bk])
                S.op("dve", lambda e: e.tensor_copy(out=idxw[:], in_=idxf[:]), reads=[B_bk], writes=[B_idxw])
                S.op("dve", lambda e: e.tensor_tensor(out=RK[:], in0=RK[:], in1=pst[:, :].unsqueeze(1).to_broadcast([128, NT, 64]), op=ALU.add),
                     reads=[B_RK, B_bk], writes=[B_RK])
                dsf = sb(pg, "dsf", [128, NT], F32)
                for (Mx, Bm, Dx, Bd) in ((M1, B_M1, D1i, B_D1), (M2, B_M2, D2i, B_D2)):
                    S.op("dve", lambda e: e.tensor_tensor(out=big[:], in0=RK[:], in1=Mx[:], op=ALU.mult), reads=[B_RK, Bm], writes=[B_big])
                    S.op("dve", lambda e: e.tensor_reduce(out=dsf[:], in_=big[:], axis=AX.X, op=ALU.add), reads=[B_big], writes=[B_bk])
                    S.op("dve", lambda e: e.tensor_copy(out=Dx[:], in_=dsf[:]), reads=[B_bk], writes=[Bd])
                if rt_dbg is not None:
                    S.op("dve", lambda e: e.tensor_copy(out=big[:, :, 0:1].rearrange("p t o -> p (t o)"), in_=D1i[:]), reads=[B_D1], writes=[B_big])
                    S.op("dve", lambda e: e.tensor_copy(out=big[:, :, 1:2].rearrange("p t o -> p (t o)"), in_=D2i[:]), reads=[B_D2], writes=[B_big])
                    S.op("dve", lambda e: e.tensor_copy(out=big[:, :, 2:4], in_=W12[:]), reads=[B_W12], writes=[B_big])
                    S.dma(rt_dbg, big[:, :, 0:4], reads=[B_big])
                    S.dma(eb_dbg, ebrow[:], reads=[B_eb])
                    if ix_dbg is not None:
                        S.dma(ix_dbg, idxw[:], reads=[B_idxw])
                for i in range(nt_run):
                    rows = slice(i * 128, (i + 1) * 128)
                    hb = h2[i % 2]
                    S.dma(hb[:], h2_d[rows, :], writes=[B_h2[i % 2]])
                    for (Dx, Bd) in ((D1i, B_D1), (D2i, B_D2)):
                        S.dma(None, None, reads=[B_h2[i % 2], Bd], eng="pool",
                              indirect=lambda e: e.indirect_dma_start(out=xperm_d[:, :], out_offset=bass.IndirectOffsetOnAxis(ap=Dx[:, i:i + 1], axis=0),
                                                                      in_=hb[:, :], in_offset=None))
                S.barrier()
            if upto in ("G", "G1"):
                S.final_wait()
                return nc

            with contextlib.ExitStack() as ph:
                w1b = [sb(ph, "w1b%d" % i, [128, 8, HID], BF16) for i in range(2)]
                w3b = [sb(ph, "w3b%d" % i, [128, 8, HID], BF16) for i in range(2)]
                w2b = [sb(ph, "w2b%d" % i, [128, 4, D], BF16) for i in range(2)]
                B_wb = [[Buf(), Buf(), Buf()] for _ in range(2)]
                xb = [sb(ph, "xb%d" % i, [128, D], BF16) for i in range(3)]
                B_xb = [Buf(), Buf(), Buf()]
                xbT = [sb(ph, "xbT%d" % i, [128, 8, 128], BF16) for i in range(2)]
                sa = [sb(ph, "sa%d" % i, [128, HID], F32) for i in range(2)]
                hh = [sb(ph, "hh%d" % i, [128, HID], BF16) for i in range(2)]
                hhT = [sb(ph, "hhT%d" % i, [128, 4, 128], BF16) for i in range(2)]
                yb = [sb(ph, "yb%d" % i, [128, D], F32) for i in range(2)]
                B_xbT, B_sa, B_hh, B_hhT, B_yb = [[Buf(), Buf()] for _ in range(5)]
                tpp = [ps(ph, "tpp%d" % i, [128, 1024], BF16) for i in range(2)]
                B_tpp = [Buf(), Buf()]
                agp = [ps(ph, "agp%d" % i, [128, 2, 512], F32) for i in range(2)]
                B_agp = [Buf(), Buf()]
                yps = ps(ph, "yps", [128, 2, 512], F32)
                B_yps = Buf()
                nb_run = NBLK if upto != "H1" else 4
                tpc = [0]
                bnd_reg = nc.gpsimd.to_reg(NE * 128 - 1)

                def load_w(b, which):
                    if b >= nb_run or b < 0:
                        return
                    q = b % 2
                    for m, wt_ in enumerate((w1b[q], w3b[q], w2b[q])):
                        if m not in which:
                            continue
                        S.dma(None, None, reads=[B_idxw], writes=[B_wb[q][m]], eng="pool",
                              indirect=lambda e: e.indirect_dma_start(out=wt_[:].rearrange("p k n -> p (k n)"), out_offset=None, in_=wbf_d[m][:, :],
                                                                      in_offset=bass.IndirectOffsetOnAxis(ap=idxw[:, b:b + 1], axis=0),
                                                                      bounds_check=bnd_reg, oob_is_err=False))

                def load_x(b):
                    if b >= nb_run:
                        return
                    S.dma(xb[b % 3][:], xperm_d[b * 128:(b + 1) * 128, :], writes=[B_xb[b % 3]])

                def st_T(b):
                    q = b % 2
                    t_ = tpc[0] % 2
                    tpc[0] += 1
                    for k in range(8):
                        S.op("pe", lambda e: e.transpose(out=tpp[t_][:, k * 128:(k + 1) * 128], in_=xb[b % 3][:, k * 128:(k + 1) * 128], identity=ident_bf[:]),
                             reads=[B_xb[b % 3], B_const], writes=[B_tpp[t_]], inc=(k == 7))
                    S.op("act", lambda e: e.activation(out=xbT[q][:, 0:4, :], in_=tpp[t_][:, 0:512].rearrange("p (k t) -> p k t", t=128), func=AF.Copy),
                         reads=[B_tpp[t_]], writes=[B_xbT[q]])
                    S.op("dve", lambda e: e.tensor_copy(out=xbT[q][:, 4:8, :], in_=tpp[t_][:, 512:1024].rearrange("p (k t) -> p k t", t=128)),
                         reads=[B_tpp[t_]], writes=[B_xbT[q]])

                def st_mm1(b):
                    q = b % 2
                    for k in range(8):
                        S.op("pe", lambda e: e.matmul(agp[q][:, 0, :], lhsT=xbT[q][:, k, :], rhs=w1b[q][:, k, :], start=(k == 0), stop=(k == 7)),
                             reads=[B_xbT[q], B_wb[q][0]], writes=[B_agp[q]], inc=False)
                    for k in range(8):
                        S.op("pe", lambda e: e.matmul(agp[q][:, 1, :], lhsT=xbT[q][:, k, :], rhs=w3b[q][:, k, :], start=(k == 0), stop=(k == 7)),
                             reads=[B_xbT[q], B_wb[q][1]], writes=[B_agp[q]], inc=(k == 7))
                    S.op("act", lambda e: e.activation(out=sa[q][:], in_=agp[q][:, 0, :], func=AF.Silu), reads=[B_agp[q]], writes=[B_sa[q]])
                    S.op("dve", lambda e: e.tensor_tensor(out=hh[q][:], in0=sa[q][:], in1=agp[q][:, 1, :], op=ALU.mult),
                         reads=[B_sa[q], B_agp[q]], writes=[B_hh[q]])

                def st_Th(b):
                    q = b % 2
                    t_ = tpc[0] % 2
                    tpc[0] += 1
                    for k in range(4):
                        S.op("pe", lambda e: e.transpose(out=tpp[t_][:, k * 128:(k + 1) * 128], in_=hh[q][:, k * 128:(k + 1) * 128], identity=ident_bf[:]),
                             reads=[B_hh[q], B_const], writes=[B_tpp[t_]], inc=(k == 3))
                    S.op("act", lambda e: e.activation(out=hhT[q][:, :, :], in_=tpp[t_][:, 0:512].rearrange("p (k t) -> p k t", t=128), func=AF.Copy),
                         reads=[B_tpp[t_]], writes=[B_hhT[q]])

                def st_mm2(b):
                    q = b % 2
                    for n in range(2):
                        for k in range(4):
                            S.op("pe", lambda e: e.matmul(yps[:, n, :], lhsT=hhT[q][:, k, :], rhs=w2b[q][:, k, n * 512:(n + 1) * 512],
                                                          start=(k == 0), stop=(k == 3)),
                                 reads=[B_hhT[q], B_wb[q][2]], writes=[B_yps], inc=(k == 3 and n == 1))
                    S.op("dve", lambda e: e.tensor_tensor(out=yb[q][:].rearrange("p (n f) -> p n f", n=2), in0=yps[:, :, :],
                                                          in1=g_b[:, 1024:2048].rearrange("p (n f) -> p n f", n=2), op=ALU.mult),
                         reads=[B_yps, B_gb], writes=[B_yb[q]])
                    S.dma(yperm_d[b * 128:(b + 1) * 128, :], yb[q][:], reads=[B_yb[q]])

                load_w(0, (0, 1))
                load_w(1, (0, 1))
                load_w(0, (2,))
                load_x(0)
                load_x(1)
                load_x(2)
                st_T(0)
                for t in range(nb_run + 1):
                    load_x(t + 3)
                    if t >= 1:
                        st_Th(t - 1)
                    if t + 1 < nb_run:
                        st_T(t + 1)
                    if t < nb_run:
                        st_mm1(t)
                        load_w(t + 2, (0, 1))
                    if t >= 1:
                        st_mm2(t - 1)
                    load_w(t + 1, (2,))
                S.barrier()
            if upto in ("H", "H1"):
                S.final_wait()
                return nc

            with contextlib.ExitStack() as pi:
                y1 = [sb(pi, "y1_%d" % i, [128, D], F32) for i in range(2)]
                y2 = [sb(pi, "y2_%d" % i, [128, D], F32) for i in range(2)]
                xmt = [sb(pi, "xmt%d" % i, [128, D], F32) for i in range(2)]
                B_y1, B_y2, B_xmt = [Buf(), Buf()], [Buf(), Buf()], [Buf(), Buf()]
                acc = sb(pi, "acc", [128, D], F32)
                ot = [sb(pi, "ot%d" % i, [128, D], F32) for i in range(2)]
                B_acc, B_ot = Buf(), [Buf(), Buf()]
                st6 = sb(pi, "ist6", [128, 2, 6], F32)
                mv = sb(pi, "imv", [128, 2], F32)
                rstd = sb(pi, "irstd", [128, 1], F32)
                nmr = sb(pi, "inmr", [128, 1], F32)
                B_s2 = Buf()

                def load_i(i):
                    if i >= NT:
                        return
                    q = i % 2
                    S.dma(xmt[q][:], xmid_d[i * 128:(i + 1) * 128, :], writes=[B_xmt[q]])
                    for (yt, By, Dx, Bd) in ((y1[q], B_y1[q], D1i, B_D1), (y2[q], B_y2[q], D2i, B_D2)):
                        S.dma(None, None, reads=[Bd], writes=[By], eng="pool",
                              indirect=lambda e: e.indirect_dma_start(out=yt[:, :], out_offset=None, in_=yperm_d[:, :],
                                                                      in_offset=bass.IndirectOffsetOnAxis(ap=Dx[:, i:i + 1], axis=0)))
                acc2 = [acc, sb(pi, "acc_b", [128, D], F32)]
                B_acc2 = [B_acc, Buf()]

                def ln_stats_i(x_ap, Bx, Bs):
                    S.op("dve", lambda e: e.bn_stats(out=st6[:, 0, :], in_=x_ap[:, 0:512]), reads=[Bx], writes=[Bs])
                    yield
                    S.op("dve", lambda e: e.bn_stats(out=st6[:, 1, :], in_=x_ap[:, 512:1024]), reads=[Bx], writes=[Bs])
                    yield
                    S.op("dve", lambda e: e.bn_aggr(out=mv[:], in_=st6[:].rearrange("p a b -> p (a b)")), reads=[Bs], writes=[Bs])
                    S.op("dve", lambda e: e.tensor_scalar(out=rstd[:], in0=mv[:, 1:2], scalar1=EPS, scalar2=None, op0=ALU.add), reads=[Bs], writes=[Bs])
                    yield
                    S.op("pool", lambda e: e.tensor_tensor(out=rstd[:], in0=rstd[:], in1=neghalf_c[:], op=ALU.pow), reads=[Bs, B_const], writes=[Bs])
                    yield
                    S.op("dve", lambda e: e.scalar_tensor_tensor(out=nmr[:], in0=mv[:, 0:1], scalar=-1.0, in1=rstd[:], op0=ALU.mult, op1=ALU.mult),
                         reads=[Bs], writes=[Bs])
                    yield

                def stage_1(i):
                    q = i % 2
                    ac, Ba = acc2[q], B_acc2[q]
                    S.op("dve", lambda e: e.tensor_scalar(out=ac[:], in0=y1[q][:], scalar1=W12[:, i, 0:1], scalar2=None, op0=ALU.mult),
                         reads=[B_y1[q], B_W12], writes=[Ba])
                    yield
                    S.op("dve", lambda e: e.scalar_tensor_tensor(out=ac[:], in0=y2[q][:], scalar=W12[:, i, 1:2], in1=ac[:], op0=ALU.mult, op1=ALU.add),
                         reads=[B_y2[q], B_W12, Ba], writes=[Ba])
                    yield
                    S.op("dve", lambda e: e.scalar_tensor_tensor(out=ac[:], in0=xmt[q][:], scalar=ALPHA, in1=ac[:], op0=ALU.mult, op1=ALU.add),
                         reads=[B_xmt[q], Ba], writes=[Ba])
                    yield
                    load_i(i + 2)
                    yield

                def stage_2(i):
                    q = i % 2
                    ac, Ba = acc2[q], B_acc2[q]
                    for _ in ln_stats_i(ac, Ba, B_s2):
                        yield
                    S.op("act", lambda e: e.activation(out=ot[q][:], in_=ac[:], func=AF.Identity, bias=nmr[:, 0:1], scale=rstd[:, 0:1]),
                         reads=[Ba, B_s2], writes=[B_ot[q]])
                    yield
                    S.op("pool", lambda e: e.tensor_tensor(out=ot[q][:], in0=ot[q][:], in1=lnp[:, 2, :], op=ALU.mult),
                         reads=[B_ot[q], B_lnp], writes=[B_ot[q]])
                    yield
                    S.op("dve", lambda e: e.tensor_tensor(out=ot[q][:], in0=ot[q][:], in1=lnp[:, 3, :], op=ALU.add),
                         reads=[B_ot[q], B_lnp], writes=[B_ot[q]])
                    yield
                    S.dma(out_d[i * 128:(i + 1) * 128, :], ot[q][:], reads=[B_ot[q]])
                    yield

                load_i(0)
                load_i(1)
                for step in range(NT + 1):
                    gens = []
                    if step < NT:
                        gens.append(stage_1(step))
                    if 0 <= step - 1 < NT:
                        gens.append(stage_2(step - 1))
                    interleave(*gens)
                S.barrier()

        S.final_wait()
    return nc


def make_in_maps(inputs, n_cores=8):
    x = np.asarray(inputs["x"], np.float32)
    c = np.asarray(inputs["c"], np.float32)
    ctx = np.asarray(inputs["ctx"], np.float32)
    c_ctx = np.asarray(inputs["c_ctx"], np.float32)
    w_ada = np.ascontiguousarray(np.asarray(inputs["w_ada"], np.float32)[0])
    b_ada = np.asarray(inputs["b_ada"], np.float32)[0]
    w_in = np.ascontiguousarray(np.asarray(inputs["w_in"], np.float32)[0])
    gate_b = np.asarray(inputs["gate_b"], np.float32)[0]
    b_ada_l = np.ascontiguousarray(np.repeat(b_ada.reshape(48, 128).T[:, :, None], 2, axis=2))
    rope = make_rope_tables()
    rperm = make_rperm()
    sidx = np.arange(128)
    cmask = np.stack([(sidx[None, :] >= sidx[:, None]), (sidx[:, None] >= sidx[None, :]), (sidx[None, :] > sidx[:, None])]).astype(np.float32).astype(ml_dtypes.bfloat16)
    conv_w = np.asarray(inputs["conv_w"], np.float32)[0]
    conv_b = np.asarray(inputs["conv_b"], np.float32)[0]
    convw_l = np.ascontiguousarray(conv_w.reshape(5, 8, 128).transpose(2, 1, 0))
    convb_l = np.ascontiguousarray(conv_b.reshape(8, 128).T)
    mlg = np.ascontiguousarray(np.broadcast_to(np.asarray(inputs["ml_norm_g"], np.float32)[0][None, :], (128, 512)))
    w_out = np.ascontiguousarray(np.asarray(inputs["w_out"], np.float32)[0])
    w_r = np.ascontiguousarray(np.concatenate([np.asarray(inputs["w_router_g"], np.float32)[0],
                                               np.asarray(inputs["w_router_e"], np.float32)[0]], axis=1))
    rb = np.concatenate([np.asarray(inputs["b_router_g"], np.float32)[0], np.asarray(inputs["b_router_e"], np.float32)[0]])
    rbias = np.ascontiguousarray(np.broadcast_to(rb[None, :], (128, 72)))
    lnp = np.stack([np.broadcast_to(np.asarray(inputs[k], np.float32)[0][None, :], (128, D))
                    for k in ("ln1_g", "ln1_b", "ln2_g", "ln2_b")]).astype(np.float32)
    bvals = np.concatenate([128.0 * (np.arange(128)[:, None] + 128 * np.arange(2)[None, :]), np.arange(128)[:, None]], axis=1).astype(np.float32)
    relay = lambda w, kc, n: np.ascontiguousarray(np.asarray(w, np.float32)[0].reshape(NE, kc, 128, n).transpose(0, 2, 1, 3).reshape(NE * 128, kc * n))
    w1 = relay(inputs["w1"], 8, HID)
    w3 = relay(inputs["w3"], 8, HID)
    w2 = relay(inputs["w2"], 4, D)
    biasT = make_bias_tables(np.asarray(inputs["rpb"], np.float32)[0])
    maps = []
    for b in range(n_cores):
        cc = np.stack([c[b].reshape(8, 128).T, c_ctx.reshape(8, 128).T], axis=-1)
        maps.append({
            "x": np.ascontiguousarray(x[b]),
            "ctx": np.ascontiguousarray(ctx[b]),
            "cc": np.ascontiguousarray(cc),
            "w_ada": w_ada,
            "b_ada_l": b_ada_l,
            "b_ada_r": np.ascontiguousarray(b_ada.reshape(1, -1)),
            "w_in": w_in,
            "gate_b": np.ascontiguousarray(gate_b.reshape(16, 1)),
            "biasT": biasT,
            "w_out": w_out, "w_r": w_r, "rbias": rbias, "lnp": np.ascontiguousarray(lnp), "bvals": bvals,
            "w1": w1, "w3": w3, "w2": w2,
            "rope": rope, "rperm": rperm, "cmask": cmask, "convw": convw_l, "convb": convb_l, "mlg": mlg,
        })
    return maps


def make_bias_tables(rpb):
    rpb = np.asarray(rpb, np.float32)
    out = np.empty((5, 128, 8, 5, 128), np.float32)
    q = np.arange(128)
    k = np.arange(128)
    for v, i in enumerate((2, 0, 1, 62, 63)):
        jb0 = min(max(i - 2, 0), 59)
        qr = 2 * i + q // 64
        qc = q % 64
        r0 = np.clip(qr - 4, 0, 120)
        c0 = np.clip(qc - 8, 0, 48)
        for n in range(5):
            kr = 2 * (jb0 + n) + k // 64
            kc = k % 64
            inside = ((kr[:, None] >= r0[None, :]) & (kr[:, None] < r0[None, :] + 8) &
                      (kc[:, None] >= c0[None, :]) & (kc[:, None] < c0[None, :] + 16))
            ri = np.clip(kr[:, None] - qr[None, :] + 7, 0, 14)
            ci = np.clip(kc[:, None] - qc[None, :] + 15, 0, 30)
            g = rpb[:, ri, ci]
            out[v, :, :, n, :] = np.where(inside[None], g, np.float32(NEG)).transpose(1, 0, 2)
    return out.reshape(5, 128, 8 * 5 * 128).astype(ml_dtypes.bfloat16)


def make_rope_tables():
    t = np.arange(T)
    row = (t // GW).astype(np.float64)
    col = (t % GW).astype(np.float64)
    f = np.arange(128)
    inv = 10000.0 ** (-(f % 32).astype(np.float64) / 32.0)
    pos = np.where((f // 64 == 0)[:, None], row[None, :], col[None, :])
    ang = (pos.astype(np.float32) * inv.astype(np.float32)[:, None]).astype(np.float32)
    cs, sn = np.cos(ang).astype(np.float32), np.sin(ang).astype(np.float32)
    ksc = np.float32(128.0 ** -0.5)
    return np.stack([cs, sn, cs * ksc, sn * ksc]).astype(np.float32)


def make_rperm():
    r = np.zeros((128, 128), np.float32)
    for f in range(128):
        if f % 64 < 32:
            r[f + 32, f] = -1.0
        else:
            r[f - 32, f] = 1.0
    return r.astype(ml_dtypes.bfloat16)


def kernel(**inputs):
    nc = build_program()
    maps = make_in_maps(inputs)
    res = run_bass_kernel_spmd(nc, maps, core_ids=list(range(8)))
    out = np.stack([np.asarray(r["out"]) for r in res.results], axis=0)
    return out.astype(np.float32)
```

```python
import contextlib
import numpy as np
import ml_dtypes
import concourse.bass as bass
import concourse.mybir as mybir
from concourse.bass_utils import run_bass_kernel_spmd

F32 = mybir.dt.float32
BF16 = mybir.dt.bfloat16
I32 = mybir.dt.int32
U32 = mybir.dt.uint32
AF = mybir.ActivationFunctionType
ALU = mybir.AluOpType
AX = mybir.AxisListType

D = 1024
T = 8192
CTX = 256
TT = T + CTX
NCOL = 3600
NT = T // 128
NCT = CTX // 128
GW = 64
ALPHA = 2.0 ** 0.25
EPS = 1e-5
NE = 64
CAP = 2 * T + NE * 128
NBLK = CAP // 128
HID = 512
NEG = -30000.0


class Buf:
    __slots__ = ("w", "r", "name")

    def __init__(self, name=""):
        self.w = None
        self.r = []
        self.name = name


class Sched:
    ENG = ("pe", "act", "dve", "pool", "sp")
    LIM = 16000
    NDS = 24
    NO_SELF_SYNC = ()
    NDS_SP = 16

    def __init__(self, nc, es):
        self.nc = nc
        self.es = es
        self.e = dict(pe=nc.tensor, act=nc.scalar, dve=nc.vector, pool=nc.gpsimd, sp=nc.sync)
        self.nsem = 0
        self.sems = {}
        self.epoch = {k: 0 for k in self.ENG}
        self.cnt = {k: 0 for k in self.ENG}
        self.seen = {k: {} for k in self.ENG}
        self.pending = {k: [] for k in self.ENG}
        self.dep = [0] * self.NDS
        self.dcnt = [0] * self.NDS
        self.dnext = 0
        self.dnext_q = {}
        self.n_inst = 0

    def semobj(self, key):
        if key not in self.sems:
            self.sems[key] = self.es.enter_context(self.nc.semaphore("s%d" % self.nsem))
            self.nsem += 1
        return self.sems[key]

    def _wait(self, eng, ev):
        key, val = ev
        if self.seen[eng].get(key, 0) >= val:
            return
        self.e[eng].wait_ge(self.semobj(key), val)
        self.seen[eng][key] = val
        self.n_inst += 1

    def _deps(self, eng, reads, writes):
        deps = set()
        for b in reads:
            if b.w is not None:
                deps.add(b.w)
        for b in writes:
            if b.w is not None:
                deps.add(b.w)
            deps.update(b.r)
        for ev in deps:
            if ev is None:
                continue
            if eng == "pe" and ev[0][0] == "pe":
                continue
            if eng in self.NO_SELF_SYNC and ev[0][0] == eng:
                continue
            self._wait(eng, ev)

    def _record(self, ev, reads, writes):
        for b in reads:
            b.r.append(ev)
            if len(b.r) > 12:
                b.r = b.r[-12:] if False else b.r
        for b in writes:
            b.w = ev
            b.r = []

    def op(self, eng, fn, reads=(), writes=(), inc=True):
        self._deps(eng, reads, writes)
        inst = fn(self.e[eng])
        self.n_inst += 1
        if not inc:
            self.pending[eng].append((list(reads), list(writes)))
            return inst
        if self.cnt[eng] >= self.LIM:
            self.epoch[eng] += 1
            self.cnt[eng] = 0
        self.cnt[eng] += 1
        key = (eng, self.epoch[eng])
        inst.then_inc(self.semobj(key), 1)
        ev = (key, self.cnt[eng])
        for (r, w) in self.pending[eng]:
            self._record(ev, r, w)
        self.pending[eng] = []
        self._record(ev, reads, writes)
        return inst

    def dma(self, out, in_, reads=(), writes=(), eng="sp", indirect=None):
        lo, hi = (0, self.NDS_SP) if eng == "sp" else (self.NDS_SP, self.NDS)
        i = self.dnext_q.get(eng, lo)
        self.dnext_q[eng] = lo + ((i + 1 - lo) % (hi - lo))
        if self.dcnt[i] > 0:
            self._wait(eng, (("d", i, self.dep[i]), 16 * self.dcnt[i]))
        if self.dcnt[i] >= self.LIM // 16:
            self.dep[i] += 1
            self.dcnt[i] = 0
        self._deps(eng, reads, writes)
        if indirect is None:
            inst = self.e[eng].dma_start(out=out, in_=in_)
        else:
            inst = indirect(self.e[eng])
        self.n_inst += 1
        self.dcnt[i] += 1
        key = ("d", i, self.dep[i])
        inst.then_inc(self.semobj(key), 16)
        ev = (key, 16 * self.dcnt[i])
        self._record(ev, reads, writes)
        return ev

    def barrier(self):
        for k in self.ENG:
            assert not self.pending[k], "pending group at barrier"
        evs = []
        for k in self.ENG:
            if self.cnt[k] > 0:
                evs.append(((k, self.epoch[k]), self.cnt[k]))
        for i in range(self.NDS):
            if self.dcnt[i] > 0:
                evs.append((("d", i, self.dep[i]), 16 * self.dcnt[i]))
        for k in self.ENG:
            for ev in evs:
                if ev[0][0] == k:
                    continue
                self._wait(k, ev)

    def final_wait(self, eng="sp"):
        for i in range(self.NDS):
            if self.dcnt[i] > 0:
                self._wait(eng, (("d", i, self.dep[i]), 16 * self.dcnt[i]))


def interleave(*gens):
    gens = [g for g in gens if g is not None]
    while gens:
        for g in list(gens):
            try:
                next(g)
            except StopIteration:
                gens.remove(g)


def weighted(gen, n):
    def g():
        done = False
        while not done:
            for _ in range(n):
                try:
                    next(gen)
                except StopIteration:
                    done = True
                    break
            yield
    return g()


def build_program(dbg=None, upto="all", phases="ABCDEFGHI"):
    dbg = dbg or []
    nc = bass.Bass("TRN2", target_bir_lowering=False)

    def din(name, shape, dt=F32):
        return nc.dram_tensor(name, list(shape), dt, kind="ExternalInput").ap()

    def dscr(name, shape, dt):
        kind = "ExternalOutput" if name in dbg else "Internal"
        return nc.dram_tensor(name, list(shape), dt, kind=kind).ap()

    x_d = din("x", [T, D])
    ctx_d = din("ctx", [CTX, D])
    cc_d = din("cc", [128, 8, 2])
    wada_d = din("w_ada", [D, 6 * D])
    bada_d = din("b_ada_l", [128, 48, 2])
    badar_d = din("b_ada_r", [1, 6 * D])
    win_d = din("w_in", [D, NCOL])
    gateb_d = din("gate_b", [16, 1])
    out_d = nc.dram_tensor("out", [T, D], F32, kind="ExternalOutput").ap()

    zT_d = dscr("zT", [16, 128, TT], BF16)
    vna_d = dscr("vna", [TT, 8 * 65], BF16)
    mlv_d = dscr("mlv", [TT, 512], BF16)
    mlo_d = dscr("mlo", [TT, 512], F32)
    gT_d = dscr("gT", [16, TT], F32)
    na_d = dscr("na", [T, 512], BF16)
    ml_d = dscr("ml", [T, 512], BF16)
    biasT_d = din("biasT", [5, 128, 8 * 5 * 128], BF16)
    qkT_d = dscr("qkT", [8, 128, TT], BF16)
    kml_d = dscr("kml", [TT, 512], BF16)
    hf_d = dscr("hf", [T, 512], F32)
    rope_d = din("rope", [4, 128, T])
    rperm_d = din("rperm", [128, 128], BF16)
    cmask_d = din("cmask", [3, 128, 128], BF16)
    convw_d = din("convw", [128, 8, 5])
    convb_d = din("convb", [128, 8])
    mlg_d = din("mlg", [128, 512])
    wout_d = din("w_out", [D, D])
    wr_d = din("w_r", [D, 72])
    rbias_d = din("rbias", [128, 72])
    lnp_d = din("lnp", [4, 128, D])
    bvals_d = din("bvals", [128, 3])
    w1_d = din("w1", [NE * 128, 8 * HID])
    w3_d = din("w3", [NE * 128, 8 * HID])
    w2_d = din("w2", [NE * 128, 4 * D])
    wbf_d = [dscr("wbf%d" % m, [NE * 128, 4096], BF16) for m in range(3)]
    xmid_d = dscr("xmid", [T, D], F32)
    h2_d = dscr("h2", [T, D], BF16)
    xperm_d = dscr("xperm", [CAP, D], BF16)
    yperm_d = dscr("yperm", [CAP, D], F32)
    rt_dbg = dscr("rt_dbg", [128, NT, 4], F32) if "rt_dbg" in dbg else None
    eb_dbg = dscr("eb_dbg", [1, 256], I32) if "eb_dbg" in dbg else None
    ix_dbg = dscr("ix_dbg", [128, 256], I32) if "ix_dbg" in dbg else None
    dbgD_d = dscr("dbgD", [2, 3, 4, TT], F32) if "dbgD" in dbg else None
    ada_dbg = dscr("ada_dbg", [128, 48, 2], F32) if "ada_dbg" in dbg else None
    gb_dbg = dscr("gb_dbg", [128, 2048], F32) if "gb_dbg" in dbg else None

    with contextlib.ExitStack() as es:
        S = Sched(nc, es)

        def sb(st, name, shape, dt):
            return st.enter_context(nc.sbuf_tensor("sb_" + name, list(shape), dt))

        def ps(st, name, shape, dt):
            return st.enter_context(nc.psum_tensor("ps_" + name, list(shape), dt))

        ident_bf = sb(es, "ident_bf", [128, 128], BF16)
        ident_f = sb(es, "ident_f", [128, 128], F32)
        ones_f = sb(es, "ones_f", [128, 128], F32)
        adaT = sb(es, "adaT", [128, 48, 2], F32)
        g_b = sb(es, "g_b", [128, 2048], F32)
        B_const = Buf("const")
        B_ada = Buf("ada")
        B_gb = Buf("gb")

        def mk_ident(tile):
            S.op("pool", lambda e: e.memset(tile[:], 1.0), writes=[B_const])
            S.op("pool", lambda e: e.affine_select(out=tile[:], in_=tile[:], pattern=[[1, 128]],
                                                   compare_op=ALU.is_equal, fill=0.0, base=0,
                                                   channel_multiplier=-1), reads=[B_const], writes=[B_const])
        mk_ident(ident_f)
        S.op("dve", lambda e: e.tensor_copy(out=ident_bf[:], in_=ident_f[:]), reads=[B_const], writes=[B_const])
        S.op("dve", lambda e: e.memset(ones_f[:], 1.0), writes=[B_const])
        neghalf_c = sb(es, "neghalf_c", [128, 1], F32)
        S.op("dve", lambda e: e.memset(neghalf_c[:], -0.5), writes=[B_const])


        def conv_task():
            srcs = (w1_d, w3_d, w2_d)
            for r0 in range(0, NE * 128, 128):
                for m in range(3):
                    S.dma(wbf_d[m][r0:r0 + 128, :], srcs[m][r0:r0 + 128, :], reads=bg_pace, eng="pool")
                    yield
        bg_task = conv_task() if "H" in phases else iter(())

        bg_pace = []

        def bg_step(n, pace=None):
            bg_pace[:] = [pace] if pace is not None else []
            for _ in range(n):
                try:
                    next(bg_task)
                except StopIteration:
                    return

        with contextlib.ExitStack() as pa:
            cc = sb(pa, "cc", [128, 8, 2], F32)
            scc = sb(pa, "scc", [128, 8, 2], F32)
            badal = sb(pa, "badal", [128, 48, 2], F32)
            badar = sb(pa, "badar", [1, 6 * D], F32)
            wp = [sb(pa, "wadap%d" % i, [128, 8, 1024], F32) for i in range(2)]
            grow = sb(pa, "grow", [1, 2048], F32)
            adaps = ps(pa, "adaps", [128, 512], F32)
            rowps = ps(pa, "rowps", [1, 512], F32)
            bcps = ps(pa, "bcps", [128, 512], F32)
            B_cc, B_scc, B_bl, B_br = Buf(), Buf(), Buf(), Buf()
            B_wp = [Buf(), Buf()]
            B_adaps, B_rowps, B_bcps, B_grow = Buf(), Buf(), Buf(), Buf()

            S.dma(cc[:], cc_d, writes=[B_cc])
            S.dma(badal[:], bada_d, writes=[B_bl])
            S.dma(badar[:], badar_d, writes=[B_br])
            S.op("act", lambda e: e.activation(out=scc[:], in_=cc[:], func=AF.Silu), reads=[B_cc], writes=[B_scc])
            wada_v = wada_d.rearrange("(k p) n -> p k n", p=128)
            for pc in range(6):
                S.dma(wp[pc % 2][:], wada_v[:, :, pc * 1024:(pc + 1) * 1024], writes=[B_wp[pc % 2]])
                w = wp[pc % 2]
                if pc in (2, 5):
                    gi = 0 if pc == 2 else 1
                    for grp in range(2):
                        for k in range(8):
                            S.op("pe", lambda e, k=k, grp=grp: e.matmul(
                                rowps[0:1, :], lhsT=scc[:, k, 0:1], rhs=w[:, k, grp * 512:(grp + 1) * 512],
                                start=(k == 0), stop=(k == 7)),
                                reads=[B_scc, B_wp[pc % 2]], writes=[B_rowps], inc=(k == 7))
                        S.op("dve", lambda e, grp=grp: e.tensor_tensor(
                            out=grow[0:1, gi * 1024 + grp * 512: gi * 1024 + (grp + 1) * 512], in0=rowps[0:1, :],
                            in1=badar[0:1, pc * 1024 + grp * 512: pc * 1024 + (grp + 1) * 512], op=ALU.add),
                            reads=[B_rowps, B_br], writes=[B_grow])
                        S.op("pe", lambda e, grp=grp: e.matmul(
                            bcps[:, :], lhsT=ones_f[0:1, :], rhs=grow[0:1, gi * 1024 + grp * 512: gi * 1024 + (grp + 1) * 512],
                            start=True, stop=True), reads=[B_grow, B_const], writes=[B_bcps])
                        S.op("act", lambda e, grp=grp: e.activation(
                            out=g_b[:, gi * 1024 + grp * 512: gi * 1024 + (grp + 1) * 512], in_=bcps[:, :], func=AF.Copy),
                            reads=[B_bcps], writes=[B_gb])
                else:
                    for jj in range(8):
                        for k in range(8):
                            S.op("pe", lambda e, k=k, jj=jj: e.matmul(
                                adaps[:, jj * 2:(jj + 1) * 2], lhsT=w[:, k, jj * 128:(jj + 1) * 128], rhs=scc[:, k, :],
                                start=(k == 0), stop=(k == 7)),
                                reads=[B_scc, B_wp[pc % 2]], writes=[B_adaps], inc=(k == 7 and jj == 7))
                    S.op("dve", lambda e: e.tensor_tensor(
                        out=adaT[:, pc * 8:(pc + 1) * 8, :], in0=adaps[:, 0:16].rearrange("p (j s) -> p j s", s=2),
                        in1=badal[:, pc * 8:(pc + 1) * 8, :], op=ALU.add),
                        reads=[B_adaps, B_bl], writes=[B_ada])
                    if pc in (1, 4):
                        S.op("dve", lambda e: e.tensor_scalar(
                            out=adaT[:, pc * 8:(pc + 1) * 8, :], in0=adaT[:, pc * 8:(pc + 1) * 8, :],
                            scalar1=1.0, scalar2=None, op0=ALU.add), reads=[B_ada], writes=[B_ada])
            if ada_dbg is not None:
                S.dma(ada_dbg, adaT[:], reads=[B_ada])
                S.dma(gb_dbg, g_b[:], reads=[B_gb])
            S.barrier()
        if upto == "A":
            S.final_wait()
            return nc

        with contextlib.ExitStack() as pb:
            win_sb = sb(pb, "win_sb", [128, 8, NCOL], BF16)
            gateb = sb(pb, "gateb", [16, 1], F32)
            B_win, B_gateb = Buf(), Buf()
            win_v = win_d.rearrange("(k p) n -> p k n", p=128)
            for k in range(8):
                S.dma(win_sb[:, k, :], win_v[:, k, :], writes=[B_win], eng="pool")
            S.dma(gateb[:], gateb_d, writes=[B_gateb])

            NXB = 3
            xt = [sb(pb, "xt%d" % i, [128, D], F32) for i in range(NXB)]
            B_xt = [Buf() for _ in range(NXB)]
            xn = [sb(pb, "xn%d" % i, [128, D], BF16) for i in range(2)]
            B_xn = [Buf(), Buf()]
            st6 = sb(pb, "st6", [128, 2, 6], F32)
            mv = sb(pb, "mv", [128, 2], F32)
            rstd = sb(pb, "rstd", [128, 1], F32)
            nmr = sb(pb, "nmr", [128, 1], F32)
            neghalf = sb(pb, "neghalf", [128, 1], F32)
            B_st, B_mv, B_rstd, B_nmr = Buf(), Buf(), Buf(), Buf()
            S.op("dve", lambda e: e.memset(neghalf[:], -0.5), writes=[B_const])
            xmT = [sb(pb, "xmT%d" % i, [128, 8, 512], BF16) for i in range(2)]
            B_xmT = [Buf(), Buf()]
            tp = [ps(pb, "tp%d" % i, [128, 1024], BF16) for i in range(2)]
            B_tp = [Buf(), Buf()]
            fps = [ps(pb, "fps%d" % i, [128, 512], F32) for i in range(2)]
            B_fps = [Buf(), Buf()]
            tps = [ps(pb, "tps%d" % i, [128, 512], F32) for i in range(2)]
            B_tps = [Buf(), Buf()]
            gps = ps(pb, "gps", [16, 512], F32)
            B_gps = Buf()
            zst = [sb(pb, "zst%d" % i, [128, 16, 512], BF16) for i in range(2)]
            B_zst = [Buf(), Buf()]
            vst = [sb(pb, "vst%d" % i, [128, 4, 8 * 65], BF16) for i in range(2)]
            B_vst = [Buf(), Buf()]
            mvst = [sb(pb, "mvst%d" % i, [128, 4, 512], BF16) for i in range(2)]
            B_mvst = [Buf(), Buf()]
            ost = [sb(pb, "ost%d" % i, [128, 4, 512], F32) for i in range(2)]
            B_ost = [Buf(), Buf()]
            gst = [sb(pb, "gst%d" % i, [16, 512], F32) for i in range(2)]
            B_gst = [Buf(), Buf()]
            for i in range(2):
                S.op("dve", lambda e, i=i: e.memset(vst[i][:], 1.0), writes=[B_vst[i]])

            supers = [(0, NCT, 1, ctx_d)] + [(CTX + s * 512, 4, 0, x_d[s * 512:(s + 1) * 512, :]) for s in range(T // 512)]
            tile_list = []
            for si, (t0, ntl, stream, src) in enumerate(supers):
                for j in range(ntl):
                    tile_list.append((si, j))
            load_idx = [0]

            def issue_load(n):
                while load_idx[0] <= n and load_idx[0] < len(tile_list):
                    si, j = tile_list[load_idx[0]]
                    src = supers[si][3]
                    b = load_idx[0] % NXB
                    S.dma(xt[b][:], src[j * 128:(j + 1) * 128, :], writes=[B_xt[b]])
                    load_idx[0] += 1

            gtile = {}
            cnt = 0
            for si, (t0, ntl, stream, src) in enumerate(supers):
                for j in range(ntl):
                    gtile[(si, j)] = cnt
                    cnt += 1

            evac_rr = [0]

            def evac(out, in_, reads, writes, scale=None, eng=None):
                if eng is None:
                    eng = ("act", "dve")[evac_rr[0] % 2]
                    evac_rr[0] += 1
                if eng == "act":
                    if scale is None:
                        S.op("act", lambda e: e.activation(out=out, in_=in_, func=AF.Copy), reads=reads, writes=writes)
                    else:
                        S.op("act", lambda e: e.activation(out=out, in_=in_, func=AF.Copy, scale=float(scale)),
                             reads=reads, writes=writes)
                else:
                    if scale is None:
                        S.op("dve", lambda e: e.tensor_copy(out=out, in_=in_), reads=reads, writes=writes)
                    else:
                        S.op("dve", lambda e: e.tensor_scalar(out=out, in0=in_, scalar1=float(scale), scalar2=None,
                                                              op0=ALU.mult), reads=reads, writes=writes)

            def prep(si):
                t0, ntl, stream, src = supers[si]
                xm = xmT[si % 2]
                Bxm = B_xmT[si % 2]
                for j in range(ntl):
                    g = gtile[(si, j)]
                    issue_load(g + 2)
                    b = g % NXB
                    x_t = xt[b]
                    S.op("dve", lambda e: e.bn_stats(out=st6[:, 0, :], in_=x_t[:, 0:512]), reads=[B_xt[b]], writes=[B_st])
                    S.op("dve", lambda e: e.bn_stats(out=st6[:, 1, :], in_=x_t[:, 512:1024]), reads=[B_xt[b]], writes=[B_st])
                    S.op("dve", lambda e: e.bn_aggr(out=mv[:], in_=st6[:].rearrange("p a b -> p (a b)")), reads=[B_st], writes=[B_mv])
                    S.op("dve", lambda e: e.tensor_scalar(out=rstd[:], in0=mv[:, 1:2], scalar1=EPS, scalar2=None, op0=ALU.add),
                         reads=[B_mv], writes=[B_rstd])
                    S.op("pool", lambda e: e.tensor_tensor(out=rstd[:], in0=rstd[:], in1=neghalf[:], op=ALU.pow),
                         reads=[B_rstd, B_const], writes=[B_rstd])
                    S.op("dve", lambda e: e.scalar_tensor_tensor(out=nmr[:], in0=mv[:, 0:1], scalar=-1.0, in1=rstd[:],
                                                                 op0=ALU.mult, op1=ALU.mult), reads=[B_mv, B_rstd], writes=[B_nmr])
                    xnb = xn[g % 2]
                    pace = Buf()
                    S.op("act", lambda e: e.activation(out=xnb[:], in_=x_t[:], func=AF.Identity, bias=nmr[:, 0:1], scale=rstd[:, 0:1]),
                         reads=[B_xt[b], B_nmr, B_rstd], writes=[B_xn[g % 2], pace])
                    bg_step(2, pace)
                    yield
                    tpp = tp[g % 2]
                    for k in range(8):
                        S.op("pe", lambda e, k=k: e.transpose(out=tpp[:, k * 128:(k + 1) * 128], in_=xnb[:, k * 128:(k + 1) * 128],
                                                              identity=ident_bf[:]),
                             reads=[B_xn[g % 2], B_const], writes=[B_tp[g % 2]], inc=(k == 7))
                    for k in range(8):
                        o = xm[:, k, j * 128:(j + 1) * 128]
                        i_ = tpp[:, k * 128:(k + 1) * 128]
                        sc = adaT[:, 8 + k, stream:stream + 1]
                        sh = adaT[:, 0 + k, stream:stream + 1]
                        if k % 2 == 0:
                            S.op("act", lambda e, o=o, i_=i_, sc=sc, sh=sh: e.activation(out=o, in_=i_, func=AF.Identity, bias=sh, scale=sc),
                                 reads=[B_tp[g % 2], B_ada], writes=[Bxm])
                        else:
                            S.op("dve", lambda e, o=o, i_=i_, sc=sc, sh=sh: e.tensor_scalar(out=o, in0=i_, scalar1=sc, scalar2=sh,
                                                                                           op0=ALU.mult, op1=ALU.add),
                                 reads=[B_tp[g % 2], B_ada], writes=[Bxm])
                    yield

            FM_CH = [(c * 128) for c in range(0, 8)] + [1536 + c * 128 for c in range(0, 8)]

            def mm(si):
                t0, ntl, stream, src = supers[si]
                ntok = ntl * 128
                xm = xmT[si % 2]
                Bxm = B_xmT[si % 2]
                zs = zst[si % 2]
                for ci, c0 in enumerate(FM_CH):
                    p = fps[ci % 2]
                    for k in range(8):
                        S.op("pe", lambda e, k=k: e.matmul(p[:, 0:ntok], lhsT=win_sb[:, k, c0:c0 + 128], rhs=xm[:, k, 0:ntok],
                                                           start=(k == 0), stop=(k == 7)),
                             reads=[B_win, Bxm], writes=[B_fps[ci % 2]], inc=(k == 7))
                    evac(zs[:, ci, 0:ntok], p[:, 0:ntok], [B_fps[ci % 2]], [B_zst[si % 2]], scale=(0.125 if ci < 4 else None))
                    yield
                S.dma(zT_d[:, :, t0:t0 + ntok].rearrange("c p t -> p c t"), zs[:, :, 0:ntok], reads=[B_zst[si % 2]])
                for k in range(8):
                    S.op("pe", lambda e, k=k: e.matmul(gps[:, 0:ntok], lhsT=win_sb[:, k, 3584:3600], rhs=xm[:, k, 0:ntok],
                                                       start=(k == 0), stop=(k == 7)),
                         reads=[B_win, Bxm], writes=[B_gps], inc=(k == 7))
                gs = gst[si % 2]
                S.op("act", lambda e: e.activation(out=gs[:, 0:ntok], in_=gps[:, 0:ntok], func=AF.Identity, bias=gateb[:, 0:1], scale=1.0),
                     reads=[B_gps, B_gateb], writes=[B_gst[si % 2]])
                S.dma(gT_d[:, t0:t0 + ntok], gs[:, 0:ntok], reads=[B_gst[si % 2]])
                yield
                for j in range(ntl):
                    lhs = lambda k: xm[:, k, j * 128:(j + 1) * 128]
                    for gi, c0 in enumerate((1024, 2560, 3072)):
                        q = (j * 3 + gi) % 2
                        p = tps[q]
                        for k in range(8):
                            S.op("pe", lambda e, k=k: e.matmul(p[:, :], lhsT=lhs(k), rhs=win_sb[:, k, c0:c0 + 512],
                                                               start=(k == 0), stop=(k == 7)),
                                 reads=[B_win, Bxm], writes=[B_tps[q]], inc=(k == 7))
                        if gi == 0:
                            evac(vst[si % 2][:, j, :].rearrange("p (h d) -> p h d", d=65)[:, :, 0:64],
                                 p[:, :].rearrange("p (h d) -> p h d", d=64), [B_tps[q]], [B_vst[si % 2]])
                        elif gi == 1:
                            evac(mvst[si % 2][:, j, :], p[:, :], [B_tps[q]], [B_mvst[si % 2]])
                        else:
                            S.op("act", lambda e: e.activation(out=ost[si % 2][:, j, :], in_=p[:, :], func=AF.Sigmoid),
                                 reads=[B_tps[q]], writes=[B_ost[si % 2]])
                        yield
                tv = lambda d_: d_[t0:t0 + ntok, :].rearrange("(j p) f -> p j f", p=128)
                S.dma(tv(vna_d), vst[si % 2][:, 0:ntl, :], reads=[B_vst[si % 2]])
                S.dma(tv(mlv_d), mvst[si % 2][:, 0:ntl, :], reads=[B_mvst[si % 2]])
                S.dma(tv(mlo_d), ost[si % 2][:, 0:ntl, :], reads=[B_ost[si % 2]])
                yield

            nsup = len(supers) if upto != "B1" else 2
            issue_load(1)
            interleave(prep(0))
            for si in range(nsup):
                interleave(mm(si), weighted(prep(si + 1), 1) if si + 1 < nsup else None)
            S.barrier()
        if upto in ("B", "B1"):
            S.final_wait()
            return nc


        if "F" in phases:
          with contextlib.ExitStack() as pf:
            kT_sb = sb(pf, "kT_sb", [128, 4, TT], BF16)
            v_sb = sb(pf, "v_sb", [128, TT // 128, 520], BF16)
            biasI = sb(pf, "biasI", [128, 8, 5, 128], BF16)
            biasE = sb(pf, "biasE", [128, 8, 5, 128], BF16)
            B_kT, B_v, B_bI, B_bE = Buf(), Buf(), Buf(), Buf()
            B_kTs = [Buf() for _ in range(4)]
            B_vs = [Buf() for _ in range(6)]
            for c in range(4):
                S.dma(kT_sb[:, c, :], zT_d[4 + c, :, :], writes=[B_kTs[c]])
            vv = vna_d.rearrange("(j p) f -> p j f", p=128)
            for j0 in range(0, TT // 128, 11):
                S.dma(v_sb[:, j0:j0 + 11, :], vv[:, j0:j0 + 11, :], writes=[B_vs[j0 // 11]])
            S.dma(biasI[:].rearrange("p h n q -> p (h n q)"), biasT_d[0], writes=[B_bI])
            qT = [sb(pf, "qT%d" % i, [128, 4, 128], BF16) for i in range(4)]
            B_qT = [Buf() for _ in range(4)]
            NPT = 3
            pT = [sb(pf, "pT%d" % i, [128, 896], BF16) for i in range(NPT)]
            B_pT = [Buf() for _ in range(NPT)]
            sT = [ps(pf, "sT%d" % i, [128, 1024], F32) for i in range(3)]
            B_sT = [Buf(), Buf(), Buf()]
            sS = [sb(pf, "sS%d" % i, [128, 896], F32) for i in range(4)]
            B_sS = [Buf(), Buf(), Buf(), Buf()]
            biasE_b = sb(pf, "biasE_b", [128, 8, 5, 128], BF16)
            biasE2 = [biasE, biasE_b]
            B_bE2 = [B_bE, Buf()]
            po = [ps(pf, "po%d" % i, [128, 2, 512], F32) for i in range(1)]
            B_po = [Buf()]
            rec = sb(pf, "rec", [128, 8], F32)
            B_rec = Buf()
            ostg = [sb(pf, "ostg%d" % i, [128, 8, 64], BF16) for i in range(2)]
            B_ostg = [Buf(), Buf()]
            ntiles_f = NT if upto != "F1" else 3
            tiles_f = list(range(NT)) if upto != "F1" else [0, 1, 2, 30, 62, 63]

            def load_q(idx):
                if idx < len(tiles_f):
                    i = tiles_f[idx]
                    t0 = CTX + i * 128
                    S.dma(qT[idx % 4][:], zT_d[0:4, :, t0:t0 + 128].rearrange("c p t -> p c t"), writes=[B_qT[idx % 4]])

            steps = [(idx, i, h) for idx, i in enumerate(tiles_f) for h in range(8)]
            tile_bias = {}
            nedge = 0
            for idx, i in enumerate(tiles_f):
                variant = {0: 1, 1: 2, 62: 3, 63: 4}.get(i, 0)
                if variant:
                    tile_bias[idx] = (biasE2[nedge % 2], B_bE2[nedge % 2], variant)
                    nedge += 1
                else:
                    tile_bias[idx] = (biasI, B_bI, 0)
            started = set()
            tile_pace = {}

            def tile_start(idx):
                if idx in started or idx >= len(tiles_f):
                    return
                started.add(idx)
                load_q(idx + 3)
                if idx in tile_pace:
                    bg_step(1, tile_pace[idx])
                bt, Bb, variant = tile_bias[idx]
                if variant:
                    S.dma(bt[:].rearrange("p h n q -> p (h n q)"), biasT_d[variant], writes=[Bb])

            def emit_qk(sidx):
                if sidx >= len(steps):
                    return
                idx, i, h = steps[sidx]
                tile_start(idx)
                jb0 = min(max(i - 2, 0), 59)
                q = qT[idx % 4]
                bt, Bb, _ = tile_bias[idx]
                c = h // 2
                pb = (h % 2) * 64
                sTb = sT[sidx % 3]
                for n in range(7):
                    if n < 5:
                        k0 = CTX + (jb0 + n) * 128
                    else:
                        k0 = (n - 5) * 128
                    S.op("pe", lambda e: e.matmul(sTb[:, n * 128:(n + 1) * 128], lhsT=kT_sb[pb:pb + 64, c, k0:k0 + 128],
                                                  rhs=q[pb:pb + 64, c, :], start=True, stop=True),
                         reads=B_kTs + [B_qT[idx % 4]], writes=[B_sT[sidx % 3]], inc=(n == 6))
                ssb = sS[sidx % 4]
                S.op("dve", lambda e: e.tensor_tensor(out=ssb[:, 0:640], in0=sTb[:, 0:640], in1=bt[:, h, :, :].rearrange("p n q -> p (n q)"), op=ALU.add),
                     reads=[B_sT[sidx % 3], Bb], writes=[B_sS[sidx % 4]])
                S.op("dve", lambda e: e.tensor_copy(out=ssb[:, 640:896], in_=sTb[:, 640:896]),
                     reads=[B_sT[sidx % 3]], writes=[B_sS[sidx % 4]])

            load_q(0)
            load_q(1)
            load_q(2)
            emit_qk(0)
            emit_qk(1)
            emit_qk(2)
            for sidx, (idx, i, h) in enumerate(steps):
                jb0 = min(max(i - 2, 0), 59)
                ssb = sS[sidx % 4]
                pTb = pT[sidx % NPT]
                wl = [B_pT[sidx % NPT]]
                if h == 0:
                    tile_pace[idx + 1] = Buf()
                    wl.append(tile_pace[idx + 1])
                S.op("act", lambda e: e.activation(out=pTb[:, :], in_=ssb[:, :], func=AF.Exp),
                     reads=[B_sS[sidx % 4]], writes=wl)
                emit_qk(sidx + 3)
                pob = po[0]
                for n in range(7):
                    blk = (2 + jb0 + n) if n < 5 else (n - 5)
                    S.op("pe", lambda e: e.matmul(pob[:, h // 4, (h % 4) * 65:(h % 4) * 65 + 65], lhsT=pTb[:, n * 128:(n + 1) * 128],
                                                  rhs=v_sb[:, blk, h * 65:(h + 1) * 65], start=(n == 0), stop=(n == 6)),
                         reads=[B_pT[sidx % NPT]] + B_vs, writes=[B_po[0]], inc=(n == 6))
                if h == 7:
                    pov = pob[:, :, 0:260].rearrange("p a (h d) -> p a h d", d=65)
                    S.op("dve", lambda e: e.reciprocal(out=rec[:].rearrange("p (a h) -> p a h", a=2), in_=pov[:, :, :, 64]),
                         reads=[B_po[0]], writes=[B_rec])
                    og = ostg[idx % 2]
                    for a_ in range(2):
                        S.op("dve", lambda e: e.tensor_tensor(out=og[:, a_ * 4:(a_ + 1) * 4, :], in0=pov[:, a_, :, 0:64],
                                                              in1=rec[:, a_ * 4:(a_ + 1) * 4].unsqueeze(2).to_broadcast([128, 4, 64]),
                                                              op=ALU.mult),
                             reads=[B_po[0], B_rec], writes=[B_ostg[idx % 2]])
                    S.dma(na_d[i * 128:(i + 1) * 128, :], og[:].rearrange("p h d -> p (h d)"), reads=[B_ostg[idx % 2]])
            S.barrier()
        if upto in ("F", "F1"):
            S.final_wait()
            return nc

        KSC = 128.0 ** -0.5
        if "C" in phases:
          with contextlib.ExitStack() as pc_:
            convw = sb(pc_, "convw", [128, 8, 5], F32)
            convb = sb(pc_, "convb", [128, 8], F32)
            diagw = sb(pc_, "diagw", [128, 8, 5, 128], BF16)
            rperm = sb(pc_, "rperm", [128, 128], BF16)
            B_cw, B_cb, B_dw, B_rp = Buf(), Buf(), Buf(), Buf()
            S.dma(convw[:], convw_d, writes=[B_cw])
            S.dma(convb[:], convb_d, writes=[B_cb])
            S.dma(rperm[:], rperm_d, writes=[B_rp])
            for ch in range(8):
                for j in range(5):
                    S.op("dve", lambda e: e.tensor_scalar(out=diagw[:, ch, j, :], in0=ident_f[:, :], scalar1=convw[:, ch, j:j + 1],
                                                          scalar2=None, op0=ALU.mult), reads=[B_cw, B_const], writes=[B_dw])
            u8 = [sb(pc_, "u8_%d" % i, [128, 8, 516], BF16) for i in range(2)]
            B_u8 = [Buf(), Buf()]
            rt = [sb(pc_, "rt%d" % i, [128, 4, 512], F32) for i in range(2)]
            B_rt = [Buf(), Buf()]
            qs = [sb(pc_, "qs%d" % i, [128, 512], BF16) for i in range(2)]
            B_qs = [Buf(), Buf()]
            t1 = [sb(pc_, "t1_%d" % i, [128, 512], F32) for i in range(2)]
            B_t1 = [Buf(), Buf()]
            t2 = [sb(pc_, "t2_%d" % i, [128, 512], F32) for i in range(2)]
            B_t2 = [Buf(), Buf()]
            qko = [sb(pc_, "qko%d" % i, [128, 8, 512], BF16) for i in range(2)]
            B_qko = [Buf(), Buf()]
            kst = [sb(pc_, "kst%d" % i, [128, 4, 4, 128], BF16) for i in range(2)]
            B_kst = [Buf(), Buf()]
            cps_ = [ps(pc_, "cvps%d" % i, [128, 512], F32) for i in range(2)]
            B_cps = [Buf(), Buf()]
            rps = [ps(pc_, "rps%d" % i, [128, 512], F32) for i in range(2)]
            B_rps = [Buf(), Buf()]
            trp = [ps(pc_, "trp%d" % i, [128, 4, 128], BF16) for i in range(2)]
            B_trp = [Buf(), Buf()]
            groups = [(0, CTX, False, 0)] + [(CTX + g * 512, 512, True, g * 512) for g in range(T // 512)]
            if upto == "C1":
                groups = groups[:2] + groups[-1:]

            def load_group(gi):
                if gi >= len(groups):
                    return
                tt0, n, lat, lo = groups[gi]
                u = u8[gi % 2]
                seg0, seg1 = (CTX, TT) if lat else (0, CTX)
                a = max(tt0 - 2, seg0)
                b_ = min(tt0 + n + 2, seg1)
                if a > tt0 - 2:
                    S.op("pool", lambda e: e.memset(u[:, :, 0:2], 0.0), writes=[B_u8[gi % 2]])
                if b_ < tt0 + n + 2:
                    S.op("pool", lambda e: e.memset(u[:, :, n + 2:n + 4], 0.0), writes=[B_u8[gi % 2]])
                S.dma(u[:, :, a - (tt0 - 2):b_ - (tt0 - 2)], zT_d[8:16, :, a:b_].rearrange("c p t -> p c t"), writes=[B_u8[gi % 2]])
                if lat:
                    S.dma(rt[gi % 2][:], rope_d[:, :, lo:lo + 512].rearrange("c p t -> p c t"), writes=[B_rt[gi % 2]])
            load_group(0)
            cc_ = 0
            for gi, (tt0, n, lat, lo) in enumerate(groups):
                load_group(gi + 1)
                u = u8[gi % 2]
                qo = qko[gi % 2]
                for ch in range(8):
                    cp = cps_[cc_ % 2]
                    for j in range(5):
                        S.op("pe", lambda e: e.matmul(cp[:, 0:n], lhsT=diagw[:, ch, j, :], rhs=u[:, ch, j:j + n], start=(j == 0), stop=(j == 4)),
                             reads=[B_dw, B_u8[gi % 2]], writes=[B_cps[cc_ % 2]], inc=(j == 4))
                    isk = ch >= 4
                    if not lat:
                        if isk:
                            S.op("act", lambda e: e.activation(out=t1[0][:, 0:n], in_=cp[:, 0:n], func=AF.Silu, bias=convb[:, ch:ch + 1], scale=1.0),
                                 reads=[B_cps[cc_ % 2], B_cb], writes=[B_t1[0]])
                            S.op("dve", lambda e: e.tensor_scalar(out=qo[:, ch, 0:n], in0=t1[0][:, 0:n], scalar1=KSC, scalar2=None, op0=ALU.mult),
                                 reads=[B_t1[0]], writes=[B_qko[gi % 2]])
                        else:
                            S.op("act", lambda e: e.activation(out=qo[:, ch, 0:n], in_=cp[:, 0:n], func=AF.Silu, bias=convb[:, ch:ch + 1], scale=1.0),
                                 reads=[B_cps[cc_ % 2], B_cb], writes=[B_qko[gi % 2]])
                    else:
                        q_ = qs[cc_ % 2]
                        S.op("act", lambda e: e.activation(out=q_[:, 0:n], in_=cp[:, 0:n], func=AF.Silu, bias=convb[:, ch:ch + 1], scale=1.0),
                             reads=[B_cps[cc_ % 2], B_cb], writes=[B_qs[cc_ % 2]])
                        rp_ = rps[cc_ % 2]
                        S.op("pe", lambda e: e.matmul(rp_[:, 0:n], lhsT=rperm[:, :], rhs=q_[:, 0:n], start=True, stop=True),
                             reads=[B_rp, B_qs[cc_ % 2]], writes=[B_rps[cc_ % 2]])
                        tb = 2 if isk else 0
                        S.op("pool", lambda e: e.tensor_tensor(out=t1[cc_ % 2][:, 0:n], in0=q_[:, 0:n], in1=rt[gi % 2][:, tb, 0:n], op=ALU.mult),
                             reads=[B_qs[cc_ % 2], B_rt[gi % 2]], writes=[B_t1[cc_ % 2]])
                        S.op("dve", lambda e: e.tensor_tensor(out=t2[cc_ % 2][:, 0:n], in0=rp_[:, 0:n], in1=rt[gi % 2][:, tb + 1, 0:n], op=ALU.mult),
                             reads=[B_rps[cc_ % 2], B_rt[gi % 2]], writes=[B_t2[cc_ % 2]])
                        S.op("dve", lambda e: e.tensor_tensor(out=qo[:, ch, 0:n], in0=t1[cc_ % 2][:, 0:n], in1=t2[cc_ % 2][:, 0:n], op=ALU.add),
                             reads=[B_t1[cc_ % 2], B_t2[cc_ % 2]], writes=[B_qko[gi % 2]])
                    cc_ += 1
                S.dma(qkT_d[:, :, tt0:tt0 + n].rearrange("c p t -> p c t"), qo[:, :, 0:n], reads=[B_qko[gi % 2]])
                ks = kst[gi % 2]
                for j in range(n // 128):
                    tr = trp[j % 2]
                    for h in range(4):
                        S.op("pe", lambda e: e.transpose(out=tr[:, h, :], in_=qo[:, 4 + h, j * 128:(j + 1) * 128], identity=ident_bf[:]),
                             reads=[B_qko[gi % 2], B_const], writes=[B_trp[j % 2]], inc=(h == 3))
                    S.op("act", lambda e: e.activation(out=ks[:, j, :, :], in_=tr[:, :, :], func=AF.Copy),
                         reads=[B_trp[j % 2]], writes=[B_kst[gi % 2]])
                S.dma(kml_d[tt0:tt0 + n, :].rearrange("(j p) f -> p j f", p=128), ks[:, 0:n // 128, :, :].rearrange("p j h d -> p j (h d)"),
                      reads=[B_kst[gi % 2]])
            S.barrier()
        if upto in ("C", "C1"):
            S.final_wait()
            return nc

        NCH = TT // 128
        if "E" in phases:
          with contextlib.ExitStack() as pde:
            wgtT = [sb(pde, "wgtT%d" % d, [128, NCH, 4], F32) for d in range(2)]
            thrT = [sb(pde, "thrT%d" % d, [128, NCH, 4], F32) for d in range(2)]
            decB = [sb(pde, "decB%d" % d, [128, 4, NCH], F32) for d in range(2)]
            B_wgtT, B_thrT, B_decB = [Buf(), Buf()], [Buf(), Buf()], [Buf(), Buf()]
            with contextlib.ExitStack() as pd:
                X1 = sb(pd, "X1", [4, TT], F32)
                X2 = sb(pd, "X2", [4, TT], F32)
                X3 = sb(pd, "X3", [4, TT], F32)
                Z0 = sb(pd, "Z0", [4, TT], F32)
                ngd = sb(pd, "ngd", [4, NCH], F32)
                dec = sb(pd, "dec", [4, NCH], F32)
                sel = sb(pd, "sel", [4, 4, 128], F32)
                ptw = ps(pd, "ptw", [128, 512], F32)
                ptt = ps(pd, "ptt", [128, 512], F32)
                pdc = ps(pd, "pdc", [128, 512], F32)
                B1, B2, B3, BZ, Bng, Bdec, Bsel, Bptw, Bptt, Bpdc = [Buf() for _ in range(10)]
                S.op("pool", lambda e: e.memset(Z0[:], 0.0), writes=[BZ])
                for h in range(4):
                    S.op("dve", lambda e: e.tensor_copy(out=sel[0:4, h, :], in_=ident_f[0:4, h:h + 1].to_broadcast([4, 128])),
                         reads=[B_const], writes=[Bsel])
                segs = [(0, CTX), (CTX, TT)]
                for d in range(2):
                    if d == 0:
                        S.dma(X1[:], gT_d[0:4, :], writes=[B1])
                        S.dma(X2[:], gT_d[4:8, :], writes=[B2])
                    else:
                        S.dma(X3[:], gT_d[8:12, :], writes=[B3])
                        for (a, b_) in segs:
                            S.op("dve", lambda e: e.tensor_copy(out=X1[:, a:b_], in_=X3[:, a:b_][:, ::-1]), reads=[B3], writes=[B1])
                        S.dma(X3[:], gT_d[12:16, :], writes=[B3])
                        for (a, b_) in segs:
                            S.op("dve", lambda e: e.tensor_copy(out=X2[:, a:b_], in_=X3[:, a:b_][:, ::-1]), reads=[B3], writes=[B2])
                    S.op("act", lambda e: e.activation(out=X2[:], in_=X2[:], func=AF.Exp, scale=-1.0), reads=[B2], writes=[B2])
                    S.op("act", lambda e: e.activation(out=X2[:], in_=X2[:], func=AF.Ln, bias=1.0, scale=1.0), reads=[B2], writes=[B2])
                    S.op("dve", lambda e: e.tensor_tensor_scan(out=X3[:], data0=Z0[:], data1=X2[:], initial=0.0, op0=ALU.add, op1=ALU.add),
                         reads=[BZ, B2], writes=[B3])
                    S.op("dve", lambda e: e.tensor_tensor(out=X1[:], in0=X1[:], in1=X3[:], op=ALU.add), reads=[B1, B3], writes=[B1])
                    S.op("dve", lambda e: e.tensor_tensor_scan(out=X2[:], data0=X1[:], data1=X1[:], initial=0.0, op0=ALU.max, op1=ALU.max),
                         reads=[B1], writes=[B2])
                    S.op("dve", lambda e: e.tensor_scalar(out=ngd[:], in0=X2[:, 127:TT:128], scalar1=-1.0, scalar2=None, op0=ALU.mult),
                         reads=[B2], writes=[Bng])
                    S.op("dve", lambda e: e.tensor_copy(out=dec[:, 0:1], in_=ngd[:, 0:1]), reads=[Bng], writes=[Bdec])
                    S.op("dve", lambda e: e.tensor_tensor(out=dec[:, 1:NCH], in0=X2[:, 127:TT - 128:128], in1=ngd[:, 1:NCH], op=ALU.add),
                         reads=[B2, Bng], writes=[Bdec])
                    S.op("act", lambda e: e.activation(out=dec[:], in_=dec[:], func=AF.Exp), reads=[Bdec], writes=[Bdec])
                    for c in range(NCH):
                        sl = slice(c * 128, (c + 1) * 128)
                        S.op("act", lambda e: e.activation(out=X1[:, sl], in_=X1[:, sl], func=AF.Exp, bias=ngd[:, c:c + 1], scale=1.0),
                             reads=[B1, Bng], writes=[B1])
                        S.op("act", lambda e: e.activation(out=X3[:, sl], in_=X3[:, sl], func=AF.Exp, bias=ngd[:, c:c + 1], scale=1.0),
                             reads=[B3, Bng], writes=[B3])
                    if d == 0:
                        Wt, Bw, Tt, Bt = X1, B1, X3, B3
                    else:
                        for (a, b_) in segs:
                            S.op("dve", lambda e: e.tensor_copy(out=X2[:, a:b_], in_=X1[:, a:b_][:, ::-1]), reads=[B1], writes=[B2])
                        for (a, b_) in segs:
                            S.op("dve", lambda e: e.tensor_copy(out=X1[:, a:b_], in_=X3[:, a:b_][:, ::-1]), reads=[B3], writes=[B1])
                        Wt, Bw, Tt, Bt = X2, B2, X1, B1
                    for blk in range(NCH):
                        sl = slice(blk * 128, (blk + 1) * 128)
                        S.op("pe", lambda e: e.matmul(ptw[:, blk * 4:(blk + 1) * 4], lhsT=Wt[0:4, sl], rhs=ident_f[0:4, 0:4], start=True, stop=True),
                             reads=[Bw, B_const], writes=[Bptw], inc=False)
                        S.op("pe", lambda e: e.matmul(ptt[:, blk * 4:(blk + 1) * 4], lhsT=Tt[0:4, sl], rhs=ident_f[0:4, 0:4], start=True, stop=True),
                             reads=[Bt, B_const], writes=[Bptt], inc=(blk == NCH - 1))
                    S.op("dve", lambda e: e.tensor_copy(out=wgtT[d][:].rearrange("p c h -> p (c h)"), in_=ptw[:, 0:NCH * 4]),
                         reads=[Bptw], writes=[B_wgtT[d]])
                    S.op("dve", lambda e: e.tensor_copy(out=thrT[d][:].rearrange("p c h -> p (c h)"), in_=ptt[:, 0:NCH * 4]),
                         reads=[Bptt], writes=[B_thrT[d]])
                    for h in range(4):
                        S.op("pe", lambda e: e.matmul(pdc[:, h * NCH:(h + 1) * NCH], lhsT=sel[0:4, h, :], rhs=dec[0:4, :], start=True, stop=True),
                             reads=[Bsel, Bdec], writes=[Bpdc], inc=(h == 3))
                    S.op("dve", lambda e: e.tensor_copy(out=decB[d][:].rearrange("p h c -> p (h c)"), in_=pdc[:, 0:4 * NCH]),
                         reads=[Bpdc], writes=[B_decB[d]])
                if dbgD_d is not None:
                    pass
                S.barrier()

            with contextlib.ExitStack() as pe_:
                cmask = sb(pe_, "cmask", [128, 3, 128], BF16)
                mlg = sb(pe_, "mlg", [128, 512], F32)
                B_cm, B_mlg = Buf(), Buf()
                S.dma(cmask[:], cmask_d.rearrange("a p q -> p a q"), writes=[B_cm])
                S.dma(mlg[:], mlg_d, writes=[B_mlg])
                QT = [sb(pe_, "QT%d" % i, [128, 4, 128], BF16) for i in range(2)]
                KT = [sb(pe_, "KT%d" % i, [128, 4, 128], BF16) for i in range(2)]
                Kt = [sb(pe_, "Kt%d" % i, [128, 4, 128], BF16) for i in range(2)]
                Vt = [sb(pe_, "Vt%d" % i, [128, 4, 128], BF16) for i in range(2)]
                HF = [sb(pe_, "HF%d" % i, [128, 512], F32) for i in range(2)]
                SO = [sb(pe_, "SO%d" % i, [128, 512], F32) for i in range(2)]
                B_QT, B_KT, B_Kt, B_Vt, B_HF, B_SO = [[Buf(), Buf()] for _ in range(6)]
                vp = [sb(pe_, "vp%d" % i, [128, 4, 129], BF16) for i in range(2)]
                B_vp = [Buf(), Buf()]
                sm = [sb(pe_, "sm%d" % i, [128, 128], BF16) for i in range(4)]
                B_sm = [Buf() for _ in range(4)]
                Cst = sb(pe_, "Cst", [128, 4, 129], F32)
                B_Cst = [Buf() for _ in range(4)]
                Cdb = [sb(pe_, "Cdb%d" % i, [128, 4, 129], BF16) for i in range(2)]
                B_Cdb = [[Buf() for _ in range(4)] for _ in range(2)]
                hst = [sb(pe_, "hst%d" % i, [128, 4, 128], F32) for i in range(2)]
                B_hst = [Buf(), Buf()]
                dn = sb(pe_, "dn", [128, 4], F32)
                B_dn = Buf()
                hsq = sb(pe_, "hsq", [128, 512], F32)
                ss = sb(pe_, "ss", [128, 4], F32)
                go = sb(pe_, "go", [128, 512], F32)
                mlo_t = [sb(pe_, "mlo_t%d" % i, [128, 512], BF16) for i in range(2)]
                B_hsq, B_ss, B_go = Buf(), Buf(), Buf()
                B_mlo = [Buf(), Buf()]
                sps = [ps(pe_, "sps%d" % i, [128, 4, 128], F32) for i in range(2)]
                B_sps = [Buf(), Buf()]
                hps = [ps(pe_, "hps%d" % i, [128, 2, 512], F32) for i in range(2)]
                B_hps = [Buf(), Buf()]
                cps2 = ps(pe_, "cps2", [128, 2, 512], F32)
                B_cps2 = [Buf() for _ in range(4)]

                def blk_of(d, c):
                    if d == 0:
                        return c
                    return (1 - c) if c < 2 else (67 - c)

                nch_run = NCH if upto != "E1" else 5
                for d in range(2):
                    S.op("dve", lambda e: e.memset(Cst[:], 0.0), writes=B_Cst)

                    def load_chunk(c):
                        if c >= nch_run:
                            return
                        blk = blk_of(d, c)
                        b = c % 2
                        rows = slice(blk * 128, (blk + 1) * 128)
                        S.dma(Kt[b][:].rearrange("p h d -> p (h d)"), kml_d[rows, :], writes=[B_Kt[b]])
                        S.dma(Vt[b][:].rearrange("p h d -> p (h d)"), mlv_d[rows, :], writes=[B_Vt[b]])
                        if blk >= 2:
                            S.dma(QT[b][:], qkT_d[0:4, :, rows].rearrange("c p t -> p c t"), writes=[B_QT[b]])
                            S.dma(KT[b][:], qkT_d[4:8, :, rows].rearrange("c p t -> p c t"), writes=[B_KT[b]])
                            if d == 1:
                                S.dma(HF[b][:], hf_d[(blk - 2) * 128:(blk - 1) * 128, :], writes=[B_HF[b]])
                                S.dma(SO[b][:], mlo_d[rows, :], writes=[B_SO[b]])
                    load_chunk(0)
                    for c in range(nch_run):
                        load_chunk(c + 1)
                        blk = blk_of(d, c)
                        b = c % 2
                        lat = blk >= 2
                        vpb = vp[b]
                        S.op("dve", lambda e: e.tensor_tensor(out=vpb[:, :, 0:128], in0=Vt[b][:, :, :],
                                                              in1=wgtT[d][:, blk, :].unsqueeze(2).to_broadcast([128, 4, 128]), op=ALU.mult),
                             reads=[B_Vt[b], B_wgtT[d]], writes=[B_vp[b]])
                        S.op("dve", lambda e: e.tensor_copy(out=vpb[:, :, 128], in_=wgtT[d][:, blk, :]),
                             reads=[B_wgtT[d]], writes=[B_vp[b]])
                        hp = hps[b]
                        cdb = Cdb[b]
                        for h in range(4):
                            S.op("act", lambda e: e.activation(out=cdb[:, h, :], in_=Cst[:, h, :], func=AF.Copy, scale=decB[d][:, h, c:c + 1]),
                                 reads=[B_Cst[h], B_decB[d]], writes=[B_Cdb[b][h]])
                            if lat:
                                S.op("pe", lambda e: e.matmul(sps[b][:, h, :], lhsT=KT[b][:, h, :], rhs=QT[b][:, h, :], start=True, stop=True),
                                     reads=[B_KT[b], B_QT[b]], writes=[B_sps[b]], inc=(h == 3))
                        for h in range(4):
                            co = cps2[:, h // 2, (h % 2) * 129:(h % 2) * 129 + 129]
                            S.op("pe", lambda e: e.matmul(co, lhsT=Kt[b][:, h, :], rhs=vpb[:, h, :], start=True, stop=True),
                                 reads=[B_Kt[b], B_vp[b]], writes=[B_cps2[h]])
                            if lat:
                                S.op("dve", lambda e: e.tensor_tensor(out=sm[h][:, :], in0=sps[b][:, h, :], in1=cmask[:, d, :], op=ALU.mult),
                                     reads=[B_sps[b], B_cm], writes=[B_sm[h]])
                        if lat:
                            for h in range(4):
                                ho = hp[:, h // 2, (h % 2) * 129:(h % 2) * 129 + 129]
                                S.op("pe", lambda e: e.matmul(ho, lhsT=sm[h][:, :], rhs=vpb[:, h, :], start=True, stop=False),
                                     reads=[B_sm[h], B_vp[b]], writes=[B_hps[b]], inc=False)
                                S.op("pe", lambda e: e.matmul(ho, lhsT=QT[b][:, h, :], rhs=cdb[:, h, :], start=False, stop=True),
                                     reads=[B_QT[b], B_Cdb[b][h]], writes=[B_hps[b]], inc=(h == 3))
                        for h in range(4):
                            co = cps2[:, h // 2, (h % 2) * 129:(h % 2) * 129 + 129]
                            S.op("dve", lambda e: e.scalar_tensor_tensor(out=Cst[:, h, :], in0=Cst[:, h, :], scalar=decB[d][:, h, c:c + 1], in1=co,
                                                                         op0=ALU.mult, op1=ALU.add),
                                 reads=[B_Cst[h], B_decB[d], B_cps2[h], B_Cdb[b][h]], writes=[B_Cst[h]])
                        if not lat:
                            continue
                        hv = hp[:, :, 0:258].rearrange("p a (h d) -> p a h d", d=129)
                        S.op("act", lambda e: e.activation(out=dn[:].rearrange("p (a h) -> p a h", a=2), in_=hv[:, :, :, 128], func=AF.Abs),
                             reads=[B_hps[b]], writes=[B_dn])
                        S.op("dve", lambda e: e.tensor_tensor(out=dn[:], in0=dn[:], in1=thrT[d][:, blk, :], op=ALU.max),
                             reads=[B_dn, B_thrT[d]], writes=[B_dn])
                        S.op("dve", lambda e: e.reciprocal(out=dn[:], in_=dn[:]), reads=[B_dn], writes=[B_dn])
                        hs = hst[b]
                        for a in range(2):
                            S.op("dve", lambda e: e.tensor_tensor(out=hs[:, a * 2:(a + 1) * 2, :], in0=hv[:, a, :, 0:128],
                                                                  in1=dn[:, a * 2:(a + 1) * 2].unsqueeze(2).to_broadcast([128, 2, 128]), op=ALU.mult),
                                 reads=[B_hps[b], B_dn], writes=[B_hst[b]])
                        lt = blk - 2
                        if d == 0:
                            S.dma(hf_d[lt * 128:(lt + 1) * 128, :], hs[:].rearrange("p h d -> p (h d)"), reads=[B_hst[b]])
                        else:
                            hsf = hs[:].rearrange("p h d -> p (h d)")
                            S.op("pool", lambda e: e.tensor_tensor(out=hsf, in0=hsf, in1=HF[b][:, :], op=ALU.add),
                                 reads=[B_hst[b], B_HF[b]], writes=[B_hst[b]])
                            S.op("pool", lambda e: e.tensor_tensor(out=hsq[:, :], in0=hsf, in1=hsf, op=ALU.mult),
                                 reads=[B_hst[b]], writes=[B_hsq])
                            S.op("dve", lambda e: e.tensor_reduce(out=ss[:, :], in_=hsq[:].rearrange("p (h d) -> p h d", d=128), axis=AX.X, op=ALU.add),
                                 reads=[B_hsq], writes=[B_ss])
                            S.op("dve", lambda e: e.tensor_scalar(out=ss[:, :], in0=ss[:, :], scalar1=1.0 / 128.0, scalar2=EPS, op0=ALU.mult, op1=ALU.add),
                                 reads=[B_ss], writes=[B_ss])
                            S.op("pool", lambda e: e.tensor_tensor(out=ss[:, :], in0=ss[:, :], in1=neghalf_c[:, 0:1].to_broadcast([128, 4]), op=ALU.pow),
                                 reads=[B_ss, B_const], writes=[B_ss])
                            S.op("pool", lambda e: e.tensor_tensor(out=go[:, :], in0=SO[b][:, :], in1=mlg[:, :], op=ALU.mult),
                                 reads=[B_SO[b], B_mlg], writes=[B_go])
                            S.op("dve", lambda e: e.tensor_tensor(out=hs[:, :, :], in0=hs[:, :, :],
                                                                  in1=ss[:, :].unsqueeze(2).to_broadcast([128, 4, 128]), op=ALU.mult),
                                 reads=[B_hst[b], B_ss], writes=[B_hst[b]])
                            S.op("dve", lambda e: e.tensor_tensor(out=mlo_t[b][:, :], in0=hsf, in1=go[:, :], op=ALU.mult),
                                 reads=[B_hst[b], B_go], writes=[B_mlo[b]])
                            S.dma(ml_d[lt * 128:(lt + 1) * 128, :], mlo_t[b][:, :], reads=[B_mlo[b]])
                    S.barrier()
        if upto in ("E", "E1"):
            S.final_wait()
            return nc

        if "G" in phases:
          bg_step(100000)
          with contextlib.ExitStack() as pg0:
            W12 = sb(pg0, "W12", [128, NT, 2], F32)
            D1i = sb(pg0, "D1i", [128, NT], I32)
            D2i = sb(pg0, "D2i", [128, NT], I32)
            ebrow = sb(pg0, "ebrow", [1, 256], I32)
            idxw = sb(pg0, "idxw", [128, 256], I32)
            B_idxw = Buf()
            lnp = sb(pg0, "lnp", [128, 4, D], F32)
            B_W12, B_D1, B_D2, B_eb, B_lnp = Buf(), Buf(), Buf(), Buf(), Buf()
            S.dma(lnp[:], lnp_d.rearrange("a p f -> p a f"), writes=[B_lnp])

            def layer_norm_stats(x_ap, Bx, st6, mv, rstd, nmr, Bs):
                S.op("dve", lambda e: e.bn_stats(out=st6[:, 0, :], in_=x_ap[:, 0:512]), reads=[Bx], writes=[Bs])
                S.op("dve", lambda e: e.bn_stats(out=st6[:, 1, :], in_=x_ap[:, 512:1024]), reads=[Bx], writes=[Bs])
                S.op("dve", lambda e: e.bn_aggr(out=mv[:], in_=st6[:].rearrange("p a b -> p (a b)")), reads=[Bs], writes=[Bs])
                S.op("dve", lambda e: e.tensor_scalar(out=rstd[:], in0=mv[:, 1:2], scalar1=EPS, scalar2=None, op0=ALU.add), reads=[Bs], writes=[Bs])
                S.op("pool", lambda e: e.tensor_tensor(out=rstd[:], in0=rstd[:], in1=neghalf_c[:], op=ALU.pow), reads=[Bs, B_const], writes=[Bs])
                S.op("dve", lambda e: e.scalar_tensor_tensor(out=nmr[:], in0=mv[:, 0:1], scalar=-1.0, in1=rstd[:], op0=ALU.mult, op1=ALU.mult),
                     reads=[Bs], writes=[Bs])

            with contextlib.ExitStack() as pg:
                wout_sb = sb(pg, "wout_sb", [128, 8, D], BF16)
                wstg = [sb(pg, "wstg%d" % i, [128, D], F32) for i in range(2)]
                B_wout, B_wstg = Buf(), [Buf(), Buf()]
                wout_v = wout_d.rearrange("(k p) n -> p k n", p=128)
                for k in range(8):
                    S.dma(wstg[k % 2][:], wout_v[:, k, :], writes=[B_wstg[k % 2]])
                    S.op("dve", lambda e: e.tensor_tensor(out=wout_sb[:, k, :], in0=wstg[k % 2][:], in1=g_b[:, 0:1024], op=ALU.mult),
                         reads=[B_wstg[k % 2], B_gb], writes=[B_wout])
                wr_sb = sb(pg, "wr_sb", [128, 8, 72], BF16)
                rbias = sb(pg, "rbias", [128, 72], F32)
                SU = sb(pg, "SU", [128, 128], BF16)
                ones_bf = sb(pg, "ones_bf", [128, 128], BF16)
                bvals = sb(pg, "bvals", [128, 3], F32)
                B_wr, B_rb, B_SU, B_bv = Buf(), Buf(), Buf(), Buf()
                S.dma(wr_sb[:], wr_d.rearrange("(k p) n -> p k n", p=128), writes=[B_wr], eng="pool")
                S.dma(rbias[:], rbias_d, writes=[B_rb])
                S.dma(SU[:], cmask_d[2], writes=[B_SU])
                S.dma(bvals[:], bvals_d, writes=[B_bv])
                S.op("dve", lambda e: e.memset(ones_bf[:], 1.0), writes=[B_const])
                M1 = sb(pg, "M1", [128, NT, 64], BF16)
                M2 = sb(pg, "M2", [128, NT, 64], BF16)
                RK = sb(pg, "RK", [128, NT, 64], F32)
                big = sb(pg, "big", [128, NT, 64], F32)
                run = sb(pg, "run", [128, 64], F32)
                B_M1, B_M2, B_RK, B_big, B_run = Buf(), Buf(), Buf(), Buf(), Buf()
                S.op("dve", lambda e: e.memset(run[:], 0.0), writes=[B_run])
                xg = [sb(pg, "xg%d" % i, [128, D], F32) for i in range(2)]
                mix = [sb(pg, "mix%d" % i, [128, D], BF16) for i in range(2)]
                B_xg, B_mix = [Buf(), Buf()], [Buf(), Buf()]
                mixT = sb(pg, "mixT", [128, 8, 128], BF16)
                xm_ = sb(pg, "xm_", [128, D], F32)
                xmid = [sb(pg, "xmid%d" % i, [128, D], F32) for i in range(2)]
                xn2 = sb(pg, "xn2", [128, D], BF16)
                h2T = sb(pg, "h2T", [128, 8, 128], BF16)
                h2 = [sb(pg, "h2_%d" % i, [128, D], BF16) for i in range(2)]
                B_mixT, B_xm, B_xmid, B_xn2, B_h2T, B_h2 = Buf(), Buf(), [Buf(), Buf()], Buf(), Buf(), [Buf(), Buf()]
                st6 = sb(pg, "gst6", [128, 2, 6], F32)
                mv = sb(pg, "gmv", [128, 2], F32)
                rstd = sb(pg, "grstd", [128, 1], F32)
                nmr = sb(pg, "gnmr", [128, 1], F32)
                B_s1 = Buf()
                lg = sb(pg, "lg", [128, 72], F32)
                sm8 = sb(pg, "sm8", [128, 16], F32)
                gm = sb(pg, "gm", [128, 8], F32)
                ge = sb(pg, "ge", [128, 8], F32)
                elm = sb(pg, "elm", [128, 64], F32)
                top8 = sb(pg, "top8", [128, 8], F32)
                m12 = sb(pg, "m12", [128, 64], BF16)
                B_lg, B_sm8, B_gm, B_elm, B_top8, B_m12 = Buf(), Buf(), Buf(), Buf(), Buf(), Buf()
                tpg = [ps(pg, "tpg%d" % i, [128, 1024], BF16) for i in range(2)]
                B_tpg = [Buf(), Buf()]
                ops_ = ps(pg, "ops_", [128, 2, 512], F32)
                B_ops = Buf()
                tpb = ps(pg, "tpb", [128, 1024], BF16)
                B_tpb = Buf()
                lps = ps(pg, "lps", [128, 512], F32)
                B_lps = Buf()
                rkps = ps(pg, "rkps", [128, 512], F32)
                B_rkps = Buf()

                nt_run = NT if upto not in ("G1",) else 2

                def load_tile(i):
                    if i >= nt_run:
                        return
                    rows = slice(i * 128, (i + 1) * 128)
                    S.dma(xg[i % 2][:], x_d[rows, :], writes=[B_xg[i % 2]])
                    S.dma(mix[i % 2][:, 0:512], na_d[rows, :], writes=[B_mix[i % 2]])
                    S.dma(mix[i % 2][:, 512:1024], ml_d[rows, :], writes=[B_mix[i % 2]])
                st6b = sb(pg, "gst6b", [128, 2, 6], F32)
                mvb = sb(pg, "gmvb", [128, 2], F32)
                rstdb = sb(pg, "grstdb", [128, 1], F32)
                nmrb = sb(pg, "gnmrb", [128, 1], F32)
                B_s1b = Buf()
                lps2 = ps(pg, "lps2", [128, 512], F32)
                lpsb = [lps, lps2]
                B_lpsb = [B_lps, Buf()]

                xm2 = [xm_, sb(pg, "xm_b", [128, D], F32)]
                B_xm2 = [B_xm, Buf()]
                h2T2 = [h2T, sb(pg, "h2T_b", [128, 8, 128], BF16)]
                B_h2T2 = [B_h2T, Buf()]
                lg2 = [lg, sb(pg, "lg_b", [128, 72], F32)]
                B_lg2 = [B_lg, Buf()]
                sm82 = [sm8, sb(pg, "sm8_b", [128, 16], F32)]
                B_sm82 = [B_sm8, Buf()]
                elm2 = [elm, sb(pg, "elm_b", [128, 64], F32)]
                B_elm2 = [B_elm, Buf()]
                top82 = [top8, sb(pg, "top8_b", [128, 8], F32)]
                B_top82 = [B_top8, Buf()]

                def stage_a1(i):
                    xm_ = xm2[i % 2]
                    B_xm = B_xm2[i % 2]
                    mx = mix[i % 2]
                    tp_ = tpg[0]
                    for k in range(8):
                        S.op("pe", lambda e: e.transpose(out=tp_[:, k * 128:(k + 1) * 128], in_=mx[:, k * 128:(k + 1) * 128], identity=ident_bf[:]),
                             reads=[B_mix[i % 2], B_const], writes=[B_tpg[0]], inc=(k == 7))
                    yield
                    S.op("act", lambda e: e.activation(out=mixT[:, 0:4, :], in_=tp_[:, 0:512].rearrange("p (k t) -> p k t", t=128), func=AF.Copy),
                         reads=[B_tpg[0]], writes=[B_mixT])
                    yield
                    S.op("dve", lambda e: e.tensor_copy(out=mixT[:, 4:8, :], in_=tp_[:, 512:1024].rearrange("p (k t) -> p k t", t=128)),
                         reads=[B_tpg[0]], writes=[B_mixT])
                    yield
                    for n in range(2):
                        for k in range(8):
                            S.op("pe", lambda e: e.matmul(ops_[:, n, :], lhsT=mixT[:, k, :], rhs=wout_sb[:, k, n * 512:(n + 1) * 512],
                                                          start=(k == 0), stop=(k == 7)),
                                 reads=[B_mixT, B_wout], writes=[B_ops], inc=(k == 7 and n == 1))
                    yield
                    S.op("dve", lambda e: e.scalar_tensor_tensor(out=xm_[:].rearrange("p (n f) -> p n f", n=2), in0=xg[i % 2][:].rearrange("p (n f) -> p n f", n=2),
                                                                 scalar=ALPHA, in1=ops_[:, :, :], op0=ALU.mult, op1=ALU.add),
                         reads=[B_xg[i % 2], B_ops], writes=[B_xm])
                    yield
                    load_tile(i + 2)
                    yield

                def stage_a2(i):
                    rows = slice(i * 128, (i + 1) * 128)
                    xm_ = xm2[i % 2]
                    B_xm = B_xm2[i % 2]
                    for _ in layer_norm_stats_g(xm_, B_xm, st6, mv, rstd, nmr, B_s1):
                        yield
                    xmd = xmid[i % 2]
                    S.op("act", lambda e: e.activation(out=xmd[:], in_=xm_[:], func=AF.Identity, bias=nmr[:, 0:1], scale=rstd[:, 0:1]),
                         reads=[B_xm, B_s1], writes=[B_xmid[i % 2]])
                    yield
                    S.op("pool", lambda e: e.tensor_tensor(out=xmd[:], in0=xmd[:], in1=lnp[:, 0, :], op=ALU.mult),
                         reads=[B_xmid[i % 2], B_lnp], writes=[B_xmid[i % 2]])
                    yield
                    S.op("dve", lambda e: e.tensor_tensor(out=xmd[:], in0=xmd[:], in1=lnp[:, 1, :], op=ALU.add),
                         reads=[B_xmid[i % 2], B_lnp], writes=[B_xmid[i % 2]])
                    yield
                    S.dma(xmid_d[rows, :], xmd[:], reads=[B_xmid[i % 2]])
                    yield

                def layer_norm_stats_g(x_ap, Bx, st6_, mv_, rstd_, nmr_, Bs):
                    S.op("dve", lambda e: e.bn_stats(out=st6_[:, 0, :], in_=x_ap[:, 0:512]), reads=[Bx], writes=[Bs])
                    yield
                    S.op("dve", lambda e: e.bn_stats(out=st6_[:, 1, :], in_=x_ap[:, 512:1024]), reads=[Bx], writes=[Bs])
                    yield
                    S.op("dve", lambda e: e.bn_aggr(out=mv_[:], in_=st6_[:].rearrange("p a b -> p (a b)")), reads=[Bs], writes=[Bs])
                    S.op("dve", lambda e: e.tensor_scalar(out=rstd_[:], in0=mv_[:, 1:2], scalar1=EPS, scalar2=None, op0=ALU.add), reads=[Bs], writes=[Bs])
                    yield
                    S.op("pool", lambda e: e.tensor_tensor(out=rstd_[:], in0=rstd_[:], in1=neghalf_c[:], op=ALU.pow), reads=[Bs, B_const], writes=[Bs])
                    yield
                    S.op("dve", lambda e: e.scalar_tensor_tensor(out=nmr_[:], in0=mv_[:, 0:1], scalar=-1.0, in1=rstd_[:], op0=ALU.mult, op1=ALU.mult),
                         reads=[Bs], writes=[Bs])
                    yield

                def stage_b1(i):
                    h2T = h2T2[i % 2]
                    B_h2T = B_h2T2[i % 2]
                    xmd = xmid[i % 2]
                    for _ in layer_norm_stats_g(xmd, B_xmid[i % 2], st6b, mvb, rstdb, nmrb, B_s1b):
                        yield
                    S.op("act", lambda e: e.activation(out=xn2[:], in_=xmd[:], func=AF.Identity, bias=nmrb[:, 0:1], scale=rstdb[:, 0:1]),
                         reads=[B_xmid[i % 2], B_s1b], writes=[B_xn2])
                    yield
                    tp2 = tpg[1]
                    for k in range(8):
                        S.op("pe", lambda e: e.transpose(out=tp2[:, k * 128:(k + 1) * 128], in_=xn2[:, k * 128:(k + 1) * 128], identity=ident_bf[:]),
                             reads=[B_xn2, B_const], writes=[B_tpg[1]], inc=(k == 7))
                    yield
                    for k in range(8):
                        o = h2T[:, k, :]
                        i_ = tp2[:, k * 128:(k + 1) * 128]
                        sc = adaT[:, 32 + k, 0:1]
                        sh = adaT[:, 24 + k, 0:1]
                        if k % 2 == 0:
                            S.op("act", lambda e: e.activation(out=o, in_=i_, func=AF.Identity, bias=sh, scale=sc),
                                 reads=[B_tpg[1], B_ada], writes=[B_h2T])
                        else:
                            S.op("dve", lambda e: e.tensor_scalar(out=o, in0=i_, scalar1=sc, scalar2=sh, op0=ALU.mult, op1=ALU.add),
                                 reads=[B_tpg[1], B_ada], writes=[B_h2T])
                        yield
                    yield

                def stage_b2(i):
                    rows = slice(i * 128, (i + 1) * 128)
                    h2T = h2T2[i % 2]
                    B_h2T = B_h2T2[i % 2]
                    for k in range(8):
                        S.op("pe", lambda e: e.transpose(out=tpb[:, k * 128:(k + 1) * 128], in_=h2T[:, k, :], identity=ident_bf[:]),
                             reads=[B_h2T, B_const], writes=[B_tpb], inc=(k == 7))
                    lp = lpsb[i % 2]
                    for k in range(8):
                        S.op("pe", lambda e: e.matmul(lp[:, 0:72], lhsT=h2T[:, k, :], rhs=wr_sb[:, k, :], start=(k == 0), stop=(k == 7)),
                             reads=[B_h2T, B_wr], writes=[B_lpsb[i % 2]], inc=(k == 7))
                    yield
                    h2b = h2[i % 2]
                    S.op("act", lambda e: e.activation(out=h2b[:, 0:512], in_=tpb[:, 0:512], func=AF.Copy), reads=[B_tpb], writes=[B_h2[i % 2]])
                    yield
                    S.op("dve", lambda e: e.tensor_copy(out=h2b[:, 512:1024], in_=tpb[:, 512:1024]), reads=[B_tpb], writes=[B_h2[i % 2]])
                    yield
                    S.dma(h2_d[rows, :], h2b[:], reads=[B_h2[i % 2]])
                    yield

                def stage_c1(i):
                    lg, B_lg = lg2[i % 2], B_lg2[i % 2]
                    sm8, B_sm8 = sm82[i % 2], B_sm82[i % 2]
                    elm, B_elm = elm2[i % 2], B_elm2[i % 2]
                    top8, B_top8 = top82[i % 2], B_top82[i % 2]
                    lp = lpsb[i % 2]
                    Bl = B_lpsb[i % 2]
                    S.op("dve", lambda e: e.tensor_tensor(out=lg[:], in0=lp[:, 0:72], in1=rbias[:], op=ALU.add), reads=[Bl, B_rb], writes=[B_lg])
                    yield
                    S.op("dve", lambda e: e.reduce_max(out=sm8[:, 0:1], in_=lg[:, 0:8], axis=AX.X), reads=[B_lg], writes=[B_sm8])
                    yield
                    S.op("dve", lambda e: e.tensor_scalar(out=sm8[:, 1:2], in0=sm8[:, 0:1], scalar1=-1.0, scalar2=None, op0=ALU.mult),
                         reads=[B_sm8], writes=[B_sm8])
                    yield
                    S.op("act", lambda e: e.activation(out=ge[:], in_=lg[:, 0:8], func=AF.Exp, bias=sm8[:, 1:2], scale=1.0, accum_out=sm8[:, 2:3]),
                         reads=[B_lg, B_sm8], writes=[B_sm8, B_gm])
                    yield
                    S.op("dve", lambda e: e.tensor_scalar(out=gm[:], in0=lg[:, 0:8], scalar1=sm8[:, 0:1], scalar2=None, op0=ALU.is_ge),
                         reads=[B_lg, B_sm8, B_gm], writes=[B_gm])
                    yield
                    S.op("dve", lambda e: e.tensor_scalar(out=gm[:], in0=gm[:], scalar1=1e9, scalar2=-1e9, op0=ALU.mult, op1=ALU.add),
                         reads=[B_gm], writes=[B_gm])
                    yield
                    S.op("dve", lambda e: e.tensor_tensor(out=elm[:].rearrange("p (g e) -> p g e", e=8), in0=lg[:, 8:72].rearrange("p (g e) -> p g e", e=8),
                                                          in1=gm[:, :].unsqueeze(2).to_broadcast([128, 8, 8]), op=ALU.add),
                         reads=[B_lg, B_gm], writes=[B_elm])
                    yield
                    S.op("dve", lambda e: e.max(out=top8[:], in_=elm[:]), reads=[B_elm], writes=[B_top8])
                    yield

                def stage_c2(i):
                    sm8, B_sm8 = sm82[i % 2], B_sm82[i % 2]
                    elm, B_elm = elm2[i % 2], B_elm2[i % 2]
                    top8, B_top8 = top82[i % 2], B_top82[i % 2]
                    S.op("dve", lambda e: e.tensor_scalar(out=M1[:, i, :], in0=elm[:], scalar1=top8[:, 0:1], scalar2=None, op0=ALU.is_ge),
                         reads=[B_elm, B_top8], writes=[B_M1])
                    yield
                    S.op("dve", lambda e: e.tensor_scalar(out=m12[:], in0=elm[:], scalar1=top8[:, 1:2], scalar2=None, op0=ALU.is_ge),
                         reads=[B_elm, B_top8], writes=[B_m12])
                    yield
                    S.op("dve", lambda e: e.tensor_tensor(out=M2[:, i, :], in0=m12[:], in1=M1[:, i, :], op=ALU.subtract),
                         reads=[B_m12, B_M1], writes=[B_M2])
                    yield
                    S.op("dve", lambda e: e.tensor_tensor(out=sm8[:, 3:4], in0=top8[:, 1:2], in1=top8[:, 0:1], op=ALU.subtract),
                         reads=[B_top8, B_sm8], writes=[B_sm8])
                    yield
                    S.op("act", lambda e: e.activation(out=sm8[:, 4:5], in_=sm8[:, 3:4], func=AF.Exp), reads=[B_sm8], writes=[B_sm8])
                    yield
                    S.op("dve", lambda e: e.tensor_scalar(out=sm8[:, 5:6], in0=sm8[:, 4:5], scalar1=1.0, scalar2=sm8[:, 2:3], op0=ALU.add, op1=ALU.mult),
                         reads=[B_sm8], writes=[B_sm8])
                    yield
                    S.op("dve", lambda e: e.reciprocal(out=W12[:, i, 0:1], in_=sm8[:, 5:6]), reads=[B_sm8], writes=[B_W12])
                    yield
                    S.op("dve", lambda e: e.tensor_tensor(out=W12[:, i, 1:2], in0=W12[:, i, 0:1], in1=sm8[:, 4:5], op=ALU.mult),
                         reads=[B_sm8, B_W12], writes=[B_W12])
                    yield
                    S.op("pe", lambda e: e.matmul(rkps[:, 0:64], lhsT=SU[:, :], rhs=m12[:, :], start=True, stop=True),
                         reads=[B_SU, B_m12], writes=[B_rkps], inc=False)
                    S.op("pe", lambda e: e.matmul(rkps[:, 64:128], lhsT=ones_bf[:, :], rhs=m12[:, :], start=True, stop=True),
                         reads=[B_const, B_m12], writes=[B_rkps])
                    yield
                    S.op("dve", lambda e: e.tensor_tensor(out=RK[:, i, :], in0=rkps[:, 0:64], in1=run[:], op=ALU.add),
                         reads=[B_rkps, B_run], writes=[B_RK])
                    yield
                    S.op("dve", lambda e: e.tensor_tensor(out=run[:], in0=rkps[:, 64:128], in1=run[:], op=ALU.add),
                         reads=[B_rkps, B_run, B_RK], writes=[B_run])
                    yield

                load_tile(0)
                load_tile(1)
                stages = (stage_a1, stage_a2, stage_b1, stage_b2, stage_c1, stage_c2)
                for step in range(nt_run + len(stages) - 1):
                    gens = []
                    for k, st in enumerate(stages):
                        if 0 <= step - k < nt_run:
                            gens.append(st(step - k))
                    interleave(*gens)

                szi = sb(pg, "szi", [128, 64], I32)
                pad = sb(pg, "pad", [128, 64], F32)
                cum = sb(pg, "cum", [128, 64], F32)
                pst = sb(pg, "pst", [128, 64], F32)
                z64 = sb(pg, "z64", [128, 64], F32)
                cmpb = sb(pg, "cmpb", [128, 64], F32)
                ebf = sb(pg, "ebf", [128, 2], F32)
                ebr = sb(pg, "ebr", [1, 256], F32)
                B_bk = Buf()
                S.op("dve", lambda e: e.memset(z64[:], 0.0), writes=[B_bk])
                S.op("dve", lambda e: e.tensor_scalar(out=szi[:], in0=run[:], scalar1=127.0, scalar2=None, op0=ALU.add), reads=[B_run], writes=[B_bk])
                S.op("dve", lambda e: e.tensor_scalar(out=szi[:], in0=szi[:], scalar1=7, scalar2=None, op0=ALU.arith_shift_right), reads=[B_bk], writes=[B_bk])
                S.op("dve", lambda e: e.tensor_scalar(out=szi[:], in0=szi[:], scalar1=7, scalar2=None, op0=ALU.logical_shift_left), reads=[B_bk], writes=[B_bk])
                S.op("dve", lambda e: e.tensor_copy(out=pad[:], in_=szi[:]), reads=[B_bk], writes=[B_bk])
                S.op("dve", lambda e: e.tensor_tensor_scan(out=cum[:], data0=z64[:], data1=pad[:], initial=0.0, op0=ALU.add, op1=ALU.add),
                     reads=[B_bk], writes=[B_bk])
                S.op("dve", lambda e: e.tensor_tensor(out=pst[:], in0=cum[:], in1=pad[:], op=ALU.subtract), reads=[B_bk], writes=[B_bk])
                for j in range(2):
                    S.op("dve", lambda e: e.tensor_scalar(out=cmpb[:], in0=cum[:], scalar1=bvals[:, j:j + 1], scalar2=None, op0=ALU.is_le),
                         reads=[B_bk, B_bv], writes=[B_bk])
                    S.op("dve", lambda e: e.reduce_sum(out=ebf[:, j:j + 1], in_=cmpb[:], axis=AX.X), reads=[B_bk], writes=[B_bk])
                S.op("dve", lambda e: e.tensor_scalar(out=ebf[:], in0=ebf[:], scalar1=63.0, scalar2=None, op0=ALU.min), reads=[B_bk], writes=[B_bk])
                for j in range(2):
                    S.op("pe", lambda e: e.matmul(lps[0:1, j * 128:(j + 1) * 128], lhsT=ebf[:, j:j + 1], rhs=ident_f[:, :], start=True, stop=True),
                         reads=[B_bk, B_const], writes=[B_lps], inc=(j == 1))
                S.op("dve", lambda e: e.tensor_copy(out=ebr[0:1, :], in_=lps[0:1, 0:256]), reads=[B_lps], writes=[B_bk])
                S.op("dve", lambda e: e.tensor_copy(out=ebrow[0:1, :], in_=ebr[0:1, :]), reads=[B_bk], writes=[B_eb])
                idr = sb(pg, "idr", [1, 256], F32)
                samer = sb(pg, "samer", [1, 256], F32)
                S.op("dve", lambda e: e.memset(samer[0:1, :], 0.0), writes=[B_bk])
                S.op("dve", lambda e: e.tensor_tensor(out=samer[0:1, 2:256], in0=ebr[0:1, 2:256], in1=ebr[0:1, 0:254], op=ALU.is_equal),
                     reads=[B_bk], writes=[B_bk])
                S.op("dve", lambda e: e.tensor_scalar(out=idr[0:1, :], in0=ebr[0:1, :], scalar1=128.0, scalar2=None, op0=ALU.mult),
                     reads=[B_bk], writes=[B_bk])
                S.op("dve", lambda e: e.scalar_tensor_tensor(out=idr[0:1, :], in0=samer[0:1, :], scalar=1048576.0, in1=idr[0:1, :],
                                                             op0=ALU.mult, op1=ALU.add), reads=[B_bk], writes=[B_bk])
                S.op("pe", lambda e: e.matmul(lps[:, 0:256], lhsT=ones_f[0:1, :], rhs=idr[0:1, :], start=True, stop=True),
                     reads=[B_bk, B_const], writes=[B_lps])
                idxf = sb(pg, "idxf", [128, 256], F32)
                S.op("dve", lambda e: e.tensor_scalar(out=idxf[:], in0=lps[:, 0:256], scalar1=bvals[:, 2:3], scalar2=None, op0=ALU.add),
                     reads=[B_lps, B_bv], writes=[B_bk])
                S.op("dve", lambda e: e.tensor_copy(out=idxw[:], in_=idxf[:]), reads=[B_bk], writes=[B_idxw])
                S.op("dve", lambda e: e.tensor_tensor(out=RK[:], in0=RK[:], in1=pst[:, :].unsqueeze(1).to_broadcast([128, NT, 64]), op=ALU.add),
                     reads=[B_RK, B_bk], writes=[B_RK])
                dsf = sb(pg, "dsf", [128, NT], F32)
                for (Mx, Bm, Dx, Bd) in ((M1, B_M1, D1i, B_D1), (M2, B_M2, D2i, B_D2)):
                    S.op("dve", lambda e: e.tensor_tensor(out=big[:], in0=RK[:], in1=Mx[:], op=ALU.mult), reads=[B_RK, Bm], writes=[B_big])
                    S.op("dve", lambda e: e.tensor_reduce(out=dsf[:], in_=big[:], axis=AX.X, op=ALU.add), reads=[B_big], writes=[B_bk])
                    S.op("dve", lambda e: e.tensor_copy(out=Dx[:], in_=dsf[:]), reads=[B_bk], writes=[Bd])
                if rt_dbg is not None:
                    S.op("dve", lambda e: e.tensor_copy(out=big[:, :, 0:1].rearrange("p t o -> p (t o)"), in_=D1i[:]), reads=[B_D1], writes=[B_big])
                    S.op("dve", lambda e: e.tensor_copy(out=big[:, :, 1:2].rearrange("p t o -> p (t o)"), in_=D2i[:]), reads=[B_D2], writes=[B_big])
                    S.op("dve", lambda e: e.tensor_copy(out=big[:, :, 2:4], in_=W12[:]), reads=[B_W12], writes=[B_big])
                    S.dma(rt_dbg, big[:, :, 0:4], reads=[B_big])
                    S.dma(eb_dbg, ebrow[:], reads=[B_eb])
                    if ix_dbg is not None:
                        S.dma(ix_dbg, idxw[:], reads=[B_idxw])
                for i in range(nt_run):
                    rows = slice(i * 128, (i + 1) * 128)
                    hb = h2[i % 2]
                    S.dma(hb[:], h2_d[rows, :], writes=[B_h2[i % 2]])
                    for (Dx, Bd) in ((D1i, B_D1), (D2i, B_D2)):
                        S.dma(None, None, reads=[B_h2[i % 2], Bd], eng="pool",
                              indirect=lambda e: e.indirect_dma_start(out=xperm_d[:, :], out_offset=bass.IndirectOffsetOnAxis(ap=Dx[:, i:i + 1], axis=0),
                                                                      in_=hb[:, :], in_offset=None))
                S.barrier()
            if upto in ("G", "G1"):
                S.final_wait()
                return nc

            with contextlib.ExitStack() as ph:
                w1b = [sb(ph, "w1b%d" % i, [128, 8, HID], BF16) for i in range(2)]
                w3b = [sb(ph, "w3b%d" % i, [128, 8, HID], BF16) for i in range(2)]
                w2b = [sb(ph, "w2b%d" % i, [128, 4, D], BF16) for i in range(2)]
                B_wb = [[Buf(), Buf(), Buf()] for _ in range(2)]
                xb = [sb(ph, "xb%d" % i, [128, D], BF16) for i in range(3)]
                B_xb = [Buf(), Buf(), Buf()]
                xbT = [sb(ph, "xbT%d" % i, [128, 8, 128], BF16) for i in range(2)]
                sa = [sb(ph, "sa%d" % i, [128, HID], F32) for i in range(2)]
                hh = [sb(ph, "hh%d" % i, [128, HID], BF16) for i in range(2)]
                hhT = [sb(ph, "hhT%d" % i, [128, 4, 128], BF16) for i in range(2)]
                yb = [sb(ph, "yb%d" % i, [128, D], F32) for i in range(2)]
                B_xbT, B_sa, B_hh, B_hhT, B_yb = [[Buf(), Buf()] for _ in range(5)]
                tpp = [ps(ph, "tpp%d" % i, [128, 1024], BF16) for i in range(2)]
                B_tpp = [Buf(), Buf()]
                agp = [ps(ph, "agp%d" % i, [128, 2, 512], F32) for i in range(2)]
                B_agp = [Buf(), Buf()]
                yps = ps(ph, "yps", [128, 2, 512], F32)
                B_yps = Buf()
                nb_run = NBLK if upto != "H1" else 4
                tpc = [0]
                bnd_reg = nc.gpsimd.to_reg(NE * 128 - 1)

                def load_w(b, which):
                    if b >= nb_run or b < 0:
                        return
                    q = b % 2
                    for m, wt_ in enumerate((w1b[q], w3b[q], w2b[q])):
                        if m not in which:
                            continue
                        S.dma(None, None, reads=[B_idxw], writes=[B_wb[q][m]], eng="pool",
                              indirect=lambda e: e.indirect_dma_start(out=wt_[:].rearrange("p k n -> p (k n)"), out_offset=None, in_=wbf_d[m][:, :],
                                                                      in_offset=bass.IndirectOffsetOnAxis(ap=idxw[:, b:b + 1], axis=0),
                                                                      bounds_check=bnd_reg, oob_is_err=False))

                def load_x(b):
                    if b >= nb_run:
                        return
                    S.dma(xb[b % 3][:], xperm_d[b * 128:(b + 1) * 128, :], writes=[B_xb[b % 3]])

                def st_T(b):
                    q = b % 2
                    t_ = tpc[0] % 2
                    tpc[0] += 1
                    for k in range(8):
                        S.op("pe", lambda e: e.transpose(out=tpp[t_][:, k * 128:(k + 1) * 128], in_=xb[b % 3][:, k * 128:(k + 1) * 128], identity=ident_bf[:]),
                             reads=[B_xb[b % 3], B_const], writes=[B_tpp[t_]], inc=(k == 7))
                    S.op("act", lambda e: e.activation(out=xbT[q][:, 0:4, :], in_=tpp[t_][:, 0:512].rearrange("p (k t) -> p k t", t=128), func=AF.Copy),
                         reads=[B_tpp[t_]], writes=[B_xbT[q]])
                    S.op("dve", lambda e: e.tensor_copy(out=xbT[q][:, 4:8, :], in_=tpp[t_][:, 512:1024].rearrange("p (k t) -> p k t", t=128)),
                         reads=[B_tpp[t_]], writes=[B_xbT[q]])

                def st_mm1(b):
                    q = b % 2
                    for k in range(8):
                        S.op("pe", lambda e: e.matmul(agp[q][:, 0, :], lhsT=xbT[q][:, k, :], rhs=w1b[q][:, k, :], start=(k == 0), stop=(k == 7)),
                             reads=[B_xbT[q], B_wb[q][0]], writes=[B_agp[q]], inc=False)
                    for k in range(8):
                        S.op("pe", lambda e: e.matmul(agp[q][:, 1, :], lhsT=xbT[q][:, k, :], rhs=w3b[q][:, k, :], start=(k == 0), stop=(k == 7)),
                             reads=[B_xbT[q], B_wb[q][1]], writes=[B_agp[q]], inc=(k == 7))
                    S.op("act", lambda e: e.activation(out=sa[q][:], in_=agp[q][:, 0, :], func=AF.Silu), reads=[B_agp[q]], writes=[B_sa[q]])
                    S.op("dve", lambda e: e.tensor_tensor(out=hh[q][:], in0=sa[q][:], in1=agp[q][:, 1, :], op=ALU.mult),
                         reads=[B_sa[q], B_agp[q]], writes=[B_hh[q]])

                def st_Th(b):
                    q = b % 2
                    t_ = tpc[0] % 2
                    tpc[0] += 1
                    for k in range(4):
                        S.op("pe", lambda e: e.transpose(out=tpp[t_][:, k * 128:(k + 1) * 128], in_=hh[q][:, k * 128:(k + 1) * 128], identity=ident_bf[:]),
                             reads=[B_hh[q], B_const], writes=[B_tpp[t_]], inc=(k == 3))
                    S.op("act", lambda e: e.activation(out=hhT[q][:, :, :], in_=tpp[t_][:, 0:512].rearrange("p (k t) -> p k t", t=128), func=AF.Copy),
                         reads=[B_tpp[t_]], writes=[B_hhT[q]])

                def st_mm2(b):
                    q = b % 2
                    for n in range(2):
                        for k in range(4):
                            S.op("pe", lambda e: e.matmul(yps[:, n, :], lhsT=hhT[q][:, k, :], rhs=w2b[q][:, k, n * 512:(n + 1) * 512],
                                                          start=(k == 0), stop=(k == 3)),
                                 reads=[B_hhT[q], B_wb[q][2]], writes=[B_yps], inc=(k == 3 and n == 1))
                    S.op("dve", lambda e: e.tensor_tensor(out=yb[q][:].rearrange("p (n f) -> p n f", n=2), in0=yps[:, :, :],
                                                          in1=g_b[:, 1024:2048].rearrange("p (n f) -> p n f", n=2), op=ALU.mult),
                         reads=[B_yps, B_gb], writes=[B_yb[q]])
                    S.dma(yperm_d[b * 128:(b + 1) * 128, :], yb[q][:], reads=[B_yb[q]])

                load_w(0, (0, 1))
                load_w(1, (0, 1))
                load_w(0, (2,))
                load_x(0)
                load_x(1)
                load_x(2)
                st_T(0)
                for t in range(nb_run + 1):
                    load_x(t + 3)
                    if t >= 1:
                        st_Th(t - 1)
                    if t + 1 < nb_run:
                        st_T(t + 1)
                    if t < nb_run:
                        st_mm1(t)
                        load_w(t + 2, (0, 1))
                    if t >= 1:
                        st_mm2(t - 1)
                    load_w(t + 1, (2,))
                S.barrier()
            if upto in ("H", "H1"):
                S.final_wait()
                return nc

            with contextlib.ExitStack() as pi:
                y1 = [sb(pi, "y1_%d" % i, [128, D], F32) for i in range(2)]
                y2 = [sb(pi, "y2_%d" % i, [128, D], F32) for i in range(2)]
                xmt = [sb(pi, "xmt%d" % i, [128, D], F32) for i in range(2)]
                B_y1, B_y2, B_xmt = [Buf(), Buf()], [Buf(), Buf()], [Buf(), Buf()]
                acc = sb(pi, "acc", [128, D], F32)
                ot = [sb(pi, "ot%d" % i, [128, D], F32) for i in range(2)]
                B_acc, B_ot = Buf(), [Buf(), Buf()]
                st6 = sb(pi, "ist6", [128, 2, 6], F32)
                mv = sb(pi, "imv", [128, 2], F32)
                rstd = sb(pi, "irstd", [128, 1], F32)
                nmr = sb(pi, "inmr", [128, 1], F32)
                B_s2 = Buf()

                def load_i(i):
                    if i >= NT:
                        return
                    q = i % 2
                    S.dma(xmt[q][:], xmid_d[i * 128:(i + 1) * 128, :], writes=[B_xmt[q]])
                    for (yt, By, Dx, Bd) in ((y1[q], B_y1[q], D1i, B_D1), (y2[q], B_y2[q], D2i, B_D2)):
                        S.dma(None, None, reads=[Bd], writes=[By], eng="pool",
                              indirect=lambda e: e.indirect_dma_start(out=yt[:, :], out_offset=None, in_=yperm_d[:, :],
                                                                      in_offset=bass.IndirectOffsetOnAxis(ap=Dx[:, i:i + 1], axis=0)))
                acc2 = [acc, sb(pi, "acc_b", [128, D], F32)]
                B_acc2 = [B_acc, Buf()]

                def ln_stats_i(x_ap, Bx, Bs):
                    S.op("dve", lambda e: e.bn_stats(out=st6[:, 0, :], in_=x_ap[:, 0:512]), reads=[Bx], writes=[Bs])
                    yield
                    S.op("dve", lambda e: e.bn_stats(out=st6[:, 1, :], in_=x_ap[:, 512:1024]), reads=[Bx], writes=[Bs])
                    yield
                    S.op("dve", lambda e: e.bn_aggr(out=mv[:], in_=st6[:].rearrange("p a b -> p (a b)")), reads=[Bs], writes=[Bs])
                    S.op("dve", lambda e: e.tensor_scalar(out=rstd[:], in0=mv[:, 1:2], scalar1=EPS, scalar2=None, op0=ALU.add), reads=[Bs], writes=[Bs])
                    yield
                    S.op("pool", lambda e: e.tensor_tensor(out=rstd[:], in0=rstd[:], in1=neghalf_c[:], op=ALU.pow), reads=[Bs, B_const], writes=[Bs])
                    yield
                    S.op("dve", lambda e: e.scalar_tensor_tensor(out=nmr[:], in0=mv[:, 0:1], scalar=-1.0, in1=rstd[:], op0=ALU.mult, op1=ALU.mult),
                         reads=[Bs], writes=[Bs])
                    yield

                def stage_1(i):
                    q = i % 2
                    ac, Ba = acc2[q], B_acc2[q]
                    S.op("dve", lambda e: e.tensor_scalar(out=ac[:], in0=y1[q][:], scalar1=W12[:, i, 0:1], scalar2=None, op0=ALU.mult),
                         reads=[B_y1[q], B_W12], writes=[Ba])
                    yield
                    S.op("dve", lambda e: e.scalar_tensor_tensor(out=ac[:], in0=y2[q][:], scalar=W12[:, i, 1:2], in1=ac[:], op0=ALU.mult, op1=ALU.add),
                         reads=[B_y2[q], B_W12, Ba], writes=[Ba])
                    yield
                    S.op("dve", lambda e: e.scalar_tensor_tensor(out=ac[:], in0=xmt[q][:], scalar=ALPHA, in1=ac[:], op0=ALU.mult, op1=ALU.add),
                         reads=[B_xmt[q], Ba], writes=[Ba])
                    yield
                    load_i(i + 2)
                    yield

                def stage_2(i):
                    q = i % 2
                    ac, Ba = acc2[q], B_acc2[q]
                    for _ in ln_stats_i(ac, Ba, B_s2):
                        yield
                    S.op("act", lambda e: e.activation(out=ot[q][:], in_=ac[:], func=AF.Identity, bias=nmr[:, 0:1], scale=rstd[:, 0:1]),
                         reads=[Ba, B_s2], writes=[B_ot[q]])
                    yield
                    S.op("pool", lambda e: e.tensor_tensor(out=ot[q][:], in0=ot[q][:], in1=lnp[:, 2, :], op=ALU.mult),
                         reads=[B_ot[q], B_lnp], writes=[B_ot[q]])
                    yield
                    S.op("dve", lambda e: e.tensor_tensor(out=ot[q][:], in0=ot[q][:], in1=lnp[:, 3, :], op=ALU.add),
                         reads=[B_ot[q], B_lnp], writes=[B_ot[q]])
                    yield
                    S.dma(out_d[i * 128:(i + 1) * 128, :], ot[q][:], reads=[B_ot[q]])
                    yield

                load_i(0)
                load_i(1)
                for step in range(NT + 1):
                    gens = []
                    if step < NT:
                        gens.append(stage_1(step))
                    if 0 <= step - 1 < NT:
                        gens.append(stage_2(step - 1))
                    interleave(*gens)
                S.barrier()

        S.final_wait()
    return nc


def make_in_maps(inputs, n_cores=8):
    x = np.asarray(inputs["x"], np.float32)
    c = np.asarray(inputs["c"], np.float32)
    ctx = np.asarray(inputs["ctx"], np.float32)
    c_ctx = np.asarray(inputs["c_ctx"], np.float32)
    w_ada = np.ascontiguousarray(np.asarray(inputs["w_ada"], np.float32)[0])
    b_ada = np.asarray(inputs["b_ada"], np.float32)[0]
    w_in = np.ascontiguousarray(np.asarray(inputs["w_in"], np.float32)[0])
    gate_b = np.asarray(inputs["gate_b"], np.float32)[0]
    b_ada_l = np.ascontiguousarray(np.repeat(b_ada.reshape(48, 128).T[:, :, None], 2, axis=2))
    rope = make_rope_tables()
    rperm = make_rperm()
    sidx = np.arange(128)
    cmask = np.stack([(sidx[None, :] >= sidx[:, None]), (sidx[:, None] >= sidx[None, :]), (sidx[None, :] > sidx[:, None])]).astype(np.float32).astype(ml_dtypes.bfloat16)
    conv_w = np.asarray(inputs["conv_w"], np.float32)[0]
    conv_b = np.asarray(inputs["conv_b"], np.float32)[0]
    convw_l = np.ascontiguousarray(conv_w.reshape(5, 8, 128).transpose(2, 1, 0))
    convb_l = np.ascontiguousarray(conv_b.reshape(8, 128).T)
    mlg = np.ascontiguousarray(np.broadcast_to(np.asarray(inputs["ml_norm_g"], np.float32)[0][None, :], (128, 512)))
    w_out = np.ascontiguousarray(np.asarray(inputs["w_out"], np.float32)[0])
    w_r = np.ascontiguousarray(np.concatenate([np.asarray(inputs["w_router_g"], np.float32)[0],
                                               np.asarray(inputs["w_router_e"], np.float32)[0]], axis=1))
    rb = np.concatenate([np.asarray(inputs["b_router_g"], np.float32)[0], np.asarray(inputs["b_router_e"], np.float32)[0]])
    rbias = np.ascontiguousarray(np.broadcast_to(rb[None, :], (128, 72)))
    lnp = np.stack([np.broadcast_to(np.asarray(inputs[k], np.float32)[0][None, :], (128, D))
                    for k in ("ln1_g", "ln1_b", "ln2_g", "ln2_b")]).astype(np.float32)
    bvals = np.concatenate([128.0 * (np.arange(128)[:, None] + 128 * np.arange(2)[None, :]), np.arange(128)[:, None]], axis=1).astype(np.float32)
    relay = lambda w, kc, n: np.ascontiguousarray(np.asarray(w, np.float32)[0].reshape(NE, kc, 128, n).transpose(0, 2, 1, 3).reshape(NE * 128, kc * n))
    w1 = relay(inputs["w1"], 8, HID)
    w3 = relay(inputs["w3"], 8, HID)
    w2 = relay(inputs["w2"], 4, D)
    biasT = make_bias_tables(np.asarray(inputs["rpb"], np.float32)[0])
    maps = []
    for b in range(n_cores):
        cc = np.stack([c[b].reshape(8, 128).T, c_ctx.reshape(8, 128).T], axis=-1)
        maps.append({
            "x": np.ascontiguousarray(x[b]),
            "ctx": np.ascontiguousarray(ctx[b]),
            "cc": np.ascontiguousarray(cc),
            "w_ada": w_ada,
            "b_ada_l": b_ada_l,
            "b_ada_r": np.ascontiguousarray(b_ada.reshape(1, -1)),
            "w_in": w_in,
            "gate_b": np.ascontiguousarray(gate_b.reshape(16, 1)),
            "biasT": biasT,
            "w_out": w_out, "w_r": w_r, "rbias": rbias, "lnp": np.ascontiguousarray(lnp), "bvals": bvals,
            "w1": w1, "w3": w3, "w2": w2,
            "rope": rope, "rperm": rperm, "cmask": cmask, "convw": convw_l, "convb": convb_l, "mlg": mlg,
        })
    return maps


def make_bias_tables(rpb):
    rpb = np.asarray(rpb, np.float32)
    out = np.empty((5, 128, 8, 5, 128), np.float32)
    q = np.arange(128)
    k = np.arange(128)
    for v, i in enumerate((2, 0, 1, 62, 63)):
        jb0 = min(max(i - 2, 0), 59)
        qr = 2 * i + q // 64
        qc = q % 64
        r0 = np.clip(qr - 4, 0, 120)
        c0 = np.clip(qc - 8, 0, 48)
        for n in range(5):
            kr = 2 * (jb0 + n) + k // 64
            kc = k % 64
            inside = ((kr[:, None] >= r0[None, :]) & (kr[:, None] < r0[None, :] + 8) &
                      (kc[:, None] >= c0[None, :]) & (kc[:, None] < c0[None, :] + 16))
            ri = np.clip(kr[:, None] - qr[None, :] + 7, 0, 14)
            ci = np.clip(kc[:, None] - qc[None, :] + 15, 0, 30)
            g = rpb[:, ri, ci]
            out[v, :, :, n, :] = np.where(inside[None], g, np.float32(NEG)).transpose(1, 0, 2)
    return out.reshape(5, 128, 8 * 5 * 128).astype(ml_dtypes.bfloat16)


def make_rope_tables():
    t = np.arange(T)
    row = (t // GW).astype(np.float64)
    col = (t % GW).astype(np.float64)
    f = np.arange(128)
    inv = 10000.0 ** (-(f % 32).astype(np.float64) / 32.0)
    pos = np.where((f // 64 == 0)[:, None], row[None, :], col[None, :])
    ang = (pos.astype(np.float32) * inv.astype(np.float32)[:, None]).astype(np.float32)
    cs, sn = np.cos(ang).astype(np.float32), np.sin(ang).astype(np.float32)
    ksc = np.float32(128.0 ** -0.5)
    return np.stack([cs, sn, cs * ksc, sn * ksc]).astype(np.float32)


def make_rperm():
    r = np.zeros((128, 128), np.float32)
    for f in range(128):
        if f % 64 < 32:
            r[f + 32, f] = -1.0
        else:
            r[f - 32, f] = 1.0
    return r.astype(ml_dtypes.bfloat16)


def kernel(**inputs):
    nc = build_program()
    maps = make_in_maps(inputs)
    res = run_bass_kernel_spmd(nc, maps, core_ids=list(range(8)))
    out = np.stack([np.asarray(r["out"]) for r in res.results], axis=0)
    return out.astype(np.float32)
```
